# Optimizing a Trainium2 kernel written in Bass

```python
import math
import jax
import jax.numpy as jnp
from jax import lax
import numpy as np


D_MODEL = 1024
BATCH = 8
SEQ = 2048
DEPTH = 2

GRID_W = 64
CTX_LEN = 256
EPS = 1e-6
ROPE_BASE = 10000.0
ROPE_DIM = 64
Q_BLOCK = 128
MASK_VALUE = -1e30

A_HEADS = 4
A_QK = 64
A_V = 2 * A_QK
B_HEADS = 4
B_Q_LORA = 256
B_KV_LORA = 128
B_NOPE = 128
B_ROPE = 64
B_V = 128
C_HEADS = 4
C_DK = 128
C_DV = 128
C_CHUNK = 64
D_HEADS = 8
D_KV_HEADS = 2
D_HD = 64
WINDOW = 128
N_BRANCH = 4
BR_W = 512
PEER_HEADS = 8
N_KEYS = 128
N_EXPERTS = N_KEYS * N_KEYS
PEER_DK = 128
PEER_TOPK = 16
PEER_TOK_BLOCK = 128

IN_SIZES = (A_HEADS * A_QK, A_HEADS * A_QK, A_HEADS * A_QK, A_HEADS * A_QK, A_HEADS * A_V,
            B_Q_LORA, B_KV_LORA, B_ROPE,
            C_HEADS * C_DK, C_HEADS * C_DK, C_HEADS * C_DK, C_HEADS * C_DV, C_HEADS * C_DV,
            D_HEADS * D_HD, D_KV_HEADS * D_HD, D_KV_HEADS * D_HD,
            N_BRANCH * D_MODEL)
P_IN = sum(IN_SIZES)
CTX_KV_ONLY = (False, False, True, True, True, False, True, True, False, True, True, True, False, False, True, True, False)

kernel_name = 'hybrid_gated_mixers_peer_dit'


def rmsnorm(x, g):
    xf = x.astype(jnp.float32)
    y = xf * lax.rsqrt(jnp.mean(xf * xf, axis=-1, keepdims=True) + EPS)
    return (y * g.astype(jnp.float32)).astype(x.dtype)


def modulate(h, shift, scale):
    return h * (1.0 + scale) + shift


def project(h, w, keep):
    outs, off = [], 0
    if keep is None:
        p = h @ w
        for size in IN_SIZES:
            outs.append(p[..., off:off + size])
            off += size
    else:
        for size, k in zip(IN_SIZES, keep):
            outs.append(h @ w[:, off:off + size] if k else None)
            off += size
    return outs


def axial_angles(n, dim):
    rows = n // GRID_W
    r = jnp.repeat(jnp.arange(rows, dtype=jnp.float32), GRID_W)
    col = jnp.tile(jnp.arange(GRID_W, dtype=jnp.float32), rows)
    m = dim // 2
    freqs = ROPE_BASE ** (-2.0 * jnp.arange(m // 2, dtype=jnp.float32) / m)
    return jnp.concatenate([r[:, None] * freqs, col[:, None] * freqs], axis=-1)


def apply_rope(x, ang):
    B, n, H, dim = x.shape
    m = dim // 2
    xs = x.reshape(B, n, H, 2, 2, m // 2)
    a = ang.reshape(n, 1, 2, m // 2)
    cos = jnp.cos(a).astype(x.dtype)
    sin = jnp.sin(a).astype(x.dtype)
    x1, x2 = xs[..., 0, :], xs[..., 1, :]
    out = jnp.stack([x1 * cos - x2 * sin, x1 * sin + x2 * cos], axis=-2)
    return out.reshape(B, n, H, dim)


def over_query_blocks(f, *qs):
    B, n = qs[0].shape[:2]
    nb = n // Q_BLOCK
    split = lambda t: jnp.moveaxis(t.reshape((B, nb, Q_BLOCK) + t.shape[2:]), 1, 0)
    out = lax.map(lambda args: f(*args), tuple(split(t) for t in qs))
    return jnp.moveaxis(out, 0, 1).reshape((B, n) + out.shape[3:])


def attn_core(q, k, v, scale):
    p = jax.nn.softmax((jnp.einsum('bqhd,bkhd->bhqk', q, k) * scale).astype(jnp.float32), axis=-1)
    return jnp.einsum('bhqk,bkhd->bqhd', p.astype(v.dtype), v)


def diff_core(q1, q2, k1, k2, v, lam):
    scale = A_QK ** -0.5
    p1 = jax.nn.softmax((jnp.einsum('bqhd,bkhd->bhqk', q1, k1) * scale).astype(jnp.float32), axis=-1)
    p2 = jax.nn.softmax((jnp.einsum('bqhd,bkhd->bhqk', q2, k2) * scale).astype(jnp.float32), axis=-1)
    return jnp.einsum('bhqk,bkhd->bqhd', (p1 - lam * p2).astype(v.dtype), v)


def mixer_diff(px, pc, lq1, lk1, lq2, lk2, norm_g, layer, ang, ctx_out):
    heads = lambda t, d: t.reshape(t.shape[0], t.shape[1], A_HEADS, d)
    q1, q2, k1, k2 = [apply_rope(heads(t, A_QK), ang) for t in px[:4]]
    v = heads(px[4], A_V)
    k1c, k2c, vc = heads(pc[2], A_QK), heads(pc[3], A_QK), heads(pc[4], A_V)
    lam_init = 0.8 - 0.6 * math.exp(-0.3 * layer)
    f32 = jnp.float32
    lam = (jnp.exp(jnp.sum(lq1.astype(f32) * lk1.astype(f32)))
           - jnp.exp(jnp.sum(lq2.astype(f32) * lk2.astype(f32))) + lam_init)
    K1 = jnp.concatenate([k1, k1c], axis=1)
    K2 = jnp.concatenate([k2, k2c], axis=1)
    V = jnp.concatenate([v, vc], axis=1)
    ox = over_query_blocks(lambda a, b: diff_core(a, b, K1, K2, V, lam), q1, q2)
    post = lambda o: (rmsnorm(o, norm_g) * (1.0 - lam_init)).reshape(o.shape[0], o.shape[1], -1)
    oc = None
    if ctx_out:
        oc = post(diff_core(heads(pc[0], A_QK), heads(pc[1], A_QK), k1c, k2c, vc, lam))
    return post(ox), oc


def mixer_mla(px, pc, qn_g, kvn_g, w_uq, w_ukv, ang, ctx_out):
    def q_of(cq, rope):
        B, n = cq.shape[:2]
        q = (rmsnorm(cq, qn_g) @ w_uq).reshape(B, n, B_HEADS, B_NOPE + B_ROPE)
        q_rope = q[..., B_NOPE:]
        if rope:
            q_rope = apply_rope(q_rope, ang)
        return jnp.concatenate([q[..., :B_NOPE], q_rope], axis=-1)

    def kv_of(ckv, kr, rope):
        B, n = ckv.shape[:2]
        kv = (rmsnorm(ckv, kvn_g) @ w_ukv).reshape(B, n, B_HEADS, B_NOPE + B_V)
        k_rope = kr.reshape(B, n, 1, B_ROPE)
        if rope:
            k_rope = apply_rope(k_rope, ang)
        k = jnp.concatenate([kv[..., :B_NOPE], jnp.broadcast_to(k_rope, (B, n, B_HEADS, B_ROPE))], axis=-1)
        return k, kv[..., B_NOPE:]

    scale = (B_NOPE + B_ROPE) ** -0.5
    kx, vx = kv_of(px[1], px[2], True)
    kc, vc = kv_of(pc[1], pc[2], False)
    K = jnp.concatenate([kx, kc], axis=1)
    V = jnp.concatenate([vx, vc], axis=1)
    ox = over_query_blocks(lambda q: attn_core(q, K, V, scale), q_of(px[0], True))
    flat = lambda o: o.reshape(o.shape[0], o.shape[1], -1)
    oc = flat(attn_core(q_of(pc[0], False), kc, vc, scale)) if ctx_out else None
    return flat(ox), oc


def gla_scan(q, k, v, logf, s0):
    B, n, H, dk = q.shape
    nc = n // C_CHUNK
    chunks = lambda t: t.astype(jnp.float32).reshape(B, nc, C_CHUNK, H, t.shape[-1]).transpose(1, 0, 3, 2, 4)
    incl = jnp.tril(jnp.ones((C_CHUNK, C_CHUNK), dtype=bool))[:, :, None]

    def step(S, inp):
        qc, kc, vc, gc = inp
        b = jnp.cumsum(gc, axis=2)
        o_prev = jnp.einsum('bhtk,bhkv->bhtv', qc * jnp.exp(b), S)
        diff = b[:, :, :, None, :] - b[:, :, None, :, :]
        dec = jnp.where(incl, jnp.exp(jnp.where(incl, diff, 0.0)), 0.0)
        att = jnp.einsum('bhtk,bhsk,bhtsk->bhts', qc, kc, dec)
        o = o_prev + jnp.einsum('bhts,bhsv->bhtv', att, vc)
        b_end = b[:, :, -1:, :]
        S = (jnp.exp(b_end[:, :, 0, :])[..., None] * S
             + jnp.einsum('bhsk,bhsv->bhkv', kc * jnp.exp(b_end - b), vc))
        return S, o

    S, o = lax.scan(step, s0, (chunks(q), chunks(k), chunks(v), chunks(logf)))
    o = o.transpose(1, 0, 3, 2, 4).reshape(B, n, H, v.shape[-1])
    return o.astype(v.dtype), S


def gla_final_state(k, v, logf):
    b = jnp.cumsum(logf.astype(jnp.float32), axis=1)
    w = k.astype(jnp.float32) * jnp.exp(b[:, -1:] - b)
    return jnp.einsum('bnhk,bnhv->bhkv', w, v.astype(jnp.float32))


def mixer_hgrn(px, pc, lb_f, lb_b, norm_g, ctx_out):
    heads = lambda t, d: t.reshape(t.shape[0], t.shape[1], C_HEADS, d)
    flip = lambda t: jnp.flip(t, axis=1)

    def decay(z, lb):
        lbh = lb.astype(jnp.float32).reshape(C_HEADS, C_DK)
        f = lbh + (1.0 - lbh) * jax.nn.sigmoid(heads(z, C_DK).astype(jnp.float32))
        return jnp.log(f), 1.0 - f

    def readout(o, g):
        B, n = o.shape[:2]
        return (rmsnorm(o, norm_g) * jax.nn.silu(heads(g, C_DV))).reshape(B, n, C_HEADS * C_DV)

    B = px[0].shape[0]
    zeros = jnp.zeros((B, C_HEADS, C_DK, C_DV), jnp.float32)
    lf_cf, k_cf = decay(pc[1], lb_f)
    lf_cb, k_cb = decay(pc[2], lb_b)
    i_c = heads(pc[3], C_DV)
    oc = None
    if ctx_out:
        q_c = heads(pc[0], C_DK)
        o_cf, S_f = gla_scan(q_c, k_cf, i_c, lf_cf, zeros)
        o_cb, S_b = gla_scan(flip(q_c), flip(k_cb), flip(i_c), flip(lf_cb), zeros)
        oc = readout(o_cf + flip(o_cb), pc[4])
    else:
        S_f = gla_final_state(k_cf, i_c, lf_cf)
        S_b = gla_final_state(flip(k_cb), flip(i_c), flip(lf_cb))
    q_x = heads(px[0], C_DK)
    lf_xf, k_xf = decay(px[1], lb_f)
    lf_xb, k_xb = decay(px[2], lb_b)
    i_x = heads(px[3], C_DV)
    o_xf, _ = gla_scan(q_x, k_xf, i_x, lf_xf, S_f)
    o_xb, _ = gla_scan(flip(q_x), flip(k_xb), flip(i_x), flip(lf_xb), S_b)
    return readout(o_xf + flip(o_xb), px[4]), oc


def mixer_window(px, pc, sink, ang, ctx_out):
    B, n = px[0].shape[:2]
    G = D_HEADS // D_KV_HEADS
    nb = n // WINDOW
    scale = D_HD ** -0.5
    q = apply_rope(px[0].reshape(B, n, D_HEADS, D_HD), ang)
    k = apply_rope(px[1].reshape(B, n, D_KV_HEADS, D_HD), ang)
    v = px[2].reshape(B, n, D_KV_HEADS, D_HD)
    m = pc[1].shape[1]
    kc = pc[1].reshape(B, m, D_KV_HEADS, D_HD)
    vc = pc[2].reshape(B, m, D_KV_HEADS, D_HD)
    sink_b = sink.astype(jnp.float32).reshape(D_KV_HEADS, G)[:, :, None, None]
    L = 3 * WINDOW

    def band(t):
        tp = jnp.pad(t, ((0, 0), (WINDOW, WINDOW), (0, 0), (0, 0))).reshape(B, nb + 2, WINDOW, D_KV_HEADS, D_HD)
        return jnp.concatenate([tp[:, :-2], tp[:, 1:-1], tp[:, 2:]], axis=2)

    j = jnp.arange(L)
    a = jnp.arange(WINDOW)
    rel = j[None, :] - WINDOW - a[:, None]

    def block(args):
        qi, ki, vi, i = args
        kpos = i * WINDOW + j - WINDOW
        valid = (jnp.abs(rel) <= WINDOW) & ((kpos >= 0) & (kpos < n))[None, :]
        s_loc = jnp.where(valid, (jnp.einsum('bqkgd,bjkd->bkgqj', qi, ki) * scale).astype(jnp.float32), MASK_VALUE)
        s_ctx = (jnp.einsum('bqkgd,bckd->bkgqc', qi, kc) * scale).astype(jnp.float32)
        s_sink = jnp.broadcast_to(sink_b, s_ctx.shape[:-1] + (1,))
        p = jax.nn.softmax(jnp.concatenate([s_loc, s_ctx, s_sink], axis=-1), axis=-1).astype(vi.dtype)
        return (jnp.einsum('bkgqj,bjkd->bqkgd', p[..., :L], vi)
                + jnp.einsum('bkgqc,bckd->bqkgd', p[..., L:L + m], vc))

    qb = jnp.moveaxis(q.reshape(B, nb, WINDOW, D_KV_HEADS, G, D_HD), 1, 0)
    o = lax.map(block, (qb, jnp.moveaxis(band(k), 1, 0), jnp.moveaxis(band(v), 1, 0), jnp.arange(nb)))
    ox = jnp.moveaxis(o, 0, 1).reshape(B, n, D_HEADS * D_HD)
    oc = None
    if ctx_out:
        qc = pc[0].reshape(B, m, D_KV_HEADS, G, D_HD)
        s = (jnp.einsum('bqkgd,bckd->bkgqc', qc, kc) * scale).astype(jnp.float32)
        s_sink = jnp.broadcast_to(sink_b, s.shape[:-1] + (1,))
        p = jax.nn.softmax(jnp.concatenate([s, s_sink], axis=-1), axis=-1)[..., :m].astype(vc.dtype)
        oc = jnp.einsum('bkgqc,bckd->bqkgd', p, vc).reshape(B, m, D_HEADS * D_HD)
    return ox, oc


def merge_branches(outs, gate_logits, w_branch, w_out):
    gl = gate_logits.reshape(gate_logits.shape[:-1] + (N_BRANCH, D_MODEL))
    y = None
    for j in range(N_BRANCH):
        term = jax.nn.sigmoid(gl[..., j, :]) * (outs[j] @ w_branch[j])
        y = term if y is None else y + term
    return y @ w_out


def peer_ffn(h, wq, keys, u, v):
    B, n, D = h.shape
    hd = PEER_DK // 2
    KK = PEER_TOPK * PEER_TOPK

    def block(t):
        tb = t.shape[0]
        q = (t @ wq).reshape(tb, PEER_HEADS, 2, hd)
        s = jnp.einsum('thpd,hpkd->thpk', q, keys).astype(jnp.float32)
        sv, si = lax.top_k(s, PEER_TOPK)
        cand_s = (sv[:, :, 0, :, None] + sv[:, :, 1, None, :]).reshape(tb, PEER_HEADS, KK)
        cand_i = (si[:, :, 0, :, None] * N_KEYS + si[:, :, 1, None, :]).reshape(tb, PEER_HEADS, KK)
        top_s, pos = lax.top_k(cand_s, PEER_TOPK)
        idx = jnp.take_along_axis(cand_i, pos, axis=-1)
        g = jax.nn.softmax(top_s, axis=-1)
        act = jax.nn.gelu(jnp.einsum('td,thkd->thk', t, u[idx]).astype(jnp.float32), approximate=False)
        return jnp.einsum('thk,thkd->td', (g * act).astype(t.dtype), v[idx])

    out = lax.map(block, h.reshape((B * n) // PEER_TOK_BLOCK, PEER_TOK_BLOCK, D))
    return out.reshape(B, n, D)


def setup_inputs(seed: int = 0) -> dict:
    key = jax.random.key(seed)
    ks = iter(jax.random.split(key, 40))
    nrm = lambda shape, scale=1.0: jax.random.normal(next(ks), shape, jnp.float32) * scale
    gain = lambda shape: 1.0 + nrm(shape, 0.02)
    L, D = DEPTH, D_MODEL
    return {
        'x': nrm((BATCH, SEQ, D)),
        'c': nrm((BATCH, D)),
        'ctx': nrm((BATCH, CTX_LEN, D)),
        'c_ctx': nrm((D,)),
        'w_mod': nrm((L, D, 6 * D), 0.5 * D ** -0.5),
        'b_mod': nrm((L, 6 * D), 0.01),
        'norm1_g': gain((L, D)),
        'norm2_g': gain((L, D)),
        'w_in': nrm((L, D, P_IN), D ** -0.5),
        'diff_lam_q1': nrm((L, A_QK), 0.1),
        'diff_lam_k1': nrm((L, A_QK), 0.1),
        'diff_lam_q2': nrm((L, A_QK), 0.1),
        'diff_lam_k2': nrm((L, A_QK), 0.1),
        'diff_norm_g': gain((L, A_V)),
        'mla_qnorm_g': gain((L, B_Q_LORA)),
        'mla_kvnorm_g': gain((L, B_KV_LORA)),
        'mla_w_uq': nrm((L, B_Q_LORA, B_HEADS * (B_NOPE + B_ROPE)), B_Q_LORA ** -0.5),
        'mla_w_ukv': nrm((L, B_KV_LORA, B_HEADS * (B_NOPE + B_V)), B_KV_LORA ** -0.5),
        'hgrn_lb': nrm((2, L, C_HEADS * C_DK), 0.5),
        'hgrn_norm_g': gain((L, C_DV)),
        'win_sink': nrm((L, D_HEADS), 0.5),
        'w_branch': nrm((L, N_BRANCH, BR_W, D), BR_W ** -0.5),
        'w_out': nrm((L, D, D), D ** -0.5),
        'peer_wq': nrm((L, D, PEER_HEADS * PEER_DK), D ** -0.5),
        'peer_keys': nrm((L, PEER_HEADS, 2, N_KEYS, PEER_DK // 2), (PEER_DK // 2) ** -0.5),
        'peer_u': nrm((L, N_EXPERTS, D), D ** -0.5),
        'peer_v': nrm((L, N_EXPERTS, D), 0.5),
        'final_g': gain((D,)),
    }


def reference(x, c, ctx, c_ctx, w_mod, b_mod, norm1_g, norm2_g, w_in, diff_lam_q1, diff_lam_k1,
              diff_lam_q2, diff_lam_k2, diff_norm_g, mla_qnorm_g, mla_kvnorm_g, mla_w_uq, mla_w_ukv,
              hgrn_lb, hgrn_norm_g, win_sink, w_branch, w_out, peer_wq, peer_keys, peer_u, peer_v, final_g):
    n = x.shape[1]
    ang = axial_angles(n, ROPE_DIM)
    lb_p = jax.nn.softmax(hgrn_lb.astype(jnp.float32), axis=1)
    lb = jnp.cumsum(lb_p, axis=1) - lb_p[:, :1]
    s_c = jax.nn.silu(c)
    s_cc = jax.nn.silu(c_ctx)
    xc = ctx
    for l in range(DEPTH):
        last = l == DEPTH - 1
        ctx_out = not last
        mod = s_c @ w_mod[l] + b_mod[l]
        sh1, sc1, g1, sh2, sc2, g2 = [t[:, None, :] for t in jnp.split(mod, 6, axis=-1)]
        n_cm = 2 if last else 6
        mod_c = jnp.split(s_cc @ w_mod[l][:, :n_cm * D_MODEL] + b_mod[l][:n_cm * D_MODEL], n_cm, axis=-1)

        hx = modulate(rmsnorm(x, norm1_g[l]), sh1, sc1)
        hc = modulate(rmsnorm(xc, norm1_g[l]), mod_c[0], mod_c[1])
        px = project(hx, w_in[l], None)
        pc = project(hc, w_in[l], CTX_KV_ONLY if last else None)
        oa = mixer_diff(px[0:5], pc[0:5], diff_lam_q1[l], diff_lam_k1[l], diff_lam_q2[l], diff_lam_k2[l],
                        diff_norm_g[l], l, ang, ctx_out)
        ob = mixer_mla(px[5:8], pc[5:8], mla_qnorm_g[l], mla_kvnorm_g[l], mla_w_uq[l], mla_w_ukv[l], ang, ctx_out)
        oh = mixer_hgrn(px[8:13], pc[8:13], lb[0, l], lb[1, l], hgrn_norm_g[l], ctx_out)
        od = mixer_window(px[13:16], pc[13:16], win_sink[l], ang, ctx_out)
        branches = (oa, ob, oh, od)
        x = x + g1 * merge_branches([o[0] for o in branches], px[16], w_branch[l], w_out[l])
        if ctx_out:
            xc = xc + mod_c[2] * merge_branches([o[1] for o in branches], pc[16], w_branch[l], w_out[l])

        x = x + g2 * peer_ffn(modulate(rmsnorm(x, norm2_g[l]), sh2, sc2),
                              peer_wq[l], peer_keys[l], peer_u[l], peer_v[l])
        if ctx_out:
            xc = xc + mod_c[5] * peer_ffn(modulate(rmsnorm(xc, norm2_g[l]), mod_c[3], mod_c[4]),
                                          peer_wq[l], peer_keys[l], peer_u[l], peer_v[l])
    return rmsnorm(x, final_g)
```

```python
import numpy as np
import math
import concourse.bass as bass
import concourse.mybir as mybir
from concourse.bass_utils import run_bass_kernel_spmd

F32 = mybir.dt.float32
BF16 = mybir.dt.bfloat16
U32 = mybir.dt.uint32
I32 = mybir.dt.int32
AF = mybir.ActivationFunctionType
ALU = mybir.AluOpType
AX = mybir.AxisListType

NDS = 40


class Buf:
    __slots__ = ("t", "w", "r", "name", "nt")

    def __init__(self, t, name="", nt=False):
        self.t = t
        self.w = None
        self.r = []
        self.name = name
        self.nt = nt

    def __getitem__(self, k):
        return self.t[k]


class KB:
    def __init__(self, nc):
        self.nc = nc
        self.E = {"pe": nc.tensor, "act": nc.scalar, "dve": nc.vector, "pool": nc.gpsimd, "sp": nc.sync}
        self.esem = {n: nc.alloc_semaphore("s_" + n) for n in self.E}
        self.ecnt = {n: 0 for n in self.E}
        self.dsems = [nc.alloc_semaphore("d%d" % i) for i in range(NDS)]
        self.dcnt = [0] * NDS
        self.dnext = 0
        self.known = {n: {} for n in self.E}
        self.nwait = 0
        self.ninst = 0

    def _sem(self, key):
        return self.esem[key] if isinstance(key, str) else self.dsems[key]

    def _need(self, eng, ev):
        key, val, _ = ev
        if self.known[eng].get(key, 0) >= val:
            return
        self.E[eng].wait_ge(self._sem(key), val)
        self.known[eng][key] = val
        self.nwait += 1

    def _deps(self, eng, reads, writes, is_dma=False):
        for b in reads:
            if b.w is not None:
                self._need(eng, b.w)
        for b in writes:
            if b.w is not None and (is_dma or b.w[2] != eng):
                self._need(eng, b.w)
            for r in b.r:
                if is_dma or r[2] != eng:
                    self._need(eng, r)

    def op(self, eng, fn, reads=(), writes=()):
        reads = [b for b in reads if not b.nt]
        writes = [b for b in writes if not b.nt]
        self._deps(eng, reads, writes)
        inst = fn(self.E[eng])
        self.ecnt[eng] += 1
        inst.then_inc(self.esem[eng], 1)
        ev = (eng, self.ecnt[eng], eng)
        for b in reads:
            b.r.append(ev)
        for b in writes:
            b.w = ev
            b.r = []
        self.ninst += 1
        return inst

    def dma(self, q, fn, reads=(), writes=()):
        reads = [b for b in reads if not b.nt]
        writes = [b for b in writes if not b.nt]
        self._deps(q, reads, writes, is_dma=True)
        i = self.dnext
        self.dnext = (self.dnext + 1) % NDS
        if self.dcnt[i] > 0:
            self._need(q, (i, self.dcnt[i], "dma"))
        inst = fn(self.E[q])
        self.dcnt[i] += 16
        inst.then_inc(self.dsems[i], 16)
        ev = (i, self.dcnt[i], "dma")
        for b in reads:
            b.r.append(ev)
        for b in writes:
            b.w = ev
            b.r = []
        self.ninst += 1
        return inst

    def barrier(self):
        evs = [(n, self.ecnt[n], n) for n in self.E if self.ecnt[n] > 0]
        evs += [(i, self.dcnt[i], "dma") for i in range(NDS) if self.dcnt[i] > 0]
        for n in self.E:
            for ev in evs:
                self._need(n, ev)

    def finish(self):
        self.barrier()


D = 1024
NX = 2048
NC_ = 256
T = NX + NC_
NT = T // 128
P_IN = 9408
EPS = 1e-6
OFF = {"a_q1": 0, "a_q2": 256, "a_k1": 512, "a_k2": 768, "a_v": 1024,
       "b_cq": 1536, "b_ckv": 1792, "b_kr": 1920,
       "c_q": 1984, "c_f": 2496, "c_b": 3008, "c_i": 3520, "c_g": 4032,
       "d_q": 4544, "d_k": 5056, "d_v": 5184, "gate": 5312}

W_NAMES = [("w_mod", [2, 1024, 6144]), ("b_mod", [2, 6144]), ("norm1_g", [2, 1024]), ("norm2_g", [2, 1024]),
           ("w_in", [2, 1024, 9408]), ("diff_lam_q1", [2, 64]), ("diff_lam_k1", [2, 64]), ("diff_lam_q2", [2, 64]),
           ("diff_lam_k2", [2, 64]), ("diff_norm_g", [2, 128]), ("mla_qnorm_g", [2, 256]), ("mla_kvnorm_g", [2, 128]),
           ("mla_w_uq", [2, 256, 768]), ("mla_w_ukv", [2, 128, 1024]), ("hgrn_lb", [2, 2, 512]), ("hgrn_norm_g", [2, 128]),
           ("win_sink", [2, 8]), ("w_branch", [2, 4, 512, 1024]), ("w_out", [2, 1024, 1024]), ("peer_wq", [2, 1024, 1024]),
           ("peer_keys", [2, 8, 2, 128, 64]), ("peer_u", [2, 16384, 1024]), ("peer_v", [2, 16384, 1024]), ("final_g", [1, 1024])]


def host_consts():
    n = NX
    rows = np.repeat(np.arange(n // 64, dtype=np.float32), 64)
    cols = np.tile(np.arange(64, dtype=np.float32), n // 64)
    m = 32
    freqs = (10000.0 ** (-2.0 * np.arange(m // 2, dtype=np.float32) / m)).astype(np.float32)
    ang = np.concatenate([rows[:, None] * freqs, cols[:, None] * freqs], axis=-1).astype(np.float32)
    cs = np.zeros((T, 64), np.float32)
    cs[:n, :32] = np.cos(ang)
    cs[:n, 32:] = np.sin(ang)
    cs[n:, :32] = 1.0
    i = np.arange(128)
    c = {}
    c["rope_cs"] = cs
    c["ident"] = np.eye(128, dtype=np.float32)
    c["mask_prev"] = (i[:, None] >= i[None, :]).astype(np.float32)
    c["mask_next"] = (i[:, None] <= i[None, :]).astype(np.float32)
    j = np.arange(64)
    tri = np.zeros((64, 4, 64), np.float32)
    tri[:, 0] = (j[:, None] <= j[None, :])
    tri[:, 1] = (j[:, None] >= j[None, :])
    tri[:, 2] = (j[:, None] > j[None, :])
    tri[:, 3] = (j[:, None] < j[None, :])
    c["tri"] = tri.reshape(64, 256)
    return c


class Ctx:
    pass


_nmc = [0]


def _nm():
    _nmc[0] += 1
    return "tmp%d" % _nmc[0]


def build_program(n_layers=2, debug=(), stop_after=None):
    nc = bass.Bass("TRN2", target_bir_lowering=False)
    kb = KB(nc)
    g = Ctx()
    g.nc, g.kb = nc, kb
    g.debug = debug

    def din(name, shape, dt=F32):
        return nc.dram_tensor(name, list(shape), dt, kind="ExternalInput").ap()

    g.x_in = din("x", [NX, D])
    g.c_in = din("c", [1, D])
    g.ctx_in = din("ctx", [NC_, D])
    g.cctx_in = din("c_ctx", [1, D])
    g.W = {n: din(n, s) for n, s in W_NAMES}
    g.rope_cs = din("rope_cs", [T, 64])
    g.ident_in = din("ident", [128, 128])
    g.mprev_in = din("mask_prev", [128, 128])
    g.mnext_in = din("mask_next", [128, 128])
    g.tri_in = din("tri", [64, 256])
    g.out = nc.dram_tensor("out", [NX, D], F32, kind="ExternalOutput").ap()

    def scratch(name, shape, dt):
        kind = "ExternalOutput" if name in debug else "Internal"
        return Buf(nc.dram_tensor(name, list(shape), dt, kind=kind).ap(), name, nt=True)

    g.P = scratch("P", [T, P_IN], BF16)
    g.OB = scratch("OB", [T, 2048], BF16)
    g.MODS = scratch("MODS", [2, 2, 6144], F32)
    g.OF = scratch("OF", [T, 512], F32)

    cnt = [0]

    def sb(shape, dt, name=None):
        cnt[0] += 1
        return Buf(nc.alloc_sbuf_tensor("%s_%d" % (name or "t", cnt[0]), list(shape), dt), name or "t")
    g.sb = sb

    g.X = [Buf(None, "X%d" % i) for i in range(NT)]
    xall = nc.alloc_sbuf_tensor("Xall", [128, NT, D], F32)
    for i in range(NT):
        g.X[i].t = xall[:, i, :]
    g.PS = [Buf(nc.alloc_psum_tensor("ps%d" % i, [128, 512], F32), "ps%d" % i) for i in range(8)]
    g.ident_f = sb([128, 128], F32, "identf")
    g.ident_b = sb([128, 128], BF16, "identb")
    g.ones_f = sb([128, 128], F32, "onesf")
    g.SCT = sb([128, 8, 2], F32, "sct")

    dma = lambda fn, r=(), w=(): kb.dma("sp", fn, r, w)

    for i in range(16):
        dma(lambda e, i=i: e.dma_start(out=g.X[i][:], in_=g.x_in[i * 128:(i + 1) * 128, :]), [], [g.X[i]])
    for i in range(2):
        dma(lambda e, i=i: e.dma_start(out=g.X[16 + i][:], in_=g.ctx_in[i * 128:(i + 1) * 128, :]), [], [g.X[16 + i]])
    dma(lambda e: e.dma_start(out=g.ident_f[:], in_=g.ident_in[:, :]), [], [g.ident_f])
    kb.op("dve", lambda e: e.tensor_copy(out=g.ident_b[:], in_=g.ident_f[:]), [g.ident_f], [g.ident_b])
    kb.op("dve", lambda e: e.memset(g.ones_f[:], 1.0), [], [g.ones_f])
    craw = sb([128, 8, 2], F32, "craw")
    with nc.allow_non_contiguous_dma(reason="tiny conditioning vector load"):
        dma(lambda e: e.dma_start(out=craw[:, :, 0], in_=g.c_in.rearrange("o (k p) -> p (o k)", p=128)), [], [craw])
        dma(lambda e: e.dma_start(out=craw[:, :, 1], in_=g.cctx_in.rearrange("o (k p) -> p (o k)", p=128)), [], [craw])
    kb.op("act", lambda e: e.activation(out=g.SCT[:], in_=craw[:], func=AF.Silu), [craw], [g.SCT])

    for l in range(n_layers):
        last = (l == 1)
        phase_mod(g, l)
        if stop_after == ("mod", l):
            break
        if "skip_proj" not in debug:
            phase_norm_proj(g, l)
        if stop_after == ("proj", l):
            break
        ctx_out = not last
        if "skip_a" not in debug:
            phase_mixer_a(g, l, ctx_out)
        if stop_after == ("a", l):
            break
        if "skip_b" not in debug:
            phase_mixer_b(g, l, ctx_out)
        if stop_after == ("b", l):
            break
        if "skip_d" not in debug:
            phase_mixer_d(g, l, ctx_out)
        if stop_after == ("d", l):
            break
        if "skip_c" not in debug:
            phase_mixer_c(g, l, ctx_out)
        if stop_after == ("c", l):
            break
        if "skip_merge" not in debug:
            phase_merge(g, l, ctx_out)
        if "Xdbg" in debug and stop_after == ("merge", l):
            xd = nc.dram_tensor("Xdbg", [NT, 128, D], F32, kind="ExternalOutput").ap()
            for i in range(NT):
                dma(lambda e, i=i: e.dma_start(out=xd[i], in_=g.X[i][:]), [g.X[i]], [])
        if stop_after == ("merge", l):
            break
        phase_peer(g, l, ctx_out)
        if "Xdbg" in debug and stop_after == ("peer", l):
            xd = nc.dram_tensor("Xdbg", [NT, 128, D], F32, kind="ExternalOutput").ap()
            for i in range(NT):
                dma(lambda e, i=i: e.dma_start(out=xd[i], in_=g.X[i][:]), [g.X[i]], [])
        if stop_after == ("peer", l):
            break

    if stop_after is None:
        phase_final(g)
    kb.finish()
    g.nc = nc
    return nc, g


def phase_mod(g, l):
    nc, kb = g.nc, g.kb
    kb.barrier()
    dma = lambda fn, r=(), w=(): kb.dma("sp", fn, r, w)
    with nc.sbuf_tensor(_nm(), [128, 2, 8, 512], F32) as wm_t, nc.sbuf_tensor(_nm(), [1, 6144], F32) as bm_t, \
            nc.sbuf_tensor(_nm(), [2, 2, 512], F32) as ev_t:
        wm = [Buf(wm_t[:, i], "wm%d" % i) for i in range(2)]
        bm = Buf(bm_t, "bm")
        ev = [Buf(ev_t[:, i], "ev%d" % i) for i in range(2)]
        dma(lambda e: e.dma_start(out=bm[:], in_=g.W["b_mod"][l:l + 1, :]), [], [bm])
        wsrc = g.W["w_mod"][l].rearrange("(k p) c -> p k c", p=128)
        for m in range(12):
            w = wm[m % 2]
            dma(lambda e, w=w, m=m: e.dma_start(out=w[:], in_=wsrc[:, :, m * 512:(m + 1) * 512]), [], [w])
            ps = g.PS[m % 2]
            for k in range(8):
                kb.op("pe", lambda e, w=w, k=k, ps=ps: e.matmul(ps[0:2, :], lhsT=g.SCT[:, k, :], rhs=w[:, k, :],
                                                              start=(k == 0), stop=False), [g.SCT, w], [ps])
            kb.op("pe", lambda e, ps=ps, m=m: e.matmul(ps[0:2, :], lhsT=g.ones_f[0:1, 0:2], rhs=bm[0:1, m * 512:(m + 1) * 512],
                                                     start=False, stop=True), [g.ones_f, bm], [ps])
            o = ev[m % 2]
            kb.op("act", lambda e, o=o, ps=ps: e.activation(out=o[:], in_=ps[0:2, :], func=AF.Copy), [ps], [o])
            dma(lambda e, o=o, m=m: e.dma_start(out=g.MODS[l, :, m * 512:(m + 1) * 512], in_=o[:]), [o], [g.MODS])
        kb.barrier()


def load_bcast(g, dst, src_row_ap):
    g.kb.dma("sp", lambda e: e.dma_start(out=dst[:], in_=src_row_ap.partition_broadcast(128)), [g.MODS], [dst])


def norm_tile(g, xin, GS, SH, hb, scr):
    kb = g.kb
    junk, st = scr
    kb.op("act", lambda e: e.activation(out=junk[:], in_=xin[:], func=AF.Square, accum_out=st[:, 0:1]), [xin], [junk, st])
    kb.op("dve", lambda e: e.tensor_scalar(out=st[:, 1:2], in0=st[:, 0:1], scalar1=1.0 / D, scalar2=EPS, op0=ALU.mult, op1=ALU.add), [st], [st])
    kb.op("act", lambda e: e.activation(out=st[:, 2:3], in_=st[:, 1:2], func=AF.Sqrt), [st], [st])
    kb.op("dve", lambda e: e.reciprocal(out=st[:, 3:4], in_=st[:, 2:3]), [st], [st])
    kb.op("dve", lambda e: e.scalar_tensor_tensor(out=junk[:], in0=xin[:], scalar=st[:, 3:4], in1=GS[:], op0=ALU.mult, op1=ALU.mult), [xin, st, GS], [junk])
    kb.op("dve", lambda e: e.tensor_tensor(out=hb[:], in0=junk[:], in1=SH[:], op=ALU.add), [junk, SH], [hb])


def phase_norm_proj(g, l):
    nc, kb = g.nc, g.kb
    dma = lambda fn, r=(), w=(): kb.dma("sp", fn, r, w)
    with nc.sbuf_tensor(_nm(), [128, 8, T], BF16) as hT_t:
        hT = Buf(hT_t, "hT")
        with nc.sbuf_tensor(_nm(), [128, 5, D], F32) as bc_t, nc.sbuf_tensor(_nm(), [128, D], F32) as junk_t, \
                nc.sbuf_tensor(_nm(), [128, 2, 8], F32) as st_t, nc.sbuf_tensor(_nm(), [128, 2, D], BF16) as hb_t:
            G1, SCx, SHx, SCc, SHc = [Buf(bc_t[:, i], "bc%d" % i) for i in range(5)]
            junk = Buf(junk_t, "junk")
            sts = [Buf(st_t[:, i], "st%d" % i) for i in range(2)]
            hbs = [Buf(hb_t[:, i], "hb%d" % i) for i in range(2)]
            dma(lambda e: e.dma_start(out=G1[:], in_=g.W["norm1_g"][l:l + 1, :].partition_broadcast(128)), [], [G1])
            load_bcast(g, SHx, g.MODS[l, 0:1, 0:D])
            load_bcast(g, SCx, g.MODS[l, 0:1, D:2 * D])
            load_bcast(g, SHc, g.MODS[l, 1:2, 0:D])
            load_bcast(g, SCc, g.MODS[l, 1:2, D:2 * D])
            for S in (SCx, SCc):
                kb.op("dve", lambda e, S=S: e.scalar_tensor_tensor(out=S[:], in0=S[:], scalar=1.0, in1=G1[:], op0=ALU.add, op1=ALU.mult), [S, G1], [S])
            for tt in range(NT):
                GS, SH = (SCx, SHx) if tt < 16 else (SCc, SHc)
                hb = hbs[tt % 2]
                norm_tile(g, g.X[tt], GS, SH, hb, (junk, sts[tt % 2]))
                ps = g.PS[tt % 2]
                psb = ps[:].bitcast(BF16)
                for k in range(8):
                    kb.op("pe", lambda e, k=k, hb=hb, psb=psb: e.transpose(out=psb[:, k * 128:(k + 1) * 128], in_=hb[:, k * 128:(k + 1) * 128], identity=g.ident_b[:]), [hb, g.ident_b], [ps])
                kb.op("act", lambda e, tt=tt, psb=psb: e.activation(out=hT[:, :, tt * 128:(tt + 1) * 128], in_=psb.rearrange("p (k t) -> p k t", k=8), func=AF.Copy), [ps], [hT])
            kb.barrier()
        with nc.sbuf_tensor(_nm(), [128, 2, 8, 512], BF16) as wb_t, nc.sbuf_tensor(_nm(), [128, 2, 6, 512], BF16) as po_t:
            wbs = [Buf(wb_t[:, i], "wb%d" % i) for i in range(2)]
            pos = [Buf(po_t[:, i], "po%d" % i) for i in range(2)]
            wsrc = g.W["w_in"][l].rearrange("(k p) c -> p k c", p=128)
            pdst = g.P.t.rearrange("(t p) c -> p t c", p=128)
            nct = (P_IN + 511) // 512
            cw = lambda ct: min(512, P_IN - ct * 512)

            def load_w(ct):
                w = wbs[ct % 2]
                kb.dma("pool", lambda e: e.dma_start(out=w[:, :, 0:cw(ct)], in_=wsrc[:, :, ct * 512:ct * 512 + cw(ct)]), [], [w])
            load_w(0)
            oi = 0
            pi = 0
            for ct in range(nct):
                if ct + 1 < nct:
                    load_w(ct + 1)
                w = wbs[ct % 2]
                n = cw(ct)
                for tg in range(3):
                    po = pos[oi % 2]
                    oi += 1
                    for j in range(6):
                        tt = tg * 6 + j
                        ps = g.PS[2 + pi % 4]
                        pi += 1
                        for k in range(8):
                            kb.op("pe", lambda e, k=k, tt=tt, ps=ps, w=w, n=n: e.matmul(ps[:, 0:n], lhsT=hT[:, k, tt * 128:(tt + 1) * 128], rhs=w[:, k, 0:n],
                                                                                     start=(k == 0), stop=(k == 7)), [hT, w], [ps])
                        if pi % 2 == 0:
                            kb.op("act", lambda e, j=j, ps=ps, po=po, n=n: e.activation(out=po[:, j, 0:n], in_=ps[:, 0:n], func=AF.Copy), [ps], [po])
                        else:
                            kb.op("dve", lambda e, j=j, ps=ps, po=po, n=n: e.tensor_copy(out=po[:, j, 0:n], in_=ps[:, 0:n]), [ps], [po])
                    dma(lambda e, po=po, tg=tg, ct=ct, n=n: e.dma_start(out=pdst[:, tg * 6:(tg + 1) * 6, ct * 512:ct * 512 + n], in_=po[:, :, 0:n]), [po], [g.P])
            kb.barrier()


def attention(g, nh, dv, scale, V, qk_chunks, groups, post, npass=1):
    nc, kb = g.nc, g.kb
    assert dv + 1 <= 256
    with nc.sbuf_tensor(_nm(), [128, 3, 512], BF16) as E_t, nc.sbuf_tensor(_nm(), [128, 4, 4, dv + 1], F32) as of_t:
        Es = [Buf(E_t[:, i], "E%d" % i) for i in range(3)]
        Ofs = [Buf(of_t[:, i], "Of%d" % i) for i in range(4)]
        it = 0
        si = 0
        for h in range(nh):
            for (q_lo, q_n, key_tiles) in groups:
                nsub = q_n // 128
                ofl = []
                for s in range(npass):
                    chunks = qk_chunks(h, s)
                    obanks = [g.PS[3 + 2 * (it % 2)], g.PS[4 + 2 * (it % 2)]]
                    Of = Ofs[(it % 2) * 2 + s] if npass == 2 else Ofs[it % 4]
                    for ki, kt in enumerate(key_tiles):
                        S = g.PS[si % 3]
                        E = Es[si % 3]
                        si += 1
                        for ci, (qf, kf, bufs) in enumerate(chunks):
                            kb.op("pe", lambda e, qf=qf, kf=kf, ci=ci, S=S, kt=kt: e.matmul(
                                S[:, 0:q_n], lhsT=kf(kt), rhs=qf(q_lo, q_n), start=(ci == 0), stop=(ci == len(chunks) - 1)),
                                bufs, [S])
                        kb.op("act", lambda e, S=S, E=E: e.activation(out=E[:, 0:q_n], in_=S[:, 0:q_n], func=AF.Exp, scale=scale), [S], [E])
                        for sub in range(nsub):
                            ob = obanks[sub // 2]
                            c0 = (sub % 2) * (dv + 1)
                            kb.op("pe", lambda e, ob=ob, c0=c0, E=E, sub=sub, kt=kt, ki=ki: e.matmul(
                                ob[:, c0:c0 + dv + 1], lhsT=E[:, sub * 128:(sub + 1) * 128], rhs=V[:, kt, h, :],
                                start=(ki == 0 and sub % 2 == 0), stop=(ki == len(key_tiles) - 1), skip_group_check=True),
                                [E, V], [ob])
                    for bi in range((nsub + 1) // 2):
                        nb = min(2, nsub - bi * 2)
                        ob = obanks[bi]
                        src = ob[:, 0:nb * (dv + 1)].rearrange("p (s d) -> p s d", s=nb)
                        kb.op("dve", lambda e, Of=Of, bi=bi, nb=nb, src=src: e.tensor_copy(out=Of[:, bi * 2:bi * 2 + nb, :], in_=src), [ob], [Of])
                    ofl.append(Of)
                    if npass == 1:
                        it += 1
                if npass == 2:
                    it += 1
                post(h, q_lo, q_n, ofl)


def phase_mixer_a(g, l, ctx_out):
    nc, kb = g.nc, g.kb
    dma = lambda fn, r=(), w=(): kb.dma("sp", fn, r, w)
    lam_init = 0.8 - 0.6 * math.exp(-0.3 * l)
    Pv = g.P.t.rearrange("(t p) c -> p t c", p=128)
    OBv = g.OB.t.rearrange("(t p) c -> p t c", p=128)
    with nc.sbuf_tensor(_nm(), [128, 8, T], BF16) as qkt_t, nc.sbuf_tensor(_nm(), [128, NT, 4, 129], BF16) as va_t, \
            nc.sbuf_tensor(_nm(), [128, NT, 64], F32) as cs_t, nc.sbuf_tensor(_nm(), [128, 8], F32) as lam_t, \
            nc.sbuf_tensor(_nm(), [128, 128], F32) as ng_t:
        QKT = Buf(qkt_t, "QKT"); VA = Buf(va_t, "VA"); CS = Buf(cs_t, "CS"); LAM = Buf(lam_t, "LAM"); NG = Buf(ng_t, "NG")
        dma(lambda e: e.dma_start(out=CS[:], in_=g.rope_cs.rearrange("(t p) c -> p t c", p=128)), [], [CS])
        for tt in range(NT):
            dma(lambda e, tt=tt: e.dma_start(out=VA[:, tt, :, 0:128], in_=Pv[:, tt, 1024:1536].rearrange("p (h d) -> p h d", h=4)), [g.P], [VA])
        kb.op("pool", lambda e: e.memset(VA[:, :, :, 128:129], 1.0), [], [VA])
        with nc.sbuf_tensor(_nm(), [128, 4, 64], F32) as lv_t:
            LV = Buf(lv_t, "LV")
            for i, nm in enumerate(["diff_lam_q1", "diff_lam_k1", "diff_lam_q2", "diff_lam_k2"]):
                dma(lambda e, i=i, nm=nm: e.dma_start(out=LV[:, i, :], in_=g.W[nm][l:l + 1, :].partition_broadcast(128)), [], [LV])
            kb.op("dve", lambda e: e.tensor_tensor(out=LV[:, 0, :], in0=LV[:, 0, :], in1=LV[:, 1, :], op=ALU.mult), [LV], [LV])
            kb.op("dve", lambda e: e.tensor_tensor(out=LV[:, 2, :], in0=LV[:, 2, :], in1=LV[:, 3, :], op=ALU.mult), [LV], [LV])
            kb.op("dve", lambda e: e.tensor_reduce(out=LAM[:, 0:1], in_=LV[:, 0, :], axis=AX.X, op=ALU.add), [LV], [LAM])
            kb.op("dve", lambda e: e.tensor_reduce(out=LAM[:, 1:2], in_=LV[:, 2, :], axis=AX.X, op=ALU.add), [LV], [LAM])
            kb.op("act", lambda e: e.activation(out=LAM[:, 2:4], in_=LAM[:, 0:2], func=AF.Exp), [LAM], [LAM])
            kb.op("dve", lambda e: e.tensor_tensor(out=LAM[:, 4:5], in0=LAM[:, 2:3], in1=LAM[:, 3:4], op=ALU.subtract), [LAM], [LAM])
            kb.op("dve", lambda e: e.tensor_scalar(out=LAM[:, 5:6], in0=LAM[:, 4:5], scalar1=lam_init, scalar2=None, op0=ALU.add), [LAM], [LAM])
            dma(lambda e: e.dma_start(out=NG[:], in_=g.W["diff_norm_g"][l:l + 1, :].partition_broadcast(128)), [], [NG])
            kb.op("dve", lambda e: e.tensor_scalar(out=NG[:], in0=NG[:], scalar1=1.0 - lam_init, scalar2=None, op0=ALU.mult), [NG], [NG])
            kb.barrier()
        lam = LAM[:, 5:6]
        with nc.sbuf_tensor(_nm(), [128, 2, 1024], BF16) as pa_t, nc.sbuf_tensor(_nm(), [128, 2, 1024], BF16) as rb_t, \
                nc.sbuf_tensor(_nm(), [128, 4, 512], F32) as tm_t:
            pas = [Buf(pa_t[:, i], "pa%d" % i) for i in range(2)]
            rbs = [Buf(rb_t[:, i], "rb%d" % i) for i in range(2)]
            tms = [Buf(tm_t[:, i], "tm%d" % i) for i in range(4)]
            for tt in range(NT):
                pa = pas[tt % 2]; rb = rbs[tt % 2]
                dma(lambda e, pa=pa, tt=tt: e.dma_start(out=pa[:], in_=Pv[:, tt, 0:1024]), [g.P], [pa])
                rope_tile(g, pa, rb, CS, tt, 16, tms)
                ps = g.PS[7]
                psb = ps[:].bitcast(BF16)
                for c in range(8):
                    kb.op("pe", lambda e, c=c, rb=rb, psb=psb: e.transpose(out=psb[:, c * 128:(c + 1) * 128], in_=rb[:, c * 128:(c + 1) * 128], identity=g.ident_b[:]), [rb, g.ident_b], [ps])
                kb.op("act", lambda e, tt=tt, psb=psb: e.activation(out=QKT[:, :, tt * 128:(tt + 1) * 128], in_=psb.rearrange("p (k t) -> p k t", k=8), func=AF.Copy), [ps], [QKT])
            kb.barrier()

        def qk_chunks(h, s):
            p0 = (h % 2) * 64
            qc = s * 2 + h // 2
            kc = 4 + s * 2 + h // 2
            return [(lambda lo, n: QKT[p0:p0 + 64, qc, lo:lo + n], lambda kt: QKT[p0:p0 + 64, kc, kt * 128:(kt + 1) * 128], [QKT])]

        with nc.sbuf_tensor(_nm(), [128, 2, 8, 4], F32) as r_t, nc.sbuf_tensor(_nm(), [128, 2, 3, 4, 128], F32) as w_t, \
                nc.sbuf_tensor(_nm(), [128, 2, 4, 128], BF16) as o_t:
            cnt = [0]
            rs_ = [Buf(r_t[:, i], "r") for i in range(2)]; obs_ = [Buf(o_t[:, i], "ob") for i in range(2)]
            ws_ = [[Buf(w_t[:, i, j], "w%d" % j) for j in range(3)] for i in range(2)]

            def post(h, q_lo, q_n, ofl):
                O1, O2 = ofl
                i = cnt[0] % 2
                cnt[0] += 1
                ns = q_n // 128
                r = rs_[i]; w0, w1, w2 = ws_[i]; ob = obs_[i]
                bc = lambda ap: ap.unsqueeze(2).to_broadcast([128, ns, 128])
                kb.op("dve", lambda e: e.reciprocal(out=r[:, 0, 0:ns], in_=O1[:, 0:ns, 128]), [O1], [r])
                kb.op("dve", lambda e: e.reciprocal(out=r[:, 1, 0:ns], in_=O2[:, 0:ns, 128]), [O2], [r])
                kb.op("dve", lambda e: e.tensor_scalar(out=r[:, 2, 0:ns], in0=r[:, 1, 0:ns], scalar1=lam, scalar2=None, op0=ALU.mult), [r, LAM], [r])
                kb.op("dve", lambda e: e.tensor_tensor(out=w0[:, 0:ns, :], in0=O1[:, 0:ns, 0:128], in1=bc(r[:, 0, 0:ns]), op=ALU.mult), [O1, r], [w0])
                kb.op("dve", lambda e: e.tensor_tensor(out=w1[:, 0:ns, :], in0=O2[:, 0:ns, 0:128], in1=bc(r[:, 2, 0:ns]), op=ALU.mult), [O2, r], [w1])
                kb.op("dve", lambda e: e.tensor_tensor(out=w0[:, 0:ns, :], in0=w0[:, 0:ns, :], in1=w1[:, 0:ns, :], op=ALU.subtract), [w0, w1], [w0])
                kb.op("pool", lambda e: e.tensor_tensor(out=w2[:, 0:ns, :], in0=w0[:, 0:ns, :], in1=w0[:, 0:ns, :], op=ALU.mult), [w0], [w2])
                kb.op("dve", lambda e: e.tensor_reduce(out=r[:, 3, 0:ns], in_=w2[:, 0:ns, :], axis=AX.X, op=ALU.add), [w2], [r])
                kb.op("dve", lambda e: e.tensor_scalar(out=r[:, 4, 0:ns], in0=r[:, 3, 0:ns], scalar1=1.0 / 128, scalar2=EPS, op0=ALU.mult, op1=ALU.add), [r], [r])
                kb.op("act", lambda e: e.activation(out=r[:, 5, 0:ns], in_=r[:, 4, 0:ns], func=AF.Sqrt), [r], [r])
                kb.op("dve", lambda e: e.reciprocal(out=r[:, 6, 0:ns], in_=r[:, 5, 0:ns]), [r], [r])
                kb.op("dve", lambda e: e.tensor_tensor(out=w1[:, 0:ns, :], in0=w0[:, 0:ns, :], in1=bc(r[:, 6, 0:ns]), op=ALU.mult), [w0, r], [w1])
                kb.op("pool", lambda e: e.tensor_tensor(out=ob[:, 0:ns, :], in0=w1[:, 0:ns, :], in1=NG[:].unsqueeze(1).to_broadcast([128, ns, 128]), op=ALU.mult), [w1, NG], [ob])
                t0 = q_lo // 128
                dma(lambda e: e.dma_start(out=OBv[:, t0:t0 + ns, h * 128:(h + 1) * 128], in_=ob[:, 0:ns, :]), [ob], [g.OB])

            groups = [(i * 512, 512, list(range(NT))) for i in range(4)]
            if ctx_out:
                groups.append((2048, 256, [16, 17]))
            attention(g, 4, 128, 1.0 / 8.0, VA, qk_chunks, groups, post, npass=2)
            kb.barrier()


def rope_tile(g, pa, rb, CS, tt, ng, tms, col0=0):
    kb = g.kb
    n = ng * 64
    xv = pa[:, col0:col0 + n].rearrange("p (g a f j) -> p g a f j", g=ng, a=2, f=2, j=16)
    ov = rb[:, col0:col0 + n].rearrange("p (g a f j) -> p g a f j", g=ng, a=2, f=2, j=16)
    x1, x2 = xv[:, :, :, 0, :], xv[:, :, :, 1, :]
    cosb = CS[:, tt, 0:32].rearrange("p (a j) -> p a j", a=2).unsqueeze(1).to_broadcast([128, ng, 2, 16])
    sinb = CS[:, tt, 32:64].rearrange("p (a j) -> p a j", a=2).unsqueeze(1).to_broadcast([128, ng, 2, 16])
    tv = [t[:, 0:ng * 32].rearrange("p (g a j) -> p g a j", g=ng, a=2) for t in tms]
    kb.op("dve", lambda e: e.tensor_tensor(out=tv[0], in0=x1, in1=cosb, op=ALU.mult), [pa, CS], [tms[0]])
    kb.op("pool", lambda e: e.tensor_tensor(out=tv[1], in0=x2, in1=sinb, op=ALU.mult), [pa, CS], [tms[1]])
    kb.op("dve", lambda e: e.tensor_tensor(out=tv[2], in0=x1, in1=sinb, op=ALU.mult), [pa, CS], [tms[2]])
    kb.op("pool", lambda e: e.tensor_tensor(out=tv[3], in0=x2, in1=cosb, op=ALU.mult), [pa, CS], [tms[3]])
    kb.op("dve", lambda e: e.tensor_tensor(out=ov[:, :, :, 0, :], in0=tv[0], in1=tv[1], op=ALU.subtract), [tms[0], tms[1]], [rb])
    kb.op("pool", lambda e: e.tensor_tensor(out=ov[:, :, :, 1, :], in0=tv[2], in1=tv[3], op=ALU.add), [tms[2], tms[3]], [rb])


def small_rms(g, src_ap, src_buf, n, nh, r, col):
    kb = g.kb
    return None


def phase_mixer_b(g, l, ctx_out):
    nc, kb = g.nc, g.kb
    dma = lambda fn, r=(), w=(): kb.dma("sp", fn, r, w)
    Pv = g.P.t.rearrange("(t p) c -> p t c", p=128)
    OBv = g.OB.t.rearrange("(t p) c -> p t c", p=128)
    TQ = T if ctx_out else NX
    with nc.sbuf_tensor(_nm(), [128, 4, T], BF16) as ft_t, nc.sbuf_tensor(_nm(), [128, 4, T], BF16) as qnt_t, \
            nc.sbuf_tensor(_nm(), [128, 4, T], BF16) as knt_t, nc.sbuf_tensor(_nm(), [128, 2, T], BF16) as qrt_t, \
            nc.sbuf_tensor(_nm(), [128, NT, 4, 129], BF16) as vb_t:
        FT = Buf(ft_t, "FT"); QNT = Buf(qnt_t, "QNT"); KNT = Buf(knt_t, "KNT"); QRT = Buf(qrt_t, "QRT"); VB = Buf(vb_t, "VB")
        kb.op("pool", lambda e: e.memset(VB[:, :, :, 128:129], 1.0), [], [VB])
        with nc.sbuf_tensor(_nm(), [128, 2, 4, 128], BF16) as wqn_t, nc.sbuf_tensor(_nm(), [128, 2, 4, 64], BF16) as wqr_t, \
                nc.sbuf_tensor(_nm(), [128, 4, 128], BF16) as wkn_t, nc.sbuf_tensor(_nm(), [128, 4, 128], BF16) as wv_t, \
                nc.sbuf_tensor(_nm(), [128, 384], F32) as gn_t, nc.sbuf_tensor(_nm(), [128, NT, 64], F32) as cs_t:
            WQN = Buf(wqn_t, "WQN"); WQR = Buf(wqr_t, "WQR"); WKN = Buf(wkn_t, "WKN"); WV = Buf(wv_t, "WV"); GN = Buf(gn_t, "GN"); CS = Buf(cs_t, "CS")
            dma(lambda e: e.dma_start(out=CS[:], in_=g.rope_cs.rearrange("(t p) c -> p t c", p=128)), [], [CS])
            wuq = g.W["mla_w_uq"][l].rearrange("(c p) (h d) -> p c h d", p=128, h=4)
            wukv = g.W["mla_w_ukv"][l].rearrange("p (h d) -> p h d", h=4)
            for c in range(2):
                kb.dma("pool", lambda e, c=c: e.dma_start(out=WQN[:, c], in_=wuq[:, c, :, 0:128]), [], [WQN])
                kb.dma("pool", lambda e, c=c: e.dma_start(out=WQR[:, c], in_=wuq[:, c, :, 128:192]), [], [WQR])
            kb.dma("pool", lambda e: e.dma_start(out=WKN[:], in_=wukv[:, :, 0:128]), [], [WKN])
            kb.dma("pool", lambda e: e.dma_start(out=WV[:], in_=wukv[:, :, 128:256]), [], [WV])
            dma(lambda e: e.dma_start(out=GN[:, 0:256], in_=g.W["mla_qnorm_g"][l:l + 1, :].partition_broadcast(128)), [], [GN])
            dma(lambda e: e.dma_start(out=GN[:, 256:384], in_=g.W["mla_kvnorm_g"][l:l + 1, :].partition_broadcast(128)), [], [GN])
            with nc.sbuf_tensor(_nm(), [128, 2, 448], BF16) as pb_t, nc.sbuf_tensor(_nm(), [128, 2, 512], BF16) as tb_t, \
                    nc.sbuf_tensor(_nm(), [128, 2, 512], F32) as jk_t, nc.sbuf_tensor(_nm(), [128, 2, 8], F32) as st_t, \
                    nc.sbuf_tensor(_nm(), [128, 4, 128], F32) as tm_t, nc.sbuf_tensor(_nm(), [128, 2, 64], BF16) as kr_t:
                tms = [Buf(tm_t[:, i], "tm%d" % i) for i in range(4)]
                pbs = [Buf(pb_t[:, i], "pb") for i in range(2)]; tbs = [Buf(tb_t[:, i], "tb") for i in range(2)]
                jks = [Buf(jk_t[:, i], "jk") for i in range(2)]; sts = [Buf(st_t[:, i], "st") for i in range(2)]
                for tt in range(NT):
                    i = tt % 2
                    pb = pbs[i]; tb = tbs[i]; jk = jks[i]; st = sts[i]
                    dma(lambda e, pb=pb, tt=tt: e.dma_start(out=pb[:], in_=Pv[:, tt, 1536:1984]), [g.P], [pb])
                    kb.op("act", lambda e: e.activation(out=jk[:, 0:256], in_=pb[:, 0:256], func=AF.Square, accum_out=st[:, 0:1]), [pb], [jk, st])
                    kb.op("act", lambda e: e.activation(out=jk[:, 256:384], in_=pb[:, 256:384], func=AF.Square, accum_out=st[:, 1:2]), [pb], [jk, st])
                    kb.op("dve", lambda e: e.tensor_scalar(out=st[:, 2:3], in0=st[:, 0:1], scalar1=1.0 / 256, scalar2=EPS, op0=ALU.mult, op1=ALU.add), [st], [st])
                    kb.op("dve", lambda e: e.tensor_scalar(out=st[:, 3:4], in0=st[:, 1:2], scalar1=1.0 / 128, scalar2=EPS, op0=ALU.mult, op1=ALU.add), [st], [st])
                    kb.op("act", lambda e: e.activation(out=st[:, 4:6], in_=st[:, 2:4], func=AF.Sqrt), [st], [st])
                    kb.op("dve", lambda e: e.reciprocal(out=st[:, 6:8], in_=st[:, 4:6]), [st], [st])
                    kb.op("dve", lambda e: e.scalar_tensor_tensor(out=tb[:, 0:256], in0=pb[:, 0:256], scalar=st[:, 6:7], in1=GN[:, 0:256], op0=ALU.mult, op1=ALU.mult), [pb, st, GN], [tb])
                    kb.op("dve", lambda e: e.scalar_tensor_tensor(out=tb[:, 256:384], in0=pb[:, 256:384], scalar=st[:, 7:8], in1=GN[:, 256:384], op0=ALU.mult, op1=ALU.mult), [pb, st, GN], [tb])
                    rope_tile(g, pb, tb, CS, tt, 1, tms, col0=384)
                    kb.op("pool", lambda e: e.tensor_copy(out=tb[:, 448:512], in_=tb[:, 384:448]), [tb], [tb])
                    ps = g.PS[7]
                    psb = ps[:].bitcast(BF16)
                    for c in range(4):
                        kb.op("pe", lambda e, c=c, tb=tb, psb=psb: e.transpose(out=psb[:, c * 128:(c + 1) * 128], in_=tb[:, c * 128:(c + 1) * 128], identity=g.ident_b[:]), [tb, g.ident_b], [ps])
                    kb.op("act", lambda e, tt=tt, psb=psb: e.activation(out=FT[:, :, tt * 128:(tt + 1) * 128], in_=psb[:, 0:512].rearrange("p (k t) -> p k t", k=4), func=AF.Copy), [ps], [FT])
                kb.barrier()
            pi = 0
            ngr = (T + 511) // 512
            for gi in range(ngr):
                lo = gi * 512
                n = min(512, T - lo)
                for h in range(4):
                    ps = g.PS[pi % 4]; pi += 1
                    for c in range(2):
                        kb.op("pe", lambda e, c=c, h=h, ps=ps, lo=lo, n=n: e.matmul(ps[:, 0:n], lhsT=WQN[:, c, h, :], rhs=FT[:, c, lo:lo + n], start=(c == 0), stop=(c == 1)), [WQN, FT], [ps])
                    kb.op("act", lambda e, h=h, ps=ps, lo=lo, n=n: e.activation(out=QNT[:, h, lo:lo + n], in_=ps[:, 0:n], func=AF.Copy), [ps], [QNT])
                    ps = g.PS[pi % 4]; pi += 1
                    kb.op("pe", lambda e, h=h, ps=ps, lo=lo, n=n: e.matmul(ps[:, 0:n], lhsT=WKN[:, h, :], rhs=FT[:, 2, lo:lo + n], start=True, stop=True), [WKN, FT], [ps])
                    kb.op("dve", lambda e, h=h, ps=ps, lo=lo, n=n: e.tensor_copy(out=KNT[:, h, lo:lo + n], in_=ps[:, 0:n]), [ps], [KNT])
            with nc.sbuf_tensor(_nm(), [128, 2, 256], BF16) as qr_t, nc.sbuf_tensor(_nm(), [128, 2, 256], BF16) as qq_t, \
                    nc.sbuf_tensor(_nm(), [128, 4, 128], F32) as tm_t:
                tms = [Buf(tm_t[:, i], "tm%d" % i) for i in range(4)]
                qrs = [Buf(qr_t[:, i], "qr") for i in range(2)]; qqs = [Buf(qq_t[:, i], "qq") for i in range(2)]
                for tt in range(NT):
                    i = tt % 2
                    qr = qrs[i]; qq = qqs[i]
                    ps = g.PS[pi % 4]; pi += 1
                    kb.op("pe", lambda e, ps=ps, tt=tt: e.matmul(ps[:, 0:512], lhsT=FT[:, 2, tt * 128:(tt + 1) * 128], rhs=WV[:].rearrange("p h d -> p (h d)"), start=True, stop=True), [FT, WV], [ps])
                    kb.op("act", lambda e, ps=ps, tt=tt: e.activation(out=VB[:, tt, :, 0:128], in_=ps[:, 0:512].rearrange("p (h d) -> p h d", h=4), func=AF.Copy), [ps], [VB])
                    ps = g.PS[pi % 4]; pi += 1
                    for c in range(2):
                        kb.op("pe", lambda e, c=c, ps=ps, tt=tt: e.matmul(ps[:, 0:256], lhsT=FT[:, c, tt * 128:(tt + 1) * 128], rhs=WQR[:, c].rearrange("p h d -> p (h d)"), start=(c == 0), stop=(c == 1)), [FT, WQR], [ps])
                    kb.op("act", lambda e, ps=ps, qq=qq: e.activation(out=qq[:], in_=ps[:, 0:256], func=AF.Copy), [ps], [qq])
                    rope_tile(g, qq, qr, CS, tt, 4, tms)
                    ps = g.PS[7]
                    psb = ps[:].bitcast(BF16)
                    for c in range(2):
                        kb.op("pe", lambda e, c=c, qr=qr, psb=psb: e.transpose(out=psb[:, c * 128:(c + 1) * 128], in_=qr[:, c * 128:(c + 1) * 128], identity=g.ident_b[:]), [qr, g.ident_b], [ps])
                    kb.op("dve", lambda e, tt=tt, psb=psb: e.tensor_copy(out=QRT[:, :, tt * 128:(tt + 1) * 128], in_=psb[:, 0:256].rearrange("p (k t) -> p k t", k=2)), [ps], [QRT])
                kb.barrier()

        if "dbgB" in g.debug:
            dft = nc.dram_tensor("dbgFT", [128, 4, T], BF16, kind="ExternalOutput").ap()
            dqr = nc.dram_tensor("dbgQRT", [128, 2, T], BF16, kind="ExternalOutput").ap()
            dqn = nc.dram_tensor("dbgQNT", [128, 4, T], BF16, kind="ExternalOutput").ap()
            dkn = nc.dram_tensor("dbgKNT", [128, 4, T], BF16, kind="ExternalOutput").ap()
            dvb = nc.dram_tensor("dbgVB", [128, NT, 4, 129], BF16, kind="ExternalOutput").ap()
            dma(lambda e: e.dma_start(out=dft[:], in_=FT[:]), [FT], [])
            dma(lambda e: e.dma_start(out=dqr[:], in_=QRT[:]), [QRT], [])
            dma(lambda e: e.dma_start(out=dqn[:], in_=QNT[:]), [QNT], [])
            dma(lambda e: e.dma_start(out=dkn[:], in_=KNT[:]), [KNT], [])
            dma(lambda e: e.dma_start(out=dvb[:], in_=VB[:]), [VB], [])

        def qk_chunks(h, s):
            p0 = (h % 2) * 64
            return [(lambda lo, n: QNT[:, h, lo:lo + n], lambda kt: KNT[:, h, kt * 128:(kt + 1) * 128], [QNT, KNT]),
                    (lambda lo, n: QRT[p0:p0 + 64, h // 2, lo:lo + n], lambda kt: FT[p0:p0 + 64, 3, kt * 128:(kt + 1) * 128], [QRT, FT])]

        with nc.sbuf_tensor(_nm(), [128, 2, 4], F32) as r_t, nc.sbuf_tensor(_nm(), [128, 2, 4, 128], BF16) as o_t:
            cnt = [0]
            rs_ = [Buf(r_t[:, i], "r") for i in range(2)]; obs_ = [Buf(o_t[:, i], "ob") for i in range(2)]

            def post(h, q_lo, q_n, ofl):
                O1 = ofl[0]
                i = cnt[0] % 2
                cnt[0] += 1
                ns = q_n // 128
                r = rs_[i]; ob = obs_[i]
                kb.op("dve", lambda e: e.reciprocal(out=r[:, 0:ns], in_=O1[:, 0:ns, 128]), [O1], [r])
                kb.op("dve", lambda e: e.tensor_tensor(out=ob[:, 0:ns, :], in0=O1[:, 0:ns, 0:128], in1=r[:, 0:ns].unsqueeze(2).to_broadcast([128, ns, 128]), op=ALU.mult), [O1, r], [ob])
                t0 = q_lo // 128
                dma(lambda e: e.dma_start(out=OBv[:, t0:t0 + ns, 512 + h * 128:512 + (h + 1) * 128], in_=ob[:, 0:ns, :]), [ob], [g.OB])

            groups = [(i * 512, 512, list(range(NT))) for i in range(4)]
            if ctx_out:
                groups.append((2048, 256, [16, 17]))
            attention(g, 4, 128, 192.0 ** -0.5, VB, qk_chunks, groups, post, npass=1)
            kb.barrier()


def phase_mixer_d(g, l, ctx_out):
    nc, kb = g.nc, g.kb
    dma = lambda fn, r=(), w=(): kb.dma("sp", fn, r, w)
    Pv = g.P.t.rearrange("(t p) c -> p t c", p=128)
    OBv = g.OB.t.rearrange("(t p) c -> p t c", p=128)
    scale = 1.0 / 8.0
    with nc.sbuf_tensor(_nm(), [128, 6, T], BF16) as qkd_t, nc.sbuf_tensor(_nm(), [128, NT, 2, 65], BF16) as vd_t, \
            nc.sbuf_tensor(_nm(), [128, 8], F32) as es_t, nc.sbuf_tensor(_nm(), [128, 2, 128], BF16) as mk_t, \
            nc.sbuf_tensor(_nm(), [128, 2, 128], F32) as mkf_t:
        QKD = Buf(qkd_t, "QKD"); VD = Buf(vd_t, "VD"); ES = Buf(es_t, "ES"); MK = Buf(mk_t, "MK"); MKF = Buf(mkf_t, "MKF")
        kb.op("pool", lambda e: e.memset(VD[:, :, :, 64:65], 1.0), [], [VD])
        for tt in range(NT):
            dma(lambda e, tt=tt: e.dma_start(out=VD[:, tt, :, 0:64], in_=Pv[:, tt, 5184:5312].rearrange("p (h d) -> p h d", h=2)), [g.P], [VD])
        dma(lambda e: e.dma_start(out=MKF[:, 0, :], in_=g.mprev_in[:, :]), [], [MKF])
        dma(lambda e: e.dma_start(out=MKF[:, 1, :], in_=g.mnext_in[:, :]), [], [MKF])
        kb.op("dve", lambda e: e.tensor_copy(out=MK[:], in_=MKF[:]), [MKF], [MK])
        dma(lambda e: e.dma_start(out=ES[:], in_=g.W["win_sink"][l:l + 1, :].partition_broadcast(128)), [], [ES])
        kb.op("act", lambda e: e.activation(out=ES[:], in_=ES[:], func=AF.Exp), [ES], [ES])
        with nc.sbuf_tensor(_nm(), [128, 2, 640], BF16) as pd_t, nc.sbuf_tensor(_nm(), [128, 2, 768], BF16) as rb_t, \
                nc.sbuf_tensor(_nm(), [128, 4, 320], F32) as tm_t, nc.sbuf_tensor(_nm(), [128, NT, 64], F32) as cs_t:
            CS = Buf(cs_t, "CS")
            dma(lambda e: e.dma_start(out=CS[:], in_=g.rope_cs.rearrange("(t p) c -> p t c", p=128)), [], [CS])
            pds = [Buf(pd_t[:, i], "pd%d" % i) for i in range(2)]
            rbs = [Buf(rb_t[:, i], "rb%d" % i) for i in range(2)]
            tms = [Buf(tm_t[:, i], "tm%d" % i) for i in range(4)]
            for tt in range(NT if "d_skip_rope" not in g.debug else 0):
                pd = pds[tt % 2]; rb = rbs[tt % 2]
                dma(lambda e, pd=pd, tt=tt: e.dma_start(out=pd[:], in_=Pv[:, tt, 4544:5184]), [g.P], [pd])
                rope_tile(g, pd, rb, CS, tt, 10, tms)
                kb.op("dve", lambda e, rb=rb: e.tensor_copy(out=rb[:, 640:768].rearrange("p (u d) -> p u d", u=2),
                                                           in_=rb[:, 576:640].unsqueeze(1).to_broadcast([128, 2, 64])), [rb], [rb])
                kb.op("dve", lambda e, rb=rb: e.tensor_copy(out=rb[:, 576:640], in_=rb[:, 512:576]), [rb], [rb])
                ps = g.PS[7]
                psb = ps[:].bitcast(BF16)
                for c in range(6):
                    kb.op("pe", lambda e, c=c, rb=rb, psb=psb: e.transpose(out=psb[:, c * 128:(c + 1) * 128], in_=rb[:, c * 128:(c + 1) * 128], identity=g.ident_b[:]), [rb, g.ident_b], [ps])
                kb.op("act", lambda e, tt=tt, psb=psb: e.activation(out=QKD[:, :, tt * 128:(tt + 1) * 128], in_=psb[:, 0:768].rearrange("p (k t) -> p k t", k=6), func=AF.Copy), [ps], [QKD])
            kb.barrier()
        with nc.sbuf_tensor(_nm(), [128, 3, 512], BF16) as E_t, nc.sbuf_tensor(_nm(), [128, 2, 8, 65], F32) as of_t, \
                nc.sbuf_tensor(_nm(), [128, 2, 2, 8], F32) as r_t, nc.sbuf_tensor(_nm(), [128, 2, 8, 64], BF16) as o_t:
            Es = [Buf(E_t[:, i], "E%d" % i) for i in range(3)]
            Ofs = [Buf(of_t[:, i], "Of%d" % i) for i in range(2)]
            rs_ = [Buf(r_t[:, i], "r%d" % i) for i in range(2)]
            obs_ = [Buf(o_t[:, i], "ob%d" % i) for i in range(2)]
            si = 0
            qtiles = list(range(16)) + ([16, 17] if ctx_out else [])
            if "d_prep_only" in g.debug:
                qtiles = []
            for qn_, qi in enumerate(qtiles):
                if qi < 16:
                    kts = ([(qi - 1, 0)] if qi > 0 else []) + [(qi, None)] + ([(qi + 1, 1)] if qi < 15 else []) + [(16, None), (17, None)]
                else:
                    kts = [(16, None), (17, None)]
                Of = Ofs[qn_ % 2]; r = rs_[qn_ % 2]; ob = obs_[qn_ % 2]
                for kvh in range(2):
                    O = g.PS[3 + (qn_ * 2 + kvh) % 4]
                    for ki, (kt, mk) in enumerate(kts):
                        Sp = [(g.PS[0], g.PS[1]), (g.PS[2], g.PS[7])][si % 2]
                        E = Es[si % 3]; si += 1
                        for gq in range(4):
                            hq = kvh * 4 + gq
                            p0 = (hq % 2) * 64
                            S = Sp[gq % 2]
                            c0 = (gq // 2) * 128
                            kb.op("pe", lambda e, S=S, c0=c0, p0=p0, kvh=kvh, kt=kt, hq=hq, qi=qi: e.matmul(
                                S[:, c0:c0 + 128], lhsT=QKD[p0:p0 + 64, 4 + kvh, kt * 128:(kt + 1) * 128],
                                rhs=QKD[p0:p0 + 64, hq // 2, qi * 128:(qi + 1) * 128], start=True, stop=True, skip_group_check=True), [QKD], [S])
                        for j in range(2):
                            kb.op("act", lambda e, j=j, E=E, Sp=Sp: e.activation(out=E[:, j * 256:(j + 1) * 256], in_=Sp[j][:, 0:256], func=AF.Exp, scale=scale), [Sp[j]], [E])
                        if mk is not None and "d_no_mask" not in g.debug:
                            kb.op("dve", lambda e, E=E, mk=mk: e.tensor_tensor(out=E[:].rearrange("p (g q) -> p g q", g=4), in0=E[:].rearrange("p (g q) -> p g q", g=4),
                                                                             in1=MKF[:, mk, :].unsqueeze(1).to_broadcast([128, 4, 128]), op=ALU.mult), [E, MKF], [E])
                        for gq in range(4 if "d_no_pv" not in g.debug else 0):
                            ei = (gq % 2) * 2 + gq // 2
                            kb.op("pe", lambda e, O=O, gq=gq, ei=ei, E=E, kt=kt, kvh=kvh, ki=ki: e.matmul(
                                O[:, gq * 65:(gq + 1) * 65], lhsT=E[:, ei * 128:(ei + 1) * 128], rhs=VD[:, kt, kvh, :],
                                start=(ki == 0 and gq == 0), stop=(ki == len(kts) - 1), skip_group_check=True), [E, VD], [O])
                    kb.op("act", lambda e, O=O, Of=Of, kvh=kvh: e.activation(out=Of[:, kvh * 4:(kvh + 1) * 4, :], in_=O[:, 0:260].rearrange("p (g d) -> p g d", g=4), func=AF.Copy), [O], [Of])
                kb.op("dve", lambda e, Of=Of, r=r: e.tensor_tensor(out=r[:, 0, :], in0=Of[:, :, 64], in1=ES[:], op=ALU.add), [Of, ES], [r])
                kb.op("dve", lambda e, r=r: e.reciprocal(out=r[:, 1, :], in_=r[:, 0, :]), [r], [r])
                kb.op("dve", lambda e, Of=Of, r=r, ob=ob: e.tensor_tensor(out=ob[:], in0=Of[:, :, 0:64], in1=r[:, 1, :].unsqueeze(2).to_broadcast([128, 8, 64]), op=ALU.mult), [Of, r], [ob])
                dma(lambda e, ob=ob, qi=qi: e.dma_start(out=OBv[:, qi, 1536:2048], in_=ob[:].rearrange("p h d -> p (h d)")), [ob], [g.OB])
            kb.barrier()


def phase_mixer_c(g, l, ctx_out):
    nc, kb = g.nc, g.kb
    dma = lambda fn, r=(), w=(): kb.dma("sp", fn, r, w)
    NCH = T // 64
    fwd_order = [32, 33, 34, 35] + list(range(32))
    bwd_order = [35, 34, 33, 32] + list(range(31, -1, -1))
    from contextlib import ExitStack
    with ExitStack() as es:
        al = lambda shape, dt: es.enter_context(nc.sbuf_tensor(_nm(), shape, dt))
        tri_t = al([64, 4, 64], F32); lb_t = al([64, 2, 2, 512], F32); ng_t = al([64, 128], F32)
        S_t = al([128, 4, 128], F32); Sb_t = al([128, 4, 128], BF16)
        TRI = Buf(tri_t, "TRI"); LB = Buf(lb_t, "LB"); NGh = Buf(ng_t, "NGh"); S = Buf(S_t, "S"); Sb = Buf(Sb_t, "Sb")
        dma(lambda e: e.dma_start(out=TRI[:], in_=g.tri_in.rearrange("p (a t) -> p a t", a=4)), [], [TRI])
        dma(lambda e: e.dma_start(out=NGh[:], in_=g.W["hgrn_norm_g"][l:l + 1, :].partition_broadcast(64)), [], [NGh])
        if l == 0:
            kb.op("dve", lambda e: e.memset(LB[:, :, 0, :], 0.0), [], [LB])
            kb.op("dve", lambda e: e.memset(LB[:, :, 1, :], 1.0), [], [LB])
        else:
            for d in range(2):
                dma(lambda e, d=d: e.dma_start(out=LB[:, d, 0, :], in_=g.W["hgrn_lb"][d, 1:2, :].partition_broadcast(64)), [], [LB])
                dma(lambda e, d=d: e.dma_start(out=LB[:, d, 1, :], in_=g.W["hgrn_lb"][d, 0:1, :].partition_broadcast(64)), [], [LB])
                kb.op("dve", lambda e, d=d: e.tensor_tensor(out=LB[:, d, 0, :], in0=LB[:, d, 0, :], in1=LB[:, d, 1, :], op=ALU.subtract), [LB], [LB])
                kb.op("act", lambda e, d=d: e.activation(out=LB[:, d, 0, :], in_=LB[:, d, 0, :], func=AF.Sigmoid), [LB], [LB])
                kb.op("dve", lambda e, d=d: e.tensor_scalar(out=LB[:, d, 1, :], in0=LB[:, d, 0, :], scalar1=-1.0, scalar2=1.0, op0=ALU.mult, op1=ALU.add), [LB], [LB])
        if True:
            pc_t = al([64, 2, 2560], BF16); f_t = al([64, 2, 512], F32); lf_t = al([64, 2, 512], F32); kf_t = al([64, 2, 512], BF16)
            bm_t = al([128, 2, 256], F32); e12_t = al([128, 2, 2, 256], F32); em_t = al([128, 2, 8], F32); qf_t = al([128, 2, 256], F32)
            qk_t = al([128, 2, 3, 256], BF16); at_t = al([64, 2, 256], BF16); ed_t = al([64, 2, 512], F32); kh_t = al([64, 2, 512], BF16)
            of_t = al([64, 2, 512], F32); ro_t = al([64, 2, 3, 512], F32); rr_t = al([64, 2, 8], F32); oo_t = al([64, 2, 512], BF16)
            mk = lambda t, nm: [Buf(t[:, i], nm + str(i)) for i in range(2)]
            PCs, Fs, LFs, KFs, BMs, E12s, EMs, QFs, QKs, ATs, EDs, KHs, OFs, ROs, RRs, OOs = [mk(t, n) for t, n in [
                (pc_t, "pc"), (f_t, "f"), (lf_t, "lf"), (kf_t, "kf"), (bm_t, "bm"), (e12_t, "e12"), (em_t, "em"), (qf_t, "qf"),
                (qk_t, "qk"), (at_t, "at"), (ed_t, "ed"), (kh_t, "kh"), (of_t, "of"), (ro_t, "ro"), (rr_t, "rr"), (oo_t, "oo")]]
            step = 0
            for d in range(2):
                order = fwd_order if d == 0 else bwd_order
                ti_incl, ti_strict = (0, 2) if d == 0 else (1, 3)
                t_end, t_mid = (63, 31) if d == 0 else (0, 32)
                zc = 512 if d == 0 else 1024
                kb.op("dve", lambda e: e.memset(S[:], 0.0), [], [S])
                kb.op("dve", lambda e: e.memset(Sb[:], 0.0), [], [Sb])
                for a_ in ATs:
                    kb.op("dve", lambda e, a_=a_: e.memset(a_[:], 0.0), [], [a_])
                for ci, ch in enumerate(order):
                    i = step % 2
                    step += 1
                    pc, f, lf, kf, bm, e12, em, qf, qk, at, ed, kh, of, ro, rr, oo = [x[i] for x in (PCs, Fs, LFs, KFs, BMs, E12s, EMs, QFs, QKs, ATs, EDs, KHs, OFs, ROs, RRs, OOs)]
                    need_out = (ch < 32) or ctx_out
                    r0 = ch * 64
                    dma(lambda e, pc=pc, r0=r0: e.dma_start(out=pc[:], in_=g.P[r0:r0 + 64, 1984:4544]), [g.P], [pc])
                    if d == 1 and need_out:
                        dma(lambda e, of=of, r0=r0: e.dma_start(out=of[:], in_=g.OF[r0:r0 + 64, :]), [g.OF], [of])
                    kb.op("act", lambda e: e.activation(out=f[:], in_=pc[:, zc:zc + 512], func=AF.Sigmoid), [pc], [f])
                    kb.op("dve", lambda e: e.tensor_tensor(out=f[:], in0=f[:], in1=LB[:, d, 1, :], op=ALU.mult), [f, LB], [f])
                    kb.op("dve", lambda e: e.tensor_tensor(out=f[:], in0=f[:], in1=LB[:, d, 0, :], op=ALU.add), [f, LB], [f])
                    kb.op("act", lambda e: e.activation(out=lf[:], in_=f[:], func=AF.Ln), [f], [lf])
                    kb.op("dve", lambda e: e.tensor_scalar(out=kf[:], in0=f[:], scalar1=-1.0, scalar2=1.0, op0=ALU.mult, op1=ALU.add), [f], [kf])
                    Bp = g.PS[0]
                    for h in range(4):
                        kb.op("pe", lambda e, h=h: e.matmul(Bp[:, h * 64:(h + 1) * 64], lhsT=lf[:, h * 128:(h + 1) * 128], rhs=TRI[:, ti_incl, :],
                                                            start=True, stop=True, skip_group_check=True), [lf, TRI], [Bp])
                    Dp = g.PS[1]
                    kb.op("pe", lambda e: e.matmul(Dp[0:64, :], lhsT=TRI[:, ti_strict, :], rhs=lf[:], start=True, stop=True), [lf, TRI], [Dp])
                    Tp = g.PS[2]
                    tpb = Tp[:].bitcast(BF16)
                    for h in range(4):
                        kb.op("pe", lambda e, h=h: e.transpose(out=tpb[:, h * 64:(h + 1) * 64], in_=pc[:, h * 128:(h + 1) * 128], identity=g.ident_b[0:64, 0:64]), [pc, g.ident_b], [Tp])
                    for h in range(4):
                        kb.op("pe", lambda e, h=h: e.transpose(out=tpb[:, 256 + h * 64:256 + (h + 1) * 64], in_=kf[:, h * 128:(h + 1) * 128], identity=g.ident_b[0:64, 0:64]), [kf, g.ident_b], [Tp])
                    b3 = Bp[:, 0:256].rearrange("p (h t) -> p h t", h=4)
                    kb.op("act", lambda e: e.activation(out=em[:, 0:4], in_=b3[:, :, t_mid], func=AF.Copy), [Bp], [em])
                    kb.op("dve", lambda e: e.tensor_tensor(out=bm[:].rearrange("p (h t) -> p h t", h=4), in0=b3, in1=em[:, 0:4].unsqueeze(2).to_broadcast([128, 4, 64]), op=ALU.subtract), [Bp, em], [bm])
                    kb.op("act", lambda e: e.activation(out=e12[:, 0, :], in_=bm[:], func=AF.Exp), [bm], [e12])
                    kb.op("act", lambda e: e.activation(out=e12[:, 1, :], in_=bm[:], func=AF.Exp, scale=-1.0), [bm], [e12])
                    kb.op("act", lambda e: e.activation(out=em[:, 4:8], in_=em[:, 0:4], func=AF.Exp), [em], [em])
                    kb.op("act", lambda e: e.activation(out=em[:, 0:4], in_=b3[:, :, t_end], func=AF.Exp), [Bp], [em])
                    kb.op("dve", lambda e: e.tensor_tensor(out=qf[:], in0=tpb[:, 0:256], in1=e12[:, 0, :], op=ALU.mult), [Tp, e12], [qf])
                    kb.op("dve", lambda e: e.tensor_copy(out=qk[:, 0, :], in_=qf[:]), [qf], [qk])
                    kb.op("dve", lambda e: e.tensor_tensor(out=qk[:, 1, :].rearrange("p (h t) -> p h t", h=4), in0=qf[:].rearrange("p (h t) -> p h t", h=4),
                                                           in1=em[:, 4:8].unsqueeze(2).to_broadcast([128, 4, 64]), op=ALU.mult), [qf, em], [qk])
                    kb.op("dve", lambda e: e.tensor_tensor(out=qk[:, 2, :], in0=tpb[:, 256:512], in1=e12[:, 1, :], op=ALU.mult), [Tp, e12], [qk])
                    Ap = g.PS[3]
                    if d == 0:
                        parts = [((0, 32), (0, 64)), ((32, 64), (32, 64))]
                    else:
                        parts = [((32, 64), (0, 64)), ((0, 32), (0, 32))]
                    for h in range(4):
                        for (s0, s1), (c0, c1) in parts:
                            kb.op("pe", lambda e, h=h, s0=s0, s1=s1, c0=c0, c1=c1: e.matmul(
                                Ap[s0:s1, h * 64 + c0:h * 64 + c1], lhsT=qk[:, 2, h * 64 + s0:h * 64 + s1], rhs=qk[:, 0, h * 64 + c0:h * 64 + c1],
                                start=True, stop=True, skip_group_check=True), [qk], [Ap])
                    for (s0, s1), (c0, c1) in parts:
                        kb.op("dve", lambda e, s0=s0, s1=s1, c0=c0, c1=c1: e.tensor_tensor(
                            out=at[s0:s1, :].rearrange("p (h t) -> p h t", h=4)[:, :, c0:c1],
                            in0=Ap[s0:s1, 0:256].rearrange("p (h t) -> p h t", h=4)[:, :, c0:c1],
                            in1=TRI[s0:s1, ti_incl, c0:c1].unsqueeze(1).to_broadcast([s1 - s0, 4, c1 - c0]), op=ALU.mult), [Ap, TRI], [at])
                    kb.op("act", lambda e: e.activation(out=ed[:], in_=Dp[0:64, :], func=AF.Exp), [Dp], [ed])
                    kb.op("dve", lambda e: e.tensor_tensor(out=kh[:], in0=kf[:], in1=ed[:], op=ALU.mult), [kf, ed], [kh])
                    if need_out:
                        Op = g.PS[4 + (step % 2)]
                        for h in range(4):
                            kb.op("pe", lambda e, h=h: e.matmul(Op[0:64, h * 128:(h + 1) * 128], lhsT=qk[:, 1, h * 64:(h + 1) * 64], rhs=Sb[:, h, :],
                                                                start=(h == 0), stop=False, skip_group_check=True), [qk, Sb], [Op])
                            kb.op("pe", lambda e, h=h: e.matmul(Op[0:64, h * 128:(h + 1) * 128], lhsT=at[:, h * 64:(h + 1) * 64], rhs=pc[:, 1536 + h * 128:1536 + (h + 1) * 128],
                                                                start=False, stop=True, skip_group_check=True), [at, pc], [Op])
                    Up = g.PS[6 + (step % 2)]
                    for h in range(4):
                        kb.op("pe", lambda e, h=h: e.matmul(Up[:, h * 128:(h + 1) * 128], lhsT=kh[:, h * 128:(h + 1) * 128], rhs=pc[:, 1536 + h * 128:1536 + (h + 1) * 128],
                                                            start=True, stop=True, skip_group_check=True), [kh, pc], [Up])
                    for h in range(4):
                        kb.op("dve", lambda e, h=h: e.scalar_tensor_tensor(out=S[:, h, :], in0=S[:, h, :], scalar=em[:, h:h + 1], in1=Up[:, h * 128:(h + 1) * 128],
                                                                           op0=ALU.mult, op1=ALU.add), [S, em, Up], [S])
                    kb.op("pool", lambda e: e.tensor_copy(out=Sb[:], in_=S[:]), [S], [Sb])
                    if need_out and d == 0:
                        kb.op("act", lambda e: e.activation(out=of[:], in_=Op[0:64, :], func=AF.Copy), [Op], [of])
                        dma(lambda e, of=of, r0=r0: e.dma_start(out=g.OF[r0:r0 + 64, :], in_=of[:]), [of], [g.OF])
                    if need_out and d == 1:
                        o3 = lambda b, j: b[:, j, :].rearrange("p (h v) -> p h v", h=4)
                        kb.op("dve", lambda e: e.tensor_tensor(out=ro[:, 0, :], in0=Op[0:64, :], in1=of[:], op=ALU.add), [Op, of], [ro])
                        kb.op("pool", lambda e: e.tensor_tensor(out=ro[:, 1, :], in0=ro[:, 0, :], in1=ro[:, 0, :], op=ALU.mult), [ro], [ro])
                        kb.op("dve", lambda e: e.tensor_reduce(out=rr[:, 0:4], in_=o3(ro, 1), axis=AX.X, op=ALU.add), [ro], [rr])
                        kb.op("dve", lambda e: e.tensor_scalar(out=rr[:, 0:4], in0=rr[:, 0:4], scalar1=1.0 / 128, scalar2=EPS, op0=ALU.mult, op1=ALU.add), [rr], [rr])
                        kb.op("act", lambda e: e.activation(out=rr[:, 4:8], in_=rr[:, 0:4], func=AF.Sqrt), [rr], [rr])
                        kb.op("dve", lambda e: e.reciprocal(out=rr[:, 0:4], in_=rr[:, 4:8]), [rr], [rr])
                        kb.op("dve", lambda e: e.tensor_tensor(out=o3(ro, 1), in0=o3(ro, 0), in1=rr[:, 0:4].unsqueeze(2).to_broadcast([64, 4, 128]), op=ALU.mult), [ro, rr], [ro])
                        kb.op("act", lambda e: e.activation(out=ro[:, 2, :], in_=pc[:, 2048:2560], func=AF.Silu), [pc], [ro])
                        kb.op("pool", lambda e: e.tensor_tensor(out=o3(ro, 0), in0=o3(ro, 1), in1=NGh[:].unsqueeze(1).to_broadcast([64, 4, 128]), op=ALU.mult), [ro, NGh], [ro])
                        kb.op("dve", lambda e: e.tensor_tensor(out=oo[:], in0=ro[:, 0, :], in1=ro[:, 2, :], op=ALU.mult), [ro], [oo])
                        dma(lambda e, oo=oo, r0=r0: e.dma_start(out=g.OB[r0:r0 + 64, 1024:1536], in_=oo[:]), [oo], [g.OB])
                kb.barrier()


def phase_merge(g, l, ctx_out):
    nc, kb = g.nc, g.kb
    from contextlib import ExitStack
    dma = lambda fn, r=(), w=(): kb.dma("sp", fn, r, w)
    Pv = g.P.t.rearrange("(t p) c -> p t c", p=128)
    OBv = g.OB.t.rearrange("(t p) c -> p t c", p=128)
    with ExitStack() as es:
        al = lambda shape, dt: es.enter_context(nc.sbuf_tensor(_nm(), shape, dt))
        WBR = Buf(al([128, 16, 1024], BF16), "WBR"); WO = Buf(al([128, 8, 1024], BF16), "WO")
        G1 = [Buf(al([128, 1024], F32), "G1x"), Buf(al([128, 1024], F32), "G1c")]
        ob_t = al([128, 2, 2048], BF16); gt_t = al([128, 2, 4096], BF16); obt_t = al([128, 2, 16, 128], BF16)
        sg_t = al([128, 3, 512], F32); tp_t = al([128, 3, 512], F32); y_t = al([128, 2, 1024], F32)
        yb_t = al([128, 2, 1024], BF16); yt_t = al([128, 2, 8, 128], BF16)
        mk = lambda t, nm, n: [Buf(t[:, i], nm + str(i)) for i in range(n)]
        OBs, GTs, OBTs, SGs, TPs, Ys, YBs, YTs = mk(ob_t, "ob", 2), mk(gt_t, "gt", 2), mk(obt_t, "obt", 2), mk(sg_t, "sg", 3), mk(tp_t, "tp", 3), mk(y_t, "y", 2), mk(yb_t, "yb", 2), mk(yt_t, "yt", 2)
        wsrc = g.W["w_branch"][l].rearrange("j (c p) n -> p j c n", p=128)
        for j in range(4):
            kb.dma("pool", lambda e, j=j: e.dma_start(out=WBR[:, j * 4:(j + 1) * 4, :], in_=wsrc[:, j]), [], [WBR])
        wo = g.W["w_out"][l].rearrange("(k p) n -> p k n", p=128)
        for j in range(2):
            kb.dma("pool", lambda e, j=j: e.dma_start(out=WO[:, j * 4:(j + 1) * 4, :], in_=wo[:, j * 4:(j + 1) * 4, :]), [], [WO])
        load_bcast(g, G1[0], g.MODS[l, 0:1, 2 * D:3 * D])
        load_bcast(g, G1[1], g.MODS[l, 1:2, 2 * D:3 * D])
        pi = 0
        si = 0
        for tt in range(NT if ctx_out else 16):
            i = tt % 2
            ob, gt, obt, y, yb, yt = OBs[i], GTs[i], OBTs[i], Ys[i], YBs[i], YTs[i]
            G = G1[0] if tt < 16 else G1[1]
            dma(lambda e, ob=ob, tt=tt: e.dma_start(out=ob[:], in_=OBv[:, tt, :]), [g.OB], [ob])
            dma(lambda e, gt=gt, tt=tt: e.dma_start(out=gt[:], in_=Pv[:, tt, 5312:9408]), [g.P], [gt])
            for hf in range(2):
                ps = g.PS[6 + hf]
                psb = ps[:].bitcast(BF16)
                for c in range(8):
                    kb.op("pe", lambda e, c=c, hf=hf, psb=psb, ob=ob: e.transpose(out=psb[:, c * 128:(c + 1) * 128], in_=ob[:, (hf * 8 + c) * 128:(hf * 8 + c + 1) * 128], identity=g.ident_b[:]), [ob, g.ident_b], [ps])
                kb.op("act", lambda e, hf=hf, psb=psb, obt=obt: e.activation(out=obt[:, hf * 8:(hf + 1) * 8, :], in_=psb.rearrange("p (k t) -> p k t", k=8), func=AF.Copy), [ps], [obt])
            for hf in range(2):
                for j in range(4):
                    ps = g.PS[pi % 4]; pi += 1
                    for c in range(4):
                        kb.op("pe", lambda e, c=c, j=j, hf=hf, ps=ps, obt=obt: e.matmul(ps[:], lhsT=obt[:, j * 4 + c, :], rhs=WBR[:, j * 4 + c, hf * 512:(hf + 1) * 512], start=(c == 0), stop=(c == 3)), [obt, WBR], [ps])
                    sg = SGs[si % 3]; tp = TPs[si % 3]; si += 1
                    kb.op("act", lambda e, sg=sg, gt=gt, j=j, hf=hf: e.activation(out=sg[:], in_=gt[:, j * 1024 + hf * 512:j * 1024 + (hf + 1) * 512], func=AF.Sigmoid), [gt], [sg])
                    ysl = (slice(None), slice(hf * 512, (hf + 1) * 512))
                    if j == 0:
                        kb.op("dve", lambda e, ps=ps, sg=sg, y=y, ysl=ysl: e.tensor_tensor(out=y[ysl], in0=ps[:], in1=sg[:], op=ALU.mult), [ps, sg], [y])
                    else:
                        kb.op("dve", lambda e, ps=ps, sg=sg, tp=tp: e.tensor_tensor(out=tp[:], in0=ps[:], in1=sg[:], op=ALU.mult), [ps, sg], [tp])
                        if j < 3:
                            kb.op("pool", lambda e, tp=tp, y=y, ysl=ysl: e.tensor_tensor(out=y[ysl], in0=y[ysl], in1=tp[:], op=ALU.add), [y, tp], [y])
                        else:
                            kb.op("pool", lambda e, tp=tp, y=y, yb=yb, ysl=ysl: e.tensor_tensor(out=yb[ysl], in0=y[ysl], in1=tp[:], op=ALU.add), [y, tp], [yb])
            ps = g.PS[6]
            psb = ps[:].bitcast(BF16)
            for c in range(8):
                kb.op("pe", lambda e, c=c, psb=psb, yb=yb: e.transpose(out=psb[:, c * 128:(c + 1) * 128], in_=yb[:, c * 128:(c + 1) * 128], identity=g.ident_b[:]), [yb, g.ident_b], [ps])
            kb.op("act", lambda e, psb=psb, yt=yt: e.activation(out=yt[:], in_=psb.rearrange("p (k t) -> p k t", k=8), func=AF.Copy), [ps], [yt])
            for hf in range(2):
                ps = g.PS[pi % 4]; pi += 1
                for k in range(8):
                    kb.op("pe", lambda e, k=k, hf=hf, ps=ps, yt=yt: e.matmul(ps[:], lhsT=yt[:, k, :], rhs=WO[:, k, hf * 512:(hf + 1) * 512], start=(k == 0), stop=(k == 7)), [yt, WO], [ps])
                tp = TPs[si % 3]; si += 1
                xs = g.X[tt]
                kb.op("dve", lambda e, ps=ps, tp=tp, G=G, hf=hf: e.tensor_tensor(out=tp[:], in0=ps[:], in1=G[:, hf * 512:(hf + 1) * 512], op=ALU.mult), [ps, G], [tp])
                kb.op("pool", lambda e, tp=tp, xs=xs, hf=hf: e.tensor_tensor(out=xs[:, hf * 512:(hf + 1) * 512], in0=xs[:, hf * 512:(hf + 1) * 512], in1=tp[:], op=ALU.add), [xs, tp], [xs])
        kb.barrier()


def phase_final(g):
    nc, kb = g.nc, g.kb
    from contextlib import ExitStack
    dma = lambda fn, r=(), w=(): kb.dma("sp", fn, r, w)
    with ExitStack() as es:
        al = lambda shape, dt: es.enter_context(nc.sbuf_tensor(_nm(), shape, dt))
        FG = Buf(al([128, 1024], F32), "FG")
        junk = Buf(al([128, 1024], F32), "junk")
        o_t = al([128, 2, 1024], F32); st_t = al([128, 2, 8], F32)
        Os = [Buf(o_t[:, i], "o%d" % i) for i in range(2)]; Ss = [Buf(st_t[:, i], "s%d" % i) for i in range(2)]
        dma(lambda e: e.dma_start(out=FG[:], in_=g.W["final_g"][0:1, :].partition_broadcast(128)), [], [FG])
        for tt in range(16):
            xin = g.X[tt]; st = Ss[tt % 2]; o = Os[tt % 2]
            kb.op("act", lambda e, xin=xin, st=st: e.activation(out=junk[:], in_=xin[:], func=AF.Square, accum_out=st[:, 0:1]), [xin], [junk, st])
            kb.op("dve", lambda e, st=st: e.tensor_scalar(out=st[:, 1:2], in0=st[:, 0:1], scalar1=1.0 / D, scalar2=EPS, op0=ALU.mult, op1=ALU.add), [st], [st])
            kb.op("act", lambda e, st=st: e.activation(out=st[:, 2:3], in_=st[:, 1:2], func=AF.Sqrt), [st], [st])
            kb.op("dve", lambda e, st=st: e.reciprocal(out=st[:, 3:4], in_=st[:, 2:3]), [st], [st])
            kb.op("dve", lambda e, xin=xin, st=st, o=o: e.scalar_tensor_tensor(out=o[:], in0=xin[:], scalar=st[:, 3:4], in1=FG[:], op0=ALU.mult, op1=ALU.mult), [xin, st, FG], [o])
            dma(lambda e, o=o, tt=tt: e.dma_start(out=g.out[tt * 128:(tt + 1) * 128, :], in_=o[:]), [o], [])
        kb.barrier()


def phase_peer(g, l, ctx_out):
    nc, kb = g.nc, g.kb
    from contextlib import ExitStack
    dma = lambda fn, r=(), w=(): kb.dma("sp", fn, r, w)
    NEG = -1.0e30
    u_flat = g.W["peer_u"].rearrange("l e d -> (l e) d")
    v_flat = g.W["peer_v"].rearrange("l e d -> (l e) d")
    with ExitStack() as es:
        al = lambda shape, dt: es.enter_context(nc.sbuf_tensor(_nm(), shape, dt))
        WQ = Buf(al([128, 8, 1024], BF16), "WQ"); KT = Buf(al([128, 8, 128], BF16), "KT")
        GS = Buf(al([128, 1024], F32), "GS"); SH = Buf(al([128, 1024], F32), "SH"); G2 = Buf(al([128, 1024], F32), "G2")
        IO16 = Buf(al([128, 16], F32), "IO16")
        kb.op("pool", lambda e: e.iota(IO16[:], pattern=[[1, 16]], base=0, channel_multiplier=0, allow_small_or_imprecise_dtypes=True), [], [IO16])
        wq = g.W["peer_wq"][l].rearrange("(k p) n -> p k n", p=128)
        for j in range(2):
            kb.dma("pool", lambda e, j=j: e.dma_start(out=WQ[:, j * 4:(j + 1) * 4, :], in_=wq[:, j * 4:(j + 1) * 4, :]), [], [WQ])
        keyf = Buf(al([128, 16, 64], F32), "keyf"); keyb = Buf(al([128, 16, 64], BF16), "keyb")
        dma(lambda e: e.dma_start(out=keyf[:], in_=g.W["peer_keys"][l].rearrange("h p n d -> n (h p) d")), [], [keyf])
        kb.op("dve", lambda e: e.tensor_copy(out=keyb[:], in_=keyf[:]), [keyf], [keyb])
        ps = g.PS[7]
        psb = ps[:].bitcast(BF16)
        for h in range(8):
            kb.op("pe", lambda e, h=h: e.transpose(out=psb[:, h * 128:(h + 1) * 128], in_=keyb[:, h * 2:(h + 1) * 2, :].rearrange("n a d -> n (a d)"), identity=g.ident_b[:]), [keyb, g.ident_b], [ps])
        kb.op("act", lambda e: e.activation(out=KT[:], in_=psb.rearrange("p (h n) -> p h n", h=8), func=AF.Copy), [ps], [KT])
        junk = Buf(al([128, 1024], F32), "junk")
        h2_t = al([128, 2, 1024], F32); hb_t = al([128, 2, 1024], BF16); hT_t = al([128, 2, 8, 128], BF16); qT_t = al([128, 2, 8, 128], BF16)
        st_t = al([128, 2, 8], F32)
        SC = Buf(al([128, 2, 8, 128], F32), "SC")
        TMP = Buf(al([128, 256], F32), "TMP")
        SV = Buf(al([128, 2, 8, 16], F32), "SV"); SI = Buf(al([128, 2, 8, 16], U32), "SI"); SIF = Buf(al([128, 2, 8, 16], F32), "SIF")
        CS_ = Buf(al([128, 8, 256], F32), "CS_")
        TS = Buf(al([128, 8, 16], F32), "TS"); POS = Buf(al([128, 8, 16], U32), "POS")
        PA = Buf(al([128, 2, 128], U32), "PA"); PAF = Buf(al([128, 2, 128], F32), "PAF")
        OH = Buf(al([128, 8, 16, 16], F32), "OH"); IAB = Buf(al([128, 2, 128], F32), "IAB")
        IDX = Buf(al([128, 128], I32), "IDX"); GW = Buf(al([128, 8, 16], F32), "GW"); RZ = Buf(al([128, 16], F32), "RZ")
        DOT = Buf(al([128, 128], F32), "DOT"); WGT = Buf(al([128, 128], F32), "WGT")
        ACC = Buf(al([128, 1024], F32), "ACC")
        NG = 6
        gb_t = al([128, NG, 1024], F32)
        GB = [Buf(gb_t[:, i], "gb%d" % i) for i in range(NG)]
        H2s = [Buf(h2_t[:, i], "h2%d" % i) for i in range(2)]; HBs = [Buf(hb_t[:, i], "hb%d" % i) for i in range(2)]
        HTs = [Buf(hT_t[:, i], "hT%d" % i) for i in range(2)]; QTs = [Buf(qT_t[:, i], "qT%d" % i) for i in range(2)]
        STs = [Buf(st_t[:, i], "st%d" % i) for i in range(2)]
        gi = 0
        tiles = list(range(NT if ctx_out else 16))
        for tt in tiles:
            if tt == 0 or tt == 16:
                r = 0 if tt < 16 else 1
                dma(lambda e: e.dma_start(out=GS[:], in_=g.W["norm2_g"][l:l + 1, :].partition_broadcast(128)), [], [GS])
                load_bcast(g, junk, g.MODS[l, r:r + 1, 4 * D:5 * D])
                kb.op("dve", lambda e: e.scalar_tensor_tensor(out=GS[:], in0=junk[:], scalar=1.0, in1=GS[:], op0=ALU.add, op1=ALU.mult), [junk, GS], [GS])
                load_bcast(g, SH, g.MODS[l, r:r + 1, 3 * D:4 * D])
                load_bcast(g, G2, g.MODS[l, r:r + 1, 5 * D:6 * D])
            i = tt % 2
            h2, hb, hT, qT, st = H2s[i], HBs[i], HTs[i], QTs[i], STs[i]
            xin = g.X[tt]
            kb.op("act", lambda e: e.activation(out=junk[:], in_=xin[:], func=AF.Square, accum_out=st[:, 0:1]), [xin], [junk, st])
            kb.op("dve", lambda e: e.tensor_scalar(out=st[:, 1:2], in0=st[:, 0:1], scalar1=1.0 / D, scalar2=EPS, op0=ALU.mult, op1=ALU.add), [st], [st])
            kb.op("act", lambda e: e.activation(out=st[:, 2:3], in_=st[:, 1:2], func=AF.Sqrt), [st], [st])
            kb.op("dve", lambda e: e.reciprocal(out=st[:, 3:4], in_=st[:, 2:3]), [st], [st])
            kb.op("dve", lambda e: e.scalar_tensor_tensor(out=junk[:], in0=xin[:], scalar=st[:, 3:4], in1=GS[:], op0=ALU.mult, op1=ALU.mult), [xin, st, GS], [junk])
            kb.op("dve", lambda e: e.tensor_tensor(out=h2[:], in0=junk[:], in1=SH[:], op=ALU.add), [junk, SH], [h2])
            kb.op("pool", lambda e: e.tensor_copy(out=hb[:], in_=h2[:]), [h2], [hb])
            ps = g.PS[7]
            psb = ps[:].bitcast(BF16)
            for k in range(8):
                kb.op("pe", lambda e, k=k: e.transpose(out=psb[:, k * 128:(k + 1) * 128], in_=hb[:, k * 128:(k + 1) * 128], identity=g.ident_b[:]), [hb, g.ident_b], [ps])
            kb.op("act", lambda e: e.activation(out=hT[:], in_=psb.rearrange("p (k t) -> p k t", k=8), func=AF.Copy), [ps], [hT])
            for hh in range(2):
                ps = g.PS[hh]
                for h4 in range(4):
                    h = hh * 4 + h4
                    for k in range(8):
                        kb.op("pe", lambda e, k=k, h=h, h4=h4, ps=ps: e.matmul(ps[:, h4 * 128:(h4 + 1) * 128], lhsT=WQ[:, k, h * 128:(h + 1) * 128], rhs=hT[:, k, :],
                                                                            start=(k == 0 and h4 == 0), stop=(k == 7), skip_group_check=True), [WQ, hT], [ps])
                kb.op("act", lambda e, hh=hh, ps=ps: e.activation(out=qT[:, hh * 4:(hh + 1) * 4, :], in_=ps[:].rearrange("p (h t) -> p h t", h=4), func=AF.Copy), [ps], [qT])
            for p in range(2):
                for hh in range(2):
                    ps = g.PS[2 + p * 2 + hh]
                    for h4 in range(4):
                        h = hh * 4 + h4
                        kb.op("pe", lambda e, p=p, h=h, h4=h4, ps=ps: e.matmul(ps[:, h4 * 128:(h4 + 1) * 128], lhsT=qT[p * 64:(p + 1) * 64, h, :], rhs=KT[p * 64:(p + 1) * 64, h, :],
                                                                            start=True, stop=True, skip_group_check=True), [qT, KT], [ps])
                    kb.op("act", lambda e, p=p, hh=hh, ps=ps: e.activation(out=SC[:, p, hh * 4:(hh + 1) * 4, :], in_=ps[:].rearrange("p (h t) -> p h t", h=4), func=AF.Copy), [ps], [SC])
            for p in range(2):
                for h in range(8):
                    s_ = SC[:, p, h, :]
                    kb.op("dve", lambda e, p=p, h=h, s_=s_: e.max(out=SV[:, p, h, 0:8], in_=s_), [SC], [SV])
                    kb.op("dve", lambda e, p=p, h=h, s_=s_: e.max_index(out=SI[:, p, h, 0:8], in_max=SV[:, p, h, 0:8], in_values=s_), [SC, SV], [SI])
                    kb.op("dve", lambda e, p=p, h=h, s_=s_: e.match_replace(out=TMP[:, 0:128], in_to_replace=SV[:, p, h, 0:8], in_values=s_, imm_value=NEG), [SC, SV], [TMP])
                    kb.op("dve", lambda e, p=p, h=h: e.max(out=SV[:, p, h, 8:16], in_=TMP[:, 0:128]), [TMP], [SV])
                    kb.op("dve", lambda e, p=p, h=h: e.max_index(out=SI[:, p, h, 8:16], in_max=SV[:, p, h, 8:16], in_values=TMP[:, 0:128]), [TMP, SV], [SI])
            kb.op("dve", lambda e: e.tensor_copy(out=SIF[:], in_=SI[:]), [SI], [SIF])
            c4 = CS_[:].rearrange("p h (a b) -> p h a b", a=16)
            kb.op("dve", lambda e: e.tensor_tensor(out=c4, in0=SV[:, 0].unsqueeze(3).to_broadcast([128, 8, 16, 16]), in1=SV[:, 1].unsqueeze(2).to_broadcast([128, 8, 16, 16]), op=ALU.add), [SV], [CS_])
            for h in range(8):
                kb.op("dve", lambda e, h=h: e.max(out=TS[:, h, 0:8], in_=CS_[:, h, :]), [CS_], [TS])
                kb.op("dve", lambda e, h=h: e.max_index(out=POS[:, h, 0:8], in_max=TS[:, h, 0:8], in_values=CS_[:, h, :]), [CS_, TS], [POS])
                kb.op("dve", lambda e, h=h: e.match_replace(out=TMP[:], in_to_replace=TS[:, h, 0:8], in_values=CS_[:, h, :], imm_value=NEG), [CS_, TS], [TMP])
                kb.op("dve", lambda e, h=h: e.max(out=TS[:, h, 8:16], in_=TMP[:]), [TMP], [TS])
                kb.op("dve", lambda e, h=h: e.max_index(out=POS[:, h, 8:16], in_max=TS[:, h, 8:16], in_values=TMP[:]), [TMP, TS], [POS])
            posf = POS[:].rearrange("p h j -> p (h j)")
            kb.op("dve", lambda e: e.tensor_single_scalar(out=PA[:, 0, :], in_=posf, scalar=4, op=ALU.logical_shift_right), [POS], [PA])
            kb.op("dve", lambda e: e.tensor_single_scalar(out=PA[:, 1, :], in_=posf, scalar=15, op=ALU.bitwise_and), [POS], [PA])
            kb.op("dve", lambda e: e.tensor_copy(out=PAF[:], in_=PA[:]), [PA], [PAF])
            for w in range(2):
                pa3 = PAF[:, w, :].rearrange("p (h j) -> p h j", h=8)
                kb.op("dve", lambda e, pa3=pa3: e.tensor_tensor(out=OH[:], in0=pa3.unsqueeze(3).to_broadcast([128, 8, 16, 16]),
                                                               in1=IO16[:].unsqueeze(1).unsqueeze(1).to_broadcast([128, 8, 16, 16]), op=ALU.is_equal), [PAF, IO16], [OH])
                kb.op("dve", lambda e, w=w: e.tensor_tensor(out=OH[:], in0=OH[:], in1=SIF[:, w].unsqueeze(2).to_broadcast([128, 8, 16, 16]), op=ALU.mult), [OH, SIF], [OH])
                kb.op("dve", lambda e, w=w: e.tensor_reduce(out=IAB[:, w, :].rearrange("p (h j) -> p h j", h=8), in_=OH[:], axis=AX.X, op=ALU.add), [OH], [IAB])
            kb.op("dve", lambda e: e.tensor_scalar(out=IAB[:, 0, :], in0=IAB[:, 0, :], scalar1=128.0, scalar2=float(l * 16384), op0=ALU.mult, op1=ALU.add), [IAB], [IAB])
            kb.op("dve", lambda e: e.tensor_tensor(out=IAB[:, 0, :], in0=IAB[:, 0, :], in1=IAB[:, 1, :], op=ALU.add), [IAB], [IAB])
            kb.op("dve", lambda e: e.tensor_copy(out=IDX[:], in_=IAB[:, 0, :]), [IAB], [IDX])
            kb.op("dve", lambda e: e.tensor_tensor(out=GW[:], in0=TS[:], in1=TS[:, :, 0:1].to_broadcast([128, 8, 16]), op=ALU.subtract), [TS], [GW])
            kb.op("act", lambda e: e.activation(out=GW[:], in_=GW[:], func=AF.Exp), [GW], [GW])
            kb.op("dve", lambda e: e.tensor_reduce(out=RZ[:, 0:8], in_=GW[:], axis=AX.X, op=ALU.add), [GW], [RZ])
            kb.op("dve", lambda e: e.reciprocal(out=RZ[:, 8:16], in_=RZ[:, 0:8]), [RZ], [RZ])
            kb.op("dve", lambda e: e.tensor_tensor(out=GW[:], in0=GW[:], in1=RZ[:, 8:16].unsqueeze(2).to_broadcast([128, 8, 16]), op=ALU.mult), [GW, RZ], [GW])
            for s in range(128):
                gbuf = GB[gi % NG]; gi += 1
                kb.dma("pool", lambda e, gbuf=gbuf, s=s: e.indirect_dma_start(out=gbuf[:], out_offset=None, in_=u_flat,
                       in_offset=bass.IndirectOffsetOnAxis(ap=IDX[:, s:s + 1], axis=0)), [IDX], [gbuf])
                kb.op("dve", lambda e, gbuf=gbuf, s=s: e.scalar_tensor_tensor(out=junk[:], in0=gbuf[:], scalar=1.0, in1=h2[:], op0=ALU.mult, op1=ALU.mult,
                                                                              accum_out=DOT[:, s:s + 1]), [gbuf, h2], [junk, DOT])
            kb.op("act", lambda e: e.activation(out=WGT[:], in_=DOT[:], func=AF.Gelu), [DOT], [WGT])
            kb.op("dve", lambda e: e.tensor_tensor(out=WGT[:], in0=WGT[:], in1=GW[:].rearrange("p h j -> p (h j)"), op=ALU.mult), [WGT, GW], [WGT])
            for s in range(128):
                gbuf = GB[gi % NG]; gi += 1
                kb.dma("pool", lambda e, gbuf=gbuf, s=s: e.indirect_dma_start(out=gbuf[:], out_offset=None, in_=v_flat,
                       in_offset=bass.IndirectOffsetOnAxis(ap=IDX[:, s:s + 1], axis=0)), [IDX], [gbuf])
                if s == 0:
                    kb.op("dve", lambda e, gbuf=gbuf, s=s: e.tensor_scalar(out=ACC[:], in0=gbuf[:], scalar1=WGT[:, s:s + 1], scalar2=None, op0=ALU.mult), [gbuf, WGT], [ACC])
                else:
                    kb.op("dve", lambda e, gbuf=gbuf, s=s: e.scalar_tensor_tensor(out=ACC[:], in0=gbuf[:], scalar=WGT[:, s:s + 1], in1=ACC[:], op0=ALU.mult, op1=ALU.add), [gbuf, WGT, ACC], [ACC])
            kb.op("dve", lambda e: e.tensor_tensor(out=junk[:], in0=ACC[:], in1=G2[:], op=ALU.mult), [ACC, G2], [junk])
            kb.op("dve", lambda e: e.tensor_tensor(out=xin[:], in0=xin[:], in1=junk[:], op=ALU.add), [xin, junk], [xin])
        kb.barrier()


def kernel(**inputs):
    nc, g = build_program()
    consts = host_consts()
    shared = {n: np.ascontiguousarray(np.asarray(inputs[n], dtype=np.float32)).reshape(s) for n, s in W_NAMES}
    shared.update(consts)
    shared["c_ctx"] = np.ascontiguousarray(np.asarray(inputs["c_ctx"], dtype=np.float32)).reshape(1, D)
    x = np.asarray(inputs["x"], dtype=np.float32)
    c = np.asarray(inputs["c"], dtype=np.float32)
    ctx = np.asarray(inputs["ctx"], dtype=np.float32)
    nb = x.shape[0]
    in_maps = []
    for b in range(nb):
        m = dict(shared)
        m["x"] = np.ascontiguousarray(x[b])
        m["ctx"] = np.ascontiguousarray(ctx[b])
        m["c"] = np.ascontiguousarray(c[b:b + 1])
        in_maps.append(m)
    res = run_bass_kernel_spmd(nc, in_maps, core_ids=list(range(nb)))
    out = np.stack([np.asarray(r["out"], dtype=np.float32) for r in res.results], axis=0)
    return out
```

```python
import numpy as np
import math
import concourse.bass as bass
import concourse.mybir as mybir
from concourse.bass_utils import run_bass_kernel_spmd

F32 = mybir.dt.float32
BF16 = mybir.dt.bfloat16
U32 = mybir.dt.uint32
I32 = mybir.dt.int32
AF = mybir.ActivationFunctionType
ALU = mybir.AluOpType
AX = mybir.AxisListType

NDS = 40


class Buf:
    __slots__ = ("t", "w", "r", "name", "nt")

    def __init__(self, t, name="", nt=False):
        self.t = t
        self.w = None
        self.r = []
        self.name = name
        self.nt = nt

    def __getitem__(self, k):
        return self.t[k]


class KB:
    def __init__(self, nc):
        self.nc = nc
        self.E = {"pe": nc.tensor, "act": nc.scalar, "dve": nc.vector, "pool": nc.gpsimd, "sp": nc.sync}
        self.esem = {n: nc.alloc_semaphore("s_" + n) for n in self.E}
        self.ecnt = {n: 0 for n in self.E}
        self.dsems = [nc.alloc_semaphore("d%d" % i) for i in range(NDS)]
        self.dcnt = [0] * NDS
        self.dnext = 0
        self.known = {n: {} for n in self.E}
        self.nwait = 0
        self.ninst = 0

    def _sem(self, key):
        return self.esem[key] if isinstance(key, str) else self.dsems[key]

    def _need(self, eng, ev):
        key, val, _ = ev
        if self.known[eng].get(key, 0) >= val:
            return
        self.E[eng].wait_ge(self._sem(key), val)
        self.known[eng][key] = val
        self.nwait += 1

    def _deps(self, eng, reads, writes, is_dma=False):
        for b in reads:
            if b.w is not None:
                self._need(eng, b.w)
        for b in writes:
            if b.w is not None and (is_dma or b.w[2] != eng):
                self._need(eng, b.w)
            for r in b.r:
                if is_dma or r[2] != eng:
                    self._need(eng, r)

    def op(self, eng, fn, reads=(), writes=()):
        reads = [b for b in reads if not b.nt]
        writes = [b for b in writes if not b.nt]
        self._deps(eng, reads, writes)
        inst = fn(self.E[eng])
        self.ecnt[eng] += 1
        inst.then_inc(self.esem[eng], 1)
        ev = (eng, self.ecnt[eng], eng)
        for b in reads:
            b.r.append(ev)
        for b in writes:
            b.w = ev
            b.r = []
        self.ninst += 1
        return inst

    def dma(self, q, fn, reads=(), writes=()):
        reads = [b for b in reads if not b.nt]
        writes = [b for b in writes if not b.nt]
        self._deps(q, reads, writes, is_dma=True)
        i = self.dnext
        self.dnext = (self.dnext + 1) % NDS
        if self.dcnt[i] > 0:
            self._need(q, (i, self.dcnt[i], "dma"))
        inst = fn(self.E[q])
        self.dcnt[i] += 16
        inst.then_inc(self.dsems[i], 16)
        ev = (i, self.dcnt[i], "dma")
        for b in reads:
            b.r.append(ev)
        for b in writes:
            b.w = ev
            b.r = []
        self.ninst += 1
        return inst

    def barrier(self):
        evs = [(n, self.ecnt[n], n) for n in self.E if self.ecnt[n] > 0]
        evs += [(i, self.dcnt[i], "dma") for i in range(NDS) if self.dcnt[i] > 0]
        for n in self.E:
            for ev in evs:
                self._need(n, ev)

    def finish(self):
        self.barrier()


D = 1024
NX = 2048
NC_ = 256
T = NX + NC_
NT = T // 128
P_IN = 9408
EPS = 1e-6
OFF = {"a_q1": 0, "a_q2": 256, "a_k1": 512, "a_k2": 768, "a_v": 1024,
       "b_cq": 1536, "b_ckv": 1792, "b_kr": 1920,
       "c_q": 1984, "c_f": 2496, "c_b": 3008, "c_i": 3520, "c_g": 4032,
       "d_q": 4544, "d_k": 5056, "d_v": 5184, "gate": 5312}

W_NAMES = [("w_mod", [2, 1024, 6144]), ("b_mod", [2, 6144]), ("norm1_g", [2, 1024]), ("norm2_g", [2, 1024]),
           ("w_in", [2, 1024, 9408]), ("diff_lam_q1", [2, 64]), ("diff_lam_k1", [2, 64]), ("diff_lam_q2", [2, 64]),
           ("diff_lam_k2", [2, 64]), ("diff_norm_g", [2, 128]), ("mla_qnorm_g", [2, 256]), ("mla_kvnorm_g", [2, 128]),
           ("mla_w_uq", [2, 256, 768]), ("mla_w_ukv", [2, 128, 1024]), ("hgrn_lb", [2, 2, 512]), ("hgrn_norm_g", [2, 128]),
           ("win_sink", [2, 8]), ("w_branch", [2, 4, 512, 1024]), ("w_out", [2, 1024, 1024]), ("peer_wq", [2, 1024, 1024]),
           ("peer_keys", [2, 8, 2, 128, 64]), ("peer_u", [2, 16384, 1024]), ("peer_v", [2, 16384, 1024]), ("final_g", [1, 1024])]


def host_consts():
    n = NX
    rows = np.repeat(np.arange(n // 64, dtype=np.float32), 64)
    cols = np.tile(np.arange(64, dtype=np.float32), n // 64)
    m = 32
    freqs = (10000.0 ** (-2.0 * np.arange(m // 2, dtype=np.float32) / m)).astype(np.float32)
    ang = np.concatenate([rows[:, None] * freqs, cols[:, None] * freqs], axis=-1).astype(np.float32)
    cs = np.zeros((T, 64), np.float32)
    cs[:n, :32] = np.cos(ang)
    cs[:n, 32:] = np.sin(ang)
    cs[n:, :32] = 1.0
    i = np.arange(128)
    c = {}
    c["rope_cs"] = cs
    c["ident"] = np.eye(128, dtype=np.float32)
    c["mask_prev"] = (i[:, None] >= i[None, :]).astype(np.float32)
    c["mask_next"] = (i[:, None] <= i[None, :]).astype(np.float32)
    j = np.arange(64)
    tri = np.zeros((64, 4, 64), np.float32)
    tri[:, 0] = (j[:, None] <= j[None, :])
    tri[:, 1] = (j[:, None] >= j[None, :])
    tri[:, 2] = (j[:, None] > j[None, :])
    tri[:, 3] = (j[:, None] < j[None, :])
    c["tri"] = tri.reshape(64, 256)
    return c


class Ctx:
    pass


_nmc = [0]


def _nm():
    _nmc[0] += 1
    return "tmp%d" % _nmc[0]


def build_program(n_layers=2, debug=(), stop_after=None):
    nc = bass.Bass("TRN2", target_bir_lowering=False)
    kb = KB(nc)
    g = Ctx()
    g.nc, g.kb = nc, kb
    g.debug = debug

    def din(name, shape, dt=F32):
        return nc.dram_tensor(name, list(shape), dt, kind="ExternalInput").ap()

    g.x_in = din("x", [NX, D])
    g.c_in = din("c", [1, D])
    g.ctx_in = din("ctx", [NC_, D])
    g.cctx_in = din("c_ctx", [1, D])
    g.W = {n: din(n, s) for n, s in W_NAMES}
    g.rope_cs = din("rope_cs", [T, 64])
    g.ident_in = din("ident", [128, 128])
    g.mprev_in = din("mask_prev", [128, 128])
    g.mnext_in = din("mask_next", [128, 128])
    g.tri_in = din("tri", [64, 256])
    g.out = nc.dram_tensor("out", [NX, D], F32, kind="ExternalOutput").ap()

    def scratch(name, shape, dt):
        kind = "ExternalOutput" if name in debug else "Internal"
        return Buf(nc.dram_tensor(name, list(shape), dt, kind=kind).ap(), name, nt=True)

    g.P = scratch("P", [T, P_IN], BF16)
    g.OB = scratch("OB", [T, 2048], BF16)
    g.MODS = scratch("MODS", [2, 2, 6144], F32)
    g.OF = scratch("OF", [T, 512], F32)
    g.UB = scratch("UB", [2 * 16384, D], BF16)
    g.VBF = scratch("VBF", [2 * 16384, D], BF16)

    cnt = [0]

    def sb(shape, dt, name=None):
        cnt[0] += 1
        return Buf(nc.alloc_sbuf_tensor("%s_%d" % (name or "t", cnt[0]), list(shape), dt), name or "t")
    g.sb = sb

    g.X = [Buf(None, "X%d" % i) for i in range(NT)]
    xall = nc.alloc_sbuf_tensor("Xall", [128, NT, D], F32)
    for i in range(NT):
        g.X[i].t = xall[:, i, :]
    psall = nc.alloc_psum_tensor("psall", [128, 8, 512], F32)
    g.PS = [Buf(psall[:, i, :], "ps%d" % i) for i in range(8)]
    g.PSP = [Buf(psall[:, 2 * i:2 * i + 2, :].rearrange("p b c -> p (b c)"), "psp%d" % i) for i in range(4)]
    g.ident_f = sb([128, 128], F32, "identf")
    g.ident_b = sb([128, 128], BF16, "identb")
    g.ones_f = sb([128, 128], F32, "onesf")
    g.SCT = sb([128, 8, 2], F32, "sct")

    dma = lambda fn, r=(), w=(): kb.dma("sp", fn, r, w)

    for i in range(16):
        dma(lambda e, i=i: e.dma_start(out=g.X[i][:], in_=g.x_in[i * 128:(i + 1) * 128, :]), [], [g.X[i]])
    for i in range(2):
        dma(lambda e, i=i: e.dma_start(out=g.X[16 + i][:], in_=g.ctx_in[i * 128:(i + 1) * 128, :]), [], [g.X[16 + i]])
    dma(lambda e: e.dma_start(out=g.ident_f[:], in_=g.ident_in[:, :]), [], [g.ident_f])
    kb.op("dve", lambda e: e.tensor_copy(out=g.ident_b[:], in_=g.ident_f[:]), [g.ident_f], [g.ident_b])
    kb.op("dve", lambda e: e.memset(g.ones_f[:], 1.0), [], [g.ones_f])
    craw = sb([128, 8, 2], F32, "craw")
    with nc.allow_non_contiguous_dma(reason="tiny conditioning vector load"):
        dma(lambda e: e.dma_start(out=craw[:, :, 0], in_=g.c_in.rearrange("o (k p) -> p (o k)", p=128)), [], [craw])
        dma(lambda e: e.dma_start(out=craw[:, :, 1], in_=g.cctx_in.rearrange("o (k p) -> p (o k)", p=128)), [], [craw])
    kb.op("act", lambda e: e.activation(out=g.SCT[:], in_=craw[:], func=AF.Silu), [craw], [g.SCT])

    if "skip_cast" not in debug:
        phase_cast_experts(g, n_layers)
    for l in range(n_layers):
        last = (l == 1)
        phase_mod(g, l)
        if stop_after == ("mod", l):
            break
        if "skip_proj" not in debug:
            phase_norm_proj(g, l)
        if stop_after == ("proj", l):
            break
        ctx_out = not last
        if "skip_a" not in debug:
            phase_mixer_a(g, l, ctx_out)
        if stop_after == ("a", l):
            break
        if "skip_b" not in debug:
            phase_mixer_b(g, l, ctx_out)
        if stop_after == ("b", l):
            break
        if "skip_d" not in debug:
            phase_mixer_d(g, l, ctx_out)
        if stop_after == ("d", l):
            break
        if "skip_c" not in debug:
            phase_mixer_c(g, l, ctx_out)
        if stop_after == ("c", l):
            break
        if "skip_merge" not in debug:
            phase_merge(g, l, ctx_out)
        if "Xdbg" in debug and stop_after == ("merge", l):
            xd = nc.dram_tensor("Xdbg", [NT, 128, D], F32, kind="ExternalOutput").ap()
            for i in range(NT):
                dma(lambda e, i=i: e.dma_start(out=xd[i], in_=g.X[i][:]), [g.X[i]], [])
        if stop_after == ("merge", l):
            break
        phase_peer(g, l, ctx_out)
        if "Xdbg" in debug and stop_after == ("peer", l):
            xd = nc.dram_tensor("Xdbg", [NT, 128, D], F32, kind="ExternalOutput").ap()
            for i in range(NT):
                dma(lambda e, i=i: e.dma_start(out=xd[i], in_=g.X[i][:]), [g.X[i]], [])
        if stop_after == ("peer", l):
            break

    if stop_after is None:
        phase_final(g)
    kb.finish()
    g.nc = nc
    return nc, g


def phase_mod(g, l):
    nc, kb = g.nc, g.kb
    kb.barrier()
    dma = lambda fn, r=(), w=(): kb.dma("sp", fn, r, w)
    with nc.sbuf_tensor(_nm(), [128, 2, 8, 512], F32) as wm_t, nc.sbuf_tensor(_nm(), [1, 6144], F32) as bm_t, \
            nc.sbuf_tensor(_nm(), [2, 2, 512], F32) as ev_t:
        wm = [Buf(wm_t[:, i], "wm%d" % i) for i in range(2)]
        bm = Buf(bm_t, "bm")
        ev = [Buf(ev_t[:, i], "ev%d" % i) for i in range(2)]
        dma(lambda e: e.dma_start(out=bm[:], in_=g.W["b_mod"][l:l + 1, :]), [], [bm])
        wsrc = g.W["w_mod"][l].rearrange("(k p) c -> p k c", p=128)
        for m in range(12):
            w = wm[m % 2]
            dma(lambda e, w=w, m=m: e.dma_start(out=w[:], in_=wsrc[:, :, m * 512:(m + 1) * 512]), [], [w])
            ps = g.PS[m % 2]
            for k in range(8):
                kb.op("pe", lambda e, w=w, k=k, ps=ps: e.matmul(ps[0:2, :], lhsT=g.SCT[:, k, :], rhs=w[:, k, :],
                                                              start=(k == 0), stop=False), [g.SCT, w], [ps])
            kb.op("pe", lambda e, ps=ps, m=m: e.matmul(ps[0:2, :], lhsT=g.ones_f[0:1, 0:2], rhs=bm[0:1, m * 512:(m + 1) * 512],
                                                     start=False, stop=True), [g.ones_f, bm], [ps])
            o = ev[m % 2]
            kb.op("act", lambda e, o=o, ps=ps: e.activation(out=o[:], in_=ps[0:2, :], func=AF.Copy), [ps], [o])
            dma(lambda e, o=o, m=m: e.dma_start(out=g.MODS[l, :, m * 512:(m + 1) * 512], in_=o[:]), [o], [g.MODS])
        kb.barrier()


def load_bcast(g, dst, src_row_ap):
    g.kb.dma("sp", lambda e: e.dma_start(out=dst[:], in_=src_row_ap.partition_broadcast(128)), [g.MODS], [dst])


def norm_tile(g, xin, GS, SH, hb, scr):
    kb = g.kb
    junk, st = scr
    kb.op("act", lambda e: e.activation(out=junk[:], in_=xin[:], func=AF.Square, accum_out=st[:, 0:1]), [xin], [junk, st])
    kb.op("dve", lambda e: e.tensor_scalar(out=st[:, 1:2], in0=st[:, 0:1], scalar1=1.0 / D, scalar2=EPS, op0=ALU.mult, op1=ALU.add), [st], [st])
    kb.op("act", lambda e: e.activation(out=st[:, 2:3], in_=st[:, 1:2], func=AF.Sqrt), [st], [st])
    kb.op("dve", lambda e: e.reciprocal(out=st[:, 3:4], in_=st[:, 2:3]), [st], [st])
    kb.op("dve", lambda e: e.scalar_tensor_tensor(out=junk[:], in0=xin[:], scalar=st[:, 3:4], in1=GS[:], op0=ALU.mult, op1=ALU.mult), [xin, st, GS], [junk])
    kb.op("dve", lambda e: e.tensor_tensor(out=hb[:], in0=junk[:], in1=SH[:], op=ALU.add), [junk, SH], [hb])


def phase_norm_proj(g, l):
    nc, kb = g.nc, g.kb
    dma = lambda fn, r=(), w=(): kb.dma("sp", fn, r, w)
    with nc.sbuf_tensor(_nm(), [128, 8, T], BF16) as hT_t:
        hT = Buf(hT_t, "hT")
        with nc.sbuf_tensor(_nm(), [128, 5, D], F32) as bc_t, nc.sbuf_tensor(_nm(), [128, D], F32) as junk_t, \
                nc.sbuf_tensor(_nm(), [128, 2, 8], F32) as st_t, nc.sbuf_tensor(_nm(), [128, 2, D], BF16) as hb_t:
            G1, SCx, SHx, SCc, SHc = [Buf(bc_t[:, i], "bc%d" % i) for i in range(5)]
            junk = Buf(junk_t, "junk")
            sts = [Buf(st_t[:, i], "st%d" % i) for i in range(2)]
            hbs = [Buf(hb_t[:, i], "hb%d" % i) for i in range(2)]
            dma(lambda e: e.dma_start(out=G1[:], in_=g.W["norm1_g"][l:l + 1, :].partition_broadcast(128)), [], [G1])
            load_bcast(g, SHx, g.MODS[l, 0:1, 0:D])
            load_bcast(g, SCx, g.MODS[l, 0:1, D:2 * D])
            load_bcast(g, SHc, g.MODS[l, 1:2, 0:D])
            load_bcast(g, SCc, g.MODS[l, 1:2, D:2 * D])
            for S in (SCx, SCc):
                kb.op("dve", lambda e, S=S: e.scalar_tensor_tensor(out=S[:], in0=S[:], scalar=1.0, in1=G1[:], op0=ALU.add, op1=ALU.mult), [S, G1], [S])
            for tt in range(NT):
                GS, SH = (SCx, SHx) if tt < 16 else (SCc, SHc)
                hb = hbs[tt % 2]
                norm_tile(g, g.X[tt], GS, SH, hb, (junk, sts[tt % 2]))
                ps = g.PS[tt % 2]
                psb = ps[:].bitcast(BF16)
                for k in range(8):
                    kb.op("pe", lambda e, k=k, hb=hb, psb=psb: e.transpose(out=psb[:, k * 128:(k + 1) * 128], in_=hb[:, k * 128:(k + 1) * 128], identity=g.ident_b[:]), [hb, g.ident_b], [ps])
                kb.op("act", lambda e, tt=tt, psb=psb: e.activation(out=hT[:, :, tt * 128:(tt + 1) * 128], in_=psb.rearrange("p (k t) -> p k t", k=8), func=AF.Copy), [ps], [hT])
            kb.barrier()
        with nc.sbuf_tensor(_nm(), [128, 2, 8, 512], BF16) as wb_t, nc.sbuf_tensor(_nm(), [128, 2, 6, 512], BF16) as po_t:
            wbs = [Buf(wb_t[:, i], "wb%d" % i) for i in range(2)]
            pos = [Buf(po_t[:, i], "po%d" % i) for i in range(2)]
            wsrc = g.W["w_in"][l].rearrange("(k p) c -> p k c", p=128)
            pdst = g.P.t.rearrange("(t p) c -> p t c", p=128)
            nct = (P_IN + 511) // 512
            cw = lambda ct: min(512, P_IN - ct * 512)

            def load_w(ct):
                w = wbs[ct % 2]
                kb.dma("pool", lambda e: e.dma_start(out=w[:, :, 0:cw(ct)], in_=wsrc[:, :, ct * 512:ct * 512 + cw(ct)]), [], [w])
            load_w(0)
            oi = 0
            pi = 0
            for ct in range(nct):
                if ct + 1 < nct:
                    load_w(ct + 1)
                w = wbs[ct % 2]
                n = cw(ct)
                for tg in range(3):
                    po = pos[oi % 2]
                    oi += 1
                    for j in range(6):
                        tt = tg * 6 + j
                        ps = g.PS[2 + pi % 4]
                        pi += 1
                        for k in range(8):
                            kb.op("pe", lambda e, k=k, tt=tt, ps=ps, w=w, n=n: e.matmul(ps[:, 0:n], lhsT=hT[:, k, tt * 128:(tt + 1) * 128], rhs=w[:, k, 0:n],
                                                                                     start=(k == 0), stop=(k == 7)), [hT, w], [ps])
                        if pi % 2 == 0:
                            kb.op("act", lambda e, j=j, ps=ps, po=po, n=n: e.activation(out=po[:, j, 0:n], in_=ps[:, 0:n], func=AF.Copy), [ps], [po])
                        else:
                            kb.op("dve", lambda e, j=j, ps=ps, po=po, n=n: e.tensor_copy(out=po[:, j, 0:n], in_=ps[:, 0:n]), [ps], [po])
                    dma(lambda e, po=po, tg=tg, ct=ct, n=n: e.dma_start(out=pdst[:, tg * 6:(tg + 1) * 6, ct * 512:ct * 512 + n], in_=po[:, :, 0:n]), [po], [g.P])
            kb.barrier()


def attention(g, nh, dv, scale, V, qk_chunks, groups, post, npass=1):
    nc, kb = g.nc, g.kb
    assert dv + 1 <= 256
    with nc.sbuf_tensor(_nm(), [128, 3, 512], BF16) as E_t, nc.sbuf_tensor(_nm(), [128, 4, 4, dv + 1], F32) as of_t:
        Es = [Buf(E_t[:, i], "E%d" % i) for i in range(3)]
        Ofs = [Buf(of_t[:, i], "Of%d" % i) for i in range(4)]
        it = 0
        si = 0
        for h in range(nh):
            for (q_lo, q_n, key_tiles) in groups:
                nsub = q_n // 128
                ofl = []
                for s in range(npass):
                    chunks = qk_chunks(h, s)
                    obanks = [g.PS[3 + 2 * (it % 2)], g.PS[4 + 2 * (it % 2)]]
                    Of = Ofs[(it % 2) * 2 + s] if npass == 2 else Ofs[it % 4]
                    for ki, kt in enumerate(key_tiles):
                        S = g.PS[si % 3]
                        E = Es[si % 3]
                        si += 1
                        for ci, (qf, kf, bufs) in enumerate(chunks):
                            kb.op("pe", lambda e, qf=qf, kf=kf, ci=ci, S=S, kt=kt: e.matmul(
                                S[:, 0:q_n], lhsT=kf(kt), rhs=qf(q_lo, q_n), start=(ci == 0), stop=(ci == len(chunks) - 1)),
                                bufs, [S])
                        kb.op("act", lambda e, S=S, E=E: e.activation(out=E[:, 0:q_n], in_=S[:, 0:q_n], func=AF.Exp, scale=scale), [S], [E])
                        for sub in range(nsub):
                            ob = obanks[sub // 2]
                            c0 = (sub % 2) * (dv + 1)
                            kb.op("pe", lambda e, ob=ob, c0=c0, E=E, sub=sub, kt=kt, ki=ki: e.matmul(
                                ob[:, c0:c0 + dv + 1], lhsT=E[:, sub * 128:(sub + 1) * 128], rhs=V[:, kt, h, :],
                                start=(ki == 0 and sub % 2 == 0), stop=(ki == len(key_tiles) - 1), skip_group_check=True),
                                [E, V], [ob])
                    for bi in range((nsub + 1) // 2):
                        nb = min(2, nsub - bi * 2)
                        ob = obanks[bi]
                        src = ob[:, 0:nb * (dv + 1)].rearrange("p (s d) -> p s d", s=nb)
                        kb.op("dve", lambda e, Of=Of, bi=bi, nb=nb, src=src: e.tensor_copy(out=Of[:, bi * 2:bi * 2 + nb, :], in_=src), [ob], [Of])
                    ofl.append(Of)
                    if npass == 1:
                        it += 1
                if npass == 2:
                    it += 1
                post(h, q_lo, q_n, ofl)


def phase_mixer_a(g, l, ctx_out):
    nc, kb = g.nc, g.kb
    dma = lambda fn, r=(), w=(): kb.dma("sp", fn, r, w)
    lam_init = 0.8 - 0.6 * math.exp(-0.3 * l)
    Pv = g.P.t.rearrange("(t p) c -> p t c", p=128)
    OBv = g.OB.t.rearrange("(t p) c -> p t c", p=128)
    with nc.sbuf_tensor(_nm(), [128, 8, T], BF16) as qkt_t, nc.sbuf_tensor(_nm(), [128, NT, 4, 129], BF16) as va_t, \
            nc.sbuf_tensor(_nm(), [128, NT, 64], F32) as cs_t, nc.sbuf_tensor(_nm(), [128, 8], F32) as lam_t, \
            nc.sbuf_tensor(_nm(), [128, 128], F32) as ng_t:
        QKT = Buf(qkt_t, "QKT"); VA = Buf(va_t, "VA"); CS = Buf(cs_t, "CS"); LAM = Buf(lam_t, "LAM"); NG = Buf(ng_t, "NG")
        dma(lambda e: e.dma_start(out=CS[:], in_=g.rope_cs.rearrange("(t p) c -> p t c", p=128)), [], [CS])
        for tt in range(NT):
            dma(lambda e, tt=tt: e.dma_start(out=VA[:, tt, :, 0:128], in_=Pv[:, tt, 1024:1536].rearrange("p (h d) -> p h d", h=4)), [g.P], [VA])
        kb.op("pool", lambda e: e.memset(VA[:, :, :, 128:129], 1.0), [], [VA])
        with nc.sbuf_tensor(_nm(), [128, 4, 64], F32) as lv_t:
            LV = Buf(lv_t, "LV")
            for i, nm in enumerate(["diff_lam_q1", "diff_lam_k1", "diff_lam_q2", "diff_lam_k2"]):
                dma(lambda e, i=i, nm=nm: e.dma_start(out=LV[:, i, :], in_=g.W[nm][l:l + 1, :].partition_broadcast(128)), [], [LV])
            kb.op("dve", lambda e: e.tensor_tensor(out=LV[:, 0, :], in0=LV[:, 0, :], in1=LV[:, 1, :], op=ALU.mult), [LV], [LV])
            kb.op("dve", lambda e: e.tensor_tensor(out=LV[:, 2, :], in0=LV[:, 2, :], in1=LV[:, 3, :], op=ALU.mult), [LV], [LV])
            kb.op("dve", lambda e: e.tensor_reduce(out=LAM[:, 0:1], in_=LV[:, 0, :], axis=AX.X, op=ALU.add), [LV], [LAM])
            kb.op("dve", lambda e: e.tensor_reduce(out=LAM[:, 1:2], in_=LV[:, 2, :], axis=AX.X, op=ALU.add), [LV], [LAM])
            kb.op("act", lambda e: e.activation(out=LAM[:, 2:4], in_=LAM[:, 0:2], func=AF.Exp), [LAM], [LAM])
            kb.op("dve", lambda e: e.tensor_tensor(out=LAM[:, 4:5], in0=LAM[:, 2:3], in1=LAM[:, 3:4], op=ALU.subtract), [LAM], [LAM])
            kb.op("dve", lambda e: e.tensor_scalar(out=LAM[:, 5:6], in0=LAM[:, 4:5], scalar1=lam_init, scalar2=None, op0=ALU.add), [LAM], [LAM])
            dma(lambda e: e.dma_start(out=NG[:], in_=g.W["diff_norm_g"][l:l + 1, :].partition_broadcast(128)), [], [NG])
            kb.op("dve", lambda e: e.tensor_scalar(out=NG[:], in0=NG[:], scalar1=1.0 - lam_init, scalar2=None, op0=ALU.mult), [NG], [NG])
            kb.barrier()
        lam = LAM[:, 5:6]
        with nc.sbuf_tensor(_nm(), [128, 2, 1024], BF16) as pa_t, nc.sbuf_tensor(_nm(), [128, 2, 1024], BF16) as rb_t, \
                nc.sbuf_tensor(_nm(), [128, 4, 512], F32) as tm_t:
            pas = [Buf(pa_t[:, i], "pa%d" % i) for i in range(2)]
            rbs = [Buf(rb_t[:, i], "rb%d" % i) for i in range(2)]
            tms = [Buf(tm_t[:, i], "tm%d" % i) for i in range(4)]
            for tt in range(NT):
                pa = pas[tt % 2]; rb = rbs[tt % 2]
                dma(lambda e, pa=pa, tt=tt: e.dma_start(out=pa[:], in_=Pv[:, tt, 0:1024]), [g.P], [pa])
                rope_tile(g, pa, rb, CS, tt, 16, tms)
                ps = g.PS[7]
                psb = ps[:].bitcast(BF16)
                for c in range(8):
                    kb.op("pe", lambda e, c=c, rb=rb, psb=psb: e.transpose(out=psb[:, c * 128:(c + 1) * 128], in_=rb[:, c * 128:(c + 1) * 128], identity=g.ident_b[:]), [rb, g.ident_b], [ps])
                kb.op("act", lambda e, tt=tt, psb=psb: e.activation(out=QKT[:, :, tt * 128:(tt + 1) * 128], in_=psb.rearrange("p (k t) -> p k t", k=8), func=AF.Copy), [ps], [QKT])
            kb.barrier()

        def qk_chunks(h, s):
            p0 = (h % 2) * 64
            qc = s * 2 + h // 2
            kc = 4 + s * 2 + h // 2
            return [(lambda lo, n: QKT[p0:p0 + 64, qc, lo:lo + n], lambda kt: QKT[p0:p0 + 64, kc, kt * 128:(kt + 1) * 128], [QKT])]

        with nc.sbuf_tensor(_nm(), [128, 2, 8, 4], F32) as r_t, nc.sbuf_tensor(_nm(), [128, 2, 3, 4, 128], F32) as w_t, \
                nc.sbuf_tensor(_nm(), [128, 2, 4, 128], BF16) as o_t:
            cnt = [0]
            rs_ = [Buf(r_t[:, i], "r") for i in range(2)]; obs_ = [Buf(o_t[:, i], "ob") for i in range(2)]
            ws_ = [[Buf(w_t[:, i, j], "w%d" % j) for j in range(3)] for i in range(2)]

            def post(h, q_lo, q_n, ofl):
                O1, O2 = ofl
                i = cnt[0] % 2
                cnt[0] += 1
                ns = q_n // 128
                r = rs_[i]; w0, w1, w2 = ws_[i]; ob = obs_[i]
                bc = lambda ap: ap.unsqueeze(2).to_broadcast([128, ns, 128])
                kb.op("dve", lambda e: e.reciprocal(out=r[:, 0, 0:ns], in_=O1[:, 0:ns, 128]), [O1], [r])
                kb.op("dve", lambda e: e.reciprocal(out=r[:, 1, 0:ns], in_=O2[:, 0:ns, 128]), [O2], [r])
                kb.op("dve", lambda e: e.tensor_scalar(out=r[:, 2, 0:ns], in0=r[:, 1, 0:ns], scalar1=lam, scalar2=None, op0=ALU.mult), [r, LAM], [r])
                kb.op("dve", lambda e: e.tensor_tensor(out=w0[:, 0:ns, :], in0=O1[:, 0:ns, 0:128], in1=bc(r[:, 0, 0:ns]), op=ALU.mult), [O1, r], [w0])
                kb.op("dve", lambda e: e.tensor_tensor(out=w1[:, 0:ns, :], in0=O2[:, 0:ns, 0:128], in1=bc(r[:, 2, 0:ns]), op=ALU.mult), [O2, r], [w1])
                kb.op("dve", lambda e: e.tensor_tensor(out=w0[:, 0:ns, :], in0=w0[:, 0:ns, :], in1=w1[:, 0:ns, :], op=ALU.subtract), [w0, w1], [w0])
                kb.op("pool", lambda e: e.tensor_tensor(out=w2[:, 0:ns, :], in0=w0[:, 0:ns, :], in1=w0[:, 0:ns, :], op=ALU.mult), [w0], [w2])
                kb.op("dve", lambda e: e.tensor_reduce(out=r[:, 3, 0:ns], in_=w2[:, 0:ns, :], axis=AX.X, op=ALU.add), [w2], [r])
                kb.op("dve", lambda e: e.tensor_scalar(out=r[:, 4, 0:ns], in0=r[:, 3, 0:ns], scalar1=1.0 / 128, scalar2=EPS, op0=ALU.mult, op1=ALU.add), [r], [r])
                kb.op("act", lambda e: e.activation(out=r[:, 5, 0:ns], in_=r[:, 4, 0:ns], func=AF.Sqrt), [r], [r])
                kb.op("dve", lambda e: e.reciprocal(out=r[:, 6, 0:ns], in_=r[:, 5, 0:ns]), [r], [r])
                kb.op("dve", lambda e: e.tensor_tensor(out=w1[:, 0:ns, :], in0=w0[:, 0:ns, :], in1=bc(r[:, 6, 0:ns]), op=ALU.mult), [w0, r], [w1])
                kb.op("pool", lambda e: e.tensor_tensor(out=ob[:, 0:ns, :], in0=w1[:, 0:ns, :], in1=NG[:].unsqueeze(1).to_broadcast([128, ns, 128]), op=ALU.mult), [w1, NG], [ob])
                t0 = q_lo // 128
                dma(lambda e: e.dma_start(out=OBv[:, t0:t0 + ns, h * 128:(h + 1) * 128], in_=ob[:, 0:ns, :]), [ob], [g.OB])

            groups = [(i * 512, 512, list(range(NT))) for i in range(4)]
            if ctx_out:
                groups.append((2048, 256, [16, 17]))
            attention(g, 4, 128, 1.0 / 8.0, VA, qk_chunks, groups, post, npass=2)
            kb.barrier()


def rope_tile(g, pa, rb, CS, tt, ng, tms, col0=0):
    kb = g.kb
    n = ng * 64
    xv = pa[:, col0:col0 + n].rearrange("p (g a f j) -> p g a f j", g=ng, a=2, f=2, j=16)
    ov = rb[:, col0:col0 + n].rearrange("p (g a f j) -> p g a f j", g=ng, a=2, f=2, j=16)
    x1, x2 = xv[:, :, :, 0, :], xv[:, :, :, 1, :]
    cosb = CS[:, tt, 0:32].rearrange("p (a j) -> p a j", a=2).unsqueeze(1).to_broadcast([128, ng, 2, 16])
    sinb = CS[:, tt, 32:64].rearrange("p (a j) -> p a j", a=2).unsqueeze(1).to_broadcast([128, ng, 2, 16])
    tv = [t[:, 0:ng * 32].rearrange("p (g a j) -> p g a j", g=ng, a=2) for t in tms]
    kb.op("dve", lambda e: e.tensor_tensor(out=tv[0], in0=x1, in1=cosb, op=ALU.mult), [pa, CS], [tms[0]])
    kb.op("pool", lambda e: e.tensor_tensor(out=tv[1], in0=x2, in1=sinb, op=ALU.mult), [pa, CS], [tms[1]])
    kb.op("dve", lambda e: e.tensor_tensor(out=tv[2], in0=x1, in1=sinb, op=ALU.mult), [pa, CS], [tms[2]])
    kb.op("pool", lambda e: e.tensor_tensor(out=tv[3], in0=x2, in1=cosb, op=ALU.mult), [pa, CS], [tms[3]])
    kb.op("dve", lambda e: e.tensor_tensor(out=ov[:, :, :, 0, :], in0=tv[0], in1=tv[1], op=ALU.subtract), [tms[0], tms[1]], [rb])
    kb.op("pool", lambda e: e.tensor_tensor(out=ov[:, :, :, 1, :], in0=tv[2], in1=tv[3], op=ALU.add), [tms[2], tms[3]], [rb])


def small_rms(g, src_ap, src_buf, n, nh, r, col):
    kb = g.kb
    return None


def phase_mixer_b(g, l, ctx_out):
    nc, kb = g.nc, g.kb
    dma = lambda fn, r=(), w=(): kb.dma("sp", fn, r, w)
    Pv = g.P.t.rearrange("(t p) c -> p t c", p=128)
    OBv = g.OB.t.rearrange("(t p) c -> p t c", p=128)
    TQ = T if ctx_out else NX
    with nc.sbuf_tensor(_nm(), [128, 4, T], BF16) as ft_t, nc.sbuf_tensor(_nm(), [128, 4, T], BF16) as qnt_t, \
            nc.sbuf_tensor(_nm(), [128, 4, T], BF16) as knt_t, nc.sbuf_tensor(_nm(), [128, 2, T], BF16) as qrt_t, \
            nc.sbuf_tensor(_nm(), [128, NT, 4, 129], BF16) as vb_t:
        FT = Buf(ft_t, "FT"); QNT = Buf(qnt_t, "QNT"); KNT = Buf(knt_t, "KNT"); QRT = Buf(qrt_t, "QRT"); VB = Buf(vb_t, "VB")
        kb.op("pool", lambda e: e.memset(VB[:, :, :, 128:129], 1.0), [], [VB])
        with nc.sbuf_tensor(_nm(), [128, 2, 4, 128], BF16) as wqn_t, nc.sbuf_tensor(_nm(), [128, 2, 4, 64], BF16) as wqr_t, \
                nc.sbuf_tensor(_nm(), [128, 4, 128], BF16) as wkn_t, nc.sbuf_tensor(_nm(), [128, 4, 128], BF16) as wv_t, \
                nc.sbuf_tensor(_nm(), [128, 384], F32) as gn_t, nc.sbuf_tensor(_nm(), [128, NT, 64], F32) as cs_t:
            WQN = Buf(wqn_t, "WQN"); WQR = Buf(wqr_t, "WQR"); WKN = Buf(wkn_t, "WKN"); WV = Buf(wv_t, "WV"); GN = Buf(gn_t, "GN"); CS = Buf(cs_t, "CS")
            dma(lambda e: e.dma_start(out=CS[:], in_=g.rope_cs.rearrange("(t p) c -> p t c", p=128)), [], [CS])
            wuq = g.W["mla_w_uq"][l].rearrange("(c p) (h d) -> p c h d", p=128, h=4)
            wukv = g.W["mla_w_ukv"][l].rearrange("p (h d) -> p h d", h=4)
            for c in range(2):
                kb.dma("pool", lambda e, c=c: e.dma_start(out=WQN[:, c], in_=wuq[:, c, :, 0:128]), [], [WQN])
                kb.dma("pool", lambda e, c=c: e.dma_start(out=WQR[:, c], in_=wuq[:, c, :, 128:192]), [], [WQR])
            kb.dma("pool", lambda e: e.dma_start(out=WKN[:], in_=wukv[:, :, 0:128]), [], [WKN])
            kb.dma("pool", lambda e: e.dma_start(out=WV[:], in_=wukv[:, :, 128:256]), [], [WV])
            dma(lambda e: e.dma_start(out=GN[:, 0:256], in_=g.W["mla_qnorm_g"][l:l + 1, :].partition_broadcast(128)), [], [GN])
            dma(lambda e: e.dma_start(out=GN[:, 256:384], in_=g.W["mla_kvnorm_g"][l:l + 1, :].partition_broadcast(128)), [], [GN])
            with nc.sbuf_tensor(_nm(), [128, 2, 448], BF16) as pb_t, nc.sbuf_tensor(_nm(), [128, 2, 512], BF16) as tb_t, \
                    nc.sbuf_tensor(_nm(), [128, 2, 512], F32) as jk_t, nc.sbuf_tensor(_nm(), [128, 2, 8], F32) as st_t, \
                    nc.sbuf_tensor(_nm(), [128, 4, 128], F32) as tm_t, nc.sbuf_tensor(_nm(), [128, 2, 64], BF16) as kr_t:
                tms = [Buf(tm_t[:, i], "tm%d" % i) for i in range(4)]
                pbs = [Buf(pb_t[:, i], "pb") for i in range(2)]; tbs = [Buf(tb_t[:, i], "tb") for i in range(2)]
                jks = [Buf(jk_t[:, i], "jk") for i in range(2)]; sts = [Buf(st_t[:, i], "st") for i in range(2)]
                for tt in range(NT):
                    i = tt % 2
                    pb = pbs[i]; tb = tbs[i]; jk = jks[i]; st = sts[i]
                    dma(lambda e, pb=pb, tt=tt: e.dma_start(out=pb[:], in_=Pv[:, tt, 1536:1984]), [g.P], [pb])
                    kb.op("act", lambda e: e.activation(out=jk[:, 0:256], in_=pb[:, 0:256], func=AF.Square, accum_out=st[:, 0:1]), [pb], [jk, st])
                    kb.op("act", lambda e: e.activation(out=jk[:, 256:384], in_=pb[:, 256:384], func=AF.Square, accum_out=st[:, 1:2]), [pb], [jk, st])
                    kb.op("dve", lambda e: e.tensor_scalar(out=st[:, 2:3], in0=st[:, 0:1], scalar1=1.0 / 256, scalar2=EPS, op0=ALU.mult, op1=ALU.add), [st], [st])
                    kb.op("dve", lambda e: e.tensor_scalar(out=st[:, 3:4], in0=st[:, 1:2], scalar1=1.0 / 128, scalar2=EPS, op0=ALU.mult, op1=ALU.add), [st], [st])
                    kb.op("act", lambda e: e.activation(out=st[:, 4:6], in_=st[:, 2:4], func=AF.Sqrt), [st], [st])
                    kb.op("dve", lambda e: e.reciprocal(out=st[:, 6:8], in_=st[:, 4:6]), [st], [st])
                    kb.op("dve", lambda e: e.scalar_tensor_tensor(out=tb[:, 0:256], in0=pb[:, 0:256], scalar=st[:, 6:7], in1=GN[:, 0:256], op0=ALU.mult, op1=ALU.mult), [pb, st, GN], [tb])
                    kb.op("dve", lambda e: e.scalar_tensor_tensor(out=tb[:, 256:384], in0=pb[:, 256:384], scalar=st[:, 7:8], in1=GN[:, 256:384], op0=ALU.mult, op1=ALU.mult), [pb, st, GN], [tb])
                    rope_tile(g, pb, tb, CS, tt, 1, tms, col0=384)
                    kb.op("pool", lambda e: e.tensor_copy(out=tb[:, 448:512], in_=tb[:, 384:448]), [tb], [tb])
                    ps = g.PS[7]
                    psb = ps[:].bitcast(BF16)
                    for c in range(4):
                        kb.op("pe", lambda e, c=c, tb=tb, psb=psb: e.transpose(out=psb[:, c * 128:(c + 1) * 128], in_=tb[:, c * 128:(c + 1) * 128], identity=g.ident_b[:]), [tb, g.ident_b], [ps])
                    kb.op("act", lambda e, tt=tt, psb=psb: e.activation(out=FT[:, :, tt * 128:(tt + 1) * 128], in_=psb[:, 0:512].rearrange("p (k t) -> p k t", k=4), func=AF.Copy), [ps], [FT])
                kb.barrier()
            pi = 0
            ngr = (T + 511) // 512
            for gi in range(ngr):
                lo = gi * 512
                n = min(512, T - lo)
                for h in range(4):
                    ps = g.PS[pi % 4]; pi += 1
                    for c in range(2):
                        kb.op("pe", lambda e, c=c, h=h, ps=ps, lo=lo, n=n: e.matmul(ps[:, 0:n], lhsT=WQN[:, c, h, :], rhs=FT[:, c, lo:lo + n], start=(c == 0), stop=(c == 1)), [WQN, FT], [ps])
                    kb.op("act", lambda e, h=h, ps=ps, lo=lo, n=n: e.activation(out=QNT[:, h, lo:lo + n], in_=ps[:, 0:n], func=AF.Copy), [ps], [QNT])
                    ps = g.PS[pi % 4]; pi += 1
                    kb.op("pe", lambda e, h=h, ps=ps, lo=lo, n=n: e.matmul(ps[:, 0:n], lhsT=WKN[:, h, :], rhs=FT[:, 2, lo:lo + n], start=True, stop=True), [WKN, FT], [ps])
                    kb.op("dve", lambda e, h=h, ps=ps, lo=lo, n=n: e.tensor_copy(out=KNT[:, h, lo:lo + n], in_=ps[:, 0:n]), [ps], [KNT])
            with nc.sbuf_tensor(_nm(), [128, 2, 256], BF16) as qr_t, nc.sbuf_tensor(_nm(), [128, 2, 256], BF16) as qq_t, \
                    nc.sbuf_tensor(_nm(), [128, 4, 128], F32) as tm_t:
                tms = [Buf(tm_t[:, i], "tm%d" % i) for i in range(4)]
                qrs = [Buf(qr_t[:, i], "qr") for i in range(2)]; qqs = [Buf(qq_t[:, i], "qq") for i in range(2)]
                for tt in range(NT):
                    i = tt % 2
                    qr = qrs[i]; qq = qqs[i]
                    ps = g.PS[pi % 4]; pi += 1
                    kb.op("pe", lambda e, ps=ps, tt=tt: e.matmul(ps[:, 0:512], lhsT=FT[:, 2, tt * 128:(tt + 1) * 128], rhs=WV[:].rearrange("p h d -> p (h d)"), start=True, stop=True), [FT, WV], [ps])
                    kb.op("act", lambda e, ps=ps, tt=tt: e.activation(out=VB[:, tt, :, 0:128], in_=ps[:, 0:512].rearrange("p (h d) -> p h d", h=4), func=AF.Copy), [ps], [VB])
                    ps = g.PS[pi % 4]; pi += 1
                    for c in range(2):
                        kb.op("pe", lambda e, c=c, ps=ps, tt=tt: e.matmul(ps[:, 0:256], lhsT=FT[:, c, tt * 128:(tt + 1) * 128], rhs=WQR[:, c].rearrange("p h d -> p (h d)"), start=(c == 0), stop=(c == 1)), [FT, WQR], [ps])
                    kb.op("act", lambda e, ps=ps, qq=qq: e.activation(out=qq[:], in_=ps[:, 0:256], func=AF.Copy), [ps], [qq])
                    rope_tile(g, qq, qr, CS, tt, 4, tms)
                    ps = g.PS[7]
                    psb = ps[:].bitcast(BF16)
                    for c in range(2):
                        kb.op("pe", lambda e, c=c, qr=qr, psb=psb: e.transpose(out=psb[:, c * 128:(c + 1) * 128], in_=qr[:, c * 128:(c + 1) * 128], identity=g.ident_b[:]), [qr, g.ident_b], [ps])
                    kb.op("dve", lambda e, tt=tt, psb=psb: e.tensor_copy(out=QRT[:, :, tt * 128:(tt + 1) * 128], in_=psb[:, 0:256].rearrange("p (k t) -> p k t", k=2)), [ps], [QRT])
                kb.barrier()

        if "dbgB" in g.debug:
            dft = nc.dram_tensor("dbgFT", [128, 4, T], BF16, kind="ExternalOutput").ap()
            dqr = nc.dram_tensor("dbgQRT", [128, 2, T], BF16, kind="ExternalOutput").ap()
            dqn = nc.dram_tensor("dbgQNT", [128, 4, T], BF16, kind="ExternalOutput").ap()
            dkn = nc.dram_tensor("dbgKNT", [128, 4, T], BF16, kind="ExternalOutput").ap()
            dvb = nc.dram_tensor("dbgVB", [128, NT, 4, 129], BF16, kind="ExternalOutput").ap()
            dma(lambda e: e.dma_start(out=dft[:], in_=FT[:]), [FT], [])
            dma(lambda e: e.dma_start(out=dqr[:], in_=QRT[:]), [QRT], [])
            dma(lambda e: e.dma_start(out=dqn[:], in_=QNT[:]), [QNT], [])
            dma(lambda e: e.dma_start(out=dkn[:], in_=KNT[:]), [KNT], [])
            dma(lambda e: e.dma_start(out=dvb[:], in_=VB[:]), [VB], [])

        def qk_chunks(h, s):
            p0 = (h % 2) * 64
            return [(lambda lo, n: QNT[:, h, lo:lo + n], lambda kt: KNT[:, h, kt * 128:(kt + 1) * 128], [QNT, KNT]),
                    (lambda lo, n: QRT[p0:p0 + 64, h // 2, lo:lo + n], lambda kt: FT[p0:p0 + 64, 3, kt * 128:(kt + 1) * 128], [QRT, FT])]

        with nc.sbuf_tensor(_nm(), [128, 2, 4], F32) as r_t, nc.sbuf_tensor(_nm(), [128, 2, 4, 128], BF16) as o_t:
            cnt = [0]
            rs_ = [Buf(r_t[:, i], "r") for i in range(2)]; obs_ = [Buf(o_t[:, i], "ob") for i in range(2)]

            def post(h, q_lo, q_n, ofl):
                O1 = ofl[0]
                i = cnt[0] % 2
                cnt[0] += 1
                ns = q_n // 128
                r = rs_[i]; ob = obs_[i]
                kb.op("dve", lambda e: e.reciprocal(out=r[:, 0:ns], in_=O1[:, 0:ns, 128]), [O1], [r])
                kb.op("dve", lambda e: e.tensor_tensor(out=ob[:, 0:ns, :], in0=O1[:, 0:ns, 0:128], in1=r[:, 0:ns].unsqueeze(2).to_broadcast([128, ns, 128]), op=ALU.mult), [O1, r], [ob])
                t0 = q_lo // 128
                dma(lambda e: e.dma_start(out=OBv[:, t0:t0 + ns, 512 + h * 128:512 + (h + 1) * 128], in_=ob[:, 0:ns, :]), [ob], [g.OB])

            groups = [(i * 512, 512, list(range(NT))) for i in range(4)]
            if ctx_out:
                groups.append((2048, 256, [16, 17]))
            attention(g, 4, 128, 192.0 ** -0.5, VB, qk_chunks, groups, post, npass=1)
            kb.barrier()


def phase_mixer_d(g, l, ctx_out):
    nc, kb = g.nc, g.kb
    dma = lambda fn, r=(), w=(): kb.dma("sp", fn, r, w)
    Pv = g.P.t.rearrange("(t p) c -> p t c", p=128)
    OBv = g.OB.t.rearrange("(t p) c -> p t c", p=128)
    scale = 1.0 / 8.0
    with nc.sbuf_tensor(_nm(), [128, 6, T], BF16) as qkd_t, nc.sbuf_tensor(_nm(), [128, NT, 2, 65], BF16) as vd_t, \
            nc.sbuf_tensor(_nm(), [128, 8], F32) as es_t, nc.sbuf_tensor(_nm(), [128, 2, 128], BF16) as mk_t, \
            nc.sbuf_tensor(_nm(), [128, 2, 128], F32) as mkf_t:
        QKD = Buf(qkd_t, "QKD"); VD = Buf(vd_t, "VD"); ES = Buf(es_t, "ES"); MK = Buf(mk_t, "MK"); MKF = Buf(mkf_t, "MKF")
        kb.op("pool", lambda e: e.memset(VD[:, :, :, 64:65], 1.0), [], [VD])
        for tt in range(NT):
            dma(lambda e, tt=tt: e.dma_start(out=VD[:, tt, :, 0:64], in_=Pv[:, tt, 5184:5312].rearrange("p (h d) -> p h d", h=2)), [g.P], [VD])
        dma(lambda e: e.dma_start(out=MKF[:, 0, :], in_=g.mprev_in[:, :]), [], [MKF])
        dma(lambda e: e.dma_start(out=MKF[:, 1, :], in_=g.mnext_in[:, :]), [], [MKF])
        kb.op("dve", lambda e: e.tensor_copy(out=MK[:], in_=MKF[:]), [MKF], [MK])
        dma(lambda e: e.dma_start(out=ES[:], in_=g.W["win_sink"][l:l + 1, :].partition_broadcast(128)), [], [ES])
        kb.op("act", lambda e: e.activation(out=ES[:], in_=ES[:], func=AF.Exp), [ES], [ES])
        with nc.sbuf_tensor(_nm(), [128, 2, 640], BF16) as pd_t, nc.sbuf_tensor(_nm(), [128, 2, 768], BF16) as rb_t, \
                nc.sbuf_tensor(_nm(), [128, 4, 320], F32) as tm_t, nc.sbuf_tensor(_nm(), [128, NT, 64], F32) as cs_t:
            CS = Buf(cs_t, "CS")
            dma(lambda e: e.dma_start(out=CS[:], in_=g.rope_cs.rearrange("(t p) c -> p t c", p=128)), [], [CS])
            pds = [Buf(pd_t[:, i], "pd%d" % i) for i in range(2)]
            rbs = [Buf(rb_t[:, i], "rb%d" % i) for i in range(2)]
            tms = [Buf(tm_t[:, i], "tm%d" % i) for i in range(4)]
            for tt in range(NT if "d_skip_rope" not in g.debug else 0):
                pd = pds[tt % 2]; rb = rbs[tt % 2]
                dma(lambda e, pd=pd, tt=tt: e.dma_start(out=pd[:], in_=Pv[:, tt, 4544:5184]), [g.P], [pd])
                rope_tile(g, pd, rb, CS, tt, 10, tms)
                kb.op("dve", lambda e, rb=rb: e.tensor_copy(out=rb[:, 640:768].rearrange("p (u d) -> p u d", u=2),
                                                           in_=rb[:, 576:640].unsqueeze(1).to_broadcast([128, 2, 64])), [rb], [rb])
                kb.op("dve", lambda e, rb=rb: e.tensor_copy(out=rb[:, 576:640], in_=rb[:, 512:576]), [rb], [rb])
                ps = g.PS[7]
                psb = ps[:].bitcast(BF16)
                for c in range(6):
                    kb.op("pe", lambda e, c=c, rb=rb, psb=psb: e.transpose(out=psb[:, c * 128:(c + 1) * 128], in_=rb[:, c * 128:(c + 1) * 128], identity=g.ident_b[:]), [rb, g.ident_b], [ps])
                kb.op("act", lambda e, tt=tt, psb=psb: e.activation(out=QKD[:, :, tt * 128:(tt + 1) * 128], in_=psb[:, 0:768].rearrange("p (k t) -> p k t", k=6), func=AF.Copy), [ps], [QKD])
            kb.barrier()
        with nc.sbuf_tensor(_nm(), [128, 3, 512], BF16) as E_t, nc.sbuf_tensor(_nm(), [128, 2, 8, 65], F32) as of_t, \
                nc.sbuf_tensor(_nm(), [128, 2, 2, 8], F32) as r_t, nc.sbuf_tensor(_nm(), [128, 2, 8, 64], BF16) as o_t:
            Es = [Buf(E_t[:, i], "E%d" % i) for i in range(3)]
            Ofs = [Buf(of_t[:, i], "Of%d" % i) for i in range(2)]
            rs_ = [Buf(r_t[:, i], "r%d" % i) for i in range(2)]
            obs_ = [Buf(o_t[:, i], "ob%d" % i) for i in range(2)]
            si = 0
            qtiles = list(range(16)) + ([16, 17] if ctx_out else [])
            if "d_prep_only" in g.debug:
                qtiles = []
            for qn_, qi in enumerate(qtiles):
                if qi < 16:
                    kts = ([(qi - 1, 0)] if qi > 0 else []) + [(qi, None)] + ([(qi + 1, 1)] if qi < 15 else []) + [(16, None), (17, None)]
                else:
                    kts = [(16, None), (17, None)]
                Of = Ofs[qn_ % 2]; r = rs_[qn_ % 2]; ob = obs_[qn_ % 2]
                for kvh in range(2):
                    O = g.PS[3 + (qn_ * 2 + kvh) % 4]
                    for ki, (kt, mk) in enumerate(kts):
                        Sp = [(g.PS[0], g.PS[1]), (g.PS[2], g.PS[7])][si % 2]
                        E = Es[si % 3]; si += 1
                        for gq in range(4):
                            hq = kvh * 4 + gq
                            p0 = (hq % 2) * 64
                            S = Sp[gq % 2]
                            c0 = (gq // 2) * 128
                            kb.op("pe", lambda e, S=S, c0=c0, p0=p0, kvh=kvh, kt=kt, hq=hq, qi=qi: e.matmul(
                                S[:, c0:c0 + 128], lhsT=QKD[p0:p0 + 64, 4 + kvh, kt * 128:(kt + 1) * 128],
                                rhs=QKD[p0:p0 + 64, hq // 2, qi * 128:(qi + 1) * 128], start=True, stop=True, skip_group_check=True), [QKD], [S])
                        for j in range(2):
                            kb.op("act", lambda e, j=j, E=E, Sp=Sp: e.activation(out=E[:, j * 256:(j + 1) * 256], in_=Sp[j][:, 0:256], func=AF.Exp, scale=scale), [Sp[j]], [E])
                        if mk is not None and "d_no_mask" not in g.debug:
                            kb.op("dve", lambda e, E=E, mk=mk: e.tensor_tensor(out=E[:].rearrange("p (g q) -> p g q", g=4), in0=E[:].rearrange("p (g q) -> p g q", g=4),
                                                                             in1=MKF[:, mk, :].unsqueeze(1).to_broadcast([128, 4, 128]), op=ALU.mult), [E, MKF], [E])
                        for gq in range(4 if "d_no_pv" not in g.debug else 0):
                            ei = (gq % 2) * 2 + gq // 2
                            kb.op("pe", lambda e, O=O, gq=gq, ei=ei, E=E, kt=kt, kvh=kvh, ki=ki: e.matmul(
                                O[:, gq * 65:(gq + 1) * 65], lhsT=E[:, ei * 128:(ei + 1) * 128], rhs=VD[:, kt, kvh, :],
                                start=(ki == 0 and gq == 0), stop=(ki == len(kts) - 1), skip_group_check=True), [E, VD], [O])
                    kb.op("act", lambda e, O=O, Of=Of, kvh=kvh: e.activation(out=Of[:, kvh * 4:(kvh + 1) * 4, :], in_=O[:, 0:260].rearrange("p (g d) -> p g d", g=4), func=AF.Copy), [O], [Of])
                kb.op("dve", lambda e, Of=Of, r=r: e.tensor_tensor(out=r[:, 0, :], in0=Of[:, :, 64], in1=ES[:], op=ALU.add), [Of, ES], [r])
                kb.op("dve", lambda e, r=r: e.reciprocal(out=r[:, 1, :], in_=r[:, 0, :]), [r], [r])
                kb.op("dve", lambda e, Of=Of, r=r, ob=ob: e.tensor_tensor(out=ob[:], in0=Of[:, :, 0:64], in1=r[:, 1, :].unsqueeze(2).to_broadcast([128, 8, 64]), op=ALU.mult), [Of, r], [ob])
                dma(lambda e, ob=ob, qi=qi: e.dma_start(out=OBv[:, qi, 1536:2048], in_=ob[:].rearrange("p h d -> p (h d)")), [ob], [g.OB])
            kb.barrier()


def phase_mixer_c(g, l, ctx_out):
    nc, kb = g.nc, g.kb
    dma = lambda fn, r=(), w=(): kb.dma("sp", fn, r, w)
    NCH = T // 64
    fwd_order = [32, 33, 34, 35] + list(range(32))
    bwd_order = [35, 34, 33, 32] + list(range(31, -1, -1))
    from contextlib import ExitStack
    with ExitStack() as es:
        al = lambda shape, dt: es.enter_context(nc.sbuf_tensor(_nm(), shape, dt))
        tri_t = al([64, 4, 64], F32); lb_t = al([64, 2, 2, 512], F32); ng_t = al([64, 128], F32)
        S_t = al([128, 4, 128], F32); Sb_t = al([128, 4, 128], BF16)
        TRI = Buf(tri_t, "TRI"); LB = Buf(lb_t, "LB"); NGh = Buf(ng_t, "NGh"); S = Buf(S_t, "S"); Sb = Buf(Sb_t, "Sb")
        dma(lambda e: e.dma_start(out=TRI[:], in_=g.tri_in.rearrange("p (a t) -> p a t", a=4)), [], [TRI])
        dma(lambda e: e.dma_start(out=NGh[:], in_=g.W["hgrn_norm_g"][l:l + 1, :].partition_broadcast(64)), [], [NGh])
        if l == 0:
            kb.op("dve", lambda e: e.memset(LB[:, :, 0, :], 0.0), [], [LB])
            kb.op("dve", lambda e: e.memset(LB[:, :, 1, :], 1.0), [], [LB])
        else:
            for d in range(2):
                dma(lambda e, d=d: e.dma_start(out=LB[:, d, 0, :], in_=g.W["hgrn_lb"][d, 1:2, :].partition_broadcast(64)), [], [LB])
                dma(lambda e, d=d: e.dma_start(out=LB[:, d, 1, :], in_=g.W["hgrn_lb"][d, 0:1, :].partition_broadcast(64)), [], [LB])
                kb.op("dve", lambda e, d=d: e.tensor_tensor(out=LB[:, d, 0, :], in0=LB[:, d, 0, :], in1=LB[:, d, 1, :], op=ALU.subtract), [LB], [LB])
                kb.op("act", lambda e, d=d: e.activation(out=LB[:, d, 0, :], in_=LB[:, d, 0, :], func=AF.Sigmoid), [LB], [LB])
                kb.op("dve", lambda e, d=d: e.tensor_scalar(out=LB[:, d, 1, :], in0=LB[:, d, 0, :], scalar1=-1.0, scalar2=1.0, op0=ALU.mult, op1=ALU.add), [LB], [LB])
        if True:
            pc_t = al([64, 2, 2560], BF16); f_t = al([64, 2, 512], F32); lf_t = al([64, 2, 512], F32); kf_t = al([64, 2, 512], BF16)
            bm_t = al([128, 2, 256], F32); e12_t = al([128, 2, 2, 256], F32); em_t = al([128, 2, 8], F32); qf_t = al([128, 2, 256], F32)
            qk_t = al([128, 2, 3, 256], BF16); at_t = al([64, 2, 256], BF16); ed_t = al([64, 2, 512], F32); kh_t = al([64, 2, 512], BF16)
            of_t = al([64, 2, 512], F32); ro_t = al([64, 2, 3, 512], F32); rr_t = al([64, 2, 8], F32); oo_t = al([64, 2, 512], BF16)
            mk = lambda t, nm: [Buf(t[:, i], nm + str(i)) for i in range(2)]
            PCs, Fs, LFs, KFs, BMs, E12s, EMs, QFs, QKs, ATs, EDs, KHs, OFs, ROs, RRs, OOs = [mk(t, n) for t, n in [
                (pc_t, "pc"), (f_t, "f"), (lf_t, "lf"), (kf_t, "kf"), (bm_t, "bm"), (e12_t, "e12"), (em_t, "em"), (qf_t, "qf"),
                (qk_t, "qk"), (at_t, "at"), (ed_t, "ed"), (kh_t, "kh"), (of_t, "of"), (ro_t, "ro"), (rr_t, "rr"), (oo_t, "oo")]]
            step = 0
            for d in range(2):
                order = fwd_order if d == 0 else bwd_order
                ti_incl, ti_strict = (0, 2) if d == 0 else (1, 3)
                t_end, t_mid = (63, 31) if d == 0 else (0, 32)
                zc = 512 if d == 0 else 1024
                kb.op("dve", lambda e: e.memset(S[:], 0.0), [], [S])
                kb.op("dve", lambda e: e.memset(Sb[:], 0.0), [], [Sb])
                for a_ in ATs:
                    kb.op("dve", lambda e, a_=a_: e.memset(a_[:], 0.0), [], [a_])
                for ci, ch in enumerate(order):
                    i = step % 2
                    step += 1
                    pc, f, lf, kf, bm, e12, em, qf, qk, at, ed, kh, of, ro, rr, oo = [x[i] for x in (PCs, Fs, LFs, KFs, BMs, E12s, EMs, QFs, QKs, ATs, EDs, KHs, OFs, ROs, RRs, OOs)]
                    need_out = (ch < 32) or ctx_out
                    r0 = ch * 64
                    dma(lambda e, pc=pc, r0=r0: e.dma_start(out=pc[:], in_=g.P[r0:r0 + 64, 1984:4544]), [g.P], [pc])
                    if d == 1 and need_out:
                        dma(lambda e, of=of, r0=r0: e.dma_start(out=of[:], in_=g.OF[r0:r0 + 64, :]), [g.OF], [of])
                    kb.op("act", lambda e: e.activation(out=f[:], in_=pc[:, zc:zc + 512], func=AF.Sigmoid), [pc], [f])
                    kb.op("dve", lambda e: e.tensor_tensor(out=f[:], in0=f[:], in1=LB[:, d, 1, :], op=ALU.mult), [f, LB], [f])
                    kb.op("dve", lambda e: e.tensor_tensor(out=f[:], in0=f[:], in1=LB[:, d, 0, :], op=ALU.add), [f, LB], [f])
                    kb.op("act", lambda e: e.activation(out=lf[:], in_=f[:], func=AF.Ln), [f], [lf])
                    kb.op("dve", lambda e: e.tensor_scalar(out=kf[:], in0=f[:], scalar1=-1.0, scalar2=1.0, op0=ALU.mult, op1=ALU.add), [f], [kf])
                    Bp = g.PS[0]
                    for h in range(4):
                        kb.op("pe", lambda e, h=h: e.matmul(Bp[:, h * 64:(h + 1) * 64], lhsT=lf[:, h * 128:(h + 1) * 128], rhs=TRI[:, ti_incl, :],
                                                            start=True, stop=True, skip_group_check=True), [lf, TRI], [Bp])
                    Dp = g.PS[1]
                    kb.op("pe", lambda e: e.matmul(Dp[0:64, :], lhsT=TRI[:, ti_strict, :], rhs=lf[:], start=True, stop=True), [lf, TRI], [Dp])
                    Tp = g.PS[2]
                    tpb = Tp[:].bitcast(BF16)
                    for h in range(4):
                        kb.op("pe", lambda e, h=h: e.transpose(out=tpb[:, h * 64:(h + 1) * 64], in_=pc[:, h * 128:(h + 1) * 128], identity=g.ident_b[0:64, 0:64]), [pc, g.ident_b], [Tp])
                    for h in range(4):
                        kb.op("pe", lambda e, h=h: e.transpose(out=tpb[:, 256 + h * 64:256 + (h + 1) * 64], in_=kf[:, h * 128:(h + 1) * 128], identity=g.ident_b[0:64, 0:64]), [kf, g.ident_b], [Tp])
                    b3 = Bp[:, 0:256].rearrange("p (h t) -> p h t", h=4)
                    kb.op("act", lambda e: e.activation(out=em[:, 0:4], in_=b3[:, :, t_mid], func=AF.Copy), [Bp], [em])
                    kb.op("dve", lambda e: e.tensor_tensor(out=bm[:].rearrange("p (h t) -> p h t", h=4), in0=b3, in1=em[:, 0:4].unsqueeze(2).to_broadcast([128, 4, 64]), op=ALU.subtract), [Bp, em], [bm])
                    kb.op("act", lambda e: e.activation(out=e12[:, 0, :], in_=bm[:], func=AF.Exp), [bm], [e12])
                    kb.op("act", lambda e: e.activation(out=e12[:, 1, :], in_=bm[:], func=AF.Exp, scale=-1.0), [bm], [e12])
                    kb.op("act", lambda e: e.activation(out=em[:, 4:8], in_=em[:, 0:4], func=AF.Exp), [em], [em])
                    kb.op("act", lambda e: e.activation(out=em[:, 0:4], in_=b3[:, :, t_end], func=AF.Exp), [Bp], [em])
                    kb.op("dve", lambda e: e.tensor_tensor(out=qf[:], in0=tpb[:, 0:256], in1=e12[:, 0, :], op=ALU.mult), [Tp, e12], [qf])
                    kb.op("dve", lambda e: e.tensor_copy(out=qk[:, 0, :], in_=qf[:]), [qf], [qk])
                    kb.op("dve", lambda e: e.tensor_tensor(out=qk[:, 1, :].rearrange("p (h t) -> p h t", h=4), in0=qf[:].rearrange("p (h t) -> p h t", h=4),
                                                           in1=em[:, 4:8].unsqueeze(2).to_broadcast([128, 4, 64]), op=ALU.mult), [qf, em], [qk])
                    kb.op("dve", lambda e: e.tensor_tensor(out=qk[:, 2, :], in0=tpb[:, 256:512], in1=e12[:, 1, :], op=ALU.mult), [Tp, e12], [qk])
                    Ap = g.PS[3]
                    if d == 0:
                        parts = [((0, 32), (0, 64)), ((32, 64), (32, 64))]
                    else:
                        parts = [((32, 64), (0, 64)), ((0, 32), (0, 32))]
                    for h in range(4):
                        for (s0, s1), (c0, c1) in parts:
                            kb.op("pe", lambda e, h=h, s0=s0, s1=s1, c0=c0, c1=c1: e.matmul(
                                Ap[s0:s1, h * 64 + c0:h * 64 + c1], lhsT=qk[:, 2, h * 64 + s0:h * 64 + s1], rhs=qk[:, 0, h * 64 + c0:h * 64 + c1],
                                start=True, stop=True, skip_group_check=True), [qk], [Ap])
                    for (s0, s1), (c0, c1) in parts:
                        kb.op("dve", lambda e, s0=s0, s1=s1, c0=c0, c1=c1: e.tensor_tensor(
                            out=at[s0:s1, :].rearrange("p (h t) -> p h t", h=4)[:, :, c0:c1],
                            in0=Ap[s0:s1, 0:256].rearrange("p (h t) -> p h t", h=4)[:, :, c0:c1],
                            in1=TRI[s0:s1, ti_incl, c0:c1].unsqueeze(1).to_broadcast([s1 - s0, 4, c1 - c0]), op=ALU.mult), [Ap, TRI], [at])
                    kb.op("act", lambda e: e.activation(out=ed[:], in_=Dp[0:64, :], func=AF.Exp), [Dp], [ed])
                    kb.op("dve", lambda e: e.tensor_tensor(out=kh[:], in0=kf[:], in1=ed[:], op=ALU.mult), [kf, ed], [kh])
                    if need_out:
                        Op = g.PS[4 + (step % 2)]
                        for h in range(4):
                            kb.op("pe", lambda e, h=h: e.matmul(Op[0:64, h * 128:(h + 1) * 128], lhsT=qk[:, 1, h * 64:(h + 1) * 64], rhs=Sb[:, h, :],
                                                                start=(h == 0), stop=False, skip_group_check=True), [qk, Sb], [Op])
                            kb.op("pe", lambda e, h=h: e.matmul(Op[0:64, h * 128:(h + 1) * 128], lhsT=at[:, h * 64:(h + 1) * 64], rhs=pc[:, 1536 + h * 128:1536 + (h + 1) * 128],
                                                                start=False, stop=True, skip_group_check=True), [at, pc], [Op])
                    Up = g.PS[6 + (step % 2)]
                    for h in range(4):
                        kb.op("pe", lambda e, h=h: e.matmul(Up[:, h * 128:(h + 1) * 128], lhsT=kh[:, h * 128:(h + 1) * 128], rhs=pc[:, 1536 + h * 128:1536 + (h + 1) * 128],
                                                            start=True, stop=True, skip_group_check=True), [kh, pc], [Up])
                    for h in range(4):
                        kb.op("dve", lambda e, h=h: e.scalar_tensor_tensor(out=S[:, h, :], in0=S[:, h, :], scalar=em[:, h:h + 1], in1=Up[:, h * 128:(h + 1) * 128],
                                                                           op0=ALU.mult, op1=ALU.add), [S, em, Up], [S])
                    kb.op("pool", lambda e: e.tensor_copy(out=Sb[:], in_=S[:]), [S], [Sb])
                    if need_out and d == 0:
                        kb.op("act", lambda e: e.activation(out=of[:], in_=Op[0:64, :], func=AF.Copy), [Op], [of])
                        dma(lambda e, of=of, r0=r0: e.dma_start(out=g.OF[r0:r0 + 64, :], in_=of[:]), [of], [g.OF])
                    if need_out and d == 1:
                        o3 = lambda b, j: b[:, j, :].rearrange("p (h v) -> p h v", h=4)
                        kb.op("dve", lambda e: e.tensor_tensor(out=ro[:, 0, :], in0=Op[0:64, :], in1=of[:], op=ALU.add), [Op, of], [ro])
                        kb.op("pool", lambda e: e.tensor_tensor(out=ro[:, 1, :], in0=ro[:, 0, :], in1=ro[:, 0, :], op=ALU.mult), [ro], [ro])
                        kb.op("dve", lambda e: e.tensor_reduce(out=rr[:, 0:4], in_=o3(ro, 1), axis=AX.X, op=ALU.add), [ro], [rr])
                        kb.op("dve", lambda e: e.tensor_scalar(out=rr[:, 0:4], in0=rr[:, 0:4], scalar1=1.0 / 128, scalar2=EPS, op0=ALU.mult, op1=ALU.add), [rr], [rr])
                        kb.op("act", lambda e: e.activation(out=rr[:, 4:8], in_=rr[:, 0:4], func=AF.Sqrt), [rr], [rr])
                        kb.op("dve", lambda e: e.reciprocal(out=rr[:, 0:4], in_=rr[:, 4:8]), [rr], [rr])
                        kb.op("dve", lambda e: e.tensor_tensor(out=o3(ro, 1), in0=o3(ro, 0), in1=rr[:, 0:4].unsqueeze(2).to_broadcast([64, 4, 128]), op=ALU.mult), [ro, rr], [ro])
                        kb.op("act", lambda e: e.activation(out=ro[:, 2, :], in_=pc[:, 2048:2560], func=AF.Silu), [pc], [ro])
                        kb.op("pool", lambda e: e.tensor_tensor(out=o3(ro, 0), in0=o3(ro, 1), in1=NGh[:].unsqueeze(1).to_broadcast([64, 4, 128]), op=ALU.mult), [ro, NGh], [ro])
                        kb.op("dve", lambda e: e.tensor_tensor(out=oo[:], in0=ro[:, 0, :], in1=ro[:, 2, :], op=ALU.mult), [ro], [oo])
                        dma(lambda e, oo=oo, r0=r0: e.dma_start(out=g.OB[r0:r0 + 64, 1024:1536], in_=oo[:]), [oo], [g.OB])
                kb.barrier()


def phase_merge(g, l, ctx_out):
    nc, kb = g.nc, g.kb
    from contextlib import ExitStack
    dma = lambda fn, r=(), w=(): kb.dma("sp", fn, r, w)
    Pv = g.P.t.rearrange("(t p) c -> p t c", p=128)
    OBv = g.OB.t.rearrange("(t p) c -> p t c", p=128)
    with ExitStack() as es:
        al = lambda shape, dt: es.enter_context(nc.sbuf_tensor(_nm(), shape, dt))
        WBR = Buf(al([128, 16, 1024], BF16), "WBR"); WO = Buf(al([128, 8, 1024], BF16), "WO")
        G1 = [Buf(al([128, 1024], F32), "G1x"), Buf(al([128, 1024], F32), "G1c")]
        ob_t = al([128, 2, 2048], BF16); gt_t = al([128, 2, 4096], BF16); obt_t = al([128, 2, 16, 128], BF16)
        sg_t = al([128, 3, 512], F32); tp_t = al([128, 3, 512], F32); y_t = al([128, 2, 1024], F32)
        yb_t = al([128, 2, 1024], BF16); yt_t = al([128, 2, 8, 128], BF16)
        mk = lambda t, nm, n: [Buf(t[:, i], nm + str(i)) for i in range(n)]
        OBs, GTs, OBTs, SGs, TPs, Ys, YBs, YTs = mk(ob_t, "ob", 2), mk(gt_t, "gt", 2), mk(obt_t, "obt", 2), mk(sg_t, "sg", 3), mk(tp_t, "tp", 3), mk(y_t, "y", 2), mk(yb_t, "yb", 2), mk(yt_t, "yt", 2)
        wsrc = g.W["w_branch"][l].rearrange("j (c p) n -> p j c n", p=128)
        for j in range(4):
            kb.dma("pool", lambda e, j=j: e.dma_start(out=WBR[:, j * 4:(j + 1) * 4, :], in_=wsrc[:, j]), [], [WBR])
        wo = g.W["w_out"][l].rearrange("(k p) n -> p k n", p=128)
        for j in range(2):
            kb.dma("pool", lambda e, j=j: e.dma_start(out=WO[:, j * 4:(j + 1) * 4, :], in_=wo[:, j * 4:(j + 1) * 4, :]), [], [WO])
        load_bcast(g, G1[0], g.MODS[l, 0:1, 2 * D:3 * D])
        load_bcast(g, G1[1], g.MODS[l, 1:2, 2 * D:3 * D])
        pi = 0
        si = 0
        for tt in range(NT if ctx_out else 16):
            i = tt % 2
            ob, gt, obt, y, yb, yt = OBs[i], GTs[i], OBTs[i], Ys[i], YBs[i], YTs[i]
            G = G1[0] if tt < 16 else G1[1]
            dma(lambda e, ob=ob, tt=tt: e.dma_start(out=ob[:], in_=OBv[:, tt, :]), [g.OB], [ob])
            dma(lambda e, gt=gt, tt=tt: e.dma_start(out=gt[:], in_=Pv[:, tt, 5312:9408]), [g.P], [gt])
            for hf in range(2):
                ps = g.PS[6 + hf]
                psb = ps[:].bitcast(BF16)
                for c in range(8):
                    kb.op("pe", lambda e, c=c, hf=hf, psb=psb, ob=ob: e.transpose(out=psb[:, c * 128:(c + 1) * 128], in_=ob[:, (hf * 8 + c) * 128:(hf * 8 + c + 1) * 128], identity=g.ident_b[:]), [ob, g.ident_b], [ps])
                kb.op("act", lambda e, hf=hf, psb=psb, obt=obt: e.activation(out=obt[:, hf * 8:(hf + 1) * 8, :], in_=psb.rearrange("p (k t) -> p k t", k=8), func=AF.Copy), [ps], [obt])
            for hf in range(2):
                for j in range(4):
                    ps = g.PS[pi % 4]; pi += 1
                    for c in range(4):
                        kb.op("pe", lambda e, c=c, j=j, hf=hf, ps=ps, obt=obt: e.matmul(ps[:], lhsT=obt[:, j * 4 + c, :], rhs=WBR[:, j * 4 + c, hf * 512:(hf + 1) * 512], start=(c == 0), stop=(c == 3)), [obt, WBR], [ps])
                    sg = SGs[si % 3]; tp = TPs[si % 3]; si += 1
                    kb.op("act", lambda e, sg=sg, gt=gt, j=j, hf=hf: e.activation(out=sg[:], in_=gt[:, j * 1024 + hf * 512:j * 1024 + (hf + 1) * 512], func=AF.Sigmoid), [gt], [sg])
                    ysl = (slice(None), slice(hf * 512, (hf + 1) * 512))
                    if j == 0:
                        kb.op("dve", lambda e, ps=ps, sg=sg, y=y, ysl=ysl: e.tensor_tensor(out=y[ysl], in0=ps[:], in1=sg[:], op=ALU.mult), [ps, sg], [y])
                    else:
                        kb.op("dve", lambda e, ps=ps, sg=sg, tp=tp: e.tensor_tensor(out=tp[:], in0=ps[:], in1=sg[:], op=ALU.mult), [ps, sg], [tp])
                        if j < 3:
                            kb.op("pool", lambda e, tp=tp, y=y, ysl=ysl: e.tensor_tensor(out=y[ysl], in0=y[ysl], in1=tp[:], op=ALU.add), [y, tp], [y])
                        else:
                            kb.op("pool", lambda e, tp=tp, y=y, yb=yb, ysl=ysl: e.tensor_tensor(out=yb[ysl], in0=y[ysl], in1=tp[:], op=ALU.add), [y, tp], [yb])
            ps = g.PS[6]
            psb = ps[:].bitcast(BF16)
            for c in range(8):
                kb.op("pe", lambda e, c=c, psb=psb, yb=yb: e.transpose(out=psb[:, c * 128:(c + 1) * 128], in_=yb[:, c * 128:(c + 1) * 128], identity=g.ident_b[:]), [yb, g.ident_b], [ps])
            kb.op("act", lambda e, psb=psb, yt=yt: e.activation(out=yt[:], in_=psb.rearrange("p (k t) -> p k t", k=8), func=AF.Copy), [ps], [yt])
            for hf in range(2):
                ps = g.PS[pi % 4]; pi += 1
                for k in range(8):
                    kb.op("pe", lambda e, k=k, hf=hf, ps=ps, yt=yt: e.matmul(ps[:], lhsT=yt[:, k, :], rhs=WO[:, k, hf * 512:(hf + 1) * 512], start=(k == 0), stop=(k == 7)), [yt, WO], [ps])
                tp = TPs[si % 3]; si += 1
                xs = g.X[tt]
                kb.op("dve", lambda e, ps=ps, tp=tp, G=G, hf=hf: e.tensor_tensor(out=tp[:], in0=ps[:], in1=G[:, hf * 512:(hf + 1) * 512], op=ALU.mult), [ps, G], [tp])
                kb.op("pool", lambda e, tp=tp, xs=xs, hf=hf: e.tensor_tensor(out=xs[:, hf * 512:(hf + 1) * 512], in0=xs[:, hf * 512:(hf + 1) * 512], in1=tp[:], op=ALU.add), [xs, tp], [xs])
        kb.barrier()


def phase_final(g):
    nc, kb = g.nc, g.kb
    from contextlib import ExitStack
    dma = lambda fn, r=(), w=(): kb.dma("sp", fn, r, w)
    with ExitStack() as es:
        al = lambda shape, dt: es.enter_context(nc.sbuf_tensor(_nm(), shape, dt))
        FG = Buf(al([128, 1024], F32), "FG")
        junk = Buf(al([128, 1024], F32), "junk")
        o_t = al([128, 2, 1024], F32); st_t = al([128, 2, 8], F32)
        Os = [Buf(o_t[:, i], "o%d" % i) for i in range(2)]; Ss = [Buf(st_t[:, i], "s%d" % i) for i in range(2)]
        dma(lambda e: e.dma_start(out=FG[:], in_=g.W["final_g"][0:1, :].partition_broadcast(128)), [], [FG])
        for tt in range(16):
            xin = g.X[tt]; st = Ss[tt % 2]; o = Os[tt % 2]
            kb.op("act", lambda e, xin=xin, st=st: e.activation(out=junk[:], in_=xin[:], func=AF.Square, accum_out=st[:, 0:1]), [xin], [junk, st])
            kb.op("dve", lambda e, st=st: e.tensor_scalar(out=st[:, 1:2], in0=st[:, 0:1], scalar1=1.0 / D, scalar2=EPS, op0=ALU.mult, op1=ALU.add), [st], [st])
            kb.op("act", lambda e, st=st: e.activation(out=st[:, 2:3], in_=st[:, 1:2], func=AF.Sqrt), [st], [st])
            kb.op("dve", lambda e, st=st: e.reciprocal(out=st[:, 3:4], in_=st[:, 2:3]), [st], [st])
            kb.op("dve", lambda e, xin=xin, st=st, o=o: e.scalar_tensor_tensor(out=o[:], in0=xin[:], scalar=st[:, 3:4], in1=FG[:], op0=ALU.mult, op1=ALU.mult), [xin, st, FG], [o])
            dma(lambda e, o=o, tt=tt: e.dma_start(out=g.out[tt * 128:(tt + 1) * 128, :], in_=o[:]), [o], [])
        kb.barrier()


def phase_peer(g, l, ctx_out):
    nc, kb = g.nc, g.kb
    from contextlib import ExitStack
    dma = lambda fn, r=(), w=(): kb.dma("sp", fn, r, w)
    NEG = -1.0e30
    u_flat = g.UB.t
    v_flat = g.VBF.t
    H2P = g.PSP[2]; ACCP = g.PSP[3]
    with ExitStack() as es:
        al = lambda shape, dt: es.enter_context(nc.sbuf_tensor(_nm(), shape, dt))
        WQ = Buf(al([128, 8, 1024], BF16), "WQ"); KT = Buf(al([128, 8, 128], BF16), "KT")
        GS = Buf(al([128, 1024], F32), "GS"); SH = Buf(al([128, 1024], F32), "SH"); G2 = Buf(al([128, 1024], F32), "G2")
        IO16 = Buf(al([128, 16], F32), "IO16")
        kb.op("pool", lambda e: e.iota(IO16[:], pattern=[[1, 16]], base=0, channel_multiplier=0, allow_small_or_imprecise_dtypes=True), [], [IO16])
        wq = g.W["peer_wq"][l].rearrange("(k p) n -> p k n", p=128)
        for j in range(2):
            kb.dma("pool", lambda e, j=j: e.dma_start(out=WQ[:, j * 4:(j + 1) * 4, :], in_=wq[:, j * 4:(j + 1) * 4, :]), [], [WQ])
        keyf = Buf(al([128, 16, 64], F32), "keyf"); keyb = Buf(al([128, 16, 64], BF16), "keyb")
        dma(lambda e: e.dma_start(out=keyf[:], in_=g.W["peer_keys"][l].rearrange("h p n d -> n (h p) d")), [], [keyf])
        kb.op("dve", lambda e: e.tensor_copy(out=keyb[:], in_=keyf[:]), [keyf], [keyb])
        ps = g.PS[3]
        psb = ps[:].bitcast(BF16)
        for h in range(8):
            kb.op("pe", lambda e, h=h: e.transpose(out=psb[:, h * 128:(h + 1) * 128], in_=keyb[:, h * 2:(h + 1) * 2, :].rearrange("n a d -> n (a d)"), identity=g.ident_b[:]), [keyb, g.ident_b], [ps])
        kb.op("act", lambda e: e.activation(out=KT[:], in_=psb.rearrange("p (h n) -> p h n", h=8), func=AF.Copy), [ps], [KT])
        junk = Buf(al([128, 1024], F32), "junk"); junkb = Buf(al([128, 1024], BF16), "junkb")
        hb_t = al([128, 2, 1024], BF16); hT_t = al([128, 2, 8, 128], BF16); qT_t = al([128, 2, 8, 128], BF16)
        st_t = al([128, 2, 8], F32)
        SC = Buf(al([128, 2, 8, 128], F32), "SC")
        TMP = Buf(al([128, 256], F32), "TMP")
        SV = Buf(al([128, 2, 8, 16], F32), "SV"); SI = Buf(al([128, 2, 8, 16], U32), "SI"); SIF = Buf(al([128, 2, 8, 16], F32), "SIF")
        CS_ = Buf(al([128, 8, 256], F32), "CS_")
        TS = Buf(al([128, 8, 16], F32), "TS"); POS = Buf(al([128, 8, 16], U32), "POS")
        PA = Buf(al([128, 2, 128], U32), "PA"); PAF = Buf(al([128, 2, 128], F32), "PAF")
        OH = Buf(al([128, 8, 16, 16], F32), "OH"); IAB = Buf(al([128, 2, 128], F32), "IAB")
        IDX = Buf(al([128, 128], I32), "IDX"); GW = Buf(al([128, 8, 16], F32), "GW"); RZ = Buf(al([128, 16], F32), "RZ")
        DOT = Buf(al([128, 128], F32), "DOT"); WGT = Buf(al([128, 128], F32), "WGT")
        NG = 8
        gb_t = al([128, NG, 1024], BF16)
        GB = [Buf(gb_t[:, i], "gb%d" % i) for i in range(NG)]
        HBs = [Buf(hb_t[:, i], "hb%d" % i) for i in range(2)]
        HTs = [Buf(hT_t[:, i], "hT%d" % i) for i in range(2)]; QTs = [Buf(qT_t[:, i], "qT%d" % i) for i in range(2)]
        STs = [Buf(st_t[:, i], "st%d" % i) for i in range(2)]
        gi = 0
        tiles = list(range(NT if ctx_out else 16))
        for tt in tiles:
            if tt == 0 or tt == 16:
                r = 0 if tt < 16 else 1
                dma(lambda e: e.dma_start(out=GS[:], in_=g.W["norm2_g"][l:l + 1, :].partition_broadcast(128)), [], [GS])
                load_bcast(g, junk, g.MODS[l, r:r + 1, 4 * D:5 * D])
                kb.op("dve", lambda e: e.scalar_tensor_tensor(out=GS[:], in0=junk[:], scalar=1.0, in1=GS[:], op0=ALU.add, op1=ALU.mult), [junk, GS], [GS])
                load_bcast(g, SH, g.MODS[l, r:r + 1, 3 * D:4 * D])
                load_bcast(g, G2, g.MODS[l, r:r + 1, 5 * D:6 * D])
            i = tt % 2
            hb, hT, qT, st = HBs[i], HTs[i], QTs[i], STs[i]
            h2 = H2P
            xin = g.X[tt]
            kb.op("act", lambda e: e.activation(out=junk[:], in_=xin[:], func=AF.Square, accum_out=st[:, 0:1]), [xin], [junk, st])
            kb.op("dve", lambda e: e.tensor_scalar(out=st[:, 1:2], in0=st[:, 0:1], scalar1=1.0 / D, scalar2=EPS, op0=ALU.mult, op1=ALU.add), [st], [st])
            kb.op("act", lambda e: e.activation(out=st[:, 2:3], in_=st[:, 1:2], func=AF.Sqrt), [st], [st])
            kb.op("dve", lambda e: e.reciprocal(out=st[:, 3:4], in_=st[:, 2:3]), [st], [st])
            kb.op("dve", lambda e: e.scalar_tensor_tensor(out=junk[:], in0=xin[:], scalar=st[:, 3:4], in1=GS[:], op0=ALU.mult, op1=ALU.mult), [xin, st, GS], [junk])
            kb.op("dve", lambda e: e.tensor_tensor(out=h2[:], in0=junk[:], in1=SH[:], op=ALU.add), [junk, SH], [h2])
            kb.op("act", lambda e: e.activation(out=hb[:], in_=h2[:], func=AF.Copy), [h2], [hb])
            ps = g.PS[3]
            psb = ps[:].bitcast(BF16)
            for k in range(8):
                kb.op("pe", lambda e, k=k: e.transpose(out=psb[:, k * 128:(k + 1) * 128], in_=hb[:, k * 128:(k + 1) * 128], identity=g.ident_b[:]), [hb, g.ident_b], [ps])
            kb.op("act", lambda e: e.activation(out=hT[:], in_=psb.rearrange("p (k t) -> p k t", k=8), func=AF.Copy), [ps], [hT])
            for hh in range(2):
                ps = g.PS[hh]
                for h4 in range(4):
                    h = hh * 4 + h4
                    for k in range(8):
                        kb.op("pe", lambda e, k=k, h=h, h4=h4, ps=ps: e.matmul(ps[:, h4 * 128:(h4 + 1) * 128], lhsT=WQ[:, k, h * 128:(h + 1) * 128], rhs=hT[:, k, :],
                                                                            start=(k == 0 and h4 == 0), stop=(k == 7), skip_group_check=True), [WQ, hT], [ps])
                kb.op("act", lambda e, hh=hh, ps=ps: e.activation(out=qT[:, hh * 4:(hh + 1) * 4, :], in_=ps[:].rearrange("p (h t) -> p h t", h=4), func=AF.Copy), [ps], [qT])
            for p in range(2):
                for hh in range(2):
                    ps = g.PS[p * 2 + hh]
                    for h4 in range(4):
                        h = hh * 4 + h4
                        kb.op("pe", lambda e, p=p, h=h, h4=h4, ps=ps: e.matmul(ps[:, h4 * 128:(h4 + 1) * 128], lhsT=qT[p * 64:(p + 1) * 64, h, :], rhs=KT[p * 64:(p + 1) * 64, h, :],
                                                                            start=True, stop=True, skip_group_check=True), [qT, KT], [ps])
                    kb.op("act", lambda e, p=p, hh=hh, ps=ps: e.activation(out=SC[:, p, hh * 4:(hh + 1) * 4, :], in_=ps[:].rearrange("p (h t) -> p h t", h=4), func=AF.Copy), [ps], [SC])
            for p in range(2):
                for h in range(8):
                    s_ = SC[:, p, h, :]
                    kb.op("dve", lambda e, p=p, h=h, s_=s_: e.max(out=SV[:, p, h, 0:8], in_=s_), [SC], [SV])
                    kb.op("dve", lambda e, p=p, h=h, s_=s_: e.max_index(out=SI[:, p, h, 0:8], in_max=SV[:, p, h, 0:8], in_values=s_), [SC, SV], [SI])
                    kb.op("dve", lambda e, p=p, h=h, s_=s_: e.match_replace(out=TMP[:, 0:128], in_to_replace=SV[:, p, h, 0:8], in_values=s_, imm_value=NEG), [SC, SV], [TMP])
                    kb.op("dve", lambda e, p=p, h=h: e.max(out=SV[:, p, h, 8:16], in_=TMP[:, 0:128]), [TMP], [SV])
                    kb.op("dve", lambda e, p=p, h=h: e.max_index(out=SI[:, p, h, 8:16], in_max=SV[:, p, h, 8:16], in_values=TMP[:, 0:128]), [TMP, SV], [SI])
            kb.op("dve", lambda e: e.tensor_copy(out=SIF[:], in_=SI[:]), [SI], [SIF])
            c4 = CS_[:].rearrange("p h (a b) -> p h a b", a=16)
            kb.op("dve", lambda e: e.tensor_tensor(out=c4, in0=SV[:, 0].unsqueeze(3).to_broadcast([128, 8, 16, 16]), in1=SV[:, 1].unsqueeze(2).to_broadcast([128, 8, 16, 16]), op=ALU.add), [SV], [CS_])
            for h in range(8):
                kb.op("dve", lambda e, h=h: e.max(out=TS[:, h, 0:8], in_=CS_[:, h, :]), [CS_], [TS])
                kb.op("dve", lambda e, h=h: e.max_index(out=POS[:, h, 0:8], in_max=TS[:, h, 0:8], in_values=CS_[:, h, :]), [CS_, TS], [POS])
                kb.op("dve", lambda e, h=h: e.match_replace(out=TMP[:], in_to_replace=TS[:, h, 0:8], in_values=CS_[:, h, :], imm_value=NEG), [CS_, TS], [TMP])
                kb.op("dve", lambda e, h=h: e.max(out=TS[:, h, 8:16], in_=TMP[:]), [TMP], [TS])
                kb.op("dve", lambda e, h=h: e.max_index(out=POS[:, h, 8:16], in_max=TS[:, h, 8:16], in_values=TMP[:]), [TMP, TS], [POS])
            posf = POS[:].rearrange("p h j -> p (h j)")
            kb.op("dve", lambda e: e.tensor_single_scalar(out=PA[:, 0, :], in_=posf, scalar=4, op=ALU.logical_shift_right), [POS], [PA])
            kb.op("dve", lambda e: e.tensor_single_scalar(out=PA[:, 1, :], in_=posf, scalar=15, op=ALU.bitwise_and), [POS], [PA])
            kb.op("dve", lambda e: e.tensor_copy(out=PAF[:], in_=PA[:]), [PA], [PAF])
            for w in range(2):
                pa3 = PAF[:, w, :].rearrange("p (h j) -> p h j", h=8)
                kb.op("dve", lambda e, pa3=pa3: e.tensor_tensor(out=OH[:], in0=pa3.unsqueeze(3).to_broadcast([128, 8, 16, 16]),
                                                               in1=IO16[:].unsqueeze(1).unsqueeze(1).to_broadcast([128, 8, 16, 16]), op=ALU.is_equal), [PAF, IO16], [OH])
                kb.op("dve", lambda e, w=w: e.tensor_tensor(out=OH[:], in0=OH[:], in1=SIF[:, w].unsqueeze(2).to_broadcast([128, 8, 16, 16]), op=ALU.mult), [OH, SIF], [OH])
                kb.op("dve", lambda e, w=w: e.tensor_reduce(out=IAB[:, w, :].rearrange("p (h j) -> p h j", h=8), in_=OH[:], axis=AX.X, op=ALU.add), [OH], [IAB])
            kb.op("dve", lambda e: e.tensor_scalar(out=IAB[:, 0, :], in0=IAB[:, 0, :], scalar1=128.0, scalar2=float(l * 16384), op0=ALU.mult, op1=ALU.add), [IAB], [IAB])
            kb.op("dve", lambda e: e.tensor_tensor(out=IAB[:, 0, :], in0=IAB[:, 0, :], in1=IAB[:, 1, :], op=ALU.add), [IAB], [IAB])
            kb.op("dve", lambda e: e.tensor_copy(out=IDX[:], in_=IAB[:, 0, :]), [IAB], [IDX])
            kb.op("dve", lambda e: e.tensor_tensor(out=GW[:], in0=TS[:], in1=TS[:, :, 0:1].to_broadcast([128, 8, 16]), op=ALU.subtract), [TS], [GW])
            kb.op("act", lambda e: e.activation(out=GW[:], in_=GW[:], func=AF.Exp), [GW], [GW])
            kb.op("dve", lambda e: e.tensor_reduce(out=RZ[:, 0:8], in_=GW[:], axis=AX.X, op=ALU.add), [GW], [RZ])
            kb.op("dve", lambda e: e.reciprocal(out=RZ[:, 8:16], in_=RZ[:, 0:8]), [RZ], [RZ])
            kb.op("dve", lambda e: e.tensor_tensor(out=GW[:], in0=GW[:], in1=RZ[:, 8:16].unsqueeze(2).to_broadcast([128, 8, 16]), op=ALU.mult), [GW, RZ], [GW])
            for s in range(128):
                gbuf = GB[gi % NG]; gi += 1
                kb.dma("pool", lambda e, gbuf=gbuf, s=s: e.indirect_dma_start(out=gbuf[:], out_offset=None, in_=u_flat,
                       in_offset=bass.IndirectOffsetOnAxis(ap=IDX[:, s:s + 1], axis=0)), [IDX], [gbuf])
                kb.op("dve", lambda e, gbuf=gbuf, s=s: e.scalar_tensor_tensor(out=junkb[:], in0=gbuf[:], scalar=1.0, in1=h2[:], op0=ALU.mult, op1=ALU.mult,
                                                                              accum_out=DOT[:, s:s + 1]), [gbuf, h2], [junkb, DOT])
            kb.op("act", lambda e: e.activation(out=WGT[:], in_=DOT[:], func=AF.Gelu), [DOT], [WGT])
            kb.op("dve", lambda e: e.tensor_tensor(out=WGT[:], in0=WGT[:], in1=GW[:].rearrange("p h j -> p (h j)"), op=ALU.mult), [WGT, GW], [WGT])
            for s in range(128):
                gbuf = GB[gi % NG]; gi += 1
                kb.dma("pool", lambda e, gbuf=gbuf, s=s: e.indirect_dma_start(out=gbuf[:], out_offset=None, in_=v_flat,
                       in_offset=bass.IndirectOffsetOnAxis(ap=IDX[:, s:s + 1], axis=0)), [IDX], [gbuf])
                if s == 0:
                    kb.op("dve", lambda e, gbuf=gbuf, s=s: e.tensor_scalar(out=ACCP[:], in0=gbuf[:], scalar1=WGT[:, s:s + 1], scalar2=None, op0=ALU.mult), [gbuf, WGT], [ACCP])
                else:
                    kb.op("dve", lambda e, gbuf=gbuf, s=s: e.scalar_tensor_tensor(out=ACCP[:], in0=gbuf[:], scalar=WGT[:, s:s + 1], in1=ACCP[:], op0=ALU.mult, op1=ALU.add), [gbuf, WGT, ACCP], [ACCP])
            kb.op("dve", lambda e: e.tensor_tensor(out=junk[:], in0=ACCP[:], in1=G2[:], op=ALU.mult), [ACCP, G2], [junk])
            kb.op("dve", lambda e: e.tensor_tensor(out=xin[:], in0=xin[:], in1=junk[:], op=ALU.add), [xin, junk], [xin])
        kb.barrier()


def kernel(**inputs):
    nc, g = build_program()
    consts = host_consts()
    shared = {n: np.ascontiguousarray(np.asarray(inputs[n], dtype=np.float32)).reshape(s) for n, s in W_NAMES}
    shared.update(consts)
    shared["c_ctx"] = np.ascontiguousarray(np.asarray(inputs["c_ctx"], dtype=np.float32)).reshape(1, D)
    x = np.asarray(inputs["x"], dtype=np.float32)
    c = np.asarray(inputs["c"], dtype=np.float32)
    ctx = np.asarray(inputs["ctx"], dtype=np.float32)
    nb = x.shape[0]
    in_maps = []
    for b in range(nb):
        m = dict(shared)
        m["x"] = np.ascontiguousarray(x[b])
        m["ctx"] = np.ascontiguousarray(ctx[b])
        m["c"] = np.ascontiguousarray(c[b:b + 1])
        in_maps.append(m)
    res = run_bass_kernel_spmd(nc, in_maps, core_ids=list(range(nb)))
    out = np.stack([np.asarray(r["out"], dtype=np.float32) for r in res.results], axis=0)
    return out


def phase_cast_experts(g, n_layers):
    nc, kb = g.nc, g.kb
    from contextlib import ExitStack
    with ExitStack() as es:
        al = lambda shape, dt: es.enter_context(nc.sbuf_tensor(_nm(), shape, dt))
        f_t = al([128, 2, 8, 1024], F32); b_t = al([128, 2, 8, 1024], BF16)
        Fs = [Buf(f_t[:, i], "cf%d" % i) for i in range(2)]; Bs = [Buf(b_t[:, i], "cb%d" % i) for i in range(2)]
        srcs = [(g.W["peer_u"].rearrange("l e d -> (l e) d"), g.UB), (g.W["peer_v"].rearrange("l e d -> (l e) d"), g.VBF)]
        n = 0
        engs = ["act", "dve", "pool"]
        for src, dst in srcs:
            for ch in range(n_layers * 16):
                r0 = ch * 1024
                f = Fs[n % 2]; b = Bs[n % 2]
                kb.dma("sp", lambda e, f=f, src=src, r0=r0: e.dma_start(out=f[:], in_=src[r0:r0 + 1024, :].rearrange("(p j) d -> p j d", p=128)), [], [f])
                for q in range(4):
                    en = engs[(n * 4 + q) % 3]
                    if en == "act":
                        kb.op("act", lambda e, f=f, b=b, q=q: e.activation(out=b[:, q * 2:(q + 1) * 2, :], in_=f[:, q * 2:(q + 1) * 2, :], func=AF.Copy), [f], [b])
                    else:
                        kb.op(en, lambda e, f=f, b=b, q=q: e.tensor_copy(out=b[:, q * 2:(q + 1) * 2, :], in_=f[:, q * 2:(q + 1) * 2, :]), [f], [b])
                kb.dma("sp", lambda e, b=b, dst=dst, r0=r0: e.dma_start(out=dst[r0:r0 + 1024, :].rearrange("(p j) d -> p j d", p=128), in_=b[:]), [b], [dst])
                n += 1
        kb.barrier()
```

```python
import numpy as np
import math
import concourse.bass as bass
import concourse.mybir as mybir
from concourse.bass_utils import run_bass_kernel_spmd

F32 = mybir.dt.float32
BF16 = mybir.dt.bfloat16
U32 = mybir.dt.uint32
I32 = mybir.dt.int32
AF = mybir.ActivationFunctionType
ALU = mybir.AluOpType
AX = mybir.AxisListType

NDS = 40


class Buf:
    __slots__ = ("t", "w", "r", "name", "nt")

    def __init__(self, t, name="", nt=False):
        self.t = t
        self.w = None
        self.r = []
        self.name = name
        self.nt = nt

    def __getitem__(self, k):
        return self.t[k]


class KB:
    def __init__(self, nc):
        self.nc = nc
        self.E = {"pe": nc.tensor, "act": nc.scalar, "dve": nc.vector, "pool": nc.gpsimd, "sp": nc.sync}
        self.esem = {n: nc.alloc_semaphore("s_" + n) for n in self.E}
        self.ecnt = {n: 0 for n in self.E}
        self.dsems = [nc.alloc_semaphore("d%d" % i) for i in range(NDS)]
        self.dcnt = [0] * NDS
        self.dnext = 0
        self.known = {n: {} for n in self.E}
        self.nwait = 0
        self.ninst = 0

    def _sem(self, key):
        return self.esem[key] if isinstance(key, str) else self.dsems[key]

    def _need(self, eng, ev):
        key, val, _ = ev
        if self.known[eng].get(key, 0) >= val:
            return
        self.E[eng].wait_ge(self._sem(key), val)
        self.known[eng][key] = val
        self.nwait += 1

    def _deps(self, eng, reads, writes, is_dma=False):
        for b in reads:
            if b.w is not None:
                self._need(eng, b.w)
        for b in writes:
            if b.w is not None and (is_dma or b.w[2] != eng):
                self._need(eng, b.w)
            for r in b.r:
                if is_dma or r[2] != eng:
                    self._need(eng, r)

    def op(self, eng, fn, reads=(), writes=()):
        reads = [b for b in reads if not b.nt]
        writes = [b for b in writes if not b.nt]
        self._deps(eng, reads, writes)
        inst = fn(self.E[eng])
        self.ecnt[eng] += 1
        inst.then_inc(self.esem[eng], 1)
        ev = (eng, self.ecnt[eng], eng)
        for b in reads:
            b.r.append(ev)
        for b in writes:
            b.w = ev
            b.r = []
        self.ninst += 1
        return inst

    def dma(self, q, fn, reads=(), writes=()):
        reads = [b for b in reads if not b.nt]
        writes = [b for b in writes if not b.nt]
        self._deps(q, reads, writes, is_dma=True)
        i = self.dnext
        self.dnext = (self.dnext + 1) % NDS
        if self.dcnt[i] > 0:
            self._need(q, (i, self.dcnt[i], "dma"))
        inst = fn(self.E[q])
        self.dcnt[i] += 16
        inst.then_inc(self.dsems[i], 16)
        ev = (i, self.dcnt[i], "dma")
        for b in reads:
            b.r.append(ev)
        for b in writes:
            b.w = ev
            b.r = []
        self.ninst += 1
        return inst

    def barrier(self):
        evs = [(n, self.ecnt[n], n) for n in self.E if self.ecnt[n] > 0]
        evs += [(i, self.dcnt[i], "dma") for i in range(NDS) if self.dcnt[i] > 0]
        for n in self.E:
            for ev in evs:
                self._need(n, ev)

    def finish(self):
        self.barrier()


D = 1024
NX = 2048
NC_ = 256
T = NX + NC_
NT = T // 128
P_IN = 9408
EPS = 1e-6
OFF = {"a_q1": 0, "a_q2": 256, "a_k1": 512, "a_k2": 768, "a_v": 1024,
       "b_cq": 1536, "b_ckv": 1792, "b_kr": 1920,
       "c_q": 1984, "c_f": 2496, "c_b": 3008, "c_i": 3520, "c_g": 4032,
       "d_q": 4544, "d_k": 5056, "d_v": 5184, "gate": 5312}

W_NAMES = [("w_mod", [2, 1024, 6144]), ("b_mod", [2, 6144]), ("norm1_g", [2, 1024]), ("norm2_g", [2, 1024]),
           ("w_in", [2, 1024, 9408]), ("diff_lam_q1", [2, 64]), ("diff_lam_k1", [2, 64]), ("diff_lam_q2", [2, 64]),
           ("diff_lam_k2", [2, 64]), ("diff_norm_g", [2, 128]), ("mla_qnorm_g", [2, 256]), ("mla_kvnorm_g", [2, 128]),
           ("mla_w_uq", [2, 256, 768]), ("mla_w_ukv", [2, 128, 1024]), ("hgrn_lb", [2, 2, 512]), ("hgrn_norm_g", [2, 128]),
           ("win_sink", [2, 8]), ("w_branch", [2, 4, 512, 1024]), ("w_out", [2, 1024, 1024]), ("peer_wq", [2, 1024, 1024]),
           ("peer_keys", [2, 8, 2, 128, 64]), ("peer_u", [2, 16384, 1024]), ("peer_v", [2, 16384, 1024]), ("final_g", [1, 1024])]


def host_consts():
    n = NX
    rows = np.repeat(np.arange(n // 64, dtype=np.float32), 64)
    cols = np.tile(np.arange(64, dtype=np.float32), n // 64)
    m = 32
    freqs = (10000.0 ** (-2.0 * np.arange(m // 2, dtype=np.float32) / m)).astype(np.float32)
    ang = np.concatenate([rows[:, None] * freqs, cols[:, None] * freqs], axis=-1).astype(np.float32)
    cs = np.zeros((T, 64), np.float32)
    cs[:n, :32] = np.cos(ang)
    cs[:n, 32:] = np.sin(ang)
    cs[n:, :32] = 1.0
    i = np.arange(128)
    c = {}
    c["rope_cs"] = cs
    c["ident"] = np.eye(128, dtype=np.float32)
    c["mask_prev"] = (i[:, None] >= i[None, :]).astype(np.float32)
    c["mask_next"] = (i[:, None] <= i[None, :]).astype(np.float32)
    j = np.arange(64)
    tri = np.zeros((64, 4, 64), np.float32)
    tri[:, 0] = (j[:, None] <= j[None, :])
    tri[:, 1] = (j[:, None] >= j[None, :])
    tri[:, 2] = (j[:, None] > j[None, :])
    tri[:, 3] = (j[:, None] < j[None, :])
    c["tri"] = tri.reshape(64, 256)
    return c


class Ctx:
    pass


_nmc = [0]


def _nm():
    _nmc[0] += 1
    return "tmp%d" % _nmc[0]


def build_program(n_layers=2, debug=(), stop_after=None):
    nc = bass.Bass("TRN2", target_bir_lowering=False)
    kb = KB(nc)
    g = Ctx()
    g.nc, g.kb = nc, kb
    g.debug = debug

    def din(name, shape, dt=F32):
        return nc.dram_tensor(name, list(shape), dt, kind="ExternalInput").ap()

    g.x_in = din("x", [NX, D])
    g.c_in = din("c", [1, D])
    g.ctx_in = din("ctx", [NC_, D])
    g.cctx_in = din("c_ctx", [1, D])
    g.W = {n: din(n, s) for n, s in W_NAMES}
    g.rope_cs = din("rope_cs", [T, 64])
    g.ident_in = din("ident", [128, 128])
    g.mprev_in = din("mask_prev", [128, 128])
    g.mnext_in = din("mask_next", [128, 128])
    g.tri_in = din("tri", [64, 256])
    g.out = nc.dram_tensor("out", [NX, D], F32, kind="ExternalOutput").ap()

    def scratch(name, shape, dt):
        kind = "ExternalOutput" if name in debug else "Internal"
        return Buf(nc.dram_tensor(name, list(shape), dt, kind=kind).ap(), name, nt=True)

    g.P = scratch("P", [T, P_IN], BF16)
    g.OB = scratch("OB", [T, 2048], BF16)
    g.MODS = scratch("MODS", [2, 2, 6144], F32)
    g.OF = scratch("OF", [T, 512], F32)
    g.UV = scratch("UV", [2 * 16384, 2 * D], BF16)

    cnt = [0]

    def sb(shape, dt, name=None):
        cnt[0] += 1
        return Buf(nc.alloc_sbuf_tensor("%s_%d" % (name or "t", cnt[0]), list(shape), dt), name or "t")
    g.sb = sb

    g.X = [Buf(None, "X%d" % i) for i in range(NT)]
    xall = nc.alloc_sbuf_tensor("Xall", [128, NT, D], F32)
    for i in range(NT):
        g.X[i].t = xall[:, i, :]
    psall = nc.alloc_psum_tensor("psall", [128, 8, 512], F32)
    g.PS = [Buf(psall[:, i, :], "ps%d" % i) for i in range(8)]
    g.PSP = [Buf(psall[:, 2 * i:2 * i + 2, :].rearrange("p b c -> p (b c)"), "psp%d" % i) for i in range(4)]
    g.ident_f = sb([128, 128], F32, "identf")
    g.ident_b = sb([128, 128], BF16, "identb")
    g.ones_f = sb([128, 128], F32, "onesf")
    g.SCT = sb([128, 8, 2], F32, "sct")

    dma = lambda fn, r=(), w=(): kb.dma("sp", fn, r, w)

    for i in range(16):
        dma(lambda e, i=i: e.dma_start(out=g.X[i][:], in_=g.x_in[i * 128:(i + 1) * 128, :]), [], [g.X[i]])
    for i in range(2):
        dma(lambda e, i=i: e.dma_start(out=g.X[16 + i][:], in_=g.ctx_in[i * 128:(i + 1) * 128, :]), [], [g.X[16 + i]])
    dma(lambda e: e.dma_start(out=g.ident_f[:], in_=g.ident_in[:, :]), [], [g.ident_f])
    kb.op("dve", lambda e: e.tensor_copy(out=g.ident_b[:], in_=g.ident_f[:]), [g.ident_f], [g.ident_b])
    kb.op("dve", lambda e: e.memset(g.ones_f[:], 1.0), [], [g.ones_f])
    craw = sb([128, 8, 2], F32, "craw")
    with nc.allow_non_contiguous_dma(reason="tiny conditioning vector load"):
        dma(lambda e: e.dma_start(out=craw[:, :, 0], in_=g.c_in.rearrange("o (k p) -> p (o k)", p=128)), [], [craw])
        dma(lambda e: e.dma_start(out=craw[:, :, 1], in_=g.cctx_in.rearrange("o (k p) -> p (o k)", p=128)), [], [craw])
    kb.op("act", lambda e: e.activation(out=g.SCT[:], in_=craw[:], func=AF.Silu), [craw], [g.SCT])

    if "skip_cast" not in debug:
        with nc.named_scope("cast"):
            phase_cast_experts(g, n_layers)
    for l in range(n_layers):
        last = (l == 1)
        with nc.named_scope("mod%d" % l):
            phase_mod(g, l)
        if stop_after == ("mod", l):
            break
        if "skip_proj" not in debug:
            with nc.named_scope("proj%d" % l):
                phase_norm_proj(g, l)
        if stop_after == ("proj", l):
            break
        ctx_out = not last
        if "skip_a" not in debug:
            with nc.named_scope("a%d" % l):
                phase_mixer_a(g, l, ctx_out)
        if stop_after == ("a", l):
            break
        if "skip_b" not in debug:
            with nc.named_scope("b%d" % l):
                phase_mixer_b(g, l, ctx_out)
        if stop_after == ("b", l):
            break
        if "skip_d" not in debug:
            with nc.named_scope("d%d" % l):
                phase_mixer_d(g, l, ctx_out)
        if stop_after == ("d", l):
            break
        if "skip_c" not in debug:
            with nc.named_scope("c%d" % l):
                phase_mixer_c(g, l, ctx_out)
        if stop_after == ("c", l):
            break
        if "skip_merge" not in debug:
            with nc.named_scope("merge%d" % l):
                phase_merge(g, l, ctx_out)
        if "Xdbg" in debug and stop_after == ("merge", l):
            xd = nc.dram_tensor("Xdbg", [NT, 128, D], F32, kind="ExternalOutput").ap()
            for i in range(NT):
                dma(lambda e, i=i: e.dma_start(out=xd[i], in_=g.X[i][:]), [g.X[i]], [])
        if stop_after == ("merge", l):
            break
        with nc.named_scope("peer%d" % l):
            phase_peer(g, l, ctx_out)
        if "Xdbg" in debug and stop_after == ("peer", l):
            xd = nc.dram_tensor("Xdbg", [NT, 128, D], F32, kind="ExternalOutput").ap()
            for i in range(NT):
                dma(lambda e, i=i: e.dma_start(out=xd[i], in_=g.X[i][:]), [g.X[i]], [])
        if stop_after == ("peer", l):
            break

    if stop_after is None:
        with nc.named_scope("final"):
            phase_final(g)
    kb.finish()
    g.nc = nc
    return nc, g


def phase_mod(g, l):
    nc, kb = g.nc, g.kb
    kb.barrier()
    dma = lambda fn, r=(), w=(): kb.dma("sp", fn, r, w)
    with nc.sbuf_tensor(_nm(), [128, 2, 8, 512], F32) as wm_t, nc.sbuf_tensor(_nm(), [1, 6144], F32) as bm_t, \
            nc.sbuf_tensor(_nm(), [2, 2, 512], F32) as ev_t:
        wm = [Buf(wm_t[:, i], "wm%d" % i) for i in range(2)]
        bm = Buf(bm_t, "bm")
        ev = [Buf(ev_t[:, i], "ev%d" % i) for i in range(2)]
        dma(lambda e: e.dma_start(out=bm[:], in_=g.W["b_mod"][l:l + 1, :]), [], [bm])
        wsrc = g.W["w_mod"][l].rearrange("(k p) c -> p k c", p=128)
        for m in range(12):
            w = wm[m % 2]
            dma(lambda e, w=w, m=m: e.dma_start(out=w[:], in_=wsrc[:, :, m * 512:(m + 1) * 512]), [], [w])
            ps = g.PS[m % 2]
            for k in range(8):
                kb.op("pe", lambda e, w=w, k=k, ps=ps: e.matmul(ps[0:2, :], lhsT=g.SCT[:, k, :], rhs=w[:, k, :],
                                                              start=(k == 0), stop=False), [g.SCT, w], [ps])
            kb.op("pe", lambda e, ps=ps, m=m: e.matmul(ps[0:2, :], lhsT=g.ones_f[0:1, 0:2], rhs=bm[0:1, m * 512:(m + 1) * 512],
                                                     start=False, stop=True), [g.ones_f, bm], [ps])
            o = ev[m % 2]
            kb.op("act", lambda e, o=o, ps=ps: e.activation(out=o[:], in_=ps[0:2, :], func=AF.Copy), [ps], [o])
            dma(lambda e, o=o, m=m: e.dma_start(out=g.MODS[l, :, m * 512:(m + 1) * 512], in_=o[:]), [o], [g.MODS])
        kb.barrier()


def load_bcast(g, dst, src_row_ap):
    g.kb.dma("sp", lambda e: e.dma_start(out=dst[:], in_=src_row_ap.partition_broadcast(128)), [g.MODS], [dst])


def norm_tile(g, xin, GS, SH, hb, scr):
    kb = g.kb
    junk, st = scr
    kb.op("act", lambda e: e.activation(out=junk[:], in_=xin[:], func=AF.Square, accum_out=st[:, 0:1]), [xin], [junk, st])
    kb.op("dve", lambda e: e.tensor_scalar(out=st[:, 1:2], in0=st[:, 0:1], scalar1=1.0 / D, scalar2=EPS, op0=ALU.mult, op1=ALU.add), [st], [st])
    kb.op("act", lambda e: e.activation(out=st[:, 2:3], in_=st[:, 1:2], func=AF.Sqrt), [st], [st])
    kb.op("dve", lambda e: e.reciprocal(out=st[:, 3:4], in_=st[:, 2:3]), [st], [st])
    kb.op("dve", lambda e: e.scalar_tensor_tensor(out=junk[:], in0=xin[:], scalar=st[:, 3:4], in1=GS[:], op0=ALU.mult, op1=ALU.mult), [xin, st, GS], [junk])
    kb.op("dve", lambda e: e.tensor_tensor(out=hb[:], in0=junk[:], in1=SH[:], op=ALU.add), [junk, SH], [hb])


def phase_norm_proj(g, l):
    nc, kb = g.nc, g.kb
    dma = lambda fn, r=(), w=(): kb.dma("sp", fn, r, w)
    with nc.sbuf_tensor(_nm(), [128, 8, T], BF16) as hT_t:
        hT = Buf(hT_t, "hT")
        with nc.sbuf_tensor(_nm(), [128, 5, D], F32) as bc_t, nc.sbuf_tensor(_nm(), [128, D], F32) as junk_t, \
                nc.sbuf_tensor(_nm(), [128, 2, 8], F32) as st_t, nc.sbuf_tensor(_nm(), [128, 2, D], BF16) as hb_t:
            G1, SCx, SHx, SCc, SHc = [Buf(bc_t[:, i], "bc%d" % i) for i in range(5)]
            junk = Buf(junk_t, "junk")
            sts = [Buf(st_t[:, i], "st%d" % i) for i in range(2)]
            hbs = [Buf(hb_t[:, i], "hb%d" % i) for i in range(2)]
            dma(lambda e: e.dma_start(out=G1[:], in_=g.W["norm1_g"][l:l + 1, :].partition_broadcast(128)), [], [G1])
            load_bcast(g, SHx, g.MODS[l, 0:1, 0:D])
            load_bcast(g, SCx, g.MODS[l, 0:1, D:2 * D])
            load_bcast(g, SHc, g.MODS[l, 1:2, 0:D])
            load_bcast(g, SCc, g.MODS[l, 1:2, D:2 * D])
            for S in (SCx, SCc):
                kb.op("dve", lambda e, S=S: e.scalar_tensor_tensor(out=S[:], in0=S[:], scalar=1.0, in1=G1[:], op0=ALU.add, op1=ALU.mult), [S, G1], [S])
            for tt in range(NT):
                GS, SH = (SCx, SHx) if tt < 16 else (SCc, SHc)
                hb = hbs[tt % 2]
                norm_tile(g, g.X[tt], GS, SH, hb, (junk, sts[tt % 2]))
                ps = g.PS[tt % 2]
                psb = ps[:].bitcast(BF16)
                for k in range(8):
                    kb.op("pe", lambda e, k=k, hb=hb, psb=psb: e.transpose(out=psb[:, k * 128:(k + 1) * 128], in_=hb[:, k * 128:(k + 1) * 128], identity=g.ident_b[:]), [hb, g.ident_b], [ps])
                kb.op("act", lambda e, tt=tt, psb=psb: e.activation(out=hT[:, :, tt * 128:(tt + 1) * 128], in_=psb.rearrange("p (k t) -> p k t", k=8), func=AF.Copy), [ps], [hT])
            kb.barrier()
        with nc.sbuf_tensor(_nm(), [128, 2, 8, 512], BF16) as wb_t, nc.sbuf_tensor(_nm(), [128, 2, 6, 512], BF16) as po_t:
            wbs = [Buf(wb_t[:, i], "wb%d" % i) for i in range(2)]
            pos = [Buf(po_t[:, i], "po%d" % i) for i in range(2)]
            wsrc = g.W["w_in"][l].rearrange("(k p) c -> p k c", p=128)
            pdst = g.P.t.rearrange("(t p) c -> p t c", p=128)
            nct = (P_IN + 511) // 512
            cw = lambda ct: min(512, P_IN - ct * 512)

            def load_w(ct):
                w = wbs[ct % 2]
                kb.dma("pool", lambda e: e.dma_start(out=w[:, :, 0:cw(ct)], in_=wsrc[:, :, ct * 512:ct * 512 + cw(ct)]), [], [w])
            load_w(0)
            oi = 0
            pi = 0
            for ct in range(nct):
                if ct + 1 < nct:
                    load_w(ct + 1)
                w = wbs[ct % 2]
                n = cw(ct)
                for tg in range(3):
                    po = pos[oi % 2]
                    oi += 1
                    for j in range(6):
                        tt = tg * 6 + j
                        ps = g.PS[2 + pi % 4]
                        pi += 1
                        for k in range(8):
                            kb.op("pe", lambda e, k=k, tt=tt, ps=ps, w=w, n=n: e.matmul(ps[:, 0:n], lhsT=hT[:, k, tt * 128:(tt + 1) * 128], rhs=w[:, k, 0:n],
                                                                                     start=(k == 0), stop=(k == 7)), [hT, w], [ps])
                        if pi % 2 == 0:
                            kb.op("act", lambda e, j=j, ps=ps, po=po, n=n: e.activation(out=po[:, j, 0:n], in_=ps[:, 0:n], func=AF.Copy), [ps], [po])
                        else:
                            kb.op("dve", lambda e, j=j, ps=ps, po=po, n=n: e.tensor_copy(out=po[:, j, 0:n], in_=ps[:, 0:n]), [ps], [po])
                    dma(lambda e, po=po, tg=tg, ct=ct, n=n: e.dma_start(out=pdst[:, tg * 6:(tg + 1) * 6, ct * 512:ct * 512 + n], in_=po[:, :, 0:n]), [po], [g.P])
            kb.barrier()


def attention(g, nh, dv, scale, V, qk_chunks, groups, post, npass=1):
    nc, kb = g.nc, g.kb
    assert dv + 1 <= 256
    with nc.sbuf_tensor(_nm(), [128, 3, 512], BF16) as E_t, nc.sbuf_tensor(_nm(), [128, 4, 4, dv + 1], F32) as of_t:
        Es = [Buf(E_t[:, i], "E%d" % i) for i in range(3)]
        Ofs = [Buf(of_t[:, i], "Of%d" % i) for i in range(4)]
        it = 0
        si = 0
        for h in range(nh):
            for (q_lo, q_n, key_tiles) in groups:
                nsub = q_n // 128
                ofl = []
                for s in range(npass):
                    chunks = qk_chunks(h, s)
                    obanks = [g.PS[3 + 2 * (it % 2)], g.PS[4 + 2 * (it % 2)]]
                    Of = Ofs[(it % 2) * 2 + s] if npass == 2 else Ofs[it % 4]
                    for ki, kt in enumerate(key_tiles):
                        S = g.PS[si % 3]
                        E = Es[si % 3]
                        si += 1
                        for ci, (qf, kf, bufs) in enumerate(chunks):
                            kb.op("pe", lambda e, qf=qf, kf=kf, ci=ci, S=S, kt=kt: e.matmul(
                                S[:, 0:q_n], lhsT=kf(kt), rhs=qf(q_lo, q_n), start=(ci == 0), stop=(ci == len(chunks) - 1)),
                                bufs, [S])
                        kb.op("act", lambda e, S=S, E=E: e.activation(out=E[:, 0:q_n], in_=S[:, 0:q_n], func=AF.Exp, scale=scale), [S], [E])
                        for sub in range(nsub):
                            ob = obanks[sub // 2]
                            c0 = (sub % 2) * (dv + 1)
                            kb.op("pe", lambda e, ob=ob, c0=c0, E=E, sub=sub, kt=kt, ki=ki: e.matmul(
                                ob[:, c0:c0 + dv + 1], lhsT=E[:, sub * 128:(sub + 1) * 128], rhs=V[:, kt, h, :],
                                start=(ki == 0 and sub % 2 == 0), stop=(ki == len(key_tiles) - 1), skip_group_check=True),
                                [E, V], [ob])
                    for bi in range((nsub + 1) // 2):
                        nb = min(2, nsub - bi * 2)
                        ob = obanks[bi]
                        src = ob[:, 0:nb * (dv + 1)].rearrange("p (s d) -> p s d", s=nb)
                        kb.op("dve", lambda e, Of=Of, bi=bi, nb=nb, src=src: e.tensor_copy(out=Of[:, bi * 2:bi * 2 + nb, :], in_=src), [ob], [Of])
                    ofl.append(Of)
                    if npass == 1:
                        it += 1
                if npass == 2:
                    it += 1
                post(h, q_lo, q_n, ofl)


def phase_mixer_a(g, l, ctx_out):
    nc, kb = g.nc, g.kb
    dma = lambda fn, r=(), w=(): kb.dma("sp", fn, r, w)
    lam_init = 0.8 - 0.6 * math.exp(-0.3 * l)
    Pv = g.P.t.rearrange("(t p) c -> p t c", p=128)
    OBv = g.OB.t.rearrange("(t p) c -> p t c", p=128)
    with nc.sbuf_tensor(_nm(), [128, 8, T], BF16) as qkt_t, nc.sbuf_tensor(_nm(), [128, NT, 4, 129], BF16) as va_t, \
            nc.sbuf_tensor(_nm(), [128, NT, 64], F32) as cs_t, nc.sbuf_tensor(_nm(), [128, 8], F32) as lam_t, \
            nc.sbuf_tensor(_nm(), [128, 128], F32) as ng_t:
        QKT = Buf(qkt_t, "QKT"); VA = Buf(va_t, "VA"); CS = Buf(cs_t, "CS"); LAM = Buf(lam_t, "LAM"); NG = Buf(ng_t, "NG")
        dma(lambda e: e.dma_start(out=CS[:], in_=g.rope_cs.rearrange("(t p) c -> p t c", p=128)), [], [CS])
        for tt in range(NT):
            dma(lambda e, tt=tt: e.dma_start(out=VA[:, tt, :, 0:128], in_=Pv[:, tt, 1024:1536].rearrange("p (h d) -> p h d", h=4)), [g.P], [VA])
        kb.op("pool", lambda e: e.memset(VA[:, :, :, 128:129], 1.0), [], [VA])
        with nc.sbuf_tensor(_nm(), [128, 4, 64], F32) as lv_t:
            LV = Buf(lv_t, "LV")
            for i, nm in enumerate(["diff_lam_q1", "diff_lam_k1", "diff_lam_q2", "diff_lam_k2"]):
                dma(lambda e, i=i, nm=nm: e.dma_start(out=LV[:, i, :], in_=g.W[nm][l:l + 1, :].partition_broadcast(128)), [], [LV])
            kb.op("dve", lambda e: e.tensor_tensor(out=LV[:, 0, :], in0=LV[:, 0, :], in1=LV[:, 1, :], op=ALU.mult), [LV], [LV])
            kb.op("dve", lambda e: e.tensor_tensor(out=LV[:, 2, :], in0=LV[:, 2, :], in1=LV[:, 3, :], op=ALU.mult), [LV], [LV])
            kb.op("dve", lambda e: e.tensor_reduce(out=LAM[:, 0:1], in_=LV[:, 0, :], axis=AX.X, op=ALU.add), [LV], [LAM])
            kb.op("dve", lambda e: e.tensor_reduce(out=LAM[:, 1:2], in_=LV[:, 2, :], axis=AX.X, op=ALU.add), [LV], [LAM])
            kb.op("act", lambda e: e.activation(out=LAM[:, 2:4], in_=LAM[:, 0:2], func=AF.Exp), [LAM], [LAM])
            kb.op("dve", lambda e: e.tensor_tensor(out=LAM[:, 4:5], in0=LAM[:, 2:3], in1=LAM[:, 3:4], op=ALU.subtract), [LAM], [LAM])
            kb.op("dve", lambda e: e.tensor_scalar(out=LAM[:, 5:6], in0=LAM[:, 4:5], scalar1=lam_init, scalar2=None, op0=ALU.add), [LAM], [LAM])
            dma(lambda e: e.dma_start(out=NG[:], in_=g.W["diff_norm_g"][l:l + 1, :].partition_broadcast(128)), [], [NG])
            kb.op("dve", lambda e: e.tensor_scalar(out=NG[:], in0=NG[:], scalar1=1.0 - lam_init, scalar2=None, op0=ALU.mult), [NG], [NG])
            kb.barrier()
        lam = LAM[:, 5:6]
        with nc.sbuf_tensor(_nm(), [128, 2, 1024], BF16) as pa_t, nc.sbuf_tensor(_nm(), [128, 2, 1024], BF16) as rb_t, \
                nc.sbuf_tensor(_nm(), [128, 4, 512], F32) as tm_t:
            pas = [Buf(pa_t[:, i], "pa%d" % i) for i in range(2)]
            rbs = [Buf(rb_t[:, i], "rb%d" % i) for i in range(2)]
            tms = [Buf(tm_t[:, i], "tm%d" % i) for i in range(4)]
            for tt in range(NT):
                pa = pas[tt % 2]; rb = rbs[tt % 2]
                dma(lambda e, pa=pa, tt=tt: e.dma_start(out=pa[:], in_=Pv[:, tt, 0:1024]), [g.P], [pa])
                rope_tile(g, pa, rb, CS, tt, 16, tms)
                ps = g.PS[7]
                psb = ps[:].bitcast(BF16)
                for c in range(8):
                    kb.op("pe", lambda e, c=c, rb=rb, psb=psb: e.transpose(out=psb[:, c * 128:(c + 1) * 128], in_=rb[:, c * 128:(c + 1) * 128], identity=g.ident_b[:]), [rb, g.ident_b], [ps])
                kb.op("act", lambda e, tt=tt, psb=psb: e.activation(out=QKT[:, :, tt * 128:(tt + 1) * 128], in_=psb.rearrange("p (k t) -> p k t", k=8), func=AF.Copy), [ps], [QKT])
            kb.barrier()

        def qk_chunks(h, s):
            p0 = (h % 2) * 64
            qc = s * 2 + h // 2
            kc = 4 + s * 2 + h // 2
            return [(lambda lo, n: QKT[p0:p0 + 64, qc, lo:lo + n], lambda kt: QKT[p0:p0 + 64, kc, kt * 128:(kt + 1) * 128], [QKT])]

        with nc.sbuf_tensor(_nm(), [128, 2, 8, 4], F32) as r_t, nc.sbuf_tensor(_nm(), [128, 2, 3, 4, 128], F32) as w_t, \
                nc.sbuf_tensor(_nm(), [128, 2, 4, 128], BF16) as o_t:
            cnt = [0]
            rs_ = [Buf(r_t[:, i], "r") for i in range(2)]; obs_ = [Buf(o_t[:, i], "ob") for i in range(2)]
            ws_ = [[Buf(w_t[:, i, j], "w%d" % j) for j in range(3)] for i in range(2)]

            def post(h, q_lo, q_n, ofl):
                O1, O2 = ofl
                i = cnt[0] % 2
                cnt[0] += 1
                ns = q_n // 128
                r = rs_[i]; w0, w1, w2 = ws_[i]; ob = obs_[i]
                bc = lambda ap: ap.unsqueeze(2).to_broadcast([128, ns, 128])
                kb.op("dve", lambda e: e.reciprocal(out=r[:, 0, 0:ns], in_=O1[:, 0:ns, 128]), [O1], [r])
                kb.op("dve", lambda e: e.reciprocal(out=r[:, 1, 0:ns], in_=O2[:, 0:ns, 128]), [O2], [r])
                kb.op("dve", lambda e: e.tensor_scalar(out=r[:, 2, 0:ns], in0=r[:, 1, 0:ns], scalar1=lam, scalar2=None, op0=ALU.mult), [r, LAM], [r])
                kb.op("dve", lambda e: e.tensor_tensor(out=w0[:, 0:ns, :], in0=O1[:, 0:ns, 0:128], in1=bc(r[:, 0, 0:ns]), op=ALU.mult), [O1, r], [w0])
                kb.op("dve", lambda e: e.tensor_tensor(out=w1[:, 0:ns, :], in0=O2[:, 0:ns, 0:128], in1=bc(r[:, 2, 0:ns]), op=ALU.mult), [O2, r], [w1])
                kb.op("dve", lambda e: e.tensor_tensor(out=w0[:, 0:ns, :], in0=w0[:, 0:ns, :], in1=w1[:, 0:ns, :], op=ALU.subtract), [w0, w1], [w0])
                kb.op("pool", lambda e: e.tensor_tensor(out=w2[:, 0:ns, :], in0=w0[:, 0:ns, :], in1=w0[:, 0:ns, :], op=ALU.mult), [w0], [w2])
                kb.op("dve", lambda e: e.tensor_reduce(out=r[:, 3, 0:ns], in_=w2[:, 0:ns, :], axis=AX.X, op=ALU.add), [w2], [r])
                kb.op("dve", lambda e: e.tensor_scalar(out=r[:, 4, 0:ns], in0=r[:, 3, 0:ns], scalar1=1.0 / 128, scalar2=EPS, op0=ALU.mult, op1=ALU.add), [r], [r])
                kb.op("act", lambda e: e.activation(out=r[:, 5, 0:ns], in_=r[:, 4, 0:ns], func=AF.Sqrt), [r], [r])
                kb.op("dve", lambda e: e.reciprocal(out=r[:, 6, 0:ns], in_=r[:, 5, 0:ns]), [r], [r])
                kb.op("dve", lambda e: e.tensor_tensor(out=w1[:, 0:ns, :], in0=w0[:, 0:ns, :], in1=bc(r[:, 6, 0:ns]), op=ALU.mult), [w0, r], [w1])
                kb.op("pool", lambda e: e.tensor_tensor(out=ob[:, 0:ns, :], in0=w1[:, 0:ns, :], in1=NG[:].unsqueeze(1).to_broadcast([128, ns, 128]), op=ALU.mult), [w1, NG], [ob])
                t0 = q_lo // 128
                dma(lambda e: e.dma_start(out=OBv[:, t0:t0 + ns, h * 128:(h + 1) * 128], in_=ob[:, 0:ns, :]), [ob], [g.OB])

            groups = [(i * 512, 512, list(range(NT))) for i in range(4)]
            if ctx_out:
                groups.append((2048, 256, [16, 17]))
            attention(g, 4, 128, 1.0 / 8.0, VA, qk_chunks, groups, post, npass=2)
            kb.barrier()


def rope_tile(g, pa, rb, CS, tt, ng, tms, col0=0):
    kb = g.kb
    n = ng * 64
    xv = pa[:, col0:col0 + n].rearrange("p (g a f j) -> p g a f j", g=ng, a=2, f=2, j=16)
    ov = rb[:, col0:col0 + n].rearrange("p (g a f j) -> p g a f j", g=ng, a=2, f=2, j=16)
    x1, x2 = xv[:, :, :, 0, :], xv[:, :, :, 1, :]
    cosb = CS[:, tt, 0:32].rearrange("p (a j) -> p a j", a=2).unsqueeze(1).to_broadcast([128, ng, 2, 16])
    sinb = CS[:, tt, 32:64].rearrange("p (a j) -> p a j", a=2).unsqueeze(1).to_broadcast([128, ng, 2, 16])
    tv = [t[:, 0:ng * 32].rearrange("p (g a j) -> p g a j", g=ng, a=2) for t in tms]
    kb.op("dve", lambda e: e.tensor_tensor(out=tv[0], in0=x1, in1=cosb, op=ALU.mult), [pa, CS], [tms[0]])
    kb.op("pool", lambda e: e.tensor_tensor(out=tv[1], in0=x2, in1=sinb, op=ALU.mult), [pa, CS], [tms[1]])
    kb.op("dve", lambda e: e.tensor_tensor(out=tv[2], in0=x1, in1=sinb, op=ALU.mult), [pa, CS], [tms[2]])
    kb.op("pool", lambda e: e.tensor_tensor(out=tv[3], in0=x2, in1=cosb, op=ALU.mult), [pa, CS], [tms[3]])
    kb.op("dve", lambda e: e.tensor_tensor(out=ov[:, :, :, 0, :], in0=tv[0], in1=tv[1], op=ALU.subtract), [tms[0], tms[1]], [rb])
    kb.op("pool", lambda e: e.tensor_tensor(out=ov[:, :, :, 1, :], in0=tv[2], in1=tv[3], op=ALU.add), [tms[2], tms[3]], [rb])


def small_rms(g, src_ap, src_buf, n, nh, r, col):
    kb = g.kb
    return None


def phase_mixer_b(g, l, ctx_out):
    nc, kb = g.nc, g.kb
    dma = lambda fn, r=(), w=(): kb.dma("sp", fn, r, w)
    Pv = g.P.t.rearrange("(t p) c -> p t c", p=128)
    OBv = g.OB.t.rearrange("(t p) c -> p t c", p=128)
    TQ = T if ctx_out else NX
    with nc.sbuf_tensor(_nm(), [128, 4, T], BF16) as ft_t, nc.sbuf_tensor(_nm(), [128, 4, T], BF16) as qnt_t, \
            nc.sbuf_tensor(_nm(), [128, 4, T], BF16) as knt_t, nc.sbuf_tensor(_nm(), [128, 2, T], BF16) as qrt_t, \
            nc.sbuf_tensor(_nm(), [128, NT, 4, 129], BF16) as vb_t:
        FT = Buf(ft_t, "FT"); QNT = Buf(qnt_t, "QNT"); KNT = Buf(knt_t, "KNT"); QRT = Buf(qrt_t, "QRT"); VB = Buf(vb_t, "VB")
        kb.op("pool", lambda e: e.memset(VB[:, :, :, 128:129], 1.0), [], [VB])
        with nc.sbuf_tensor(_nm(), [128, 2, 4, 128], BF16) as wqn_t, nc.sbuf_tensor(_nm(), [128, 2, 4, 64], BF16) as wqr_t, \
                nc.sbuf_tensor(_nm(), [128, 4, 128], BF16) as wkn_t, nc.sbuf_tensor(_nm(), [128, 4, 128], BF16) as wv_t, \
                nc.sbuf_tensor(_nm(), [128, 384], F32) as gn_t, nc.sbuf_tensor(_nm(), [128, NT, 64], F32) as cs_t:
            WQN = Buf(wqn_t, "WQN"); WQR = Buf(wqr_t, "WQR"); WKN = Buf(wkn_t, "WKN"); WV = Buf(wv_t, "WV"); GN = Buf(gn_t, "GN"); CS = Buf(cs_t, "CS")
            dma(lambda e: e.dma_start(out=CS[:], in_=g.rope_cs.rearrange("(t p) c -> p t c", p=128)), [], [CS])
            wuq = g.W["mla_w_uq"][l].rearrange("(c p) (h d) -> p c h d", p=128, h=4)
            wukv = g.W["mla_w_ukv"][l].rearrange("p (h d) -> p h d", h=4)
            for c in range(2):
                kb.dma("pool", lambda e, c=c: e.dma_start(out=WQN[:, c], in_=wuq[:, c, :, 0:128]), [], [WQN])
                kb.dma("pool", lambda e, c=c: e.dma_start(out=WQR[:, c], in_=wuq[:, c, :, 128:192]), [], [WQR])
            kb.dma("pool", lambda e: e.dma_start(out=WKN[:], in_=wukv[:, :, 0:128]), [], [WKN])
            kb.dma("pool", lambda e: e.dma_start(out=WV[:], in_=wukv[:, :, 128:256]), [], [WV])
            dma(lambda e: e.dma_start(out=GN[:, 0:256], in_=g.W["mla_qnorm_g"][l:l + 1, :].partition_broadcast(128)), [], [GN])
            dma(lambda e: e.dma_start(out=GN[:, 256:384], in_=g.W["mla_kvnorm_g"][l:l + 1, :].partition_broadcast(128)), [], [GN])
            with nc.sbuf_tensor(_nm(), [128, 2, 448], BF16) as pb_t, nc.sbuf_tensor(_nm(), [128, 2, 512], BF16) as tb_t, \
                    nc.sbuf_tensor(_nm(), [128, 2, 512], F32) as jk_t, nc.sbuf_tensor(_nm(), [128, 2, 8], F32) as st_t, \
                    nc.sbuf_tensor(_nm(), [128, 4, 128], F32) as tm_t, nc.sbuf_tensor(_nm(), [128, 2, 64], BF16) as kr_t:
                tms = [Buf(tm_t[:, i], "tm%d" % i) for i in range(4)]
                pbs = [Buf(pb_t[:, i], "pb") for i in range(2)]; tbs = [Buf(tb_t[:, i], "tb") for i in range(2)]
                jks = [Buf(jk_t[:, i], "jk") for i in range(2)]; sts = [Buf(st_t[:, i], "st") for i in range(2)]
                for tt in range(NT):
                    i = tt % 2
                    pb = pbs[i]; tb = tbs[i]; jk = jks[i]; st = sts[i]
                    dma(lambda e, pb=pb, tt=tt: e.dma_start(out=pb[:], in_=Pv[:, tt, 1536:1984]), [g.P], [pb])
                    kb.op("act", lambda e: e.activation(out=jk[:, 0:256], in_=pb[:, 0:256], func=AF.Square, accum_out=st[:, 0:1]), [pb], [jk, st])
                    kb.op("act", lambda e: e.activation(out=jk[:, 256:384], in_=pb[:, 256:384], func=AF.Square, accum_out=st[:, 1:2]), [pb], [jk, st])
                    kb.op("dve", lambda e: e.tensor_scalar(out=st[:, 2:3], in0=st[:, 0:1], scalar1=1.0 / 256, scalar2=EPS, op0=ALU.mult, op1=ALU.add), [st], [st])
                    kb.op("dve", lambda e: e.tensor_scalar(out=st[:, 3:4], in0=st[:, 1:2], scalar1=1.0 / 128, scalar2=EPS, op0=ALU.mult, op1=ALU.add), [st], [st])
                    kb.op("act", lambda e: e.activation(out=st[:, 4:6], in_=st[:, 2:4], func=AF.Sqrt), [st], [st])
                    kb.op("dve", lambda e: e.reciprocal(out=st[:, 6:8], in_=st[:, 4:6]), [st], [st])
                    kb.op("dve", lambda e: e.scalar_tensor_tensor(out=tb[:, 0:256], in0=pb[:, 0:256], scalar=st[:, 6:7], in1=GN[:, 0:256], op0=ALU.mult, op1=ALU.mult), [pb, st, GN], [tb])
                    kb.op("dve", lambda e: e.scalar_tensor_tensor(out=tb[:, 256:384], in0=pb[:, 256:384], scalar=st[:, 7:8], in1=GN[:, 256:384], op0=ALU.mult, op1=ALU.mult), [pb, st, GN], [tb])
                    rope_tile(g, pb, tb, CS, tt, 1, tms, col0=384)
                    kb.op("pool", lambda e: e.tensor_copy(out=tb[:, 448:512], in_=tb[:, 384:448]), [tb], [tb])
                    ps = g.PS[7]
                    psb = ps[:].bitcast(BF16)
                    for c in range(4):
                        kb.op("pe", lambda e, c=c, tb=tb, psb=psb: e.transpose(out=psb[:, c * 128:(c + 1) * 128], in_=tb[:, c * 128:(c + 1) * 128], identity=g.ident_b[:]), [tb, g.ident_b], [ps])
                    kb.op("act", lambda e, tt=tt, psb=psb: e.activation(out=FT[:, :, tt * 128:(tt + 1) * 128], in_=psb[:, 0:512].rearrange("p (k t) -> p k t", k=4), func=AF.Copy), [ps], [FT])
                kb.barrier()
            pi = 0
            ngr = (T + 511) // 512
            for gi in range(ngr):
                lo = gi * 512
                n = min(512, T - lo)
                for h in range(4):
                    ps = g.PS[pi % 4]; pi += 1
                    for c in range(2):
                        kb.op("pe", lambda e, c=c, h=h, ps=ps, lo=lo, n=n: e.matmul(ps[:, 0:n], lhsT=WQN[:, c, h, :], rhs=FT[:, c, lo:lo + n], start=(c == 0), stop=(c == 1)), [WQN, FT], [ps])
                    kb.op("act", lambda e, h=h, ps=ps, lo=lo, n=n: e.activation(out=QNT[:, h, lo:lo + n], in_=ps[:, 0:n], func=AF.Copy), [ps], [QNT])
                    ps = g.PS[pi % 4]; pi += 1
                    kb.op("pe", lambda e, h=h, ps=ps, lo=lo, n=n: e.matmul(ps[:, 0:n], lhsT=WKN[:, h, :], rhs=FT[:, 2, lo:lo + n], start=True, stop=True), [WKN, FT], [ps])
                    kb.op("dve", lambda e, h=h, ps=ps, lo=lo, n=n: e.tensor_copy(out=KNT[:, h, lo:lo + n], in_=ps[:, 0:n]), [ps], [KNT])
            with nc.sbuf_tensor(_nm(), [128, 2, 256], BF16) as qr_t, nc.sbuf_tensor(_nm(), [128, 2, 256], BF16) as qq_t, \
                    nc.sbuf_tensor(_nm(), [128, 4, 128], F32) as tm_t:
                tms = [Buf(tm_t[:, i], "tm%d" % i) for i in range(4)]
                qrs = [Buf(qr_t[:, i], "qr") for i in range(2)]; qqs = [Buf(qq_t[:, i], "qq") for i in range(2)]
                for tt in range(NT):
                    i = tt % 2
                    qr = qrs[i]; qq = qqs[i]
                    ps = g.PS[pi % 4]; pi += 1
                    kb.op("pe", lambda e, ps=ps, tt=tt: e.matmul(ps[:, 0:512], lhsT=FT[:, 2, tt * 128:(tt + 1) * 128], rhs=WV[:].rearrange("p h d -> p (h d)"), start=True, stop=True), [FT, WV], [ps])
                    kb.op("act", lambda e, ps=ps, tt=tt: e.activation(out=VB[:, tt, :, 0:128], in_=ps[:, 0:512].rearrange("p (h d) -> p h d", h=4), func=AF.Copy), [ps], [VB])
                    ps = g.PS[pi % 4]; pi += 1
                    for c in range(2):
                        kb.op("pe", lambda e, c=c, ps=ps, tt=tt: e.matmul(ps[:, 0:256], lhsT=FT[:, c, tt * 128:(tt + 1) * 128], rhs=WQR[:, c].rearrange("p h d -> p (h d)"), start=(c == 0), stop=(c == 1)), [FT, WQR], [ps])
                    kb.op("act", lambda e, ps=ps, qq=qq: e.activation(out=qq[:], in_=ps[:, 0:256], func=AF.Copy), [ps], [qq])
                    rope_tile(g, qq, qr, CS, tt, 4, tms)
                    ps = g.PS[7]
                    psb = ps[:].bitcast(BF16)
                    for c in range(2):
                        kb.op("pe", lambda e, c=c, qr=qr, psb=psb: e.transpose(out=psb[:, c * 128:(c + 1) * 128], in_=qr[:, c * 128:(c + 1) * 128], identity=g.ident_b[:]), [qr, g.ident_b], [ps])
                    kb.op("dve", lambda e, tt=tt, psb=psb: e.tensor_copy(out=QRT[:, :, tt * 128:(tt + 1) * 128], in_=psb[:, 0:256].rearrange("p (k t) -> p k t", k=2)), [ps], [QRT])
                kb.barrier()

        if "dbgB" in g.debug:
            dft = nc.dram_tensor("dbgFT", [128, 4, T], BF16, kind="ExternalOutput").ap()
            dqr = nc.dram_tensor("dbgQRT", [128, 2, T], BF16, kind="ExternalOutput").ap()
            dqn = nc.dram_tensor("dbgQNT", [128, 4, T], BF16, kind="ExternalOutput").ap()
            dkn = nc.dram_tensor("dbgKNT", [128, 4, T], BF16, kind="ExternalOutput").ap()
            dvb = nc.dram_tensor("dbgVB", [128, NT, 4, 129], BF16, kind="ExternalOutput").ap()
            dma(lambda e: e.dma_start(out=dft[:], in_=FT[:]), [FT], [])
            dma(lambda e: e.dma_start(out=dqr[:], in_=QRT[:]), [QRT], [])
            dma(lambda e: e.dma_start(out=dqn[:], in_=QNT[:]), [QNT], [])
            dma(lambda e: e.dma_start(out=dkn[:], in_=KNT[:]), [KNT], [])
            dma(lambda e: e.dma_start(out=dvb[:], in_=VB[:]), [VB], [])

        def qk_chunks(h, s):
            p0 = (h % 2) * 64
            return [(lambda lo, n: QNT[:, h, lo:lo + n], lambda kt: KNT[:, h, kt * 128:(kt + 1) * 128], [QNT, KNT]),
                    (lambda lo, n: QRT[p0:p0 + 64, h // 2, lo:lo + n], lambda kt: FT[p0:p0 + 64, 3, kt * 128:(kt + 1) * 128], [QRT, FT])]

        with nc.sbuf_tensor(_nm(), [128, 2, 4], F32) as r_t, nc.sbuf_tensor(_nm(), [128, 2, 4, 128], BF16) as o_t:
            cnt = [0]
            rs_ = [Buf(r_t[:, i], "r") for i in range(2)]; obs_ = [Buf(o_t[:, i], "ob") for i in range(2)]

            def post(h, q_lo, q_n, ofl):
                O1 = ofl[0]
                i = cnt[0] % 2
                cnt[0] += 1
                ns = q_n // 128
                r = rs_[i]; ob = obs_[i]
                kb.op("dve", lambda e: e.reciprocal(out=r[:, 0:ns], in_=O1[:, 0:ns, 128]), [O1], [r])
                kb.op("dve", lambda e: e.tensor_tensor(out=ob[:, 0:ns, :], in0=O1[:, 0:ns, 0:128], in1=r[:, 0:ns].unsqueeze(2).to_broadcast([128, ns, 128]), op=ALU.mult), [O1, r], [ob])
                t0 = q_lo // 128
                dma(lambda e: e.dma_start(out=OBv[:, t0:t0 + ns, 512 + h * 128:512 + (h + 1) * 128], in_=ob[:, 0:ns, :]), [ob], [g.OB])

            groups = [(i * 512, 512, list(range(NT))) for i in range(4)]
            if ctx_out:
                groups.append((2048, 256, [16, 17]))
            attention(g, 4, 128, 192.0 ** -0.5, VB, qk_chunks, groups, post, npass=1)
            kb.barrier()


def phase_mixer_d(g, l, ctx_out):
    nc, kb = g.nc, g.kb
    dma = lambda fn, r=(), w=(): kb.dma("sp", fn, r, w)
    Pv = g.P.t.rearrange("(t p) c -> p t c", p=128)
    OBv = g.OB.t.rearrange("(t p) c -> p t c", p=128)
    scale = 1.0 / 8.0
    with nc.sbuf_tensor(_nm(), [128, 6, T], BF16) as qkd_t, nc.sbuf_tensor(_nm(), [128, NT, 2, 65], BF16) as vd_t, \
            nc.sbuf_tensor(_nm(), [128, 8], F32) as es_t, nc.sbuf_tensor(_nm(), [128, 2, 128], BF16) as mk_t, \
            nc.sbuf_tensor(_nm(), [128, 2, 128], F32) as mkf_t:
        QKD = Buf(qkd_t, "QKD"); VD = Buf(vd_t, "VD"); ES = Buf(es_t, "ES"); MK = Buf(mk_t, "MK"); MKF = Buf(mkf_t, "MKF")
        kb.op("pool", lambda e: e.memset(VD[:, :, :, 64:65], 1.0), [], [VD])
        for tt in range(NT):
            dma(lambda e, tt=tt: e.dma_start(out=VD[:, tt, :, 0:64], in_=Pv[:, tt, 5184:5312].rearrange("p (h d) -> p h d", h=2)), [g.P], [VD])
        dma(lambda e: e.dma_start(out=MKF[:, 0, :], in_=g.mprev_in[:, :]), [], [MKF])
        dma(lambda e: e.dma_start(out=MKF[:, 1, :], in_=g.mnext_in[:, :]), [], [MKF])
        kb.op("dve", lambda e: e.tensor_copy(out=MK[:], in_=MKF[:]), [MKF], [MK])
        dma(lambda e: e.dma_start(out=ES[:], in_=g.W["win_sink"][l:l + 1, :].partition_broadcast(128)), [], [ES])
        kb.op("act", lambda e: e.activation(out=ES[:], in_=ES[:], func=AF.Exp), [ES], [ES])
        with nc.sbuf_tensor(_nm(), [128, 2, 640], BF16) as pd_t, nc.sbuf_tensor(_nm(), [128, 2, 768], BF16) as rb_t, \
                nc.sbuf_tensor(_nm(), [128, 4, 320], F32) as tm_t, nc.sbuf_tensor(_nm(), [128, NT, 64], F32) as cs_t:
            CS = Buf(cs_t, "CS")
            dma(lambda e: e.dma_start(out=CS[:], in_=g.rope_cs.rearrange("(t p) c -> p t c", p=128)), [], [CS])
            pds = [Buf(pd_t[:, i], "pd%d" % i) for i in range(2)]
            rbs = [Buf(rb_t[:, i], "rb%d" % i) for i in range(2)]
            tms = [Buf(tm_t[:, i], "tm%d" % i) for i in range(4)]
            for tt in range(NT if "d_skip_rope" not in g.debug else 0):
                pd = pds[tt % 2]; rb = rbs[tt % 2]
                dma(lambda e, pd=pd, tt=tt: e.dma_start(out=pd[:], in_=Pv[:, tt, 4544:5184]), [g.P], [pd])
                rope_tile(g, pd, rb, CS, tt, 10, tms)
                kb.op("dve", lambda e, rb=rb: e.tensor_copy(out=rb[:, 640:768].rearrange("p (u d) -> p u d", u=2),
                                                           in_=rb[:, 576:640].unsqueeze(1).to_broadcast([128, 2, 64])), [rb], [rb])
                kb.op("dve", lambda e, rb=rb: e.tensor_copy(out=rb[:, 576:640], in_=rb[:, 512:576]), [rb], [rb])
                ps = g.PS[7]
                psb = ps[:].bitcast(BF16)
                for c in range(6):
                    kb.op("pe", lambda e, c=c, rb=rb, psb=psb: e.transpose(out=psb[:, c * 128:(c + 1) * 128], in_=rb[:, c * 128:(c + 1) * 128], identity=g.ident_b[:]), [rb, g.ident_b], [ps])
                kb.op("act", lambda e, tt=tt, psb=psb: e.activation(out=QKD[:, :, tt * 128:(tt + 1) * 128], in_=psb[:, 0:768].rearrange("p (k t) -> p k t", k=6), func=AF.Copy), [ps], [QKD])
            kb.barrier()
        with nc.sbuf_tensor(_nm(), [128, 3, 512], BF16) as E_t, nc.sbuf_tensor(_nm(), [128, 2, 8, 65], F32) as of_t, \
                nc.sbuf_tensor(_nm(), [128, 2, 2, 8], F32) as r_t, nc.sbuf_tensor(_nm(), [128, 2, 8, 64], BF16) as o_t:
            Es = [Buf(E_t[:, i], "E%d" % i) for i in range(3)]
            Ofs = [Buf(of_t[:, i], "Of%d" % i) for i in range(2)]
            rs_ = [Buf(r_t[:, i], "r%d" % i) for i in range(2)]
            obs_ = [Buf(o_t[:, i], "ob%d" % i) for i in range(2)]
            si = 0
            qtiles = list(range(16)) + ([16, 17] if ctx_out else [])
            if "d_prep_only" in g.debug:
                qtiles = []
            for qn_, qi in enumerate(qtiles):
                if qi < 16:
                    kts = ([(qi - 1, 0)] if qi > 0 else []) + [(qi, None)] + ([(qi + 1, 1)] if qi < 15 else []) + [(16, None), (17, None)]
                else:
                    kts = [(16, None), (17, None)]
                Of = Ofs[qn_ % 2]; r = rs_[qn_ % 2]; ob = obs_[qn_ % 2]
                for kvh in range(2):
                    O = g.PS[3 + (qn_ * 2 + kvh) % 4]
                    for ki, (kt, mk) in enumerate(kts):
                        Sp = [(g.PS[0], g.PS[1]), (g.PS[2], g.PS[7])][si % 2]
                        E = Es[si % 3]; si += 1
                        for gq in range(4):
                            hq = kvh * 4 + gq
                            p0 = (hq % 2) * 64
                            S = Sp[gq % 2]
                            c0 = (gq // 2) * 128
                            kb.op("pe", lambda e, S=S, c0=c0, p0=p0, kvh=kvh, kt=kt, hq=hq, qi=qi: e.matmul(
                                S[:, c0:c0 + 128], lhsT=QKD[p0:p0 + 64, 4 + kvh, kt * 128:(kt + 1) * 128],
                                rhs=QKD[p0:p0 + 64, hq // 2, qi * 128:(qi + 1) * 128], start=True, stop=True, skip_group_check=True), [QKD], [S])
                        for j in range(2):
                            kb.op("act", lambda e, j=j, E=E, Sp=Sp: e.activation(out=E[:, j * 256:(j + 1) * 256], in_=Sp[j][:, 0:256], func=AF.Exp, scale=scale), [Sp[j]], [E])
                        if mk is not None and "d_no_mask" not in g.debug:
                            kb.op("dve", lambda e, E=E, mk=mk: e.tensor_tensor(out=E[:].rearrange("p (g q) -> p g q", g=4), in0=E[:].rearrange("p (g q) -> p g q", g=4),
                                                                             in1=MKF[:, mk, :].unsqueeze(1).to_broadcast([128, 4, 128]), op=ALU.mult), [E, MKF], [E])
                        for gq in range(4 if "d_no_pv" not in g.debug else 0):
                            ei = (gq % 2) * 2 + gq // 2
                            kb.op("pe", lambda e, O=O, gq=gq, ei=ei, E=E, kt=kt, kvh=kvh, ki=ki: e.matmul(
                                O[:, gq * 65:(gq + 1) * 65], lhsT=E[:, ei * 128:(ei + 1) * 128], rhs=VD[:, kt, kvh, :],
                                start=(ki == 0 and gq == 0), stop=(ki == len(kts) - 1), skip_group_check=True), [E, VD], [O])
                    kb.op("act", lambda e, O=O, Of=Of, kvh=kvh: e.activation(out=Of[:, kvh * 4:(kvh + 1) * 4, :], in_=O[:, 0:260].rearrange("p (g d) -> p g d", g=4), func=AF.Copy), [O], [Of])
                kb.op("dve", lambda e, Of=Of, r=r: e.tensor_tensor(out=r[:, 0, :], in0=Of[:, :, 64], in1=ES[:], op=ALU.add), [Of, ES], [r])
                kb.op("dve", lambda e, r=r: e.reciprocal(out=r[:, 1, :], in_=r[:, 0, :]), [r], [r])
                kb.op("dve", lambda e, Of=Of, r=r, ob=ob: e.tensor_tensor(out=ob[:], in0=Of[:, :, 0:64], in1=r[:, 1, :].unsqueeze(2).to_broadcast([128, 8, 64]), op=ALU.mult), [Of, r], [ob])
                dma(lambda e, ob=ob, qi=qi: e.dma_start(out=OBv[:, qi, 1536:2048], in_=ob[:].rearrange("p h d -> p (h d)")), [ob], [g.OB])
            kb.barrier()


def phase_mixer_c(g, l, ctx_out):
    nc, kb = g.nc, g.kb
    dma = lambda fn, r=(), w=(): kb.dma("sp", fn, r, w)
    NCH = T // 64
    fwd_order = [32, 33, 34, 35] + list(range(32))
    bwd_order = [35, 34, 33, 32] + list(range(31, -1, -1))
    from contextlib import ExitStack
    with ExitStack() as es:
        al = lambda shape, dt: es.enter_context(nc.sbuf_tensor(_nm(), shape, dt))
        tri_t = al([64, 4, 64], F32); lb_t = al([64, 2, 2, 512], F32); ng_t = al([64, 128], F32)
        S_t = al([128, 4, 128], F32); Sb_t = al([128, 4, 128], BF16)
        TRI = Buf(tri_t, "TRI"); LB = Buf(lb_t, "LB"); NGh = Buf(ng_t, "NGh"); S = Buf(S_t, "S"); Sb = Buf(Sb_t, "Sb")
        dma(lambda e: e.dma_start(out=TRI[:], in_=g.tri_in.rearrange("p (a t) -> p a t", a=4)), [], [TRI])
        dma(lambda e: e.dma_start(out=NGh[:], in_=g.W["hgrn_norm_g"][l:l + 1, :].partition_broadcast(64)), [], [NGh])
        if l == 0:
            kb.op("dve", lambda e: e.memset(LB[:, :, 0, :], 0.0), [], [LB])
            kb.op("dve", lambda e: e.memset(LB[:, :, 1, :], 1.0), [], [LB])
        else:
            for d in range(2):
                dma(lambda e, d=d: e.dma_start(out=LB[:, d, 0, :], in_=g.W["hgrn_lb"][d, 1:2, :].partition_broadcast(64)), [], [LB])
                dma(lambda e, d=d: e.dma_start(out=LB[:, d, 1, :], in_=g.W["hgrn_lb"][d, 0:1, :].partition_broadcast(64)), [], [LB])
                kb.op("dve", lambda e, d=d: e.tensor_tensor(out=LB[:, d, 0, :], in0=LB[:, d, 0, :], in1=LB[:, d, 1, :], op=ALU.subtract), [LB], [LB])
                kb.op("act", lambda e, d=d: e.activation(out=LB[:, d, 0, :], in_=LB[:, d, 0, :], func=AF.Sigmoid), [LB], [LB])
                kb.op("dve", lambda e, d=d: e.tensor_scalar(out=LB[:, d, 1, :], in0=LB[:, d, 0, :], scalar1=-1.0, scalar2=1.0, op0=ALU.mult, op1=ALU.add), [LB], [LB])
        if True:
            pc_t = al([64, 2, 2560], BF16); f_t = al([64, 2, 512], F32); lf_t = al([64, 2, 512], F32); kf_t = al([64, 2, 512], BF16)
            bm_t = al([128, 2, 256], F32); e12_t = al([128, 2, 2, 256], F32); em_t = al([128, 2, 8], F32); qf_t = al([128, 2, 256], F32)
            qk_t = al([128, 2, 3, 256], BF16); at_t = al([64, 2, 256], BF16); ed_t = al([64, 2, 512], F32); kh_t = al([64, 2, 512], BF16)
            of_t = al([64, 2, 512], F32); ro_t = al([64, 2, 3, 512], F32); rr_t = al([64, 2, 8], F32); oo_t = al([64, 2, 512], BF16)
            mk = lambda t, nm: [Buf(t[:, i], nm + str(i)) for i in range(2)]
            PCs, Fs, LFs, KFs, BMs, E12s, EMs, QFs, QKs, ATs, EDs, KHs, OFs, ROs, RRs, OOs = [mk(t, n) for t, n in [
                (pc_t, "pc"), (f_t, "f"), (lf_t, "lf"), (kf_t, "kf"), (bm_t, "bm"), (e12_t, "e12"), (em_t, "em"), (qf_t, "qf"),
                (qk_t, "qk"), (at_t, "at"), (ed_t, "ed"), (kh_t, "kh"), (of_t, "of"), (ro_t, "ro"), (rr_t, "rr"), (oo_t, "oo")]]
            step = 0
            for d in range(2):
                order = fwd_order if d == 0 else bwd_order
                ti_incl, ti_strict = (0, 2) if d == 0 else (1, 3)
                t_end, t_mid = (63, 31) if d == 0 else (0, 32)
                zc = 512 if d == 0 else 1024
                kb.op("dve", lambda e: e.memset(S[:], 0.0), [], [S])
                kb.op("dve", lambda e: e.memset(Sb[:], 0.0), [], [Sb])
                for a_ in ATs:
                    kb.op("dve", lambda e, a_=a_: e.memset(a_[:], 0.0), [], [a_])
                for ci, ch in enumerate(order):
                    i = step % 2
                    step += 1
                    pc, f, lf, kf, bm, e12, em, qf, qk, at, ed, kh, of, ro, rr, oo = [x[i] for x in (PCs, Fs, LFs, KFs, BMs, E12s, EMs, QFs, QKs, ATs, EDs, KHs, OFs, ROs, RRs, OOs)]
                    need_out = (ch < 32) or ctx_out
                    r0 = ch * 64
                    dma(lambda e, pc=pc, r0=r0: e.dma_start(out=pc[:], in_=g.P[r0:r0 + 64, 1984:4544]), [g.P], [pc])
                    if d == 1 and need_out:
                        dma(lambda e, of=of, r0=r0: e.dma_start(out=of[:], in_=g.OF[r0:r0 + 64, :]), [g.OF], [of])
                    kb.op("act", lambda e: e.activation(out=f[:], in_=pc[:, zc:zc + 512], func=AF.Sigmoid), [pc], [f])
                    kb.op("dve", lambda e: e.tensor_tensor(out=f[:], in0=f[:], in1=LB[:, d, 1, :], op=ALU.mult), [f, LB], [f])
                    kb.op("dve", lambda e: e.tensor_tensor(out=f[:], in0=f[:], in1=LB[:, d, 0, :], op=ALU.add), [f, LB], [f])
                    kb.op("act", lambda e: e.activation(out=lf[:], in_=f[:], func=AF.Ln), [f], [lf])
                    kb.op("dve", lambda e: e.tensor_scalar(out=kf[:], in0=f[:], scalar1=-1.0, scalar2=1.0, op0=ALU.mult, op1=ALU.add), [f], [kf])
                    Bp = g.PS[0]
                    for h in range(4):
                        kb.op("pe", lambda e, h=h: e.matmul(Bp[:, h * 64:(h + 1) * 64], lhsT=lf[:, h * 128:(h + 1) * 128], rhs=TRI[:, ti_incl, :],
                                                            start=True, stop=True, skip_group_check=True), [lf, TRI], [Bp])
                    Dp = g.PS[1]
                    kb.op("pe", lambda e: e.matmul(Dp[0:64, :], lhsT=TRI[:, ti_strict, :], rhs=lf[:], start=True, stop=True), [lf, TRI], [Dp])
                    Tp = g.PS[2]
                    tpb = Tp[:].bitcast(BF16)
                    for h in range(4):
                        kb.op("pe", lambda e, h=h: e.transpose(out=tpb[:, h * 64:(h + 1) * 64], in_=pc[:, h * 128:(h + 1) * 128], identity=g.ident_b[0:64, 0:64]), [pc, g.ident_b], [Tp])
                    for h in range(4):
                        kb.op("pe", lambda e, h=h: e.transpose(out=tpb[:, 256 + h * 64:256 + (h + 1) * 64], in_=kf[:, h * 128:(h + 1) * 128], identity=g.ident_b[0:64, 0:64]), [kf, g.ident_b], [Tp])
                    b3 = Bp[:, 0:256].rearrange("p (h t) -> p h t", h=4)
                    kb.op("act", lambda e: e.activation(out=em[:, 0:4], in_=b3[:, :, t_mid], func=AF.Copy), [Bp], [em])
                    kb.op("dve", lambda e: e.tensor_tensor(out=bm[:].rearrange("p (h t) -> p h t", h=4), in0=b3, in1=em[:, 0:4].unsqueeze(2).to_broadcast([128, 4, 64]), op=ALU.subtract), [Bp, em], [bm])
                    kb.op("act", lambda e: e.activation(out=e12[:, 0, :], in_=bm[:], func=AF.Exp), [bm], [e12])
                    kb.op("act", lambda e: e.activation(out=e12[:, 1, :], in_=bm[:], func=AF.Exp, scale=-1.0), [bm], [e12])
                    kb.op("act", lambda e: e.activation(out=em[:, 4:8], in_=em[:, 0:4], func=AF.Exp), [em], [em])
                    kb.op("act", lambda e: e.activation(out=em[:, 0:4], in_=b3[:, :, t_end], func=AF.Exp), [Bp], [em])
                    kb.op("dve", lambda e: e.tensor_tensor(out=qf[:], in0=tpb[:, 0:256], in1=e12[:, 0, :], op=ALU.mult), [Tp, e12], [qf])
                    kb.op("dve", lambda e: e.tensor_copy(out=qk[:, 0, :], in_=qf[:]), [qf], [qk])
                    kb.op("dve", lambda e: e.tensor_tensor(out=qk[:, 1, :].rearrange("p (h t) -> p h t", h=4), in0=qf[:].rearrange("p (h t) -> p h t", h=4),
                                                           in1=em[:, 4:8].unsqueeze(2).to_broadcast([128, 4, 64]), op=ALU.mult), [qf, em], [qk])
                    kb.op("dve", lambda e: e.tensor_tensor(out=qk[:, 2, :], in0=tpb[:, 256:512], in1=e12[:, 1, :], op=ALU.mult), [Tp, e12], [qk])
                    Ap = g.PS[3]
                    if d == 0:
                        parts = [((0, 32), (0, 64)), ((32, 64), (32, 64))]
                    else:
                        parts = [((32, 64), (0, 64)), ((0, 32), (0, 32))]
                    for h in range(4):
                        for (s0, s1), (c0, c1) in parts:
                            kb.op("pe", lambda e, h=h, s0=s0, s1=s1, c0=c0, c1=c1: e.matmul(
                                Ap[s0:s1, h * 64 + c0:h * 64 + c1], lhsT=qk[:, 2, h * 64 + s0:h * 64 + s1], rhs=qk[:, 0, h * 64 + c0:h * 64 + c1],
                                start=True, stop=True, skip_group_check=True), [qk], [Ap])
                    for (s0, s1), (c0, c1) in parts:
                        kb.op("dve", lambda e, s0=s0, s1=s1, c0=c0, c1=c1: e.tensor_tensor(
                            out=at[s0:s1, :].rearrange("p (h t) -> p h t", h=4)[:, :, c0:c1],
                            in0=Ap[s0:s1, 0:256].rearrange("p (h t) -> p h t", h=4)[:, :, c0:c1],
                            in1=TRI[s0:s1, ti_incl, c0:c1].unsqueeze(1).to_broadcast([s1 - s0, 4, c1 - c0]), op=ALU.mult), [Ap, TRI], [at])
                    kb.op("act", lambda e: e.activation(out=ed[:], in_=Dp[0:64, :], func=AF.Exp), [Dp], [ed])
                    kb.op("dve", lambda e: e.tensor_tensor(out=kh[:], in0=kf[:], in1=ed[:], op=ALU.mult), [kf, ed], [kh])
                    if need_out:
                        Op = g.PS[4 + (step % 2)]
                        for h in range(4):
                            kb.op("pe", lambda e, h=h: e.matmul(Op[0:64, h * 128:(h + 1) * 128], lhsT=qk[:, 1, h * 64:(h + 1) * 64], rhs=Sb[:, h, :],
                                                                start=(h == 0), stop=False, skip_group_check=True), [qk, Sb], [Op])
                            kb.op("pe", lambda e, h=h: e.matmul(Op[0:64, h * 128:(h + 1) * 128], lhsT=at[:, h * 64:(h + 1) * 64], rhs=pc[:, 1536 + h * 128:1536 + (h + 1) * 128],
                                                                start=False, stop=True, skip_group_check=True), [at, pc], [Op])
                    Up = g.PS[6 + (step % 2)]
                    for h in range(4):
                        kb.op("pe", lambda e, h=h: e.matmul(Up[:, h * 128:(h + 1) * 128], lhsT=kh[:, h * 128:(h + 1) * 128], rhs=pc[:, 1536 + h * 128:1536 + (h + 1) * 128],
                                                            start=True, stop=True, skip_group_check=True), [kh, pc], [Up])
                    for h in range(4):
                        kb.op("dve", lambda e, h=h: e.scalar_tensor_tensor(out=S[:, h, :], in0=S[:, h, :], scalar=em[:, h:h + 1], in1=Up[:, h * 128:(h + 1) * 128],
                                                                           op0=ALU.mult, op1=ALU.add), [S, em, Up], [S])
                    kb.op("pool", lambda e: e.tensor_copy(out=Sb[:], in_=S[:]), [S], [Sb])
                    if need_out and d == 0:
                        kb.op("act", lambda e: e.activation(out=of[:], in_=Op[0:64, :], func=AF.Copy), [Op], [of])
                        dma(lambda e, of=of, r0=r0: e.dma_start(out=g.OF[r0:r0 + 64, :], in_=of[:]), [of], [g.OF])
                    if need_out and d == 1:
                        o3 = lambda b, j: b[:, j, :].rearrange("p (h v) -> p h v", h=4)
                        kb.op("dve", lambda e: e.tensor_tensor(out=ro[:, 0, :], in0=Op[0:64, :], in1=of[:], op=ALU.add), [Op, of], [ro])
                        kb.op("pool", lambda e: e.tensor_tensor(out=ro[:, 1, :], in0=ro[:, 0, :], in1=ro[:, 0, :], op=ALU.mult), [ro], [ro])
                        kb.op("dve", lambda e: e.tensor_reduce(out=rr[:, 0:4], in_=o3(ro, 1), axis=AX.X, op=ALU.add), [ro], [rr])
                        kb.op("dve", lambda e: e.tensor_scalar(out=rr[:, 0:4], in0=rr[:, 0:4], scalar1=1.0 / 128, scalar2=EPS, op0=ALU.mult, op1=ALU.add), [rr], [rr])
                        kb.op("act", lambda e: e.activation(out=rr[:, 4:8], in_=rr[:, 0:4], func=AF.Sqrt), [rr], [rr])
                        kb.op("dve", lambda e: e.reciprocal(out=rr[:, 0:4], in_=rr[:, 4:8]), [rr], [rr])
                        kb.op("dve", lambda e: e.tensor_tensor(out=o3(ro, 1), in0=o3(ro, 0), in1=rr[:, 0:4].unsqueeze(2).to_broadcast([64, 4, 128]), op=ALU.mult), [ro, rr], [ro])
                        kb.op("act", lambda e: e.activation(out=ro[:, 2, :], in_=pc[:, 2048:2560], func=AF.Silu), [pc], [ro])
                        kb.op("pool", lambda e: e.tensor_tensor(out=o3(ro, 0), in0=o3(ro, 1), in1=NGh[:].unsqueeze(1).to_broadcast([64, 4, 128]), op=ALU.mult), [ro, NGh], [ro])
                        kb.op("dve", lambda e: e.tensor_tensor(out=oo[:], in0=ro[:, 0, :], in1=ro[:, 2, :], op=ALU.mult), [ro], [oo])
                        dma(lambda e, oo=oo, r0=r0: e.dma_start(out=g.OB[r0:r0 + 64, 1024:1536], in_=oo[:]), [oo], [g.OB])
                kb.barrier()


def phase_merge(g, l, ctx_out):
    nc, kb = g.nc, g.kb
    from contextlib import ExitStack
    dma = lambda fn, r=(), w=(): kb.dma("sp", fn, r, w)
    Pv = g.P.t.rearrange("(t p) c -> p t c", p=128)
    OBv = g.OB.t.rearrange("(t p) c -> p t c", p=128)
    with ExitStack() as es:
        al = lambda shape, dt: es.enter_context(nc.sbuf_tensor(_nm(), shape, dt))
        WBR = Buf(al([128, 16, 1024], BF16), "WBR"); WO = Buf(al([128, 8, 1024], BF16), "WO")
        G1 = [Buf(al([128, 1024], F32), "G1x"), Buf(al([128, 1024], F32), "G1c")]
        ob_t = al([128, 2, 2048], BF16); gt_t = al([128, 2, 4096], BF16); obt_t = al([128, 2, 16, 128], BF16)
        sg_t = al([128, 3, 512], F32); tp_t = al([128, 3, 512], F32); y_t = al([128, 2, 1024], F32)
        yb_t = al([128, 2, 1024], BF16); yt_t = al([128, 2, 8, 128], BF16)
        mk = lambda t, nm, n: [Buf(t[:, i], nm + str(i)) for i in range(n)]
        OBs, GTs, OBTs, SGs, TPs, Ys, YBs, YTs = mk(ob_t, "ob", 2), mk(gt_t, "gt", 2), mk(obt_t, "obt", 2), mk(sg_t, "sg", 3), mk(tp_t, "tp", 3), mk(y_t, "y", 2), mk(yb_t, "yb", 2), mk(yt_t, "yt", 2)
        wsrc = g.W["w_branch"][l].rearrange("j (c p) n -> p j c n", p=128)
        for j in range(4):
            kb.dma("pool", lambda e, j=j: e.dma_start(out=WBR[:, j * 4:(j + 1) * 4, :], in_=wsrc[:, j]), [], [WBR])
        wo = g.W["w_out"][l].rearrange("(k p) n -> p k n", p=128)
        for j in range(2):
            kb.dma("pool", lambda e, j=j: e.dma_start(out=WO[:, j * 4:(j + 1) * 4, :], in_=wo[:, j * 4:(j + 1) * 4, :]), [], [WO])
        load_bcast(g, G1[0], g.MODS[l, 0:1, 2 * D:3 * D])
        load_bcast(g, G1[1], g.MODS[l, 1:2, 2 * D:3 * D])
        pi = 0
        si = 0
        for tt in range(NT if ctx_out else 16):
            i = tt % 2
            ob, gt, obt, y, yb, yt = OBs[i], GTs[i], OBTs[i], Ys[i], YBs[i], YTs[i]
            G = G1[0] if tt < 16 else G1[1]
            dma(lambda e, ob=ob, tt=tt: e.dma_start(out=ob[:], in_=OBv[:, tt, :]), [g.OB], [ob])
            dma(lambda e, gt=gt, tt=tt: e.dma_start(out=gt[:], in_=Pv[:, tt, 5312:9408]), [g.P], [gt])
            for hf in range(2):
                ps = g.PS[6 + hf]
                psb = ps[:].bitcast(BF16)
                for c in range(8):
                    kb.op("pe", lambda e, c=c, hf=hf, psb=psb, ob=ob: e.transpose(out=psb[:, c * 128:(c + 1) * 128], in_=ob[:, (hf * 8 + c) * 128:(hf * 8 + c + 1) * 128], identity=g.ident_b[:]), [ob, g.ident_b], [ps])
                kb.op("act", lambda e, hf=hf, psb=psb, obt=obt: e.activation(out=obt[:, hf * 8:(hf + 1) * 8, :], in_=psb.rearrange("p (k t) -> p k t", k=8), func=AF.Copy), [ps], [obt])
            for hf in range(2):
                for j in range(4):
                    ps = g.PS[pi % 4]; pi += 1
                    for c in range(4):
                        kb.op("pe", lambda e, c=c, j=j, hf=hf, ps=ps, obt=obt: e.matmul(ps[:], lhsT=obt[:, j * 4 + c, :], rhs=WBR[:, j * 4 + c, hf * 512:(hf + 1) * 512], start=(c == 0), stop=(c == 3)), [obt, WBR], [ps])
                    sg = SGs[si % 3]; tp = TPs[si % 3]; si += 1
                    kb.op("act", lambda e, sg=sg, gt=gt, j=j, hf=hf: e.activation(out=sg[:], in_=gt[:, j * 1024 + hf * 512:j * 1024 + (hf + 1) * 512], func=AF.Sigmoid), [gt], [sg])
                    ysl = (slice(None), slice(hf * 512, (hf + 1) * 512))
                    if j == 0:
                        kb.op("dve", lambda e, ps=ps, sg=sg, y=y, ysl=ysl: e.tensor_tensor(out=y[ysl], in0=ps[:], in1=sg[:], op=ALU.mult), [ps, sg], [y])
                    else:
                        kb.op("dve", lambda e, ps=ps, sg=sg, tp=tp: e.tensor_tensor(out=tp[:], in0=ps[:], in1=sg[:], op=ALU.mult), [ps, sg], [tp])
                        if j < 3:
                            kb.op("pool", lambda e, tp=tp, y=y, ysl=ysl: e.tensor_tensor(out=y[ysl], in0=y[ysl], in1=tp[:], op=ALU.add), [y, tp], [y])
                        else:
                            kb.op("pool", lambda e, tp=tp, y=y, yb=yb, ysl=ysl: e.tensor_tensor(out=yb[ysl], in0=y[ysl], in1=tp[:], op=ALU.add), [y, tp], [yb])
            ps = g.PS[6]
            psb = ps[:].bitcast(BF16)
            for c in range(8):
                kb.op("pe", lambda e, c=c, psb=psb, yb=yb: e.transpose(out=psb[:, c * 128:(c + 1) * 128], in_=yb[:, c * 128:(c + 1) * 128], identity=g.ident_b[:]), [yb, g.ident_b], [ps])
            kb.op("act", lambda e, psb=psb, yt=yt: e.activation(out=yt[:], in_=psb.rearrange("p (k t) -> p k t", k=8), func=AF.Copy), [ps], [yt])
            for hf in range(2):
                ps = g.PS[pi % 4]; pi += 1
                for k in range(8):
                    kb.op("pe", lambda e, k=k, hf=hf, ps=ps, yt=yt: e.matmul(ps[:], lhsT=yt[:, k, :], rhs=WO[:, k, hf * 512:(hf + 1) * 512], start=(k == 0), stop=(k == 7)), [yt, WO], [ps])
                tp = TPs[si % 3]; si += 1
                xs = g.X[tt]
                kb.op("dve", lambda e, ps=ps, tp=tp, G=G, hf=hf: e.tensor_tensor(out=tp[:], in0=ps[:], in1=G[:, hf * 512:(hf + 1) * 512], op=ALU.mult), [ps, G], [tp])
                kb.op("pool", lambda e, tp=tp, xs=xs, hf=hf: e.tensor_tensor(out=xs[:, hf * 512:(hf + 1) * 512], in0=xs[:, hf * 512:(hf + 1) * 512], in1=tp[:], op=ALU.add), [xs, tp], [xs])
        kb.barrier()


def phase_final(g):
    nc, kb = g.nc, g.kb
    from contextlib import ExitStack
    dma = lambda fn, r=(), w=(): kb.dma("sp", fn, r, w)
    with ExitStack() as es:
        al = lambda shape, dt: es.enter_context(nc.sbuf_tensor(_nm(), shape, dt))
        FG = Buf(al([128, 1024], F32), "FG")
        junk = Buf(al([128, 1024], F32), "junk")
        o_t = al([128, 2, 1024], F32); st_t = al([128, 2, 8], F32)
        Os = [Buf(o_t[:, i], "o%d" % i) for i in range(2)]; Ss = [Buf(st_t[:, i], "s%d" % i) for i in range(2)]
        dma(lambda e: e.dma_start(out=FG[:], in_=g.W["final_g"][0:1, :].partition_broadcast(128)), [], [FG])
        for tt in range(16):
            xin = g.X[tt]; st = Ss[tt % 2]; o = Os[tt % 2]
            kb.op("act", lambda e, xin=xin, st=st: e.activation(out=junk[:], in_=xin[:], func=AF.Square, accum_out=st[:, 0:1]), [xin], [junk, st])
            kb.op("dve", lambda e, st=st: e.tensor_scalar(out=st[:, 1:2], in0=st[:, 0:1], scalar1=1.0 / D, scalar2=EPS, op0=ALU.mult, op1=ALU.add), [st], [st])
            kb.op("act", lambda e, st=st: e.activation(out=st[:, 2:3], in_=st[:, 1:2], func=AF.Sqrt), [st], [st])
            kb.op("dve", lambda e, st=st: e.reciprocal(out=st[:, 3:4], in_=st[:, 2:3]), [st], [st])
            kb.op("dve", lambda e, xin=xin, st=st, o=o: e.scalar_tensor_tensor(out=o[:], in0=xin[:], scalar=st[:, 3:4], in1=FG[:], op0=ALU.mult, op1=ALU.mult), [xin, st, FG], [o])
            dma(lambda e, o=o, tt=tt: e.dma_start(out=g.out[tt * 128:(tt + 1) * 128, :], in_=o[:]), [o], [])
        kb.barrier()


def phase_peer(g, l, ctx_out):
    nc, kb = g.nc, g.kb
    from contextlib import ExitStack
    dma = lambda fn, r=(), w=(): kb.dma("sp", fn, r, w)
    NEG = -1.0e30
    uv_flat = g.UV.t
    H2P = g.PSP[2]; ACCP = g.PSP[3]
    with ExitStack() as es:
        al = lambda shape, dt: es.enter_context(nc.sbuf_tensor(_nm(), shape, dt))
        WQ = Buf(al([128, 8, 1024], BF16), "WQ"); KT = Buf(al([128, 8, 128], BF16), "KT")
        GS = Buf(al([128, 1024], F32), "GS"); SH = Buf(al([128, 1024], F32), "SH"); G2 = Buf(al([128, 1024], F32), "G2")
        IO16 = Buf(al([128, 16], F32), "IO16")
        kb.op("pool", lambda e: e.iota(IO16[:], pattern=[[1, 16]], base=0, channel_multiplier=0, allow_small_or_imprecise_dtypes=True), [], [IO16])
        wq = g.W["peer_wq"][l].rearrange("(k p) n -> p k n", p=128)
        for j in range(2):
            kb.dma("pool", lambda e, j=j: e.dma_start(out=WQ[:, j * 4:(j + 1) * 4, :], in_=wq[:, j * 4:(j + 1) * 4, :]), [], [WQ])
        junk = Buf(al([128, 1024], F32), "junk"); junkb = Buf(al([128, 1024], BF16), "junkb")
        keyf = junk[:].rearrange("p (a d) -> p a d", a=16); keyb = junkb[:].rearrange("p (a d) -> p a d", a=16)
        dma(lambda e: e.dma_start(out=keyf, in_=g.W["peer_keys"][l].rearrange("h p n d -> n (h p) d")), [], [junk])
        kb.op("dve", lambda e: e.tensor_copy(out=keyb, in_=keyf), [junk], [junkb])
        ps = g.PS[3]
        psb = ps[:].bitcast(BF16)
        for h in range(8):
            kb.op("pe", lambda e, h=h: e.transpose(out=psb[:, h * 128:(h + 1) * 128], in_=keyb[:, h * 2:(h + 1) * 2, :].rearrange("n a d -> n (a d)"), identity=g.ident_b[:]), [junkb, g.ident_b], [ps])
        kb.op("act", lambda e: e.activation(out=KT[:], in_=psb.rearrange("p (h n) -> p h n", h=8), func=AF.Copy), [ps], [KT])
        hb_t = al([128, 2, 1024], BF16); hT_t = al([128, 2, 8, 128], BF16); qT_t = al([128, 2, 8, 128], BF16)
        st_t = al([128, 2, 8], F32)
        SC = Buf(al([128, 2, 8, 128], F32), "SC")
        TMP = Buf(al([128, 256], F32), "TMP")
        SV = Buf(al([128, 2, 8, 16], F32), "SV"); SI = Buf(al([128, 2, 8, 16], U32), "SI"); SIF = Buf(al([128, 2, 8, 16], F32), "SIF")
        CS_ = Buf(al([128, 8, 256], F32), "CS_")
        TS = Buf(al([128, 8, 16], F32), "TS"); POS = Buf(al([128, 8, 16], U32), "POS")
        PA = Buf(al([128, 2, 128], U32), "PA"); PAF = Buf(al([128, 2, 128], F32), "PAF")
        OH = Buf(al([128, 8, 16, 16], F32), "OH"); IAB = Buf(al([128, 2, 128], F32), "IAB")
        IDX = Buf(al([128, 128], I32), "IDX"); GW = Buf(al([128, 8, 16], F32), "GW"); RZ = Buf(al([128, 16], F32), "RZ")
        DOT = Buf(al([128, 128], F32), "DOT"); WGT = Buf(al([128, 128], F32), "WGT")
        NGRP = 3
        GSZ = 4
        gb_t = al([128, NGRP, GSZ, 2048], BF16)
        GB = [[Buf(gb_t[:, i, j], "gb%d_%d" % (i, j)) for j in range(GSZ)] for i in range(NGRP)]
        WG = Buf(al([128, 128], F32), "WG")
        dg_t = al([128, 4, 128], BF16)
        DGs = [Buf(dg_t[:, i], "dg%d" % i) for i in range(4)]
        ACCB = [g.PS[6], g.PS[7]]
        HBs = [Buf(hb_t[:, i], "hb%d" % i) for i in range(2)]
        HTs = [Buf(hT_t[:, i], "hT%d" % i) for i in range(2)]; QTs = [Buf(qT_t[:, i], "qT%d" % i) for i in range(2)]
        STs = [Buf(st_t[:, i], "st%d" % i) for i in range(2)]
        gi = 0
        tiles = list(range(NT if ctx_out else 16))
        for tt in tiles:
            if tt == 0 or tt == 16:
                r = 0 if tt < 16 else 1
                dma(lambda e: e.dma_start(out=GS[:], in_=g.W["norm2_g"][l:l + 1, :].partition_broadcast(128)), [], [GS])
                load_bcast(g, junk, g.MODS[l, r:r + 1, 4 * D:5 * D])
                kb.op("dve", lambda e: e.scalar_tensor_tensor(out=GS[:], in0=junk[:], scalar=1.0, in1=GS[:], op0=ALU.add, op1=ALU.mult), [junk, GS], [GS])
                load_bcast(g, SH, g.MODS[l, r:r + 1, 3 * D:4 * D])
                load_bcast(g, G2, g.MODS[l, r:r + 1, 5 * D:6 * D])
            i = tt % 2
            hb, hT, qT, st = HBs[i], HTs[i], QTs[i], STs[i]
            h2 = H2P
            xin = g.X[tt]
            kb.op("act", lambda e: e.activation(out=junk[:], in_=xin[:], func=AF.Square, accum_out=st[:, 0:1]), [xin], [junk, st])
            kb.op("dve", lambda e: e.tensor_scalar(out=st[:, 1:2], in0=st[:, 0:1], scalar1=1.0 / D, scalar2=EPS, op0=ALU.mult, op1=ALU.add), [st], [st])
            kb.op("act", lambda e: e.activation(out=st[:, 2:3], in_=st[:, 1:2], func=AF.Sqrt), [st], [st])
            kb.op("dve", lambda e: e.reciprocal(out=st[:, 3:4], in_=st[:, 2:3]), [st], [st])
            kb.op("dve", lambda e: e.scalar_tensor_tensor(out=junk[:], in0=xin[:], scalar=st[:, 3:4], in1=GS[:], op0=ALU.mult, op1=ALU.mult), [xin, st, GS], [junk])
            kb.op("dve", lambda e: e.tensor_tensor(out=h2[:], in0=junk[:], in1=SH[:], op=ALU.add), [junk, SH], [h2])
            kb.op("act", lambda e: e.activation(out=hb[:], in_=h2[:], func=AF.Copy), [h2], [hb])
            ps = g.PS[3]
            psb = ps[:].bitcast(BF16)
            for k in range(8):
                kb.op("pe", lambda e, k=k: e.transpose(out=psb[:, k * 128:(k + 1) * 128], in_=hb[:, k * 128:(k + 1) * 128], identity=g.ident_b[:]), [hb, g.ident_b], [ps])
            kb.op("act", lambda e: e.activation(out=hT[:], in_=psb.rearrange("p (k t) -> p k t", k=8), func=AF.Copy), [ps], [hT])
            for hh in range(2):
                ps = g.PS[hh]
                for h4 in range(4):
                    h = hh * 4 + h4
                    for k in range(8):
                        kb.op("pe", lambda e, k=k, h=h, h4=h4, ps=ps: e.matmul(ps[:, h4 * 128:(h4 + 1) * 128], lhsT=WQ[:, k, h * 128:(h + 1) * 128], rhs=hT[:, k, :],
                                                                            start=(k == 0 and h4 == 0), stop=(k == 7), skip_group_check=True), [WQ, hT], [ps])
                kb.op("act", lambda e, hh=hh, ps=ps: e.activation(out=qT[:, hh * 4:(hh + 1) * 4, :], in_=ps[:].rearrange("p (h t) -> p h t", h=4), func=AF.Copy), [ps], [qT])
            for p in range(2):
                for hh in range(2):
                    ps = g.PS[p * 2 + hh]
                    for h4 in range(4):
                        h = hh * 4 + h4
                        kb.op("pe", lambda e, p=p, h=h, h4=h4, ps=ps: e.matmul(ps[:, h4 * 128:(h4 + 1) * 128], lhsT=qT[p * 64:(p + 1) * 64, h, :], rhs=KT[p * 64:(p + 1) * 64, h, :],
                                                                            start=True, stop=True, skip_group_check=True), [qT, KT], [ps])
                    kb.op("act", lambda e, p=p, hh=hh, ps=ps: e.activation(out=SC[:, p, hh * 4:(hh + 1) * 4, :], in_=ps[:].rearrange("p (h t) -> p h t", h=4), func=AF.Copy), [ps], [SC])
            for p in range(2):
                for h in range(8):
                    s_ = SC[:, p, h, :]
                    kb.op("dve", lambda e, p=p, h=h, s_=s_: e.max(out=SV[:, p, h, 0:8], in_=s_), [SC], [SV])
                    kb.op("dve", lambda e, p=p, h=h, s_=s_: e.max_index(out=SI[:, p, h, 0:8], in_max=SV[:, p, h, 0:8], in_values=s_), [SC, SV], [SI])
                    kb.op("dve", lambda e, p=p, h=h, s_=s_: e.match_replace(out=TMP[:, 0:128], in_to_replace=SV[:, p, h, 0:8], in_values=s_, imm_value=NEG), [SC, SV], [TMP])
                    kb.op("dve", lambda e, p=p, h=h: e.max(out=SV[:, p, h, 8:16], in_=TMP[:, 0:128]), [TMP], [SV])
                    kb.op("dve", lambda e, p=p, h=h: e.max_index(out=SI[:, p, h, 8:16], in_max=SV[:, p, h, 8:16], in_values=TMP[:, 0:128]), [TMP, SV], [SI])
            kb.op("dve", lambda e: e.tensor_copy(out=SIF[:], in_=SI[:]), [SI], [SIF])
            c4 = CS_[:].rearrange("p h (a b) -> p h a b", a=16)
            kb.op("dve", lambda e: e.tensor_tensor(out=c4, in0=SV[:, 0].unsqueeze(3).to_broadcast([128, 8, 16, 16]), in1=SV[:, 1].unsqueeze(2).to_broadcast([128, 8, 16, 16]), op=ALU.add), [SV], [CS_])
            for h in range(8):
                kb.op("dve", lambda e, h=h: e.max(out=TS[:, h, 0:8], in_=CS_[:, h, :]), [CS_], [TS])
                kb.op("dve", lambda e, h=h: e.max_index(out=POS[:, h, 0:8], in_max=TS[:, h, 0:8], in_values=CS_[:, h, :]), [CS_, TS], [POS])
                kb.op("dve", lambda e, h=h: e.match_replace(out=TMP[:], in_to_replace=TS[:, h, 0:8], in_values=CS_[:, h, :], imm_value=NEG), [CS_, TS], [TMP])
                kb.op("dve", lambda e, h=h: e.max(out=TS[:, h, 8:16], in_=TMP[:]), [TMP], [TS])
                kb.op("dve", lambda e, h=h: e.max_index(out=POS[:, h, 8:16], in_max=TS[:, h, 8:16], in_values=TMP[:]), [TMP, TS], [POS])
            posf = POS[:].rearrange("p h j -> p (h j)")
            kb.op("dve", lambda e: e.tensor_single_scalar(out=PA[:, 0, :], in_=posf, scalar=4, op=ALU.logical_shift_right), [POS], [PA])
            kb.op("dve", lambda e: e.tensor_single_scalar(out=PA[:, 1, :], in_=posf, scalar=15, op=ALU.bitwise_and), [POS], [PA])
            kb.op("dve", lambda e: e.tensor_copy(out=PAF[:], in_=PA[:]), [PA], [PAF])
            for w in range(2):
                pa3 = PAF[:, w, :].rearrange("p (h j) -> p h j", h=8)
                kb.op("dve", lambda e, pa3=pa3: e.tensor_tensor(out=OH[:], in0=pa3.unsqueeze(3).to_broadcast([128, 8, 16, 16]),
                                                               in1=IO16[:].unsqueeze(1).unsqueeze(1).to_broadcast([128, 8, 16, 16]), op=ALU.is_equal), [PAF, IO16], [OH])
                kb.op("dve", lambda e, w=w: e.tensor_tensor(out=OH[:], in0=OH[:], in1=SIF[:, w].unsqueeze(2).to_broadcast([128, 8, 16, 16]), op=ALU.mult), [OH, SIF], [OH])
                kb.op("dve", lambda e, w=w: e.tensor_reduce(out=IAB[:, w, :].rearrange("p (h j) -> p h j", h=8), in_=OH[:], axis=AX.X, op=ALU.add), [OH], [IAB])
            kb.op("dve", lambda e: e.tensor_scalar(out=IAB[:, 0, :], in0=IAB[:, 0, :], scalar1=128.0, scalar2=float(l * 16384), op0=ALU.mult, op1=ALU.add), [IAB], [IAB])
            kb.op("dve", lambda e: e.tensor_tensor(out=IAB[:, 0, :], in0=IAB[:, 0, :], in1=IAB[:, 1, :], op=ALU.add), [IAB], [IAB])
            kb.op("dve", lambda e: e.tensor_copy(out=IDX[:], in_=IAB[:, 0, :]), [IAB], [IDX])
            kb.op("dve", lambda e: e.tensor_tensor(out=GW[:], in0=TS[:], in1=TS[:, :, 0:1].to_broadcast([128, 8, 16]), op=ALU.subtract), [TS], [GW])
            kb.op("act", lambda e: e.activation(out=GW[:], in_=GW[:], func=AF.Exp), [GW], [GW])
            kb.op("dve", lambda e: e.tensor_reduce(out=RZ[:, 0:8], in_=GW[:], axis=AX.X, op=ALU.add), [GW], [RZ])
            kb.op("dve", lambda e: e.reciprocal(out=RZ[:, 8:16], in_=RZ[:, 0:8]), [RZ], [RZ])
            kb.op("dve", lambda e: e.tensor_tensor(out=GW[:], in0=GW[:], in1=RZ[:, 8:16].unsqueeze(2).to_broadcast([128, 8, 16]), op=ALU.mult), [GW, RZ], [GW])
            ngr = 128 // GSZ

            def acc_group(k):
                gb = GB[k % NGRP]
                sl = slice(k * GSZ, (k + 1) * GSZ)
                kb.op("dve", lambda e: e.tensor_tensor(out=WGT[:, sl], in0=WG[:, sl], in1=GW[:].rearrange("p h j -> p (h j)")[:, sl], op=ALU.mult), [WG, GW], [WGT])
                for j in range(GSZ):
                    s = k * GSZ + j
                    dg = DGs[s % 4]
                    kb.op("act", lambda e, dg=dg, s=s: e.activation(out=dg[:], in_=g.ident_b[:], func=AF.Copy, scale=WGT[:, s:s + 1]), [g.ident_b, WGT], [dg])
                    for hf in range(2):
                        kb.op("pe", lambda e, dg=dg, j=j, hf=hf, s=s: e.matmul(ACCB[hf][:], lhsT=dg[:], rhs=gb[j][:, 1024 + hf * 512:1024 + (hf + 1) * 512],
                                                                            start=(s == 0), stop=(s == 127)), [dg, gb[j]], [ACCB[hf]])

            for k in range(ngr):
                gb = GB[k % NGRP]
                for j in range(GSZ):
                    s = k * GSZ + j
                    kb.dma("pool", lambda e, gb=gb, j=j, s=s: e.indirect_dma_start(out=gb[j][:], out_offset=None, in_=uv_flat,
                           in_offset=bass.IndirectOffsetOnAxis(ap=IDX[:, s:s + 1], axis=0)), [IDX], [gb[j]])
                for j in range(GSZ):
                    s = k * GSZ + j
                    kb.op("dve", lambda e, gb=gb, j=j, s=s: e.scalar_tensor_tensor(out=junkb[:], in0=gb[j][:, 0:1024], scalar=1.0, in1=h2[:], op0=ALU.mult, op1=ALU.mult,
                                                                                  accum_out=DOT[:, s:s + 1]), [gb[j], h2], [junkb, DOT])
                sl = slice(k * GSZ, (k + 1) * GSZ)
                kb.op("act", lambda e, sl=sl: e.activation(out=WG[:, sl], in_=DOT[:, sl], func=AF.Gelu), [DOT], [WG])
                if k >= 1:
                    acc_group(k - 1)
            acc_group(ngr - 1)
            for hf in range(2):
                kb.op("dve", lambda e, hf=hf: e.tensor_tensor(out=junk[:, hf * 512:(hf + 1) * 512], in0=ACCB[hf][:], in1=G2[:, hf * 512:(hf + 1) * 512], op=ALU.mult), [ACCB[hf], G2], [junk])
            kb.op("dve", lambda e: e.tensor_tensor(out=xin[:], in0=xin[:], in1=junk[:], op=ALU.add), [xin, junk], [xin])
        kb.barrier()


def kernel(**inputs):
    nc, g = build_program()
    consts = host_consts()
    shared = {n: np.ascontiguousarray(np.asarray(inputs[n], dtype=np.float32)).reshape(s) for n, s in W_NAMES}
    shared.update(consts)
    shared["c_ctx"] = np.ascontiguousarray(np.asarray(inputs["c_ctx"], dtype=np.float32)).reshape(1, D)
    x = np.asarray(inputs["x"], dtype=np.float32)
    c = np.asarray(inputs["c"], dtype=np.float32)
    ctx = np.asarray(inputs["ctx"], dtype=np.float32)
    nb = x.shape[0]
    in_maps = []
    for b in range(nb):
        m = dict(shared)
        m["x"] = np.ascontiguousarray(x[b])
        m["ctx"] = np.ascontiguousarray(ctx[b])
        m["c"] = np.ascontiguousarray(c[b:b + 1])
        in_maps.append(m)
    res = run_bass_kernel_spmd(nc, in_maps, core_ids=list(range(nb)))
    out = np.stack([np.asarray(r["out"], dtype=np.float32) for r in res.results], axis=0)
    return out


def phase_cast_experts(g, n_layers):
    nc, kb = g.nc, g.kb
    from contextlib import ExitStack
    with ExitStack() as es:
        al = lambda shape, dt: es.enter_context(nc.sbuf_tensor(_nm(), shape, dt))
        f_t = al([128, 2, 8, 1024], F32); b_t = al([128, 2, 8, 1024], BF16)
        Fs = [Buf(f_t[:, i], "cf%d" % i) for i in range(2)]; Bs = [Buf(b_t[:, i], "cb%d" % i) for i in range(2)]
        srcs = [(g.W["peer_u"].rearrange("l e d -> (l e) d"), g.UV[:, 0:D]), (g.W["peer_v"].rearrange("l e d -> (l e) d"), g.UV[:, D:2 * D])]
        n = 0
        engs = ["act", "dve", "pool"]
        for src, dst in srcs:
            for ch in range(n_layers * 16):
                r0 = ch * 1024
                f = Fs[n % 2]; b = Bs[n % 2]
                kb.dma("sp", lambda e, f=f, src=src, r0=r0: e.dma_start(out=f[:], in_=src[r0:r0 + 1024, :].rearrange("(p j) d -> p j d", p=128)), [], [f])
                for q in range(4):
                    en = engs[(n * 4 + q) % 3]
                    if en == "act":
                        kb.op("act", lambda e, f=f, b=b, q=q: e.activation(out=b[:, q * 2:(q + 1) * 2, :], in_=f[:, q * 2:(q + 1) * 2, :], func=AF.Copy), [f], [b])
                    else:
                        kb.op(en, lambda e, f=f, b=b, q=q: e.tensor_copy(out=b[:, q * 2:(q + 1) * 2, :], in_=f[:, q * 2:(q + 1) * 2, :]), [f], [b])
                kb.dma("sp", lambda e, b=b, dst=dst, r0=r0: e.dma_start(out=dst[r0:r0 + 1024, :].rearrange("(p j) d -> p j d", p=128), in_=b[:]), [b], [])
                n += 1
        kb.barrier()
```

```python
import numpy as np
import math
import concourse.bass as bass
import concourse.mybir as mybir
from concourse.bass_utils import run_bass_kernel_spmd

F32 = mybir.dt.float32
BF16 = mybir.dt.bfloat16
U32 = mybir.dt.uint32
I32 = mybir.dt.int32
AF = mybir.ActivationFunctionType
ALU = mybir.AluOpType
AX = mybir.AxisListType

NDS = 40


class Buf:
    __slots__ = ("t", "w", "r", "name", "nt")

    def __init__(self, t, name="", nt=False):
        self.t = t
        self.w = None
        self.r = []
        self.name = name
        self.nt = nt

    def __getitem__(self, k):
        return self.t[k]


class KB:
    def __init__(self, nc):
        self.nc = nc
        self.E = {"pe": nc.tensor, "act": nc.scalar, "dve": nc.vector, "pool": nc.gpsimd, "sp": nc.sync}
        self.esem = {n: nc.alloc_semaphore("s_" + n) for n in self.E}
        self.ecnt = {n: 0 for n in self.E}
        self.dsems = [nc.alloc_semaphore("d%d" % i) for i in range(NDS)]
        self.dcnt = [0] * NDS
        self.dnext = 0
        self.known = {n: {} for n in self.E}
        self.nwait = 0
        self.ninst = 0

    def _sem(self, key):
        return self.esem[key] if isinstance(key, str) else self.dsems[key]

    def _need(self, eng, ev):
        key, val, _ = ev
        if self.known[eng].get(key, 0) >= val:
            return
        self.E[eng].wait_ge(self._sem(key), val)
        self.known[eng][key] = val
        self.nwait += 1

    def _deps(self, eng, reads, writes, is_dma=False):
        for b in reads:
            if b.w is not None:
                self._need(eng, b.w)
        for b in writes:
            if b.w is not None and (is_dma or b.w[2] != eng):
                self._need(eng, b.w)
            for r in b.r:
                if is_dma or r[2] != eng:
                    self._need(eng, r)

    def op(self, eng, fn, reads=(), writes=()):
        reads = [b for b in reads if not b.nt]
        writes = [b for b in writes if not b.nt]
        self._deps(eng, reads, writes)
        inst = fn(self.E[eng])
        self.ecnt[eng] += 1
        inst.then_inc(self.esem[eng], 1)
        ev = (eng, self.ecnt[eng], eng)
        for b in reads:
            b.r.append(ev)
        for b in writes:
            b.w = ev
            b.r = []
        self.ninst += 1
        return inst

    def dma(self, q, fn, reads=(), writes=()):
        reads = [b for b in reads if not b.nt]
        writes = [b for b in writes if not b.nt]
        self._deps(q, reads, writes, is_dma=True)
        i = self.dnext
        self.dnext = (self.dnext + 1) % NDS
        if self.dcnt[i] > 0:
            self._need(q, (i, self.dcnt[i], "dma"))
        inst = fn(self.E[q])
        self.dcnt[i] += 16
        inst.then_inc(self.dsems[i], 16)
        ev = (i, self.dcnt[i], "dma")
        for b in reads:
            b.r.append(ev)
        for b in writes:
            b.w = ev
            b.r = []
        self.ninst += 1
        return inst

    def barrier(self):
        evs = [(n, self.ecnt[n], n) for n in self.E if self.ecnt[n] > 0]
        evs += [(i, self.dcnt[i], "dma") for i in range(NDS) if self.dcnt[i] > 0]
        for n in self.E:
            for ev in evs:
                self._need(n, ev)

    def finish(self):
        self.barrier()


D = 1024
NX = 2048
NC_ = 256
T = NX + NC_
NT = T // 128
P_IN = 9408
EPS = 1e-6
OFF = {"a_q1": 0, "a_q2": 256, "a_k1": 512, "a_k2": 768, "a_v": 1024,
       "b_cq": 1536, "b_ckv": 1792, "b_kr": 1920,
       "c_q": 1984, "c_f": 2496, "c_b": 3008, "c_i": 3520, "c_g": 4032,
       "d_q": 4544, "d_k": 5056, "d_v": 5184, "gate": 5312}

W_NAMES = [("w_mod", [2, 1024, 6144]), ("b_mod", [2, 6144]), ("norm1_g", [2, 1024]), ("norm2_g", [2, 1024]),
           ("w_in", [2, 1024, 9408]), ("diff_lam_q1", [2, 64]), ("diff_lam_k1", [2, 64]), ("diff_lam_q2", [2, 64]),
           ("diff_lam_k2", [2, 64]), ("diff_norm_g", [2, 128]), ("mla_qnorm_g", [2, 256]), ("mla_kvnorm_g", [2, 128]),
           ("mla_w_uq", [2, 256, 768]), ("mla_w_ukv", [2, 128, 1024]), ("hgrn_lb", [2, 2, 512]), ("hgrn_norm_g", [2, 128]),
           ("win_sink", [2, 8]), ("w_branch", [2, 4, 512, 1024]), ("w_out", [2, 1024, 1024]), ("peer_wq", [2, 1024, 1024]),
           ("peer_keys", [2, 8, 2, 128, 64]), ("peer_u", [2, 16384, 1024]), ("peer_v", [2, 16384, 1024]), ("final_g", [1, 1024])]


def host_consts():
    n = NX
    rows = np.repeat(np.arange(n // 64, dtype=np.float32), 64)
    cols = np.tile(np.arange(64, dtype=np.float32), n // 64)
    m = 32
    freqs = (10000.0 ** (-2.0 * np.arange(m // 2, dtype=np.float32) / m)).astype(np.float32)
    ang = np.concatenate([rows[:, None] * freqs, cols[:, None] * freqs], axis=-1).astype(np.float32)
    cs = np.zeros((T, 64), np.float32)
    cs[:n, :32] = np.cos(ang)
    cs[:n, 32:] = np.sin(ang)
    cs[n:, :32] = 1.0
    i = np.arange(128)
    c = {}
    c["rope_cs"] = cs
    c["ident"] = np.eye(128, dtype=np.float32)
    c["mask_prev"] = (i[:, None] >= i[None, :]).astype(np.float32)
    c["mask_next"] = (i[:, None] <= i[None, :]).astype(np.float32)
    j = np.arange(64)
    tri = np.zeros((64, 4, 64), np.float32)
    tri[:, 0] = (j[:, None] <= j[None, :])
    tri[:, 1] = (j[:, None] >= j[None, :])
    tri[:, 2] = (j[:, None] > j[None, :])
    tri[:, 3] = (j[:, None] < j[None, :])
    c["tri"] = tri.reshape(64, 256)
    return c


class Ctx:
    pass


_nmc = [0]


def _nm():
    _nmc[0] += 1
    return "tmp%d" % _nmc[0]


def build_program(n_layers=2, debug=(), stop_after=None):
    nc = bass.Bass("TRN2", target_bir_lowering=False)
    kb = KB(nc)
    g = Ctx()
    g.nc, g.kb = nc, kb
    g.debug = debug

    def din(name, shape, dt=F32):
        return nc.dram_tensor(name, list(shape), dt, kind="ExternalInput").ap()

    g.x_in = din("x", [NX, D])
    g.c_in = din("c", [1, D])
    g.ctx_in = din("ctx", [NC_, D])
    g.cctx_in = din("c_ctx", [1, D])
    g.W = {n: din(n, s) for n, s in W_NAMES}
    g.rope_cs = din("rope_cs", [T, 64])
    g.ident_in = din("ident", [128, 128])
    g.mprev_in = din("mask_prev", [128, 128])
    g.mnext_in = din("mask_next", [128, 128])
    g.tri_in = din("tri", [64, 256])
    g.out = nc.dram_tensor("out", [NX, D], F32, kind="ExternalOutput").ap()

    def scratch(name, shape, dt):
        kind = "ExternalOutput" if name in debug else "Internal"
        return Buf(nc.dram_tensor(name, list(shape), dt, kind=kind).ap(), name, nt=True)

    g.P = scratch("P", [T, P_IN], BF16)
    g.OB = scratch("OB", [T, 2048], BF16)
    g.MODS = scratch("MODS", [2, 2, 6144], F32)
    g.OF = scratch("OF", [T, 512], F32)
    g.OBK = scratch("OBK", [T, 512], F32)
    g.UV = scratch("UV", [2 * 16384, 2 * D], BF16)

    cnt = [0]

    def sb(shape, dt, name=None):
        cnt[0] += 1
        return Buf(nc.alloc_sbuf_tensor("%s_%d" % (name or "t", cnt[0]), list(shape), dt), name or "t")
    g.sb = sb

    g.X = [Buf(None, "X%d" % i) for i in range(NT)]
    xall = nc.alloc_sbuf_tensor("Xall", [128, NT, D], F32)
    for i in range(NT):
        g.X[i].t = xall[:, i, :]
    psall = nc.alloc_psum_tensor("psall", [128, 8, 512], F32)
    g.psall = psall
    g.PS = [Buf(psall[:, i, :], "ps%d" % i) for i in range(8)]
    g.PSP = [Buf(psall[:, 2 * i:2 * i + 2, :].rearrange("p b c -> p (b c)"), "psp%d" % i) for i in range(4)]
    g.ident_f = sb([128, 128], F32, "identf")
    g.ident_b = sb([128, 128], BF16, "identb")
    g.ones_f = sb([128, 128], F32, "onesf")
    g.SCT = sb([128, 8, 2], F32, "sct")

    dma = lambda fn, r=(), w=(): kb.dma("sp", fn, r, w)

    for i in range(16):
        dma(lambda e, i=i: e.dma_start(out=g.X[i][:], in_=g.x_in[i * 128:(i + 1) * 128, :]), [], [g.X[i]])
    for i in range(2):
        dma(lambda e, i=i: e.dma_start(out=g.X[16 + i][:], in_=g.ctx_in[i * 128:(i + 1) * 128, :]), [], [g.X[16 + i]])
    dma(lambda e: e.dma_start(out=g.ident_f[:], in_=g.ident_in[:, :]), [], [g.ident_f])
    kb.op("dve", lambda e: e.tensor_copy(out=g.ident_b[:], in_=g.ident_f[:]), [g.ident_f], [g.ident_b])
    kb.op("dve", lambda e: e.memset(g.ones_f[:], 1.0), [], [g.ones_f])
    craw = sb([128, 8, 2], F32, "craw")
    with nc.allow_non_contiguous_dma(reason="tiny conditioning vector load"):
        dma(lambda e: e.dma_start(out=craw[:, :, 0], in_=g.c_in.rearrange("o (k p) -> p (o k)", p=128)), [], [craw])
        dma(lambda e: e.dma_start(out=craw[:, :, 1], in_=g.cctx_in.rearrange("o (k p) -> p (o k)", p=128)), [], [craw])
    kb.op("act", lambda e: e.activation(out=g.SCT[:], in_=craw[:], func=AF.Silu), [craw], [g.SCT])

    if "skip_cast" not in debug:
        with nc.named_scope("cast"):
            phase_cast_experts(g, n_layers)
    for l in range(n_layers):
        last = (l == 1)
        with nc.named_scope("mod%d" % l):
            phase_mod(g, l)
        if stop_after == ("mod", l):
            break
        if "skip_proj" not in debug:
            with nc.named_scope("proj%d" % l):
                phase_norm_proj(g, l)
        if stop_after == ("proj", l):
            break
        ctx_out = not last
        if "skip_a" not in debug:
            with nc.named_scope("a%d" % l):
                phase_mixer_a(g, l, ctx_out)
        if stop_after == ("a", l):
            break
        if "skip_b" not in debug:
            with nc.named_scope("b%d" % l):
                phase_mixer_b(g, l, ctx_out)
        if stop_after == ("b", l):
            break
        if "skip_d" not in debug:
            with nc.named_scope("d%d" % l):
                phase_mixer_d(g, l, ctx_out)
        if stop_after == ("d", l):
            break
        if "skip_c" not in debug:
            with nc.named_scope("c%d" % l):
                phase_mixer_c(g, l, ctx_out)
        if stop_after == ("c", l):
            break
        if "skip_merge" not in debug:
            with nc.named_scope("merge%d" % l):
                phase_merge(g, l, ctx_out)
        if "Xdbg" in debug and stop_after == ("merge", l):
            xd = nc.dram_tensor("Xdbg", [NT, 128, D], F32, kind="ExternalOutput").ap()
            for i in range(NT):
                dma(lambda e, i=i: e.dma_start(out=xd[i], in_=g.X[i][:]), [g.X[i]], [])
        if stop_after == ("merge", l):
            break
        with nc.named_scope("peer%d" % l):
            phase_peer(g, l, ctx_out)
        if "Xdbg" in debug and stop_after == ("peer", l):
            xd = nc.dram_tensor("Xdbg", [NT, 128, D], F32, kind="ExternalOutput").ap()
            for i in range(NT):
                dma(lambda e, i=i: e.dma_start(out=xd[i], in_=g.X[i][:]), [g.X[i]], [])
        if stop_after == ("peer", l):
            break

    if stop_after is None:
        with nc.named_scope("final"):
            phase_final(g)
    kb.finish()
    g.nc = nc
    return nc, g


def phase_mod(g, l):
    nc, kb = g.nc, g.kb
    kb.barrier()
    dma = lambda fn, r=(), w=(): kb.dma("sp", fn, r, w)
    with nc.sbuf_tensor(_nm(), [128, 2, 8, 512], F32) as wm_t, nc.sbuf_tensor(_nm(), [1, 6144], F32) as bm_t, \
            nc.sbuf_tensor(_nm(), [2, 2, 512], F32) as ev_t:
        wm = [Buf(wm_t[:, i], "wm%d" % i) for i in range(2)]
        bm = Buf(bm_t, "bm")
        ev = [Buf(ev_t[:, i], "ev%d" % i) for i in range(2)]
        dma(lambda e: e.dma_start(out=bm[:], in_=g.W["b_mod"][l:l + 1, :]), [], [bm])
        wsrc = g.W["w_mod"][l].rearrange("(k p) c -> p k c", p=128)
        for m in range(12):
            w = wm[m % 2]
            dma(lambda e, w=w, m=m: e.dma_start(out=w[:], in_=wsrc[:, :, m * 512:(m + 1) * 512]), [], [w])
            ps = g.PS[m % 2]
            for k in range(8):
                kb.op("pe", lambda e, w=w, k=k, ps=ps: e.matmul(ps[0:2, :], lhsT=g.SCT[:, k, :], rhs=w[:, k, :],
                                                              start=(k == 0), stop=False), [g.SCT, w], [ps])
            kb.op("pe", lambda e, ps=ps, m=m: e.matmul(ps[0:2, :], lhsT=g.ones_f[0:1, 0:2], rhs=bm[0:1, m * 512:(m + 1) * 512],
                                                     start=False, stop=True), [g.ones_f, bm], [ps])
            o = ev[m % 2]
            kb.op("act", lambda e, o=o, ps=ps: e.activation(out=o[:], in_=ps[0:2, :], func=AF.Copy), [ps], [o])
            dma(lambda e, o=o, m=m: e.dma_start(out=g.MODS[l, :, m * 512:(m + 1) * 512], in_=o[:]), [o], [g.MODS])
        kb.barrier()


def load_bcast(g, dst, src_row_ap):
    g.kb.dma("sp", lambda e: e.dma_start(out=dst[:], in_=src_row_ap.partition_broadcast(128)), [g.MODS], [dst])


def norm_tile(g, xin, GS, SH, hb, scr):
    kb = g.kb
    junk, st = scr
    kb.op("act", lambda e: e.activation(out=junk[:], in_=xin[:], func=AF.Square, accum_out=st[:, 0:1]), [xin], [junk, st])
    kb.op("dve", lambda e: e.tensor_scalar(out=st[:, 1:2], in0=st[:, 0:1], scalar1=1.0 / D, scalar2=EPS, op0=ALU.mult, op1=ALU.add), [st], [st])
    kb.op("act", lambda e: e.activation(out=st[:, 2:3], in_=st[:, 1:2], func=AF.Sqrt), [st], [st])
    kb.op("dve", lambda e: e.reciprocal(out=st[:, 3:4], in_=st[:, 2:3]), [st], [st])
    kb.op("dve", lambda e: e.scalar_tensor_tensor(out=junk[:], in0=xin[:], scalar=st[:, 3:4], in1=GS[:], op0=ALU.mult, op1=ALU.mult), [xin, st, GS], [junk])
    kb.op("dve", lambda e: e.tensor_tensor(out=hb[:], in0=junk[:], in1=SH[:], op=ALU.add), [junk, SH], [hb])


def phase_norm_proj(g, l):
    nc, kb = g.nc, g.kb
    dma = lambda fn, r=(), w=(): kb.dma("sp", fn, r, w)
    with nc.sbuf_tensor(_nm(), [128, 8, T], BF16) as hT_t:
        hT = Buf(hT_t, "hT")
        with nc.sbuf_tensor(_nm(), [128, 5, D], F32) as bc_t, nc.sbuf_tensor(_nm(), [128, D], F32) as junk_t, \
                nc.sbuf_tensor(_nm(), [128, 2, 8], F32) as st_t, nc.sbuf_tensor(_nm(), [128, 2, D], BF16) as hb_t:
            G1, SCx, SHx, SCc, SHc = [Buf(bc_t[:, i], "bc%d" % i) for i in range(5)]
            junk = Buf(junk_t, "junk")
            sts = [Buf(st_t[:, i], "st%d" % i) for i in range(2)]
            hbs = [Buf(hb_t[:, i], "hb%d" % i) for i in range(2)]
            dma(lambda e: e.dma_start(out=G1[:], in_=g.W["norm1_g"][l:l + 1, :].partition_broadcast(128)), [], [G1])
            load_bcast(g, SHx, g.MODS[l, 0:1, 0:D])
            load_bcast(g, SCx, g.MODS[l, 0:1, D:2 * D])
            load_bcast(g, SHc, g.MODS[l, 1:2, 0:D])
            load_bcast(g, SCc, g.MODS[l, 1:2, D:2 * D])
            for S in (SCx, SCc):
                kb.op("dve", lambda e, S=S: e.scalar_tensor_tensor(out=S[:], in0=S[:], scalar=1.0, in1=G1[:], op0=ALU.add, op1=ALU.mult), [S, G1], [S])
            for tt in range(NT):
                GS, SH = (SCx, SHx) if tt < 16 else (SCc, SHc)
                hb = hbs[tt % 2]
                norm_tile(g, g.X[tt], GS, SH, hb, (junk, sts[tt % 2]))
                ps = g.PS[tt % 2]
                psb = ps[:].bitcast(BF16)
                for k in range(8):
                    kb.op("pe", lambda e, k=k, hb=hb, psb=psb: e.transpose(out=psb[:, k * 128:(k + 1) * 128], in_=hb[:, k * 128:(k + 1) * 128], identity=g.ident_b[:]), [hb, g.ident_b], [ps])
                kb.op("act", lambda e, tt=tt, psb=psb: e.activation(out=hT[:, :, tt * 128:(tt + 1) * 128], in_=psb.rearrange("p (k t) -> p k t", k=8), func=AF.Copy), [ps], [hT])
            kb.barrier()
        with nc.sbuf_tensor(_nm(), [128, 2, 8, 512], BF16) as wb_t, nc.sbuf_tensor(_nm(), [128, 2, 6, 512], BF16) as po_t:
            wbs = [Buf(wb_t[:, i], "wb%d" % i) for i in range(2)]
            pos = [Buf(po_t[:, i], "po%d" % i) for i in range(2)]
            wsrc = g.W["w_in"][l].rearrange("(k p) c -> p k c", p=128)
            pdst = g.P.t.rearrange("(t p) c -> p t c", p=128)
            nct = (P_IN + 511) // 512
            cw = lambda ct: min(512, P_IN - ct * 512)

            def load_w(ct):
                w = wbs[ct % 2]
                kb.dma("pool", lambda e: e.dma_start(out=w[:, :, 0:cw(ct)], in_=wsrc[:, :, ct * 512:ct * 512 + cw(ct)]), [], [w])
            load_w(0)
            oi = 0
            pi = 0
            for ct in range(nct):
                if ct + 1 < nct:
                    load_w(ct + 1)
                w = wbs[ct % 2]
                n = cw(ct)
                for tg in range(3):
                    po = pos[oi % 2]
                    oi += 1
                    for j in range(6):
                        tt = tg * 6 + j
                        ps = g.PS[2 + pi % 4]
                        pi += 1
                        for k in range(8):
                            kb.op("pe", lambda e, k=k, tt=tt, ps=ps, w=w, n=n: e.matmul(ps[:, 0:n], lhsT=hT[:, k, tt * 128:(tt + 1) * 128], rhs=w[:, k, 0:n],
                                                                                     start=(k == 0), stop=(k == 7)), [hT, w], [ps])
                        if pi % 2 == 0:
                            kb.op("act", lambda e, j=j, ps=ps, po=po, n=n: e.activation(out=po[:, j, 0:n], in_=ps[:, 0:n], func=AF.Copy), [ps], [po])
                        else:
                            kb.op("dve", lambda e, j=j, ps=ps, po=po, n=n: e.tensor_copy(out=po[:, j, 0:n], in_=ps[:, 0:n]), [ps], [po])
                    dma(lambda e, po=po, tg=tg, ct=ct, n=n: e.dma_start(out=pdst[:, tg * 6:(tg + 1) * 6, ct * 512:ct * 512 + n], in_=po[:, :, 0:n]), [po], [g.P])
            kb.barrier()


def attention(g, nh, dv, scale, V, qk_chunks, groups, post, npass=1):
    nc, kb = g.nc, g.kb
    assert dv + 1 <= 256
    with nc.sbuf_tensor(_nm(), [128, 3, 512], BF16) as E_t, nc.sbuf_tensor(_nm(), [128, 4, 4, dv + 1], F32) as of_t:
        Es = [Buf(E_t[:, i], "E%d" % i) for i in range(3)]
        Ofs = [Buf(of_t[:, i], "Of%d" % i) for i in range(4)]
        iters = []
        for h in range(nh):
            for (q_lo, q_n, key_tiles) in groups:
                for s in range(npass):
                    iters.append((h, q_lo, q_n, key_tiles, s))
        steps = [(ii, ki) for ii, it_ in enumerate(iters) for ki in range(len(it_[3]))]

        def emit_S(i):
            ii, ki = steps[i]
            h, q_lo, q_n, kts, s = iters[ii]
            kt = kts[ki]
            S = g.PS[i % 3]
            chunks = qk_chunks(h, s)
            for ci, (qf, kf, bufs) in enumerate(chunks):
                kb.op("pe", lambda e, qf=qf, kf=kf, ci=ci: e.matmul(
                    S[:, 0:q_n], lhsT=kf(kt), rhs=qf(q_lo, q_n), start=(ci == 0), stop=(ci == len(chunks) - 1)), bufs, [S])

        ofl = []

        def emit_rest(i):
            ii, ki = steps[i]
            h, q_lo, q_n, kts, s = iters[ii]
            kt = kts[ki]
            nsub = q_n // 128
            S = g.PS[i % 3]; E = Es[i % 3]
            obanks = [g.PS[3 + 2 * (ii % 2)], g.PS[4 + 2 * (ii % 2)]]
            Of = Ofs[ii % 4]
            kb.op("act", lambda e: e.activation(out=E[:, 0:q_n], in_=S[:, 0:q_n], func=AF.Exp, scale=scale), [S], [E])
            for sub in range(nsub):
                ob = obanks[sub // 2]
                c0 = (sub % 2) * (dv + 1)
                kb.op("pe", lambda e, ob=ob, c0=c0, sub=sub: e.matmul(
                    ob[:, c0:c0 + dv + 1], lhsT=E[:, sub * 128:(sub + 1) * 128], rhs=V[:, kt, h, :],
                    start=(ki == 0 and sub % 2 == 0), stop=(ki == len(kts) - 1), skip_group_check=True), [E, V], [ob])
            if ki == len(kts) - 1:
                for bi in range((nsub + 1) // 2):
                    nb = min(2, nsub - bi * 2)
                    ob = obanks[bi]
                    src = ob[:, 0:nb * (dv + 1)].rearrange("p (s d) -> p s d", s=nb)
                    kb.op("dve", lambda e, bi=bi, nb=nb, src=src: e.tensor_copy(out=Of[:, bi * 2:bi * 2 + nb, :], in_=src), [ob], [Of])
                ofl.append(Of)
                if s == npass - 1:
                    post(h, q_lo, q_n, list(ofl))
                    del ofl[:]

        emit_S(0)
        for i in range(len(steps)):
            if i + 1 < len(steps):
                emit_S(i + 1)
            emit_rest(i)


def phase_mixer_a(g, l, ctx_out):
    nc, kb = g.nc, g.kb
    dma = lambda fn, r=(), w=(): kb.dma("sp", fn, r, w)
    lam_init = 0.8 - 0.6 * math.exp(-0.3 * l)
    Pv = g.P.t.rearrange("(t p) c -> p t c", p=128)
    OBv = g.OB.t.rearrange("(t p) c -> p t c", p=128)
    with nc.sbuf_tensor(_nm(), [128, 8, T], BF16) as qkt_t, nc.sbuf_tensor(_nm(), [128, NT, 4, 129], BF16) as va_t, \
            nc.sbuf_tensor(_nm(), [128, NT, 64], F32) as cs_t, nc.sbuf_tensor(_nm(), [128, 8], F32) as lam_t, \
            nc.sbuf_tensor(_nm(), [128, 128], F32) as ng_t:
        QKT = Buf(qkt_t, "QKT"); VA = Buf(va_t, "VA"); CS = Buf(cs_t, "CS"); LAM = Buf(lam_t, "LAM"); NG = Buf(ng_t, "NG")
        dma(lambda e: e.dma_start(out=CS[:], in_=g.rope_cs.rearrange("(t p) c -> p t c", p=128)), [], [CS])
        for tt in range(NT):
            dma(lambda e, tt=tt: e.dma_start(out=VA[:, tt, :, 0:128], in_=Pv[:, tt, 1024:1536].rearrange("p (h d) -> p h d", h=4)), [g.P], [VA])
        kb.op("pool", lambda e: e.memset(VA[:, :, :, 128:129], 1.0), [], [VA])
        with nc.sbuf_tensor(_nm(), [128, 4, 64], F32) as lv_t:
            LV = Buf(lv_t, "LV")
            for i, nm in enumerate(["diff_lam_q1", "diff_lam_k1", "diff_lam_q2", "diff_lam_k2"]):
                dma(lambda e, i=i, nm=nm: e.dma_start(out=LV[:, i, :], in_=g.W[nm][l:l + 1, :].partition_broadcast(128)), [], [LV])
            kb.op("dve", lambda e: e.tensor_tensor(out=LV[:, 0, :], in0=LV[:, 0, :], in1=LV[:, 1, :], op=ALU.mult), [LV], [LV])
            kb.op("dve", lambda e: e.tensor_tensor(out=LV[:, 2, :], in0=LV[:, 2, :], in1=LV[:, 3, :], op=ALU.mult), [LV], [LV])
            kb.op("dve", lambda e: e.tensor_reduce(out=LAM[:, 0:1], in_=LV[:, 0, :], axis=AX.X, op=ALU.add), [LV], [LAM])
            kb.op("dve", lambda e: e.tensor_reduce(out=LAM[:, 1:2], in_=LV[:, 2, :], axis=AX.X, op=ALU.add), [LV], [LAM])
            kb.op("act", lambda e: e.activation(out=LAM[:, 2:4], in_=LAM[:, 0:2], func=AF.Exp), [LAM], [LAM])
            kb.op("dve", lambda e: e.tensor_tensor(out=LAM[:, 4:5], in0=LAM[:, 2:3], in1=LAM[:, 3:4], op=ALU.subtract), [LAM], [LAM])
            kb.op("dve", lambda e: e.tensor_scalar(out=LAM[:, 5:6], in0=LAM[:, 4:5], scalar1=lam_init, scalar2=None, op0=ALU.add), [LAM], [LAM])
            dma(lambda e: e.dma_start(out=NG[:], in_=g.W["diff_norm_g"][l:l + 1, :].partition_broadcast(128)), [], [NG])
            kb.op("dve", lambda e: e.tensor_scalar(out=NG[:], in0=NG[:], scalar1=1.0 - lam_init, scalar2=None, op0=ALU.mult), [NG], [NG])
            kb.barrier()
        lam = LAM[:, 5:6]
        with nc.sbuf_tensor(_nm(), [128, 2, 1024], BF16) as pa_t, nc.sbuf_tensor(_nm(), [128, 2, 1024], BF16) as rb_t, \
                nc.sbuf_tensor(_nm(), [128, 4, 512], F32) as tm_t:
            pas = [Buf(pa_t[:, i], "pa%d" % i) for i in range(2)]
            rbs = [Buf(rb_t[:, i], "rb%d" % i) for i in range(2)]
            tms = [Buf(tm_t[:, i], "tm%d" % i) for i in range(4)]
            for tt in range(NT):
                pa = pas[tt % 2]; rb = rbs[tt % 2]
                dma(lambda e, pa=pa, tt=tt: e.dma_start(out=pa[:], in_=Pv[:, tt, 0:1024]), [g.P], [pa])
                rope_tile(g, pa, rb, CS, tt, 16, tms)
                ps = g.PS[7]
                psb = ps[:].bitcast(BF16)
                for c in range(8):
                    kb.op("pe", lambda e, c=c, rb=rb, psb=psb: e.transpose(out=psb[:, c * 128:(c + 1) * 128], in_=rb[:, c * 128:(c + 1) * 128], identity=g.ident_b[:]), [rb, g.ident_b], [ps])
                kb.op("act", lambda e, tt=tt, psb=psb: e.activation(out=QKT[:, :, tt * 128:(tt + 1) * 128], in_=psb.rearrange("p (k t) -> p k t", k=8), func=AF.Copy), [ps], [QKT])
            kb.barrier()

        def qk_chunks(h, s):
            p0 = (h % 2) * 64
            qc = s * 2 + h // 2
            kc = 4 + s * 2 + h // 2
            return [(lambda lo, n: QKT[p0:p0 + 64, qc, lo:lo + n], lambda kt: QKT[p0:p0 + 64, kc, kt * 128:(kt + 1) * 128], [QKT])]

        with nc.sbuf_tensor(_nm(), [128, 2, 8, 4], F32) as r_t, nc.sbuf_tensor(_nm(), [128, 2, 3, 4, 128], F32) as w_t, \
                nc.sbuf_tensor(_nm(), [128, 2, 4, 128], BF16) as o_t:
            cnt = [0]
            rs_ = [Buf(r_t[:, i], "r") for i in range(2)]; obs_ = [Buf(o_t[:, i], "ob") for i in range(2)]
            ws_ = [[Buf(w_t[:, i, j], "w%d" % j) for j in range(3)] for i in range(2)]

            def post(h, q_lo, q_n, ofl):
                O1, O2 = ofl
                i = cnt[0] % 2
                cnt[0] += 1
                ns = q_n // 128
                r = rs_[i]; w0, w1, w2 = ws_[i]; ob = obs_[i]
                bc = lambda ap: ap.unsqueeze(2).to_broadcast([128, ns, 128])
                kb.op("dve", lambda e: e.reciprocal(out=r[:, 0, 0:ns], in_=O1[:, 0:ns, 128]), [O1], [r])
                kb.op("dve", lambda e: e.reciprocal(out=r[:, 1, 0:ns], in_=O2[:, 0:ns, 128]), [O2], [r])
                kb.op("dve", lambda e: e.tensor_scalar(out=r[:, 2, 0:ns], in0=r[:, 1, 0:ns], scalar1=lam, scalar2=None, op0=ALU.mult), [r, LAM], [r])
                kb.op("dve", lambda e: e.tensor_tensor(out=w0[:, 0:ns, :], in0=O1[:, 0:ns, 0:128], in1=bc(r[:, 0, 0:ns]), op=ALU.mult), [O1, r], [w0])
                kb.op("dve", lambda e: e.tensor_tensor(out=w1[:, 0:ns, :], in0=O2[:, 0:ns, 0:128], in1=bc(r[:, 2, 0:ns]), op=ALU.mult), [O2, r], [w1])
                kb.op("dve", lambda e: e.tensor_tensor(out=w0[:, 0:ns, :], in0=w0[:, 0:ns, :], in1=w1[:, 0:ns, :], op=ALU.subtract), [w0, w1], [w0])
                kb.op("pool", lambda e: e.tensor_tensor(out=w2[:, 0:ns, :], in0=w0[:, 0:ns, :], in1=w0[:, 0:ns, :], op=ALU.mult), [w0], [w2])
                kb.op("dve", lambda e: e.tensor_reduce(out=r[:, 3, 0:ns], in_=w2[:, 0:ns, :], axis=AX.X, op=ALU.add), [w2], [r])
                kb.op("dve", lambda e: e.tensor_scalar(out=r[:, 4, 0:ns], in0=r[:, 3, 0:ns], scalar1=1.0 / 128, scalar2=EPS, op0=ALU.mult, op1=ALU.add), [r], [r])
                kb.op("act", lambda e: e.activation(out=r[:, 5, 0:ns], in_=r[:, 4, 0:ns], func=AF.Sqrt), [r], [r])
                kb.op("dve", lambda e: e.reciprocal(out=r[:, 6, 0:ns], in_=r[:, 5, 0:ns]), [r], [r])
                kb.op("dve", lambda e: e.tensor_tensor(out=w1[:, 0:ns, :], in0=w0[:, 0:ns, :], in1=bc(r[:, 6, 0:ns]), op=ALU.mult), [w0, r], [w1])
                kb.op("pool", lambda e: e.tensor_tensor(out=ob[:, 0:ns, :], in0=w1[:, 0:ns, :], in1=NG[:].unsqueeze(1).to_broadcast([128, ns, 128]), op=ALU.mult), [w1, NG], [ob])
                t0 = q_lo // 128
                dma(lambda e: e.dma_start(out=OBv[:, t0:t0 + ns, h * 128:(h + 1) * 128], in_=ob[:, 0:ns, :]), [ob], [g.OB])

            groups = [(i * 512, 512, list(range(NT))) for i in range(4)]
            if ctx_out:
                groups.append((2048, 256, [16, 17]))
            attention(g, 4, 128, 1.0 / 8.0, VA, qk_chunks, groups, post, npass=2)
            kb.barrier()


def rope_tile(g, pa, rb, CS, tt, ng, tms, col0=0):
    kb = g.kb
    n = ng * 64
    xv = pa[:, col0:col0 + n].rearrange("p (g a f j) -> p g a f j", g=ng, a=2, f=2, j=16)
    ov = rb[:, col0:col0 + n].rearrange("p (g a f j) -> p g a f j", g=ng, a=2, f=2, j=16)
    x1, x2 = xv[:, :, :, 0, :], xv[:, :, :, 1, :]
    cosb = CS[:, tt, 0:32].rearrange("p (a j) -> p a j", a=2).unsqueeze(1).to_broadcast([128, ng, 2, 16])
    sinb = CS[:, tt, 32:64].rearrange("p (a j) -> p a j", a=2).unsqueeze(1).to_broadcast([128, ng, 2, 16])
    tv = [t[:, 0:ng * 32].rearrange("p (g a j) -> p g a j", g=ng, a=2) for t in tms]
    kb.op("dve", lambda e: e.tensor_tensor(out=tv[0], in0=x1, in1=cosb, op=ALU.mult), [pa, CS], [tms[0]])
    kb.op("pool", lambda e: e.tensor_tensor(out=tv[1], in0=x2, in1=sinb, op=ALU.mult), [pa, CS], [tms[1]])
    kb.op("dve", lambda e: e.tensor_tensor(out=tv[2], in0=x1, in1=sinb, op=ALU.mult), [pa, CS], [tms[2]])
    kb.op("pool", lambda e: e.tensor_tensor(out=tv[3], in0=x2, in1=cosb, op=ALU.mult), [pa, CS], [tms[3]])
    kb.op("dve", lambda e: e.tensor_tensor(out=ov[:, :, :, 0, :], in0=tv[0], in1=tv[1], op=ALU.subtract), [tms[0], tms[1]], [rb])
    kb.op("pool", lambda e: e.tensor_tensor(out=ov[:, :, :, 1, :], in0=tv[2], in1=tv[3], op=ALU.add), [tms[2], tms[3]], [rb])


def small_rms(g, src_ap, src_buf, n, nh, r, col):
    kb = g.kb
    return None


def phase_mixer_b(g, l, ctx_out):
    nc, kb = g.nc, g.kb
    dma = lambda fn, r=(), w=(): kb.dma("sp", fn, r, w)
    Pv = g.P.t.rearrange("(t p) c -> p t c", p=128)
    OBv = g.OB.t.rearrange("(t p) c -> p t c", p=128)
    TQ = T if ctx_out else NX
    with nc.sbuf_tensor(_nm(), [128, 4, T], BF16) as ft_t, nc.sbuf_tensor(_nm(), [128, 4, T], BF16) as qnt_t, \
            nc.sbuf_tensor(_nm(), [128, 4, T], BF16) as knt_t, nc.sbuf_tensor(_nm(), [128, 2, T], BF16) as qrt_t, \
            nc.sbuf_tensor(_nm(), [128, NT, 4, 129], BF16) as vb_t:
        FT = Buf(ft_t, "FT"); QNT = Buf(qnt_t, "QNT"); KNT = Buf(knt_t, "KNT"); QRT = Buf(qrt_t, "QRT"); VB = Buf(vb_t, "VB")
        kb.op("pool", lambda e: e.memset(VB[:, :, :, 128:129], 1.0), [], [VB])
        with nc.sbuf_tensor(_nm(), [128, 2, 4, 128], BF16) as wqn_t, nc.sbuf_tensor(_nm(), [128, 2, 4, 64], BF16) as wqr_t, \
                nc.sbuf_tensor(_nm(), [128, 4, 128], BF16) as wkn_t, nc.sbuf_tensor(_nm(), [128, 4, 128], BF16) as wv_t, \
                nc.sbuf_tensor(_nm(), [128, 384], F32) as gn_t, nc.sbuf_tensor(_nm(), [128, NT, 64], F32) as cs_t:
            WQN = Buf(wqn_t, "WQN"); WQR = Buf(wqr_t, "WQR"); WKN = Buf(wkn_t, "WKN"); WV = Buf(wv_t, "WV"); GN = Buf(gn_t, "GN"); CS = Buf(cs_t, "CS")
            dma(lambda e: e.dma_start(out=CS[:], in_=g.rope_cs.rearrange("(t p) c -> p t c", p=128)), [], [CS])
            wuq = g.W["mla_w_uq"][l].rearrange("(c p) (h d) -> p c h d", p=128, h=4)
            wukv = g.W["mla_w_ukv"][l].rearrange("p (h d) -> p h d", h=4)
            for c in range(2):
                kb.dma("pool", lambda e, c=c: e.dma_start(out=WQN[:, c], in_=wuq[:, c, :, 0:128]), [], [WQN])
                kb.dma("pool", lambda e, c=c: e.dma_start(out=WQR[:, c], in_=wuq[:, c, :, 128:192]), [], [WQR])
            kb.dma("pool", lambda e: e.dma_start(out=WKN[:], in_=wukv[:, :, 0:128]), [], [WKN])
            kb.dma("pool", lambda e: e.dma_start(out=WV[:], in_=wukv[:, :, 128:256]), [], [WV])
            dma(lambda e: e.dma_start(out=GN[:, 0:256], in_=g.W["mla_qnorm_g"][l:l + 1, :].partition_broadcast(128)), [], [GN])
            dma(lambda e: e.dma_start(out=GN[:, 256:384], in_=g.W["mla_kvnorm_g"][l:l + 1, :].partition_broadcast(128)), [], [GN])
            with nc.sbuf_tensor(_nm(), [128, 2, 448], BF16) as pb_t, nc.sbuf_tensor(_nm(), [128, 2, 512], BF16) as tb_t, \
                    nc.sbuf_tensor(_nm(), [128, 2, 512], F32) as jk_t, nc.sbuf_tensor(_nm(), [128, 2, 8], F32) as st_t, \
                    nc.sbuf_tensor(_nm(), [128, 4, 128], F32) as tm_t, nc.sbuf_tensor(_nm(), [128, 2, 64], BF16) as kr_t:
                tms = [Buf(tm_t[:, i], "tm%d" % i) for i in range(4)]
                pbs = [Buf(pb_t[:, i], "pb") for i in range(2)]; tbs = [Buf(tb_t[:, i], "tb") for i in range(2)]
                jks = [Buf(jk_t[:, i], "jk") for i in range(2)]; sts = [Buf(st_t[:, i], "st") for i in range(2)]
                for tt in range(NT):
                    i = tt % 2
                    pb = pbs[i]; tb = tbs[i]; jk = jks[i]; st = sts[i]
                    dma(lambda e, pb=pb, tt=tt: e.dma_start(out=pb[:], in_=Pv[:, tt, 1536:1984]), [g.P], [pb])
                    kb.op("act", lambda e: e.activation(out=jk[:, 0:256], in_=pb[:, 0:256], func=AF.Square, accum_out=st[:, 0:1]), [pb], [jk, st])
                    kb.op("act", lambda e: e.activation(out=jk[:, 256:384], in_=pb[:, 256:384], func=AF.Square, accum_out=st[:, 1:2]), [pb], [jk, st])
                    kb.op("dve", lambda e: e.tensor_scalar(out=st[:, 2:3], in0=st[:, 0:1], scalar1=1.0 / 256, scalar2=EPS, op0=ALU.mult, op1=ALU.add), [st], [st])
                    kb.op("dve", lambda e: e.tensor_scalar(out=st[:, 3:4], in0=st[:, 1:2], scalar1=1.0 / 128, scalar2=EPS, op0=ALU.mult, op1=ALU.add), [st], [st])
                    kb.op("act", lambda e: e.activation(out=st[:, 4:6], in_=st[:, 2:4], func=AF.Sqrt), [st], [st])
                    kb.op("dve", lambda e: e.reciprocal(out=st[:, 6:8], in_=st[:, 4:6]), [st], [st])
                    kb.op("dve", lambda e: e.scalar_tensor_tensor(out=tb[:, 0:256], in0=pb[:, 0:256], scalar=st[:, 6:7], in1=GN[:, 0:256], op0=ALU.mult, op1=ALU.mult), [pb, st, GN], [tb])
                    kb.op("dve", lambda e: e.scalar_tensor_tensor(out=tb[:, 256:384], in0=pb[:, 256:384], scalar=st[:, 7:8], in1=GN[:, 256:384], op0=ALU.mult, op1=ALU.mult), [pb, st, GN], [tb])
                    rope_tile(g, pb, tb, CS, tt, 1, tms, col0=384)
                    kb.op("pool", lambda e: e.tensor_copy(out=tb[:, 448:512], in_=tb[:, 384:448]), [tb], [tb])
                    ps = g.PS[7]
                    psb = ps[:].bitcast(BF16)
                    for c in range(4):
                        kb.op("pe", lambda e, c=c, tb=tb, psb=psb: e.transpose(out=psb[:, c * 128:(c + 1) * 128], in_=tb[:, c * 128:(c + 1) * 128], identity=g.ident_b[:]), [tb, g.ident_b], [ps])
                    kb.op("act", lambda e, tt=tt, psb=psb: e.activation(out=FT[:, :, tt * 128:(tt + 1) * 128], in_=psb[:, 0:512].rearrange("p (k t) -> p k t", k=4), func=AF.Copy), [ps], [FT])
                kb.barrier()
            pi = 0
            ngr = (T + 511) // 512
            for gi in range(ngr):
                lo = gi * 512
                n = min(512, T - lo)
                for h in range(4):
                    ps = g.PS[pi % 4]; pi += 1
                    for c in range(2):
                        kb.op("pe", lambda e, c=c, h=h, ps=ps, lo=lo, n=n: e.matmul(ps[:, 0:n], lhsT=WQN[:, c, h, :], rhs=FT[:, c, lo:lo + n], start=(c == 0), stop=(c == 1)), [WQN, FT], [ps])
                    kb.op("act", lambda e, h=h, ps=ps, lo=lo, n=n: e.activation(out=QNT[:, h, lo:lo + n], in_=ps[:, 0:n], func=AF.Copy), [ps], [QNT])
                    ps = g.PS[pi % 4]; pi += 1
                    kb.op("pe", lambda e, h=h, ps=ps, lo=lo, n=n: e.matmul(ps[:, 0:n], lhsT=WKN[:, h, :], rhs=FT[:, 2, lo:lo + n], start=True, stop=True), [WKN, FT], [ps])
                    kb.op("dve", lambda e, h=h, ps=ps, lo=lo, n=n: e.tensor_copy(out=KNT[:, h, lo:lo + n], in_=ps[:, 0:n]), [ps], [KNT])
            with nc.sbuf_tensor(_nm(), [128, 2, 256], BF16) as qr_t, nc.sbuf_tensor(_nm(), [128, 2, 256], BF16) as qq_t, \
                    nc.sbuf_tensor(_nm(), [128, 4, 128], F32) as tm_t:
                tms = [Buf(tm_t[:, i], "tm%d" % i) for i in range(4)]
                qrs = [Buf(qr_t[:, i], "qr") for i in range(2)]; qqs = [Buf(qq_t[:, i], "qq") for i in range(2)]
                for tt in range(NT):
                    i = tt % 2
                    qr = qrs[i]; qq = qqs[i]
                    ps = g.PS[pi % 4]; pi += 1
                    kb.op("pe", lambda e, ps=ps, tt=tt: e.matmul(ps[:, 0:512], lhsT=FT[:, 2, tt * 128:(tt + 1) * 128], rhs=WV[:].rearrange("p h d -> p (h d)"), start=True, stop=True), [FT, WV], [ps])
                    kb.op("act", lambda e, ps=ps, tt=tt: e.activation(out=VB[:, tt, :, 0:128], in_=ps[:, 0:512].rearrange("p (h d) -> p h d", h=4), func=AF.Copy), [ps], [VB])
                    ps = g.PS[pi % 4]; pi += 1
                    for c in range(2):
                        kb.op("pe", lambda e, c=c, ps=ps, tt=tt: e.matmul(ps[:, 0:256], lhsT=FT[:, c, tt * 128:(tt + 1) * 128], rhs=WQR[:, c].rearrange("p h d -> p (h d)"), start=(c == 0), stop=(c == 1)), [FT, WQR], [ps])
                    kb.op("act", lambda e, ps=ps, qq=qq: e.activation(out=qq[:], in_=ps[:, 0:256], func=AF.Copy), [ps], [qq])
                    rope_tile(g, qq, qr, CS, tt, 4, tms)
                    ps = g.PS[7]
                    psb = ps[:].bitcast(BF16)
                    for c in range(2):
                        kb.op("pe", lambda e, c=c, qr=qr, psb=psb: e.transpose(out=psb[:, c * 128:(c + 1) * 128], in_=qr[:, c * 128:(c + 1) * 128], identity=g.ident_b[:]), [qr, g.ident_b], [ps])
                    kb.op("dve", lambda e, tt=tt, psb=psb: e.tensor_copy(out=QRT[:, :, tt * 128:(tt + 1) * 128], in_=psb[:, 0:256].rearrange("p (k t) -> p k t", k=2)), [ps], [QRT])
                kb.barrier()

        if "dbgB" in g.debug:
            dft = nc.dram_tensor("dbgFT", [128, 4, T], BF16, kind="ExternalOutput").ap()
            dqr = nc.dram_tensor("dbgQRT", [128, 2, T], BF16, kind="ExternalOutput").ap()
            dqn = nc.dram_tensor("dbgQNT", [128, 4, T], BF16, kind="ExternalOutput").ap()
            dkn = nc.dram_tensor("dbgKNT", [128, 4, T], BF16, kind="ExternalOutput").ap()
            dvb = nc.dram_tensor("dbgVB", [128, NT, 4, 129], BF16, kind="ExternalOutput").ap()
            dma(lambda e: e.dma_start(out=dft[:], in_=FT[:]), [FT], [])
            dma(lambda e: e.dma_start(out=dqr[:], in_=QRT[:]), [QRT], [])
            dma(lambda e: e.dma_start(out=dqn[:], in_=QNT[:]), [QNT], [])
            dma(lambda e: e.dma_start(out=dkn[:], in_=KNT[:]), [KNT], [])
            dma(lambda e: e.dma_start(out=dvb[:], in_=VB[:]), [VB], [])

        def qk_chunks(h, s):
            p0 = (h % 2) * 64
            return [(lambda lo, n: QNT[:, h, lo:lo + n], lambda kt: KNT[:, h, kt * 128:(kt + 1) * 128], [QNT, KNT]),
                    (lambda lo, n: QRT[p0:p0 + 64, h // 2, lo:lo + n], lambda kt: FT[p0:p0 + 64, 3, kt * 128:(kt + 1) * 128], [QRT, FT])]

        with nc.sbuf_tensor(_nm(), [128, 2, 4], F32) as r_t, nc.sbuf_tensor(_nm(), [128, 2, 4, 128], BF16) as o_t:
            cnt = [0]
            rs_ = [Buf(r_t[:, i], "r") for i in range(2)]; obs_ = [Buf(o_t[:, i], "ob") for i in range(2)]

            def post(h, q_lo, q_n, ofl):
                O1 = ofl[0]
                i = cnt[0] % 2
                cnt[0] += 1
                ns = q_n // 128
                r = rs_[i]; ob = obs_[i]
                kb.op("dve", lambda e: e.reciprocal(out=r[:, 0:ns], in_=O1[:, 0:ns, 128]), [O1], [r])
                kb.op("dve", lambda e: e.tensor_tensor(out=ob[:, 0:ns, :], in0=O1[:, 0:ns, 0:128], in1=r[:, 0:ns].unsqueeze(2).to_broadcast([128, ns, 128]), op=ALU.mult), [O1, r], [ob])
                t0 = q_lo // 128
                dma(lambda e: e.dma_start(out=OBv[:, t0:t0 + ns, 512 + h * 128:512 + (h + 1) * 128], in_=ob[:, 0:ns, :]), [ob], [g.OB])

            groups = [(i * 512, 512, list(range(NT))) for i in range(4)]
            if ctx_out:
                groups.append((2048, 256, [16, 17]))
            attention(g, 4, 128, 192.0 ** -0.5, VB, qk_chunks, groups, post, npass=1)
            kb.barrier()


def phase_mixer_d(g, l, ctx_out):
    nc, kb = g.nc, g.kb
    dma = lambda fn, r=(), w=(): kb.dma("sp", fn, r, w)
    Pv = g.P.t.rearrange("(t p) c -> p t c", p=128)
    OBv = g.OB.t.rearrange("(t p) c -> p t c", p=128)
    scale = 1.0 / 8.0
    with nc.sbuf_tensor(_nm(), [128, 6, T], BF16) as qkd_t, nc.sbuf_tensor(_nm(), [128, NT, 2, 65], BF16) as vd_t, \
            nc.sbuf_tensor(_nm(), [128, 8], F32) as es_t, nc.sbuf_tensor(_nm(), [128, 2, 128], BF16) as mk_t, \
            nc.sbuf_tensor(_nm(), [128, 2, 128], F32) as mkf_t:
        QKD = Buf(qkd_t, "QKD"); VD = Buf(vd_t, "VD"); ES = Buf(es_t, "ES"); MK = Buf(mk_t, "MK"); MKF = Buf(mkf_t, "MKF")
        kb.op("pool", lambda e: e.memset(VD[:, :, :, 64:65], 1.0), [], [VD])
        for tt in range(NT):
            dma(lambda e, tt=tt: e.dma_start(out=VD[:, tt, :, 0:64], in_=Pv[:, tt, 5184:5312].rearrange("p (h d) -> p h d", h=2)), [g.P], [VD])
        dma(lambda e: e.dma_start(out=MKF[:, 0, :], in_=g.mprev_in[:, :]), [], [MKF])
        dma(lambda e: e.dma_start(out=MKF[:, 1, :], in_=g.mnext_in[:, :]), [], [MKF])
        kb.op("dve", lambda e: e.tensor_copy(out=MK[:], in_=MKF[:]), [MKF], [MK])
        dma(lambda e: e.dma_start(out=ES[:], in_=g.W["win_sink"][l:l + 1, :].partition_broadcast(128)), [], [ES])
        kb.op("act", lambda e: e.activation(out=ES[:], in_=ES[:], func=AF.Exp), [ES], [ES])
        with nc.sbuf_tensor(_nm(), [128, 2, 640], BF16) as pd_t, nc.sbuf_tensor(_nm(), [128, 2, 768], BF16) as rb_t, \
                nc.sbuf_tensor(_nm(), [128, 4, 320], F32) as tm_t, nc.sbuf_tensor(_nm(), [128, NT, 64], F32) as cs_t:
            CS = Buf(cs_t, "CS")
            dma(lambda e: e.dma_start(out=CS[:], in_=g.rope_cs.rearrange("(t p) c -> p t c", p=128)), [], [CS])
            pds = [Buf(pd_t[:, i], "pd%d" % i) for i in range(2)]
            rbs = [Buf(rb_t[:, i], "rb%d" % i) for i in range(2)]
            tms = [Buf(tm_t[:, i], "tm%d" % i) for i in range(4)]
            for tt in range(NT if "d_skip_rope" not in g.debug else 0):
                pd = pds[tt % 2]; rb = rbs[tt % 2]
                dma(lambda e, pd=pd, tt=tt: e.dma_start(out=pd[:], in_=Pv[:, tt, 4544:5184]), [g.P], [pd])
                rope_tile(g, pd, rb, CS, tt, 10, tms)
                kb.op("dve", lambda e, rb=rb: e.tensor_copy(out=rb[:, 640:768].rearrange("p (u d) -> p u d", u=2),
                                                           in_=rb[:, 576:640].unsqueeze(1).to_broadcast([128, 2, 64])), [rb], [rb])
                kb.op("dve", lambda e, rb=rb: e.tensor_copy(out=rb[:, 576:640], in_=rb[:, 512:576]), [rb], [rb])
                ps = g.PS[7]
                psb = ps[:].bitcast(BF16)
                for c in range(6):
                    kb.op("pe", lambda e, c=c, rb=rb, psb=psb: e.transpose(out=psb[:, c * 128:(c + 1) * 128], in_=rb[:, c * 128:(c + 1) * 128], identity=g.ident_b[:]), [rb, g.ident_b], [ps])
                kb.op("act", lambda e, tt=tt, psb=psb: e.activation(out=QKD[:, :, tt * 128:(tt + 1) * 128], in_=psb[:, 0:768].rearrange("p (k t) -> p k t", k=6), func=AF.Copy), [ps], [QKD])
            kb.barrier()
        with nc.sbuf_tensor(_nm(), [128, 3, 512], BF16) as E_t, nc.sbuf_tensor(_nm(), [128, 2, 8, 65], F32) as of_t, \
                nc.sbuf_tensor(_nm(), [128, 2, 2, 8], F32) as r_t, nc.sbuf_tensor(_nm(), [128, 2, 8, 64], BF16) as o_t:
            Es = [Buf(E_t[:, i], "E%d" % i) for i in range(3)]
            Ofs = [Buf(of_t[:, i], "Of%d" % i) for i in range(2)]
            rs_ = [Buf(r_t[:, i], "r%d" % i) for i in range(2)]
            obs_ = [Buf(o_t[:, i], "ob%d" % i) for i in range(2)]
            qtiles = list(range(16)) + ([16, 17] if ctx_out else [])
            if "d_prep_only" in g.debug:
                qtiles = []
            iters = []
            for qn_, qi in enumerate(qtiles):
                if qi < 16:
                    kts = ([(qi - 1, 0)] if qi > 0 else []) + [(qi, None)] + ([(qi + 1, 1)] if qi < 15 else []) + [(16, None), (17, None)]
                else:
                    kts = [(16, None), (17, None)]
                for kvh in range(2):
                    iters.append((qn_, qi, kvh, kts))
            steps = [(ii, ki) for ii, it_ in enumerate(iters) for ki in range(len(it_[3]))]
            Spairs = [(g.PS[0], g.PS[1]), (g.PS[2], g.PS[7])]

            def emit_S(i):
                ii, ki = steps[i]
                qn_, qi, kvh, kts = iters[ii]
                kt, mk = kts[ki]
                Sp = Spairs[i % 2]
                for gq in range(4):
                    hq = kvh * 4 + gq
                    p0 = (hq % 2) * 64
                    S = Sp[gq % 2]
                    c0 = (gq // 2) * 128
                    kb.op("pe", lambda e, S=S, c0=c0, p0=p0, hq=hq: e.matmul(
                        S[:, c0:c0 + 128], lhsT=QKD[p0:p0 + 64, 4 + kvh, kt * 128:(kt + 1) * 128],
                        rhs=QKD[p0:p0 + 64, hq // 2, qi * 128:(qi + 1) * 128], start=True, stop=True, skip_group_check=True), [QKD], [S])

            def emit_rest(i):
                ii, ki = steps[i]
                qn_, qi, kvh, kts = iters[ii]
                kt, mk = kts[ki]
                Sp = Spairs[i % 2]
                E = Es[i % 3]
                Of = Ofs[qn_ % 2]; r = rs_[qn_ % 2]; ob = obs_[qn_ % 2]
                O = g.PS[3 + ii % 4]
                for j in range(2):
                    kb.op("act", lambda e, j=j: e.activation(out=E[:, j * 256:(j + 1) * 256], in_=Sp[j][:, 0:256], func=AF.Exp, scale=scale), [Sp[j]], [E])
                if mk is not None:
                    kb.op("dve", lambda e: e.tensor_tensor(out=E[:].rearrange("p (g q) -> p g q", g=4), in0=E[:].rearrange("p (g q) -> p g q", g=4),
                                                           in1=MKF[:, mk, :].unsqueeze(1).to_broadcast([128, 4, 128]), op=ALU.mult), [E, MKF], [E])
                for gq in range(4):
                    ei = (gq % 2) * 2 + gq // 2
                    kb.op("pe", lambda e, gq=gq, ei=ei: e.matmul(
                        O[:, gq * 65:(gq + 1) * 65], lhsT=E[:, ei * 128:(ei + 1) * 128], rhs=VD[:, kt, kvh, :],
                        start=(ki == 0 and gq == 0), stop=(ki == len(kts) - 1), skip_group_check=True), [E, VD], [O])
                if ki == len(kts) - 1:
                    kb.op("act", lambda e: e.activation(out=Of[:, kvh * 4:(kvh + 1) * 4, :], in_=O[:, 0:260].rearrange("p (g d) -> p g d", g=4), func=AF.Copy), [O], [Of])
                    if kvh == 1:
                        kb.op("dve", lambda e: e.tensor_tensor(out=r[:, 0, :], in0=Of[:, :, 64], in1=ES[:], op=ALU.add), [Of, ES], [r])
                        kb.op("dve", lambda e: e.reciprocal(out=r[:, 1, :], in_=r[:, 0, :]), [r], [r])
                        kb.op("dve", lambda e: e.tensor_tensor(out=ob[:], in0=Of[:, :, 0:64], in1=r[:, 1, :].unsqueeze(2).to_broadcast([128, 8, 64]), op=ALU.mult), [Of, r], [ob])
                        dma(lambda e: e.dma_start(out=OBv[:, qi, 1536:2048], in_=ob[:].rearrange("p h d -> p (h d)")), [ob], [g.OB])

            if steps:
                emit_S(0)
            for i in range(len(steps)):
                if i + 1 < len(steps):
                    emit_S(i + 1)
                emit_rest(i)
            kb.barrier()


def phase_mixer_c(g, l, ctx_out):
    nc, kb = g.nc, g.kb
    from contextlib import ExitStack
    dma = lambda fn, r=(), w=(): kb.dma("sp", fn, r, w)
    orders = [[32, 33, 34, 35] + list(range(32)), [35, 34, 33, 32] + list(range(31, -1, -1))]
    ODST = [g.OF, g.OBK]
    with ExitStack() as es:
        al = lambda shape, dt: es.enter_context(nc.sbuf_tensor(_nm(), shape, dt))
        TRI = Buf(al([64, 4, 64], F32), "TRI"); LB = Buf(al([64, 2, 2, 512], F32), "LB")
        S_t = al([128, 2, 4, 128], F32); Sb_t = al([128, 2, 4, 128], BF16)
        Ss = [Buf(S_t[:, d], "S%d" % d) for d in range(2)]; Sbs = [Buf(Sb_t[:, d], "Sb%d" % d) for d in range(2)]
        dma(lambda e: e.dma_start(out=TRI[:], in_=g.tri_in.rearrange("p (a t) -> p a t", a=4)), [], [TRI])
        if l == 0:
            kb.op("dve", lambda e: e.memset(LB[:, :, 0, :], 0.0), [], [LB])
            kb.op("dve", lambda e: e.memset(LB[:, :, 1, :], 1.0), [], [LB])
        else:
            for d in range(2):
                dma(lambda e, d=d: e.dma_start(out=LB[:, d, 0, :], in_=g.W["hgrn_lb"][d, 1:2, :].partition_broadcast(64)), [], [LB])
                dma(lambda e, d=d: e.dma_start(out=LB[:, d, 1, :], in_=g.W["hgrn_lb"][d, 0:1, :].partition_broadcast(64)), [], [LB])
                kb.op("dve", lambda e, d=d: e.tensor_tensor(out=LB[:, d, 0, :], in0=LB[:, d, 0, :], in1=LB[:, d, 1, :], op=ALU.subtract), [LB], [LB])
                kb.op("act", lambda e, d=d: e.activation(out=LB[:, d, 0, :], in_=LB[:, d, 0, :], func=AF.Sigmoid), [LB], [LB])
                kb.op("dve", lambda e, d=d: e.tensor_scalar(out=LB[:, d, 1, :], in0=LB[:, d, 0, :], scalar1=-1.0, scalar2=1.0, op0=ALU.mult, op1=ALU.add), [LB], [LB])
        NB = 4
        pc_t = al([64, NB, 2560], BF16); f_t = al([64, NB, 512], F32); lf_t = al([64, NB, 512], F32); kf_t = al([64, NB, 512], BF16)
        bm_t = al([128, NB, 256], F32); e12_t = al([128, NB, 2, 256], F32); em_t = al([128, NB, 8], F32); qf_t = al([128, NB, 256], F32)
        qk_t = al([128, NB, 3, 256], BF16); at_t = al([64, NB, 256], BF16); ed_t = al([64, NB, 512], F32); kh_t = al([64, NB, 512], BF16)
        of_t = al([64, NB, 512], F32)
        mk = lambda t, nm: [Buf(t[:, i], nm + str(i)) for i in range(NB)]
        PCs, Fs, LFs, KFs, BMs, E12s, EMs, QFs, QKs, ATs, EDs, KHs, OFs = [mk(t, n) for t, n in [
            (pc_t, "pc"), (f_t, "f"), (lf_t, "lf"), (kf_t, "kf"), (bm_t, "bm"), (e12_t, "e12"), (em_t, "em"), (qf_t, "qf"),
            (qk_t, "qk"), (at_t, "at"), (ed_t, "ed"), (kh_t, "kh"), (of_t, "of")]]
        psall = g.psall
        BPs = [Buf(psall[:, d, 0:256], "Bp%d" % d) for d in range(2)]
        TPs = [Buf(psall[:, 4 + d, 0:256], "Tp%d" % d) for d in range(2)]
        for d in range(2):
            kb.op("dve", lambda e, d=d: e.memset(Ss[d][:], 0.0), [], [Ss[d]])
            kb.op("dve", lambda e, d=d: e.memset(Sbs[d][:], 0.0), [], [Sbs[d]])
        for a_ in ATs:
            kb.op("dve", lambda e, a_=a_: e.memset(a_[:], 0.0), [], [a_])

        def stage_b(d, S, Sb, pc, qk, at, kh, em, of, need_out, r0):
            if need_out:
                Op = g.PS[6]
                for h in range(4):
                    kb.op("pe", lambda e, h=h: e.matmul(Op[0:64, h * 128:(h + 1) * 128], lhsT=qk[:, 1, h * 64:(h + 1) * 64], rhs=Sb[:, h, :],
                                                        start=(h == 0), stop=False, skip_group_check=True), [qk, Sb], [Op])
                    yield
                    kb.op("pe", lambda e, h=h: e.matmul(Op[0:64, h * 128:(h + 1) * 128], lhsT=at[:, h * 64:(h + 1) * 64], rhs=pc[:, 1536 + h * 128:1536 + (h + 1) * 128],
                                                        start=False, stop=True, skip_group_check=True), [at, pc], [Op])
                    yield
            Up = g.PS[7]
            for h in range(4):
                kb.op("pe", lambda e, h=h: e.matmul(Up[:, h * 128:(h + 1) * 128], lhsT=kh[:, h * 128:(h + 1) * 128], rhs=pc[:, 1536 + h * 128:1536 + (h + 1) * 128],
                                                    start=True, stop=True, skip_group_check=True), [kh, pc], [Up])
                yield
            for h in range(4):
                kb.op("dve", lambda e, h=h: e.scalar_tensor_tensor(out=S[:, h, :], in0=S[:, h, :], scalar=em[:, h:h + 1], in1=Up[:, h * 128:(h + 1) * 128],
                                                                   op0=ALU.mult, op1=ALU.add), [S, em, Up], [S])
                yield
            kb.op("pool", lambda e: e.tensor_copy(out=Sb[:], in_=S[:]), [S], [Sb])
            yield
            if need_out:
                kb.op("act", lambda e: e.activation(out=of[:], in_=Op[0:64, :], func=AF.Copy), [Op], [of])
                yield
                dma(lambda e: e.dma_start(out=ODST[d][r0:r0 + 64, :], in_=of[:]), [of], [ODST[d]])
                yield


        def step(d, ci, part):
            ch = orders[d][ci]
            ti_incl, ti_strict = (0, 2) if d == 0 else (1, 3)
            t_end, t_mid = (63, 31) if d == 0 else (0, 32)
            zc = 512 if d == 0 else 1024
            S, Sb = Ss[d], Sbs[d]
            i = d * 2 + (ci % 2)
            pc, f, lf, kf, bm, e12, em, qf, qk, at, ed, kh, of = [x[i] for x in (PCs, Fs, LFs, KFs, BMs, E12s, EMs, QFs, QKs, ATs, EDs, KHs, OFs)]
            need_out = (ch < 32) or ctx_out
            r0 = ch * 64
            if part == 1:
                yield from stage_b(d, S, Sb, pc, qk, at, kh, em, of, need_out, r0)
                return
            dma(lambda e: e.dma_start(out=pc[:], in_=g.P[r0:r0 + 64, 1984:4544]), [g.P], [pc])
            yield
            yield
            kb.op("act", lambda e: e.activation(out=f[:], in_=pc[:, zc:zc + 512], func=AF.Sigmoid), [pc], [f])
            yield
            kb.op("dve", lambda e: e.tensor_tensor(out=f[:], in0=f[:], in1=LB[:, d, 1, :], op=ALU.mult), [f, LB], [f])
            yield
            kb.op("dve", lambda e: e.tensor_tensor(out=f[:], in0=f[:], in1=LB[:, d, 0, :], op=ALU.add), [f, LB], [f])
            yield
            kb.op("act", lambda e: e.activation(out=lf[:], in_=f[:], func=AF.Ln), [f], [lf])
            yield
            kb.op("pool", lambda e: e.tensor_scalar(out=kf[:], in0=f[:], scalar1=-1.0, scalar2=1.0, op0=ALU.mult, op1=ALU.add), [f], [kf])
            yield
            yield
            Bp = BPs[d]
            for h in range(4):
                kb.op("pe", lambda e, h=h: e.matmul(Bp[:, h * 64:(h + 1) * 64], lhsT=lf[:, h * 128:(h + 1) * 128], rhs=TRI[:, ti_incl, :],
                                                    start=True, stop=True, skip_group_check=True), [lf, TRI], [Bp])
            Dp = g.PS[2 + d]
            kb.op("pe", lambda e: e.matmul(Dp[0:64, :], lhsT=TRI[:, ti_strict, :], rhs=lf[:], start=True, stop=True), [lf, TRI], [Dp])
            yield
            yield
            Tp = TPs[d]
            tpb = Tp[:].bitcast(BF16)
            for h in range(4):
                kb.op("pe", lambda e, h=h: e.transpose(out=tpb[:, h * 64:(h + 1) * 64], in_=pc[:, h * 128:(h + 1) * 128], identity=g.ident_b[0:64, 0:64]), [pc, g.ident_b], [Tp])
            for h in range(4):
                kb.op("pe", lambda e, h=h: e.transpose(out=tpb[:, 256 + h * 64:256 + (h + 1) * 64], in_=kf[:, h * 128:(h + 1) * 128], identity=g.ident_b[0:64, 0:64]), [kf, g.ident_b], [Tp])
            b3 = Bp[:].rearrange("p (h t) -> p h t", h=4)
            kb.op("act", lambda e: e.activation(out=em[:, 0:4], in_=b3[:, :, t_mid], func=AF.Copy), [Bp], [em])
            yield
            kb.op("dve", lambda e: e.tensor_tensor(out=bm[:].rearrange("p (h t) -> p h t", h=4), in0=b3, in1=em[:, 0:4].unsqueeze(2).to_broadcast([128, 4, 64]), op=ALU.subtract), [Bp, em], [bm])
            yield
            kb.op("act", lambda e: e.activation(out=e12[:, 0, :], in_=bm[:], func=AF.Exp), [bm], [e12])
            yield
            kb.op("act", lambda e: e.activation(out=e12[:, 1, :], in_=bm[:], func=AF.Exp, scale=-1.0), [bm], [e12])
            yield
            kb.op("act", lambda e: e.activation(out=em[:, 4:8], in_=em[:, 0:4], func=AF.Exp), [em], [em])
            yield
            kb.op("act", lambda e: e.activation(out=em[:, 0:4], in_=b3[:, :, t_end], func=AF.Exp), [Bp], [em])
            yield
            yield
            kb.op("dve", lambda e: e.tensor_tensor(out=qf[:], in0=tpb[:, 0:256], in1=e12[:, 0, :], op=ALU.mult), [Tp, e12], [qf])
            yield
            kb.op("pool", lambda e: e.tensor_copy(out=qk[:, 0, :], in_=qf[:]), [qf], [qk])
            yield
            kb.op("dve", lambda e: e.tensor_tensor(out=qk[:, 1, :].rearrange("p (h t) -> p h t", h=4), in0=qf[:].rearrange("p (h t) -> p h t", h=4),
                                                   in1=em[:, 4:8].unsqueeze(2).to_broadcast([128, 4, 64]), op=ALU.mult), [qf, em], [qk])
            yield
            kb.op("dve", lambda e: e.tensor_tensor(out=qk[:, 2, :], in0=tpb[:, 256:512], in1=e12[:, 1, :], op=ALU.mult), [Tp, e12], [qk])
            yield
            yield
            yield
            Ap = Tp
            if d == 0:
                parts = [((0, 32), (0, 64)), ((32, 64), (32, 64))]
            else:
                parts = [((32, 64), (0, 64)), ((0, 32), (0, 32))]
            for h in range(4):
                for (s0, s1), (c0, c1) in parts:
                    kb.op("pe", lambda e, h=h, s0=s0, s1=s1, c0=c0, c1=c1: e.matmul(
                        Ap[s0:s1, h * 64 + c0:h * 64 + c1], lhsT=qk[:, 2, h * 64 + s0:h * 64 + s1], rhs=qk[:, 0, h * 64 + c0:h * 64 + c1],
                        start=True, stop=True, skip_group_check=True), [qk], [Ap])
            for (s0, s1), (c0, c1) in parts:
                kb.op("dve", lambda e, s0=s0, s1=s1, c0=c0, c1=c1: e.tensor_tensor(
                    out=at[s0:s1, :].rearrange("p (h t) -> p h t", h=4)[:, :, c0:c1],
                    in0=Ap[s0:s1, 0:256].rearrange("p (h t) -> p h t", h=4)[:, :, c0:c1],
                    in1=TRI[s0:s1, ti_incl, c0:c1].unsqueeze(1).to_broadcast([s1 - s0, 4, c1 - c0]), op=ALU.mult), [Ap, TRI], [at])
            yield
            kb.op("act", lambda e: e.activation(out=ed[:], in_=Dp[0:64, :], func=AF.Exp), [Dp], [ed])
            yield
            kb.op("pool", lambda e: e.tensor_tensor(out=kh[:], in0=kf[:], in1=ed[:], op=ALU.mult), [kf, ed], [kh])
            yield

        def lockstep(gens):
            gens = list(gens)
            while gens:
                for g_ in list(gens):
                    try:
                        next(g_)
                    except StopIteration:
                        gens.remove(g_)

        for ci in range(37):
            if ci < 36:
                if "c_nolock" in g.debug:
                    lockstep([step(0, ci, 0)])
                    lockstep([step(1, ci, 0)])
                else:
                    lockstep([step(0, ci, 0), step(1, ci, 0)])
            if ci >= 1:
                lockstep([step(0, ci - 1, 1)])
                lockstep([step(1, ci - 1, 1)])
        kb.barrier()
    with ExitStack() as es:
        al = lambda shape, dt: es.enter_context(nc.sbuf_tensor(_nm(), shape, dt))
        NGh = Buf(al([128, 128], F32), "NGh")
        dma(lambda e: e.dma_start(out=NGh[:], in_=g.W["hgrn_norm_g"][l:l + 1, :].partition_broadcast(128)), [], [NGh])
        a_t = al([128, 2, 512], F32); b_t = al([128, 2, 512], F32); gt_t = al([128, 2, 512], BF16); ro_t = al([128, 2, 3, 512], F32)
        rr_t = al([128, 2, 8], F32); oo_t = al([128, 2, 512], BF16)
        mk2 = lambda t, nm: [Buf(t[:, i], nm + str(i)) for i in range(2)]
        As, Bs, GTs, ROs, RRs, OOs = mk2(a_t, "a"), mk2(b_t, "b"), mk2(gt_t, "gt"), mk2(ro_t, "ro"), mk2(rr_t, "rr"), mk2(oo_t, "oo")
        for tt in range(NT if ctx_out else 16):
            i = tt % 2
            a, b, gt, ro, rr, oo = As[i], Bs[i], GTs[i], ROs[i], RRs[i], OOs[i]
            r0 = tt * 128
            dma(lambda e, a=a, r0=r0: e.dma_start(out=a[:], in_=g.OF[r0:r0 + 128, :]), [g.OF], [a])
            dma(lambda e, b=b, r0=r0: e.dma_start(out=b[:], in_=g.OBK[r0:r0 + 128, :]), [g.OBK], [b])
            dma(lambda e, gt=gt, r0=r0: e.dma_start(out=gt[:], in_=g.P[r0:r0 + 128, 4032:4544]), [g.P], [gt])
            o3 = lambda bb, j: bb[:, j, :].rearrange("p (h v) -> p h v", h=4)
            kb.op("dve", lambda e, a=a, b=b, ro=ro: e.tensor_tensor(out=ro[:, 0, :], in0=a[:], in1=b[:], op=ALU.add), [a, b], [ro])
            kb.op("pool", lambda e, ro=ro: e.tensor_tensor(out=ro[:, 1, :], in0=ro[:, 0, :], in1=ro[:, 0, :], op=ALU.mult), [ro], [ro])
            kb.op("dve", lambda e, ro=ro, rr=rr: e.tensor_reduce(out=rr[:, 0:4], in_=o3(ro, 1), axis=AX.X, op=ALU.add), [ro], [rr])
            kb.op("dve", lambda e, rr=rr: e.tensor_scalar(out=rr[:, 0:4], in0=rr[:, 0:4], scalar1=1.0 / 128, scalar2=EPS, op0=ALU.mult, op1=ALU.add), [rr], [rr])
            kb.op("act", lambda e, rr=rr: e.activation(out=rr[:, 4:8], in_=rr[:, 0:4], func=AF.Sqrt), [rr], [rr])
            kb.op("dve", lambda e, rr=rr: e.reciprocal(out=rr[:, 0:4], in_=rr[:, 4:8]), [rr], [rr])
            kb.op("dve", lambda e, ro=ro, rr=rr: e.tensor_tensor(out=o3(ro, 1), in0=o3(ro, 0), in1=rr[:, 0:4].unsqueeze(2).to_broadcast([128, 4, 128]), op=ALU.mult), [ro, rr], [ro])
            kb.op("act", lambda e, ro=ro, gt=gt: e.activation(out=ro[:, 2, :], in_=gt[:], func=AF.Silu), [gt], [ro])
            kb.op("pool", lambda e, ro=ro: e.tensor_tensor(out=o3(ro, 0), in0=o3(ro, 1), in1=NGh[:].unsqueeze(1).to_broadcast([128, 4, 128]), op=ALU.mult), [ro, NGh], [ro])
            kb.op("dve", lambda e, ro=ro, oo=oo: e.tensor_tensor(out=oo[:], in0=ro[:, 0, :], in1=ro[:, 2, :], op=ALU.mult), [ro], [oo])
            dma(lambda e, oo=oo, r0=r0: e.dma_start(out=g.OB[r0:r0 + 128, 1024:1536], in_=oo[:]), [oo], [g.OB])
        kb.barrier()


def phase_merge(g, l, ctx_out):
    nc, kb = g.nc, g.kb
    from contextlib import ExitStack
    dma = lambda fn, r=(), w=(): kb.dma("sp", fn, r, w)
    Pv = g.P.t.rearrange("(t p) c -> p t c", p=128)
    OBv = g.OB.t.rearrange("(t p) c -> p t c", p=128)
    with ExitStack() as es:
        al = lambda shape, dt: es.enter_context(nc.sbuf_tensor(_nm(), shape, dt))
        WBR = Buf(al([128, 16, 1024], BF16), "WBR"); WO = Buf(al([128, 8, 1024], BF16), "WO")
        G1 = [Buf(al([128, 1024], F32), "G1x"), Buf(al([128, 1024], F32), "G1c")]
        ob_t = al([128, 2, 2048], BF16); gt_t = al([128, 2, 4096], BF16); obt_t = al([128, 2, 16, 128], BF16)
        sg_t = al([128, 3, 512], F32); tp_t = al([128, 3, 512], F32); y_t = al([128, 2, 1024], F32)
        yb_t = al([128, 2, 1024], BF16); yt_t = al([128, 2, 8, 128], BF16)
        mk = lambda t, nm, n: [Buf(t[:, i], nm + str(i)) for i in range(n)]
        OBs, GTs, OBTs, SGs, TPs, Ys, YBs, YTs = mk(ob_t, "ob", 2), mk(gt_t, "gt", 2), mk(obt_t, "obt", 2), mk(sg_t, "sg", 3), mk(tp_t, "tp", 3), mk(y_t, "y", 2), mk(yb_t, "yb", 2), mk(yt_t, "yt", 2)
        wsrc = g.W["w_branch"][l].rearrange("j (c p) n -> p j c n", p=128)
        for j in range(4):
            kb.dma("pool", lambda e, j=j: e.dma_start(out=WBR[:, j * 4:(j + 1) * 4, :], in_=wsrc[:, j]), [], [WBR])
        wo = g.W["w_out"][l].rearrange("(k p) n -> p k n", p=128)
        for j in range(2):
            kb.dma("pool", lambda e, j=j: e.dma_start(out=WO[:, j * 4:(j + 1) * 4, :], in_=wo[:, j * 4:(j + 1) * 4, :]), [], [WO])
        load_bcast(g, G1[0], g.MODS[l, 0:1, 2 * D:3 * D])
        load_bcast(g, G1[1], g.MODS[l, 1:2, 2 * D:3 * D])
        pi = 0
        si = 0
        for tt in range(NT if ctx_out else 16):
            i = tt % 2
            ob, gt, obt, y, yb, yt = OBs[i], GTs[i], OBTs[i], Ys[i], YBs[i], YTs[i]
            G = G1[0] if tt < 16 else G1[1]
            dma(lambda e, ob=ob, tt=tt: e.dma_start(out=ob[:], in_=OBv[:, tt, :]), [g.OB], [ob])
            dma(lambda e, gt=gt, tt=tt: e.dma_start(out=gt[:], in_=Pv[:, tt, 5312:9408]), [g.P], [gt])
            for hf in range(2):
                ps = g.PS[6 + hf]
                psb = ps[:].bitcast(BF16)
                for c in range(8):
                    kb.op("pe", lambda e, c=c, hf=hf, psb=psb, ob=ob: e.transpose(out=psb[:, c * 128:(c + 1) * 128], in_=ob[:, (hf * 8 + c) * 128:(hf * 8 + c + 1) * 128], identity=g.ident_b[:]), [ob, g.ident_b], [ps])
                kb.op("act", lambda e, hf=hf, psb=psb, obt=obt: e.activation(out=obt[:, hf * 8:(hf + 1) * 8, :], in_=psb.rearrange("p (k t) -> p k t", k=8), func=AF.Copy), [ps], [obt])
            for hf in range(2):
                for j in range(4):
                    ps = g.PS[pi % 4]; pi += 1
                    for c in range(4):
                        kb.op("pe", lambda e, c=c, j=j, hf=hf, ps=ps, obt=obt: e.matmul(ps[:], lhsT=obt[:, j * 4 + c, :], rhs=WBR[:, j * 4 + c, hf * 512:(hf + 1) * 512], start=(c == 0), stop=(c == 3)), [obt, WBR], [ps])
                    sg = SGs[si % 3]; tp = TPs[si % 3]; si += 1
                    kb.op("act", lambda e, sg=sg, gt=gt, j=j, hf=hf: e.activation(out=sg[:], in_=gt[:, j * 1024 + hf * 512:j * 1024 + (hf + 1) * 512], func=AF.Sigmoid), [gt], [sg])
                    ysl = (slice(None), slice(hf * 512, (hf + 1) * 512))
                    if j == 0:
                        kb.op("dve", lambda e, ps=ps, sg=sg, y=y, ysl=ysl: e.tensor_tensor(out=y[ysl], in0=ps[:], in1=sg[:], op=ALU.mult), [ps, sg], [y])
                    else:
                        kb.op("dve", lambda e, ps=ps, sg=sg, tp=tp: e.tensor_tensor(out=tp[:], in0=ps[:], in1=sg[:], op=ALU.mult), [ps, sg], [tp])
                        if j < 3:
                            kb.op("pool", lambda e, tp=tp, y=y, ysl=ysl: e.tensor_tensor(out=y[ysl], in0=y[ysl], in1=tp[:], op=ALU.add), [y, tp], [y])
                        else:
                            kb.op("pool", lambda e, tp=tp, y=y, yb=yb, ysl=ysl: e.tensor_tensor(out=yb[ysl], in0=y[ysl], in1=tp[:], op=ALU.add), [y, tp], [yb])
            ps = g.PS[6]
            psb = ps[:].bitcast(BF16)
            for c in range(8):
                kb.op("pe", lambda e, c=c, psb=psb, yb=yb: e.transpose(out=psb[:, c * 128:(c + 1) * 128], in_=yb[:, c * 128:(c + 1) * 128], identity=g.ident_b[:]), [yb, g.ident_b], [ps])
            kb.op("act", lambda e, psb=psb, yt=yt: e.activation(out=yt[:], in_=psb.rearrange("p (k t) -> p k t", k=8), func=AF.Copy), [ps], [yt])
            for hf in range(2):
                ps = g.PS[pi % 4]; pi += 1
                for k in range(8):
                    kb.op("pe", lambda e, k=k, hf=hf, ps=ps, yt=yt: e.matmul(ps[:], lhsT=yt[:, k, :], rhs=WO[:, k, hf * 512:(hf + 1) * 512], start=(k == 0), stop=(k == 7)), [yt, WO], [ps])
                tp = TPs[si % 3]; si += 1
                xs = g.X[tt]
                kb.op("dve", lambda e, ps=ps, tp=tp, G=G, hf=hf: e.tensor_tensor(out=tp[:], in0=ps[:], in1=G[:, hf * 512:(hf + 1) * 512], op=ALU.mult), [ps, G], [tp])
                kb.op("pool", lambda e, tp=tp, xs=xs, hf=hf: e.tensor_tensor(out=xs[:, hf * 512:(hf + 1) * 512], in0=xs[:, hf * 512:(hf + 1) * 512], in1=tp[:], op=ALU.add), [xs, tp], [xs])
        kb.barrier()


def phase_final(g):
    nc, kb = g.nc, g.kb
    from contextlib import ExitStack
    dma = lambda fn, r=(), w=(): kb.dma("sp", fn, r, w)
    with ExitStack() as es:
        al = lambda shape, dt: es.enter_context(nc.sbuf_tensor(_nm(), shape, dt))
        FG = Buf(al([128, 1024], F32), "FG")
        junk = Buf(al([128, 1024], F32), "junk")
        o_t = al([128, 2, 1024], F32); st_t = al([128, 2, 8], F32)
        Os = [Buf(o_t[:, i], "o%d" % i) for i in range(2)]; Ss = [Buf(st_t[:, i], "s%d" % i) for i in range(2)]
        dma(lambda e: e.dma_start(out=FG[:], in_=g.W["final_g"][0:1, :].partition_broadcast(128)), [], [FG])
        for tt in range(16):
            xin = g.X[tt]; st = Ss[tt % 2]; o = Os[tt % 2]
            kb.op("act", lambda e, xin=xin, st=st: e.activation(out=junk[:], in_=xin[:], func=AF.Square, accum_out=st[:, 0:1]), [xin], [junk, st])
            kb.op("dve", lambda e, st=st: e.tensor_scalar(out=st[:, 1:2], in0=st[:, 0:1], scalar1=1.0 / D, scalar2=EPS, op0=ALU.mult, op1=ALU.add), [st], [st])
            kb.op("act", lambda e, st=st: e.activation(out=st[:, 2:3], in_=st[:, 1:2], func=AF.Sqrt), [st], [st])
            kb.op("dve", lambda e, st=st: e.reciprocal(out=st[:, 3:4], in_=st[:, 2:3]), [st], [st])
            kb.op("dve", lambda e, xin=xin, st=st, o=o: e.scalar_tensor_tensor(out=o[:], in0=xin[:], scalar=st[:, 3:4], in1=FG[:], op0=ALU.mult, op1=ALU.mult), [xin, st, FG], [o])
            dma(lambda e, o=o, tt=tt: e.dma_start(out=g.out[tt * 128:(tt + 1) * 128, :], in_=o[:]), [o], [])
        kb.barrier()


def phase_peer(g, l, ctx_out):
    nc, kb = g.nc, g.kb
    from contextlib import ExitStack
    dma = lambda fn, r=(), w=(): kb.dma("sp", fn, r, w)
    NEG = -1.0e30
    uv_flat = g.UV.t
    H2P = g.PSP[2]; ACCP = g.PSP[3]
    with ExitStack() as es:
        al = lambda shape, dt: es.enter_context(nc.sbuf_tensor(_nm(), shape, dt))
        WQ = Buf(al([128, 8, 1024], BF16), "WQ"); KT = Buf(al([128, 8, 128], BF16), "KT")
        GS = Buf(al([128, 1024], F32), "GS"); SH = Buf(al([128, 1024], F32), "SH"); G2 = Buf(al([128, 1024], F32), "G2")
        IO16 = Buf(al([128, 16], F32), "IO16")
        kb.op("pool", lambda e: e.iota(IO16[:], pattern=[[1, 16]], base=0, channel_multiplier=0, allow_small_or_imprecise_dtypes=True), [], [IO16])
        wq = g.W["peer_wq"][l].rearrange("(k p) n -> p k n", p=128)
        for j in range(2):
            kb.dma("pool", lambda e, j=j: e.dma_start(out=WQ[:, j * 4:(j + 1) * 4, :], in_=wq[:, j * 4:(j + 1) * 4, :]), [], [WQ])
        junk = Buf(al([128, 1024], F32), "junk"); junkb = Buf(al([128, 1024], BF16), "junkb")
        keyf = junk[:].rearrange("p (a d) -> p a d", a=16); keyb = junkb[:].rearrange("p (a d) -> p a d", a=16)
        dma(lambda e: e.dma_start(out=keyf, in_=g.W["peer_keys"][l].rearrange("h p n d -> n (h p) d")), [], [junk])
        kb.op("dve", lambda e: e.tensor_copy(out=keyb, in_=keyf), [junk], [junkb])
        ps = g.PS[3]
        psb = ps[:].bitcast(BF16)
        for h in range(8):
            kb.op("pe", lambda e, h=h: e.transpose(out=psb[:, h * 128:(h + 1) * 128], in_=keyb[:, h * 2:(h + 1) * 2, :].rearrange("n a d -> n (a d)"), identity=g.ident_b[:]), [junkb, g.ident_b], [ps])
        kb.op("act", lambda e: e.activation(out=KT[:], in_=psb.rearrange("p (h n) -> p h n", h=8), func=AF.Copy), [ps], [KT])
        hb_t = al([128, 2, 1024], BF16); hT_t = al([128, 2, 8, 128], BF16); qT_t = al([128, 2, 8, 128], BF16)
        st_t = al([128, 2, 8], F32)
        SC = Buf(al([128, 2, 8, 128], F32), "SC")
        TMP = Buf(al([128, 256], F32), "TMP")
        SV = Buf(al([128, 2, 8, 16], F32), "SV"); SI = Buf(al([128, 2, 8, 16], U32), "SI"); SIF = Buf(al([128, 2, 8, 16], F32), "SIF")
        CS_ = Buf(al([128, 8, 256], F32), "CS_")
        TS = Buf(al([128, 8, 16], F32), "TS"); POS = Buf(al([128, 8, 16], U32), "POS")
        PA = Buf(al([128, 2, 128], U32), "PA"); PAF = Buf(al([128, 2, 128], F32), "PAF")
        OH = Buf(al([128, 8, 16, 16], F32), "OH"); IAB = Buf(al([128, 2, 128], F32), "IAB")
        IDX = Buf(al([128, 128], I32), "IDX"); GW = Buf(al([128, 8, 16], F32), "GW"); RZ = Buf(al([128, 16], F32), "RZ")
        DOT = Buf(al([128, 128], F32), "DOT"); WGT = Buf(al([128, 128], F32), "WGT")
        NGRP = 3
        GSZ = 4
        gb_t = al([128, NGRP, GSZ, 2048], BF16)
        GB = [[Buf(gb_t[:, i, j], "gb%d_%d" % (i, j)) for j in range(GSZ)] for i in range(NGRP)]
        WG = Buf(al([128, 128], F32), "WG")
        dg_t = al([128, 4, 128], BF16)
        DGs = [Buf(dg_t[:, i], "dg%d" % i) for i in range(4)]
        ACCB = [g.PS[6], g.PS[7]]
        HBs = [Buf(hb_t[:, i], "hb%d" % i) for i in range(2)]
        HTs = [Buf(hT_t[:, i], "hT%d" % i) for i in range(2)]; QTs = [Buf(qT_t[:, i], "qT%d" % i) for i in range(2)]
        STs = [Buf(st_t[:, i], "st%d" % i) for i in range(2)]
        gi = 0
        tiles = list(range(NT if ctx_out else 16))
        for tt in tiles:
            if tt == 0 or tt == 16:
                r = 0 if tt < 16 else 1
                dma(lambda e: e.dma_start(out=GS[:], in_=g.W["norm2_g"][l:l + 1, :].partition_broadcast(128)), [], [GS])
                load_bcast(g, junk, g.MODS[l, r:r + 1, 4 * D:5 * D])
                kb.op("dve", lambda e: e.scalar_tensor_tensor(out=GS[:], in0=junk[:], scalar=1.0, in1=GS[:], op0=ALU.add, op1=ALU.mult), [junk, GS], [GS])
                load_bcast(g, SH, g.MODS[l, r:r + 1, 3 * D:4 * D])
                load_bcast(g, G2, g.MODS[l, r:r + 1, 5 * D:6 * D])
            i = tt % 2
            hb, hT, qT, st = HBs[i], HTs[i], QTs[i], STs[i]
            h2 = H2P
            xin = g.X[tt]
            kb.op("act", lambda e: e.activation(out=junk[:], in_=xin[:], func=AF.Square, accum_out=st[:, 0:1]), [xin], [junk, st])
            kb.op("dve", lambda e: e.tensor_scalar(out=st[:, 1:2], in0=st[:, 0:1], scalar1=1.0 / D, scalar2=EPS, op0=ALU.mult, op1=ALU.add), [st], [st])
            kb.op("act", lambda e: e.activation(out=st[:, 2:3], in_=st[:, 1:2], func=AF.Sqrt), [st], [st])
            kb.op("dve", lambda e: e.reciprocal(out=st[:, 3:4], in_=st[:, 2:3]), [st], [st])
            kb.op("dve", lambda e: e.scalar_tensor_tensor(out=junk[:], in0=xin[:], scalar=st[:, 3:4], in1=GS[:], op0=ALU.mult, op1=ALU.mult), [xin, st, GS], [junk])
            kb.op("dve", lambda e: e.tensor_tensor(out=h2[:], in0=junk[:], in1=SH[:], op=ALU.add), [junk, SH], [h2])
            kb.op("act", lambda e: e.activation(out=hb[:], in_=h2[:], func=AF.Copy), [h2], [hb])
            ps = g.PS[3]
            psb = ps[:].bitcast(BF16)
            for k in range(8):
                kb.op("pe", lambda e, k=k: e.transpose(out=psb[:, k * 128:(k + 1) * 128], in_=hb[:, k * 128:(k + 1) * 128], identity=g.ident_b[:]), [hb, g.ident_b], [ps])
            kb.op("act", lambda e: e.activation(out=hT[:], in_=psb.rearrange("p (k t) -> p k t", k=8), func=AF.Copy), [ps], [hT])
            for hh in range(2):
                ps = g.PS[hh]
                for h4 in range(4):
                    h = hh * 4 + h4
                    for k in range(8):
                        kb.op("pe", lambda e, k=k, h=h, h4=h4, ps=ps: e.matmul(ps[:, h4 * 128:(h4 + 1) * 128], lhsT=WQ[:, k, h * 128:(h + 1) * 128], rhs=hT[:, k, :],
                                                                            start=(k == 0 and h4 == 0), stop=(k == 7), skip_group_check=True), [WQ, hT], [ps])
                kb.op("act", lambda e, hh=hh, ps=ps: e.activation(out=qT[:, hh * 4:(hh + 1) * 4, :], in_=ps[:].rearrange("p (h t) -> p h t", h=4), func=AF.Copy), [ps], [qT])
            for p in range(2):
                for hh in range(2):
                    ps = g.PS[p * 2 + hh]
                    for h4 in range(4):
                        h = hh * 4 + h4
                        kb.op("pe", lambda e, p=p, h=h, h4=h4, ps=ps: e.matmul(ps[:, h4 * 128:(h4 + 1) * 128], lhsT=qT[p * 64:(p + 1) * 64, h, :], rhs=KT[p * 64:(p + 1) * 64, h, :],
                                                                            start=True, stop=True, skip_group_check=True), [qT, KT], [ps])
                    kb.op("act", lambda e, p=p, hh=hh, ps=ps: e.activation(out=SC[:, p, hh * 4:(hh + 1) * 4, :], in_=ps[:].rearrange("p (h t) -> p h t", h=4), func=AF.Copy), [ps], [SC])
            for p in range(2):
                for h in range(8):
                    s_ = SC[:, p, h, :]
                    kb.op("dve", lambda e, p=p, h=h, s_=s_: e.max(out=SV[:, p, h, 0:8], in_=s_), [SC], [SV])
                    kb.op("dve", lambda e, p=p, h=h, s_=s_: e.max_index(out=SI[:, p, h, 0:8], in_max=SV[:, p, h, 0:8], in_values=s_), [SC, SV], [SI])
                    kb.op("dve", lambda e, p=p, h=h, s_=s_: e.match_replace(out=TMP[:, 0:128], in_to_replace=SV[:, p, h, 0:8], in_values=s_, imm_value=NEG), [SC, SV], [TMP])
                    kb.op("dve", lambda e, p=p, h=h: e.max(out=SV[:, p, h, 8:16], in_=TMP[:, 0:128]), [TMP], [SV])
                    kb.op("dve", lambda e, p=p, h=h: e.max_index(out=SI[:, p, h, 8:16], in_max=SV[:, p, h, 8:16], in_values=TMP[:, 0:128]), [TMP, SV], [SI])
            kb.op("dve", lambda e: e.tensor_copy(out=SIF[:], in_=SI[:]), [SI], [SIF])
            c4 = CS_[:].rearrange("p h (a b) -> p h a b", a=16)
            kb.op("dve", lambda e: e.tensor_tensor(out=c4, in0=SV[:, 0].unsqueeze(3).to_broadcast([128, 8, 16, 16]), in1=SV[:, 1].unsqueeze(2).to_broadcast([128, 8, 16, 16]), op=ALU.add), [SV], [CS_])
            for h in range(8):
                kb.op("dve", lambda e, h=h: e.max(out=TS[:, h, 0:8], in_=CS_[:, h, :]), [CS_], [TS])
                kb.op("dve", lambda e, h=h: e.max_index(out=POS[:, h, 0:8], in_max=TS[:, h, 0:8], in_values=CS_[:, h, :]), [CS_, TS], [POS])
                kb.op("dve", lambda e, h=h: e.match_replace(out=TMP[:], in_to_replace=TS[:, h, 0:8], in_values=CS_[:, h, :], imm_value=NEG), [CS_, TS], [TMP])
                kb.op("dve", lambda e, h=h: e.max(out=TS[:, h, 8:16], in_=TMP[:]), [TMP], [TS])
                kb.op("dve", lambda e, h=h: e.max_index(out=POS[:, h, 8:16], in_max=TS[:, h, 8:16], in_values=TMP[:]), [TMP, TS], [POS])
            posf = POS[:].rearrange("p h j -> p (h j)")
            kb.op("dve", lambda e: e.tensor_single_scalar(out=PA[:, 0, :], in_=posf, scalar=4, op=ALU.logical_shift_right), [POS], [PA])
            kb.op("dve", lambda e: e.tensor_single_scalar(out=PA[:, 1, :], in_=posf, scalar=15, op=ALU.bitwise_and), [POS], [PA])
            kb.op("dve", lambda e: e.tensor_copy(out=PAF[:], in_=PA[:]), [PA], [PAF])
            for w in range(2):
                pa3 = PAF[:, w, :].rearrange("p (h j) -> p h j", h=8)
                kb.op("dve", lambda e, pa3=pa3: e.tensor_tensor(out=OH[:], in0=pa3.unsqueeze(3).to_broadcast([128, 8, 16, 16]),
                                                               in1=IO16[:].unsqueeze(1).unsqueeze(1).to_broadcast([128, 8, 16, 16]), op=ALU.is_equal), [PAF, IO16], [OH])
                kb.op("dve", lambda e, w=w: e.tensor_tensor(out=OH[:], in0=OH[:], in1=SIF[:, w].unsqueeze(2).to_broadcast([128, 8, 16, 16]), op=ALU.mult), [OH, SIF], [OH])
                kb.op("dve", lambda e, w=w: e.tensor_reduce(out=IAB[:, w, :].rearrange("p (h j) -> p h j", h=8), in_=OH[:], axis=AX.X, op=ALU.add), [OH], [IAB])
            kb.op("dve", lambda e: e.tensor_scalar(out=IAB[:, 0, :], in0=IAB[:, 0, :], scalar1=128.0, scalar2=float(l * 16384), op0=ALU.mult, op1=ALU.add), [IAB], [IAB])
            kb.op("dve", lambda e: e.tensor_tensor(out=IAB[:, 0, :], in0=IAB[:, 0, :], in1=IAB[:, 1, :], op=ALU.add), [IAB], [IAB])
            kb.op("dve", lambda e: e.tensor_copy(out=IDX[:], in_=IAB[:, 0, :]), [IAB], [IDX])
            kb.op("dve", lambda e: e.tensor_tensor(out=GW[:], in0=TS[:], in1=TS[:, :, 0:1].to_broadcast([128, 8, 16]), op=ALU.subtract), [TS], [GW])
            kb.op("act", lambda e: e.activation(out=GW[:], in_=GW[:], func=AF.Exp), [GW], [GW])
            kb.op("dve", lambda e: e.tensor_reduce(out=RZ[:, 0:8], in_=GW[:], axis=AX.X, op=ALU.add), [GW], [RZ])
            kb.op("dve", lambda e: e.reciprocal(out=RZ[:, 8:16], in_=RZ[:, 0:8]), [RZ], [RZ])
            kb.op("dve", lambda e: e.tensor_tensor(out=GW[:], in0=GW[:], in1=RZ[:, 8:16].unsqueeze(2).to_broadcast([128, 8, 16]), op=ALU.mult), [GW, RZ], [GW])
            ngr = 128 // GSZ

            def acc_group(k):
                gb = GB[k % NGRP]
                sl = slice(k * GSZ, (k + 1) * GSZ)
                kb.op("dve", lambda e: e.tensor_tensor(out=WGT[:, sl], in0=WG[:, sl], in1=GW[:].rearrange("p h j -> p (h j)")[:, sl], op=ALU.mult), [WG, GW], [WGT])
                for j in range(GSZ):
                    s = k * GSZ + j
                    dg = DGs[s % 4]
                    kb.op("act", lambda e, dg=dg, s=s: e.activation(out=dg[:], in_=g.ident_b[:], func=AF.Copy, scale=WGT[:, s:s + 1]), [g.ident_b, WGT], [dg])
                    for hf in range(2):
                        kb.op("pe", lambda e, dg=dg, j=j, hf=hf, s=s: e.matmul(ACCB[hf][:], lhsT=dg[:], rhs=gb[j][:, 1024 + hf * 512:1024 + (hf + 1) * 512],
                                                                            start=(s == 0), stop=(s == 127)), [dg, gb[j]], [ACCB[hf]])

            for k in range(ngr):
                gb = GB[k % NGRP]
                for j in range(GSZ):
                    s = k * GSZ + j
                    kb.dma("pool", lambda e, gb=gb, j=j, s=s: e.indirect_dma_start(out=gb[j][:], out_offset=None, in_=uv_flat,
                           in_offset=bass.IndirectOffsetOnAxis(ap=IDX[:, s:s + 1], axis=0)), [IDX], [gb[j]])
                for j in range(GSZ):
                    s = k * GSZ + j
                    kb.op("dve", lambda e, gb=gb, j=j, s=s: e.scalar_tensor_tensor(out=junkb[:], in0=gb[j][:, 0:1024], scalar=1.0, in1=h2[:], op0=ALU.mult, op1=ALU.mult,
                                                                                  accum_out=DOT[:, s:s + 1]), [gb[j], h2], [junkb, DOT])
                sl = slice(k * GSZ, (k + 1) * GSZ)
                kb.op("act", lambda e, sl=sl: e.activation(out=WG[:, sl], in_=DOT[:, sl], func=AF.Gelu), [DOT], [WG])
                if k >= 1:
                    acc_group(k - 1)
            acc_group(ngr - 1)
            for hf in range(2):
                kb.op("dve", lambda e, hf=hf: e.tensor_tensor(out=junk[:, hf * 512:(hf + 1) * 512], in0=ACCB[hf][:], in1=G2[:, hf * 512:(hf + 1) * 512], op=ALU.mult), [ACCB[hf], G2], [junk])
            kb.op("dve", lambda e: e.tensor_tensor(out=xin[:], in0=xin[:], in1=junk[:], op=ALU.add), [xin, junk], [xin])
        kb.barrier()


def kernel(**inputs):
    nc, g = build_program()
    consts = host_consts()
    shared = {n: np.ascontiguousarray(np.asarray(inputs[n], dtype=np.float32)).reshape(s) for n, s in W_NAMES}
    shared.update(consts)
    shared["c_ctx"] = np.ascontiguousarray(np.asarray(inputs["c_ctx"], dtype=np.float32)).reshape(1, D)
    x = np.asarray(inputs["x"], dtype=np.float32)
    c = np.asarray(inputs["c"], dtype=np.float32)
    ctx = np.asarray(inputs["ctx"], dtype=np.float32)
    nb = x.shape[0]
    in_maps = []
    for b in range(nb):
        m = dict(shared)
        m["x"] = np.ascontiguousarray(x[b])
        m["ctx"] = np.ascontiguousarray(ctx[b])
        m["c"] = np.ascontiguousarray(c[b:b + 1])
        in_maps.append(m)
    res = run_bass_kernel_spmd(nc, in_maps, core_ids=list(range(nb)))
    out = np.stack([np.asarray(r["out"], dtype=np.float32) for r in res.results], axis=0)
    return out


def phase_cast_experts(g, n_layers):
    nc, kb = g.nc, g.kb
    from contextlib import ExitStack
    with ExitStack() as es:
        al = lambda shape, dt: es.enter_context(nc.sbuf_tensor(_nm(), shape, dt))
        f_t = al([128, 2, 8, 1024], F32); b_t = al([128, 2, 8, 1024], BF16)
        Fs = [Buf(f_t[:, i], "cf%d" % i) for i in range(2)]; Bs = [Buf(b_t[:, i], "cb%d" % i) for i in range(2)]
        srcs = [(g.W["peer_u"].rearrange("l e d -> (l e) d"), g.UV[:, 0:D]), (g.W["peer_v"].rearrange("l e d -> (l e) d"), g.UV[:, D:2 * D])]
        n = 0
        engs = ["act", "dve", "pool"]
        for src, dst in srcs:
            for ch in range(n_layers * 16):
                r0 = ch * 1024
                f = Fs[n % 2]; b = Bs[n % 2]
                kb.dma("sp", lambda e, f=f, src=src, r0=r0: e.dma_start(out=f[:], in_=src[r0:r0 + 1024, :].rearrange("(p j) d -> p j d", p=128)), [], [f])
                for q in range(4):
                    en = engs[(n * 4 + q) % 3]
                    if en == "act":
                        kb.op("act", lambda e, f=f, b=b, q=q: e.activation(out=b[:, q * 2:(q + 1) * 2, :], in_=f[:, q * 2:(q + 1) * 2, :], func=AF.Copy), [f], [b])
                    else:
                        kb.op(en, lambda e, f=f, b=b, q=q: e.tensor_copy(out=b[:, q * 2:(q + 1) * 2, :], in_=f[:, q * 2:(q + 1) * 2, :]), [f], [b])
                kb.dma("sp", lambda e, b=b, dst=dst, r0=r0: e.dma_start(out=dst[r0:r0 + 1024, :].rearrange("(p j) d -> p j d", p=128), in_=b[:]), [b], [])
                n += 1
        kb.barrier()
```

```python
import numpy as np
import math
import concourse.bass as bass
import concourse.mybir as mybir
from concourse.bass_utils import run_bass_kernel_spmd

F32 = mybir.dt.float32
BF16 = mybir.dt.bfloat16
U32 = mybir.dt.uint32
I32 = mybir.dt.int32
AF = mybir.ActivationFunctionType
ALU = mybir.AluOpType
AX = mybir.AxisListType

NDS = 40


class Buf:
    __slots__ = ("t", "w", "r", "name", "nt")

    def __init__(self, t, name="", nt=False):
        self.t = t
        self.w = None
        self.r = []
        self.name = name
        self.nt = nt

    def __getitem__(self, k):
        return self.t[k]


class KB:
    def __init__(self, nc):
        self.nc = nc
        self.E = {"pe": nc.tensor, "act": nc.scalar, "dve": nc.vector, "pool": nc.gpsimd, "sp": nc.sync}
        self.esem = {n: nc.alloc_semaphore("s_" + n) for n in self.E}
        self.ecnt = {n: 0 for n in self.E}
        self.dsems = [nc.alloc_semaphore("d%d" % i) for i in range(NDS)]
        self.dcnt = [0] * NDS
        self.dnext = 0
        self.known = {n: {} for n in self.E}
        self.nwait = 0
        self.ninst = 0

    def _sem(self, key):
        return self.esem[key] if isinstance(key, str) else self.dsems[key]

    def _need(self, eng, ev):
        key, val, _ = ev
        if self.known[eng].get(key, 0) >= val:
            return
        self.E[eng].wait_ge(self._sem(key), val)
        self.known[eng][key] = val
        self.nwait += 1

    def _deps(self, eng, reads, writes, is_dma=False):
        for b in reads:
            if b.w is not None:
                self._need(eng, b.w)
        for b in writes:
            if b.w is not None and (is_dma or b.w[2] != eng):
                self._need(eng, b.w)
            for r in b.r:
                if is_dma or r[2] != eng:
                    self._need(eng, r)

    def op(self, eng, fn, reads=(), writes=()):
        reads = [b for b in reads if not b.nt]
        writes = [b for b in writes if not b.nt]
        self._deps(eng, reads, writes)
        inst = fn(self.E[eng])
        self.ecnt[eng] += 1
        inst.then_inc(self.esem[eng], 1)
        ev = (eng, self.ecnt[eng], eng)
        for b in reads:
            b.r.append(ev)
        for b in writes:
            b.w = ev
            b.r = []
        self.ninst += 1
        return inst

    def dma(self, q, fn, reads=(), writes=()):
        reads = [b for b in reads if not b.nt]
        writes = [b for b in writes if not b.nt]
        self._deps(q, reads, writes, is_dma=True)
        i = self.dnext
        self.dnext = (self.dnext + 1) % NDS
        if self.dcnt[i] > 0:
            self._need(q, (i, self.dcnt[i], "dma"))
        inst = fn(self.E[q])
        self.dcnt[i] += 16
        inst.then_inc(self.dsems[i], 16)
        ev = (i, self.dcnt[i], "dma")
        for b in reads:
            b.r.append(ev)
        for b in writes:
            b.w = ev
            b.r = []
        self.ninst += 1
        return inst

    def barrier(self):
        evs = [(n, self.ecnt[n], n) for n in self.E if self.ecnt[n] > 0]
        evs += [(i, self.dcnt[i], "dma") for i in range(NDS) if self.dcnt[i] > 0]
        for n in self.E:
            for ev in evs:
                self._need(n, ev)

    def finish(self):
        self.barrier()


D = 1024
NX = 2048
NC_ = 256
T = NX + NC_
NT = T // 128
P_IN = 9408
EPS = 1e-6
OFF = {"a_q1": 0, "a_q2": 256, "a_k1": 512, "a_k2": 768, "a_v": 1024,
       "b_cq": 1536, "b_ckv": 1792, "b_kr": 1920,
       "c_q": 1984, "c_f": 2496, "c_b": 3008, "c_i": 3520, "c_g": 4032,
       "d_q": 4544, "d_k": 5056, "d_v": 5184, "gate": 5312}

W_NAMES = [("w_mod", [2, 1024, 6144]), ("b_mod", [2, 6144]), ("norm1_g", [2, 1024]), ("norm2_g", [2, 1024]),
           ("w_in", [2, 1024, 9408]), ("diff_lam_q1", [2, 64]), ("diff_lam_k1", [2, 64]), ("diff_lam_q2", [2, 64]),
           ("diff_lam_k2", [2, 64]), ("diff_norm_g", [2, 128]), ("mla_qnorm_g", [2, 256]), ("mla_kvnorm_g", [2, 128]),
           ("mla_w_uq", [2, 256, 768]), ("mla_w_ukv", [2, 128, 1024]), ("hgrn_lb", [2, 2, 512]), ("hgrn_norm_g", [2, 128]),
           ("win_sink", [2, 8]), ("w_branch", [2, 4, 512, 1024]), ("w_out", [2, 1024, 1024]), ("peer_wq", [2, 1024, 1024]),
           ("peer_keys", [2, 8, 2, 128, 64]), ("peer_u", [2, 16384, 1024]), ("peer_v", [2, 16384, 1024]), ("final_g", [1, 1024])]


def host_consts():
    n = NX
    rows = np.repeat(np.arange(n // 64, dtype=np.float32), 64)
    cols = np.tile(np.arange(64, dtype=np.float32), n // 64)
    m = 32
    freqs = (10000.0 ** (-2.0 * np.arange(m // 2, dtype=np.float32) / m)).astype(np.float32)
    ang = np.concatenate([rows[:, None] * freqs, cols[:, None] * freqs], axis=-1).astype(np.float32)
    cs = np.zeros((T, 64), np.float32)
    cs[:n, :32] = np.cos(ang)
    cs[:n, 32:] = np.sin(ang)
    cs[n:, :32] = 1.0
    i = np.arange(128)
    c = {}
    c["rope_cs"] = cs
    c["ident"] = np.eye(128, dtype=np.float32)
    c["mask_prev"] = (i[:, None] >= i[None, :]).astype(np.float32)
    c["mask_next"] = (i[:, None] <= i[None, :]).astype(np.float32)
    j = np.arange(64)
    tri = np.zeros((64, 4, 64), np.float32)
    tri[:, 0] = (j[:, None] <= j[None, :])
    tri[:, 1] = (j[:, None] >= j[None, :])
    tri[:, 2] = (j[:, None] > j[None, :])
    tri[:, 3] = (j[:, None] < j[None, :])
    c["tri"] = tri.reshape(64, 256)
    return c


class Ctx:
    pass


_nmc = [0]


def _nm():
    _nmc[0] += 1
    return "tmp%d" % _nmc[0]


def build_program(n_layers=2, debug=(), stop_after=None):
    nc = bass.Bass("TRN2", target_bir_lowering=False)
    kb = KB(nc)
    g = Ctx()
    g.nc, g.kb = nc, kb
    g.debug = debug

    def din(name, shape, dt=F32):
        return nc.dram_tensor(name, list(shape), dt, kind="ExternalInput").ap()

    g.x_in = din("x", [NX, D])
    g.c_in = din("c", [1, D])
    g.ctx_in = din("ctx", [NC_, D])
    g.cctx_in = din("c_ctx", [1, D])
    g.W = {n: din(n, s) for n, s in W_NAMES}
    g.rope_cs = din("rope_cs", [T, 64])
    g.ident_in = din("ident", [128, 128])
    g.mprev_in = din("mask_prev", [128, 128])
    g.mnext_in = din("mask_next", [128, 128])
    g.tri_in = din("tri", [64, 256])
    g.out = nc.dram_tensor("out", [NX, D], F32, kind="ExternalOutput").ap()

    def scratch(name, shape, dt):
        kind = "ExternalOutput" if name in debug else "Internal"
        return Buf(nc.dram_tensor(name, list(shape), dt, kind=kind).ap(), name, nt=True)

    g.P = scratch("P", [T, P_IN], BF16)
    g.OB = scratch("OB", [T, 2048], BF16)
    g.MODS = scratch("MODS", [2, 2, 6144], F32)
    g.OF = scratch("OF", [T, 512], F32)
    g.OBK = scratch("OBK", [T, 512], F32)
    g.UV = scratch("UV", [2 * 16384, 2 * D], BF16)

    cnt = [0]

    def sb(shape, dt, name=None):
        cnt[0] += 1
        return Buf(nc.alloc_sbuf_tensor("%s_%d" % (name or "t", cnt[0]), list(shape), dt), name or "t")
    g.sb = sb

    g.X = [Buf(None, "X%d" % i) for i in range(NT)]
    xall = nc.alloc_sbuf_tensor("Xall", [128, NT, D], F32)
    for i in range(NT):
        g.X[i].t = xall[:, i, :]
    psall = nc.alloc_psum_tensor("psall", [128, 8, 512], F32)
    g.psall = psall
    g.PS = [Buf(psall[:, i, :], "ps%d" % i) for i in range(8)]
    g.PSP = [Buf(psall[:, 2 * i:2 * i + 2, :].rearrange("p b c -> p (b c)"), "psp%d" % i) for i in range(4)]
    g.ident_f = sb([128, 128], F32, "identf")
    g.ident_b = sb([128, 128], BF16, "identb")
    g.ones_f = sb([128, 128], F32, "onesf")
    g.SCT = sb([128, 8, 2], F32, "sct")

    dma = lambda fn, r=(), w=(): kb.dma("sp", fn, r, w)

    for i in range(16):
        dma(lambda e, i=i: e.dma_start(out=g.X[i][:], in_=g.x_in[i * 128:(i + 1) * 128, :]), [], [g.X[i]])
    for i in range(2):
        dma(lambda e, i=i: e.dma_start(out=g.X[16 + i][:], in_=g.ctx_in[i * 128:(i + 1) * 128, :]), [], [g.X[16 + i]])
    dma(lambda e: e.dma_start(out=g.ident_f[:], in_=g.ident_in[:, :]), [], [g.ident_f])
    kb.op("dve", lambda e: e.tensor_copy(out=g.ident_b[:], in_=g.ident_f[:]), [g.ident_f], [g.ident_b])
    kb.op("dve", lambda e: e.memset(g.ones_f[:], 1.0), [], [g.ones_f])
    craw = sb([128, 8, 2], F32, "craw")
    with nc.allow_non_contiguous_dma(reason="tiny conditioning vector load"):
        dma(lambda e: e.dma_start(out=craw[:, :, 0], in_=g.c_in.rearrange("o (k p) -> p (o k)", p=128)), [], [craw])
        dma(lambda e: e.dma_start(out=craw[:, :, 1], in_=g.cctx_in.rearrange("o (k p) -> p (o k)", p=128)), [], [craw])
    kb.op("act", lambda e: e.activation(out=g.SCT[:], in_=craw[:], func=AF.Silu), [craw], [g.SCT])

    for l in range(n_layers):
        last = (l == 1)
        from contextlib import ExitStack
        bg_es = ExitStack()
        g.bg = cast_bg(g, l, cast_alloc(g, bg_es))
        with nc.named_scope("mod%d" % l):
            phase_mod(g, l)
        if stop_after == ("mod", l):
            break
        if "skip_proj" not in debug:
            with nc.named_scope("proj%d" % l):
                phase_norm_proj(g, l)
        if stop_after == ("proj", l):
            break
        ctx_out = not last
        if "skip_a" not in debug:
            with nc.named_scope("a%d" % l):
                phase_mixer_a(g, l, ctx_out)
        if stop_after == ("a", l):
            break
        if "skip_b" not in debug:
            with nc.named_scope("b%d" % l):
                phase_mixer_b(g, l, ctx_out)
        if stop_after == ("b", l):
            break
        if "skip_d" not in debug:
            with nc.named_scope("d%d" % l):
                phase_mixer_d(g, l, ctx_out)
        if stop_after == ("d", l):
            break
        if "skip_c" not in debug:
            with nc.named_scope("c%d" % l):
                phase_mixer_c(g, l, ctx_out)
        if stop_after == ("c", l):
            break
        if "skip_merge" not in debug:
            with nc.named_scope("merge%d" % l):
                phase_merge(g, l, ctx_out)
        if "Xdbg" in debug and stop_after == ("merge", l):
            xd = nc.dram_tensor("Xdbg", [NT, 128, D], F32, kind="ExternalOutput").ap()
            for i in range(NT):
                dma(lambda e, i=i: e.dma_start(out=xd[i], in_=g.X[i][:]), [g.X[i]], [])
        if stop_after == ("merge", l):
            break
        with nc.named_scope("castdrain%d" % l):
            bgstep(g, 100000)
            kb.barrier()
            bg_es.close()
        with nc.named_scope("peer%d" % l):
            phase_peer(g, l, ctx_out)
        if "Xdbg" in debug and stop_after == ("peer", l):
            xd = nc.dram_tensor("Xdbg", [NT, 128, D], F32, kind="ExternalOutput").ap()
            for i in range(NT):
                dma(lambda e, i=i: e.dma_start(out=xd[i], in_=g.X[i][:]), [g.X[i]], [])
        if stop_after == ("peer", l):
            break

    if stop_after is None:
        with nc.named_scope("final"):
            phase_final(g)
    kb.finish()
    g.nc = nc
    return nc, g


def phase_mod(g, l):
    nc, kb = g.nc, g.kb
    kb.barrier()
    dma = lambda fn, r=(), w=(): kb.dma("sp", fn, r, w)
    with nc.sbuf_tensor(_nm(), [128, 2, 8, 512], F32) as wm_t, nc.sbuf_tensor(_nm(), [1, 6144], F32) as bm_t, \
            nc.sbuf_tensor(_nm(), [2, 2, 512], F32) as ev_t:
        wm = [Buf(wm_t[:, i], "wm%d" % i) for i in range(2)]
        bm = Buf(bm_t, "bm")
        ev = [Buf(ev_t[:, i], "ev%d" % i) for i in range(2)]
        dma(lambda e: e.dma_start(out=bm[:], in_=g.W["b_mod"][l:l + 1, :]), [], [bm])
        wsrc = g.W["w_mod"][l].rearrange("(k p) c -> p k c", p=128)
        for m in range(12):
            w = wm[m % 2]
            dma(lambda e, w=w, m=m: e.dma_start(out=w[:], in_=wsrc[:, :, m * 512:(m + 1) * 512]), [], [w])
            ps = g.PS[m % 2]
            for k in range(8):
                kb.op("pe", lambda e, w=w, k=k, ps=ps: e.matmul(ps[0:2, :], lhsT=g.SCT[:, k, :], rhs=w[:, k, :],
                                                              start=(k == 0), stop=False), [g.SCT, w], [ps])
            kb.op("pe", lambda e, ps=ps, m=m: e.matmul(ps[0:2, :], lhsT=g.ones_f[0:1, 0:2], rhs=bm[0:1, m * 512:(m + 1) * 512],
                                                     start=False, stop=True), [g.ones_f, bm], [ps])
            o = ev[m % 2]
            kb.op("act", lambda e, o=o, ps=ps: e.activation(out=o[:], in_=ps[0:2, :], func=AF.Copy), [ps], [o])
            dma(lambda e, o=o, m=m: e.dma_start(out=g.MODS[l, :, m * 512:(m + 1) * 512], in_=o[:]), [o], [g.MODS])
        kb.barrier()


def load_bcast(g, dst, src_row_ap):
    g.kb.dma("sp", lambda e: e.dma_start(out=dst[:], in_=src_row_ap.partition_broadcast(128)), [g.MODS], [dst])


def norm_tile(g, xin, GS, SH, hb, scr):
    kb = g.kb
    junk, st = scr
    kb.op("act", lambda e: e.activation(out=junk[:], in_=xin[:], func=AF.Square, accum_out=st[:, 0:1]), [xin], [junk, st])
    kb.op("dve", lambda e: e.tensor_scalar(out=st[:, 1:2], in0=st[:, 0:1], scalar1=1.0 / D, scalar2=EPS, op0=ALU.mult, op1=ALU.add), [st], [st])
    kb.op("act", lambda e: e.activation(out=st[:, 2:3], in_=st[:, 1:2], func=AF.Sqrt), [st], [st])
    kb.op("dve", lambda e: e.reciprocal(out=st[:, 3:4], in_=st[:, 2:3]), [st], [st])
    kb.op("dve", lambda e: e.scalar_tensor_tensor(out=junk[:], in0=xin[:], scalar=st[:, 3:4], in1=GS[:], op0=ALU.mult, op1=ALU.mult), [xin, st, GS], [junk])
    kb.op("dve", lambda e: e.tensor_tensor(out=hb[:], in0=junk[:], in1=SH[:], op=ALU.add), [junk, SH], [hb])


def phase_norm_proj(g, l):
    nc, kb = g.nc, g.kb
    dma = lambda fn, r=(), w=(): kb.dma("sp", fn, r, w)
    with nc.sbuf_tensor(_nm(), [128, 8, T], BF16) as hT_t:
        hT = Buf(hT_t, "hT")
        with nc.sbuf_tensor(_nm(), [128, 5, D], F32) as bc_t, nc.sbuf_tensor(_nm(), [128, D], F32) as junk_t, \
                nc.sbuf_tensor(_nm(), [128, 2, 8], F32) as st_t, nc.sbuf_tensor(_nm(), [128, 2, D], BF16) as hb_t:
            G1, SCx, SHx, SCc, SHc = [Buf(bc_t[:, i], "bc%d" % i) for i in range(5)]
            junk = Buf(junk_t, "junk")
            sts = [Buf(st_t[:, i], "st%d" % i) for i in range(2)]
            hbs = [Buf(hb_t[:, i], "hb%d" % i) for i in range(2)]
            dma(lambda e: e.dma_start(out=G1[:], in_=g.W["norm1_g"][l:l + 1, :].partition_broadcast(128)), [], [G1])
            load_bcast(g, SHx, g.MODS[l, 0:1, 0:D])
            load_bcast(g, SCx, g.MODS[l, 0:1, D:2 * D])
            load_bcast(g, SHc, g.MODS[l, 1:2, 0:D])
            load_bcast(g, SCc, g.MODS[l, 1:2, D:2 * D])
            for S in (SCx, SCc):
                kb.op("dve", lambda e, S=S: e.scalar_tensor_tensor(out=S[:], in0=S[:], scalar=1.0, in1=G1[:], op0=ALU.add, op1=ALU.mult), [S, G1], [S])
            for tt in range(NT):
                GS, SH = (SCx, SHx) if tt < 16 else (SCc, SHc)
                hb = hbs[tt % 2]
                norm_tile(g, g.X[tt], GS, SH, hb, (junk, sts[tt % 2]))
                ps = g.PS[tt % 2]
                psb = ps[:].bitcast(BF16)
                for k in range(8):
                    kb.op("pe", lambda e, k=k, hb=hb, psb=psb: e.transpose(out=psb[:, k * 128:(k + 1) * 128], in_=hb[:, k * 128:(k + 1) * 128], identity=g.ident_b[:]), [hb, g.ident_b], [ps])
                kb.op("act", lambda e, tt=tt, psb=psb: e.activation(out=hT[:, :, tt * 128:(tt + 1) * 128], in_=psb.rearrange("p (k t) -> p k t", k=8), func=AF.Copy), [ps], [hT])
            kb.barrier()
        with nc.sbuf_tensor(_nm(), [128, 2, 8, 512], BF16) as wb_t, nc.sbuf_tensor(_nm(), [128, 2, 6, 512], BF16) as po_t:
            wbs = [Buf(wb_t[:, i], "wb%d" % i) for i in range(2)]
            pos = [Buf(po_t[:, i], "po%d" % i) for i in range(2)]
            wsrc = g.W["w_in"][l].rearrange("(k p) c -> p k c", p=128)
            pdst = g.P.t.rearrange("(t p) c -> p t c", p=128)
            nct = (P_IN + 511) // 512
            cw = lambda ct: min(512, P_IN - ct * 512)

            def load_w(ct):
                w = wbs[ct % 2]
                kb.dma("pool", lambda e: e.dma_start(out=w[:, :, 0:cw(ct)], in_=wsrc[:, :, ct * 512:ct * 512 + cw(ct)]), [], [w])
            load_w(0)
            oi = 0
            pi = 0
            for ct in range(nct):
                if ct + 1 < nct:
                    load_w(ct + 1)
                w = wbs[ct % 2]
                n = cw(ct)
                for tg in range(3):
                    po = pos[oi % 2]
                    oi += 1
                    for j in range(6):
                        tt = tg * 6 + j
                        ps = g.PS[2 + pi % 4]
                        pi += 1
                        for k in range(8):
                            kb.op("pe", lambda e, k=k, tt=tt, ps=ps, w=w, n=n: e.matmul(ps[:, 0:n], lhsT=hT[:, k, tt * 128:(tt + 1) * 128], rhs=w[:, k, 0:n],
                                                                                     start=(k == 0), stop=(k == 7)), [hT, w], [ps])
                        if pi % 2 == 0:
                            kb.op("act", lambda e, j=j, ps=ps, po=po, n=n: e.activation(out=po[:, j, 0:n], in_=ps[:, 0:n], func=AF.Copy), [ps], [po])
                        else:
                            kb.op("dve", lambda e, j=j, ps=ps, po=po, n=n: e.tensor_copy(out=po[:, j, 0:n], in_=ps[:, 0:n]), [ps], [po])
                    dma(lambda e, po=po, tg=tg, ct=ct, n=n: e.dma_start(out=pdst[:, tg * 6:(tg + 1) * 6, ct * 512:ct * 512 + n], in_=po[:, :, 0:n]), [po], [g.P])
            kb.barrier()


def attention(g, nh, dv, scale, V, qk_chunks, groups, post, npass=1):
    nc, kb = g.nc, g.kb
    assert dv + 1 <= 256
    with nc.sbuf_tensor(_nm(), [128, 3, 512], BF16) as E_t, nc.sbuf_tensor(_nm(), [128, 4, 4, dv + 1], F32) as of_t:
        Es = [Buf(E_t[:, i], "E%d" % i) for i in range(3)]
        Ofs = [Buf(of_t[:, i], "Of%d" % i) for i in range(4)]
        iters = []
        for h in range(nh):
            for (q_lo, q_n, key_tiles) in groups:
                for s in range(npass):
                    iters.append((h, q_lo, q_n, key_tiles, s))
        steps = [(ii, ki) for ii, it_ in enumerate(iters) for ki in range(len(it_[3]))]

        def emit_S(i):
            ii, ki = steps[i]
            h, q_lo, q_n, kts, s = iters[ii]
            kt = kts[ki]
            S = g.PS[i % 3]
            chunks = qk_chunks(h, s)
            for ci, (qf, kf, bufs) in enumerate(chunks):
                kb.op("pe", lambda e, qf=qf, kf=kf, ci=ci: e.matmul(
                    S[:, 0:q_n], lhsT=kf(kt), rhs=qf(q_lo, q_n), start=(ci == 0), stop=(ci == len(chunks) - 1)), bufs, [S])

        ofl = []

        def emit_rest(i):
            ii, ki = steps[i]
            h, q_lo, q_n, kts, s = iters[ii]
            kt = kts[ki]
            nsub = q_n // 128
            S = g.PS[i % 3]; E = Es[i % 3]
            obanks = [g.PS[3 + 2 * (ii % 2)], g.PS[4 + 2 * (ii % 2)]]
            Of = Ofs[ii % 4]
            kb.op("act", lambda e: e.activation(out=E[:, 0:q_n], in_=S[:, 0:q_n], func=AF.Exp, scale=scale), [S], [E])
            for sub in range(nsub):
                ob = obanks[sub // 2]
                c0 = (sub % 2) * (dv + 1)
                kb.op("pe", lambda e, ob=ob, c0=c0, sub=sub: e.matmul(
                    ob[:, c0:c0 + dv + 1], lhsT=E[:, sub * 128:(sub + 1) * 128], rhs=V[:, kt, h, :],
                    start=(ki == 0 and sub % 2 == 0), stop=(ki == len(kts) - 1), skip_group_check=True), [E, V], [ob])
            if ki == len(kts) - 1:
                for bi in range((nsub + 1) // 2):
                    nb = min(2, nsub - bi * 2)
                    ob = obanks[bi]
                    src = ob[:, 0:nb * (dv + 1)].rearrange("p (s d) -> p s d", s=nb)
                    kb.op("dve", lambda e, bi=bi, nb=nb, src=src: e.tensor_copy(out=Of[:, bi * 2:bi * 2 + nb, :], in_=src), [ob], [Of])
                ofl.append(Of)
                if s == npass - 1:
                    post(h, q_lo, q_n, list(ofl))
                    del ofl[:]

        emit_S(0)
        for i in range(len(steps)):
            if i + 1 < len(steps):
                emit_S(i + 1)
            emit_rest(i)
            if i % 4 == 3:
                bgstep(g)


def phase_mixer_a(g, l, ctx_out):
    nc, kb = g.nc, g.kb
    dma = lambda fn, r=(), w=(): kb.dma("sp", fn, r, w)
    lam_init = 0.8 - 0.6 * math.exp(-0.3 * l)
    Pv = g.P.t.rearrange("(t p) c -> p t c", p=128)
    OBv = g.OB.t.rearrange("(t p) c -> p t c", p=128)
    with nc.sbuf_tensor(_nm(), [128, 8, T], BF16) as qkt_t, nc.sbuf_tensor(_nm(), [128, NT, 4, 129], BF16) as va_t, \
            nc.sbuf_tensor(_nm(), [128, NT, 64], F32) as cs_t, nc.sbuf_tensor(_nm(), [128, 8], F32) as lam_t, \
            nc.sbuf_tensor(_nm(), [128, 128], F32) as ng_t:
        QKT = Buf(qkt_t, "QKT"); VA = Buf(va_t, "VA"); CS = Buf(cs_t, "CS"); LAM = Buf(lam_t, "LAM"); NG = Buf(ng_t, "NG")
        dma(lambda e: e.dma_start(out=CS[:], in_=g.rope_cs.rearrange("(t p) c -> p t c", p=128)), [], [CS])
        for tt in range(NT):
            dma(lambda e, tt=tt: e.dma_start(out=VA[:, tt, :, 0:128], in_=Pv[:, tt, 1024:1536].rearrange("p (h d) -> p h d", h=4)), [g.P], [VA])
        kb.op("pool", lambda e: e.memset(VA[:, :, :, 128:129], 1.0), [], [VA])
        with nc.sbuf_tensor(_nm(), [128, 4, 64], F32) as lv_t:
            LV = Buf(lv_t, "LV")
            for i, nm in enumerate(["diff_lam_q1", "diff_lam_k1", "diff_lam_q2", "diff_lam_k2"]):
                dma(lambda e, i=i, nm=nm: e.dma_start(out=LV[:, i, :], in_=g.W[nm][l:l + 1, :].partition_broadcast(128)), [], [LV])
            kb.op("dve", lambda e: e.tensor_tensor(out=LV[:, 0, :], in0=LV[:, 0, :], in1=LV[:, 1, :], op=ALU.mult), [LV], [LV])
            kb.op("dve", lambda e: e.tensor_tensor(out=LV[:, 2, :], in0=LV[:, 2, :], in1=LV[:, 3, :], op=ALU.mult), [LV], [LV])
            kb.op("dve", lambda e: e.tensor_reduce(out=LAM[:, 0:1], in_=LV[:, 0, :], axis=AX.X, op=ALU.add), [LV], [LAM])
            kb.op("dve", lambda e: e.tensor_reduce(out=LAM[:, 1:2], in_=LV[:, 2, :], axis=AX.X, op=ALU.add), [LV], [LAM])
            kb.op("act", lambda e: e.activation(out=LAM[:, 2:4], in_=LAM[:, 0:2], func=AF.Exp), [LAM], [LAM])
            kb.op("dve", lambda e: e.tensor_tensor(out=LAM[:, 4:5], in0=LAM[:, 2:3], in1=LAM[:, 3:4], op=ALU.subtract), [LAM], [LAM])
            kb.op("dve", lambda e: e.tensor_scalar(out=LAM[:, 5:6], in0=LAM[:, 4:5], scalar1=lam_init, scalar2=None, op0=ALU.add), [LAM], [LAM])
            dma(lambda e: e.dma_start(out=NG[:], in_=g.W["diff_norm_g"][l:l + 1, :].partition_broadcast(128)), [], [NG])
            kb.op("dve", lambda e: e.tensor_scalar(out=NG[:], in0=NG[:], scalar1=1.0 - lam_init, scalar2=None, op0=ALU.mult), [NG], [NG])
            kb.barrier()
        lam = LAM[:, 5:6]
        with nc.sbuf_tensor(_nm(), [128, 2, 1024], BF16) as pa_t, nc.sbuf_tensor(_nm(), [128, 2, 1024], BF16) as rb_t, \
                nc.sbuf_tensor(_nm(), [128, 4, 512], F32) as tm_t:
            pas = [Buf(pa_t[:, i], "pa%d" % i) for i in range(2)]
            rbs = [Buf(rb_t[:, i], "rb%d" % i) for i in range(2)]
            tms = [Buf(tm_t[:, i], "tm%d" % i) for i in range(4)]
            for tt in range(NT):
                pa = pas[tt % 2]; rb = rbs[tt % 2]
                dma(lambda e, pa=pa, tt=tt: e.dma_start(out=pa[:], in_=Pv[:, tt, 0:1024]), [g.P], [pa])
                rope_tile(g, pa, rb, CS, tt, 16, tms)
                ps = g.PS[7]
                psb = ps[:].bitcast(BF16)
                for c in range(8):
                    kb.op("pe", lambda e, c=c, rb=rb, psb=psb: e.transpose(out=psb[:, c * 128:(c + 1) * 128], in_=rb[:, c * 128:(c + 1) * 128], identity=g.ident_b[:]), [rb, g.ident_b], [ps])
                kb.op("act", lambda e, tt=tt, psb=psb: e.activation(out=QKT[:, :, tt * 128:(tt + 1) * 128], in_=psb.rearrange("p (k t) -> p k t", k=8), func=AF.Copy), [ps], [QKT])
            kb.barrier()

        def qk_chunks(h, s):
            p0 = (h % 2) * 64
            qc = s * 2 + h // 2
            kc = 4 + s * 2 + h // 2
            return [(lambda lo, n: QKT[p0:p0 + 64, qc, lo:lo + n], lambda kt: QKT[p0:p0 + 64, kc, kt * 128:(kt + 1) * 128], [QKT])]

        with nc.sbuf_tensor(_nm(), [128, 2, 8, 4], F32) as r_t, nc.sbuf_tensor(_nm(), [128, 2, 3, 4, 128], F32) as w_t, \
                nc.sbuf_tensor(_nm(), [128, 2, 4, 128], BF16) as o_t:
            cnt = [0]
            rs_ = [Buf(r_t[:, i], "r") for i in range(2)]; obs_ = [Buf(o_t[:, i], "ob") for i in range(2)]
            ws_ = [[Buf(w_t[:, i, j], "w%d" % j) for j in range(3)] for i in range(2)]

            def post(h, q_lo, q_n, ofl):
                O1, O2 = ofl
                i = cnt[0] % 2
                cnt[0] += 1
                ns = q_n // 128
                r = rs_[i]; w0, w1, w2 = ws_[i]; ob = obs_[i]
                bc = lambda ap: ap.unsqueeze(2).to_broadcast([128, ns, 128])
                kb.op("dve", lambda e: e.reciprocal(out=r[:, 0, 0:ns], in_=O1[:, 0:ns, 128]), [O1], [r])
                kb.op("dve", lambda e: e.reciprocal(out=r[:, 1, 0:ns], in_=O2[:, 0:ns, 128]), [O2], [r])
                kb.op("dve", lambda e: e.tensor_scalar(out=r[:, 2, 0:ns], in0=r[:, 1, 0:ns], scalar1=lam, scalar2=None, op0=ALU.mult), [r, LAM], [r])
                kb.op("dve", lambda e: e.tensor_tensor(out=w0[:, 0:ns, :], in0=O1[:, 0:ns, 0:128], in1=bc(r[:, 0, 0:ns]), op=ALU.mult), [O1, r], [w0])
                kb.op("dve", lambda e: e.tensor_tensor(out=w1[:, 0:ns, :], in0=O2[:, 0:ns, 0:128], in1=bc(r[:, 2, 0:ns]), op=ALU.mult), [O2, r], [w1])
                kb.op("dve", lambda e: e.tensor_tensor(out=w0[:, 0:ns, :], in0=w0[:, 0:ns, :], in1=w1[:, 0:ns, :], op=ALU.subtract), [w0, w1], [w0])
                kb.op("pool", lambda e: e.tensor_tensor(out=w2[:, 0:ns, :], in0=w0[:, 0:ns, :], in1=w0[:, 0:ns, :], op=ALU.mult), [w0], [w2])
                kb.op("dve", lambda e: e.tensor_reduce(out=r[:, 3, 0:ns], in_=w2[:, 0:ns, :], axis=AX.X, op=ALU.add), [w2], [r])
                kb.op("dve", lambda e: e.tensor_scalar(out=r[:, 4, 0:ns], in0=r[:, 3, 0:ns], scalar1=1.0 / 128, scalar2=EPS, op0=ALU.mult, op1=ALU.add), [r], [r])
                kb.op("act", lambda e: e.activation(out=r[:, 5, 0:ns], in_=r[:, 4, 0:ns], func=AF.Sqrt), [r], [r])
                kb.op("dve", lambda e: e.reciprocal(out=r[:, 6, 0:ns], in_=r[:, 5, 0:ns]), [r], [r])
                kb.op("dve", lambda e: e.tensor_tensor(out=w1[:, 0:ns, :], in0=w0[:, 0:ns, :], in1=bc(r[:, 6, 0:ns]), op=ALU.mult), [w0, r], [w1])
                kb.op("pool", lambda e: e.tensor_tensor(out=ob[:, 0:ns, :], in0=w1[:, 0:ns, :], in1=NG[:].unsqueeze(1).to_broadcast([128, ns, 128]), op=ALU.mult), [w1, NG], [ob])
                t0 = q_lo // 128
                dma(lambda e: e.dma_start(out=OBv[:, t0:t0 + ns, h * 128:(h + 1) * 128], in_=ob[:, 0:ns, :]), [ob], [g.OB])

            groups = [(i * 512, 512, list(range(NT))) for i in range(4)]
            if ctx_out:
                groups.append((2048, 256, [16, 17]))
            attention(g, 4, 128, 1.0 / 8.0, VA, qk_chunks, groups, post, npass=2)
            kb.barrier()


def rope_tile(g, pa, rb, CS, tt, ng, tms, col0=0):
    kb = g.kb
    n = ng * 64
    xv = pa[:, col0:col0 + n].rearrange("p (g a f j) -> p g a f j", g=ng, a=2, f=2, j=16)
    ov = rb[:, col0:col0 + n].rearrange("p (g a f j) -> p g a f j", g=ng, a=2, f=2, j=16)
    x1, x2 = xv[:, :, :, 0, :], xv[:, :, :, 1, :]
    cosb = CS[:, tt, 0:32].rearrange("p (a j) -> p a j", a=2).unsqueeze(1).to_broadcast([128, ng, 2, 16])
    sinb = CS[:, tt, 32:64].rearrange("p (a j) -> p a j", a=2).unsqueeze(1).to_broadcast([128, ng, 2, 16])
    tv = [t[:, 0:ng * 32].rearrange("p (g a j) -> p g a j", g=ng, a=2) for t in tms]
    kb.op("dve", lambda e: e.tensor_tensor(out=tv[0], in0=x1, in1=cosb, op=ALU.mult), [pa, CS], [tms[0]])
    kb.op("pool", lambda e: e.tensor_tensor(out=tv[1], in0=x2, in1=sinb, op=ALU.mult), [pa, CS], [tms[1]])
    kb.op("dve", lambda e: e.tensor_tensor(out=tv[2], in0=x1, in1=sinb, op=ALU.mult), [pa, CS], [tms[2]])
    kb.op("pool", lambda e: e.tensor_tensor(out=tv[3], in0=x2, in1=cosb, op=ALU.mult), [pa, CS], [tms[3]])
    kb.op("dve", lambda e: e.tensor_tensor(out=ov[:, :, :, 0, :], in0=tv[0], in1=tv[1], op=ALU.subtract), [tms[0], tms[1]], [rb])
    kb.op("pool", lambda e: e.tensor_tensor(out=ov[:, :, :, 1, :], in0=tv[2], in1=tv[3], op=ALU.add), [tms[2], tms[3]], [rb])


def small_rms(g, src_ap, src_buf, n, nh, r, col):
    kb = g.kb
    return None


def phase_mixer_b(g, l, ctx_out):
    nc, kb = g.nc, g.kb
    dma = lambda fn, r=(), w=(): kb.dma("sp", fn, r, w)
    Pv = g.P.t.rearrange("(t p) c -> p t c", p=128)
    OBv = g.OB.t.rearrange("(t p) c -> p t c", p=128)
    TQ = T if ctx_out else NX
    with nc.sbuf_tensor(_nm(), [128, 4, T], BF16) as ft_t, nc.sbuf_tensor(_nm(), [128, 4, T], BF16) as qnt_t, \
            nc.sbuf_tensor(_nm(), [128, 4, T], BF16) as knt_t, nc.sbuf_tensor(_nm(), [128, 2, T], BF16) as qrt_t, \
            nc.sbuf_tensor(_nm(), [128, NT, 4, 129], BF16) as vb_t:
        FT = Buf(ft_t, "FT"); QNT = Buf(qnt_t, "QNT"); KNT = Buf(knt_t, "KNT"); QRT = Buf(qrt_t, "QRT"); VB = Buf(vb_t, "VB")
        kb.op("pool", lambda e: e.memset(VB[:, :, :, 128:129], 1.0), [], [VB])
        with nc.sbuf_tensor(_nm(), [128, 2, 4, 128], BF16) as wqn_t, nc.sbuf_tensor(_nm(), [128, 2, 4, 64], BF16) as wqr_t, \
                nc.sbuf_tensor(_nm(), [128, 4, 128], BF16) as wkn_t, nc.sbuf_tensor(_nm(), [128, 4, 128], BF16) as wv_t, \
                nc.sbuf_tensor(_nm(), [128, 384], F32) as gn_t, nc.sbuf_tensor(_nm(), [128, NT, 64], F32) as cs_t:
            WQN = Buf(wqn_t, "WQN"); WQR = Buf(wqr_t, "WQR"); WKN = Buf(wkn_t, "WKN"); WV = Buf(wv_t, "WV"); GN = Buf(gn_t, "GN"); CS = Buf(cs_t, "CS")
            dma(lambda e: e.dma_start(out=CS[:], in_=g.rope_cs.rearrange("(t p) c -> p t c", p=128)), [], [CS])
            wuq = g.W["mla_w_uq"][l].rearrange("(c p) (h d) -> p c h d", p=128, h=4)
            wukv = g.W["mla_w_ukv"][l].rearrange("p (h d) -> p h d", h=4)
            for c in range(2):
                kb.dma("pool", lambda e, c=c: e.dma_start(out=WQN[:, c], in_=wuq[:, c, :, 0:128]), [], [WQN])
                kb.dma("pool", lambda e, c=c: e.dma_start(out=WQR[:, c], in_=wuq[:, c, :, 128:192]), [], [WQR])
            kb.dma("pool", lambda e: e.dma_start(out=WKN[:], in_=wukv[:, :, 0:128]), [], [WKN])
            kb.dma("pool", lambda e: e.dma_start(out=WV[:], in_=wukv[:, :, 128:256]), [], [WV])
            dma(lambda e: e.dma_start(out=GN[:, 0:256], in_=g.W["mla_qnorm_g"][l:l + 1, :].partition_broadcast(128)), [], [GN])
            dma(lambda e: e.dma_start(out=GN[:, 256:384], in_=g.W["mla_kvnorm_g"][l:l + 1, :].partition_broadcast(128)), [], [GN])
            with nc.sbuf_tensor(_nm(), [128, 2, 448], BF16) as pb_t, nc.sbuf_tensor(_nm(), [128, 2, 512], BF16) as tb_t, \
                    nc.sbuf_tensor(_nm(), [128, 2, 512], F32) as jk_t, nc.sbuf_tensor(_nm(), [128, 2, 8], F32) as st_t, \
                    nc.sbuf_tensor(_nm(), [128, 4, 128], F32) as tm_t, nc.sbuf_tensor(_nm(), [128, 2, 64], BF16) as kr_t:
                tms = [Buf(tm_t[:, i], "tm%d" % i) for i in range(4)]
                pbs = [Buf(pb_t[:, i], "pb") for i in range(2)]; tbs = [Buf(tb_t[:, i], "tb") for i in range(2)]
                jks = [Buf(jk_t[:, i], "jk") for i in range(2)]; sts = [Buf(st_t[:, i], "st") for i in range(2)]
                for tt in range(NT):
                    i = tt % 2
                    pb = pbs[i]; tb = tbs[i]; jk = jks[i]; st = sts[i]
                    dma(lambda e, pb=pb, tt=tt: e.dma_start(out=pb[:], in_=Pv[:, tt, 1536:1984]), [g.P], [pb])
                    kb.op("act", lambda e: e.activation(out=jk[:, 0:256], in_=pb[:, 0:256], func=AF.Square, accum_out=st[:, 0:1]), [pb], [jk, st])
                    kb.op("act", lambda e: e.activation(out=jk[:, 256:384], in_=pb[:, 256:384], func=AF.Square, accum_out=st[:, 1:2]), [pb], [jk, st])
                    kb.op("dve", lambda e: e.tensor_scalar(out=st[:, 2:3], in0=st[:, 0:1], scalar1=1.0 / 256, scalar2=EPS, op0=ALU.mult, op1=ALU.add), [st], [st])
                    kb.op("dve", lambda e: e.tensor_scalar(out=st[:, 3:4], in0=st[:, 1:2], scalar1=1.0 / 128, scalar2=EPS, op0=ALU.mult, op1=ALU.add), [st], [st])
                    kb.op("act", lambda e: e.activation(out=st[:, 4:6], in_=st[:, 2:4], func=AF.Sqrt), [st], [st])
                    kb.op("dve", lambda e: e.reciprocal(out=st[:, 6:8], in_=st[:, 4:6]), [st], [st])
                    kb.op("dve", lambda e: e.scalar_tensor_tensor(out=tb[:, 0:256], in0=pb[:, 0:256], scalar=st[:, 6:7], in1=GN[:, 0:256], op0=ALU.mult, op1=ALU.mult), [pb, st, GN], [tb])
                    kb.op("dve", lambda e: e.scalar_tensor_tensor(out=tb[:, 256:384], in0=pb[:, 256:384], scalar=st[:, 7:8], in1=GN[:, 256:384], op0=ALU.mult, op1=ALU.mult), [pb, st, GN], [tb])
                    rope_tile(g, pb, tb, CS, tt, 1, tms, col0=384)
                    kb.op("pool", lambda e: e.tensor_copy(out=tb[:, 448:512], in_=tb[:, 384:448]), [tb], [tb])
                    ps = g.PS[7]
                    psb = ps[:].bitcast(BF16)
                    for c in range(4):
                        kb.op("pe", lambda e, c=c, tb=tb, psb=psb: e.transpose(out=psb[:, c * 128:(c + 1) * 128], in_=tb[:, c * 128:(c + 1) * 128], identity=g.ident_b[:]), [tb, g.ident_b], [ps])
                    kb.op("act", lambda e, tt=tt, psb=psb: e.activation(out=FT[:, :, tt * 128:(tt + 1) * 128], in_=psb[:, 0:512].rearrange("p (k t) -> p k t", k=4), func=AF.Copy), [ps], [FT])
                kb.barrier()
            pi = 0
            ngr = (T + 511) // 512
            for gi in range(ngr):
                lo = gi * 512
                n = min(512, T - lo)
                for h in range(4):
                    ps = g.PS[pi % 4]; pi += 1
                    for c in range(2):
                        kb.op("pe", lambda e, c=c, h=h, ps=ps, lo=lo, n=n: e.matmul(ps[:, 0:n], lhsT=WQN[:, c, h, :], rhs=FT[:, c, lo:lo + n], start=(c == 0), stop=(c == 1)), [WQN, FT], [ps])
                    kb.op("act", lambda e, h=h, ps=ps, lo=lo, n=n: e.activation(out=QNT[:, h, lo:lo + n], in_=ps[:, 0:n], func=AF.Copy), [ps], [QNT])
                    ps = g.PS[pi % 4]; pi += 1
                    kb.op("pe", lambda e, h=h, ps=ps, lo=lo, n=n: e.matmul(ps[:, 0:n], lhsT=WKN[:, h, :], rhs=FT[:, 2, lo:lo + n], start=True, stop=True), [WKN, FT], [ps])
                    kb.op("dve", lambda e, h=h, ps=ps, lo=lo, n=n: e.tensor_copy(out=KNT[:, h, lo:lo + n], in_=ps[:, 0:n]), [ps], [KNT])
            with nc.sbuf_tensor(_nm(), [128, 2, 256], BF16) as qr_t, nc.sbuf_tensor(_nm(), [128, 2, 256], BF16) as qq_t, \
                    nc.sbuf_tensor(_nm(), [128, 4, 128], F32) as tm_t:
                tms = [Buf(tm_t[:, i], "tm%d" % i) for i in range(4)]
                qrs = [Buf(qr_t[:, i], "qr") for i in range(2)]; qqs = [Buf(qq_t[:, i], "qq") for i in range(2)]
                for tt in range(NT):
                    i = tt % 2
                    qr = qrs[i]; qq = qqs[i]
                    ps = g.PS[pi % 4]; pi += 1
                    kb.op("pe", lambda e, ps=ps, tt=tt: e.matmul(ps[:, 0:512], lhsT=FT[:, 2, tt * 128:(tt + 1) * 128], rhs=WV[:].rearrange("p h d -> p (h d)"), start=True, stop=True), [FT, WV], [ps])
                    kb.op("act", lambda e, ps=ps, tt=tt: e.activation(out=VB[:, tt, :, 0:128], in_=ps[:, 0:512].rearrange("p (h d) -> p h d", h=4), func=AF.Copy), [ps], [VB])
                    ps = g.PS[pi % 4]; pi += 1
                    for c in range(2):
                        kb.op("pe", lambda e, c=c, ps=ps, tt=tt: e.matmul(ps[:, 0:256], lhsT=FT[:, c, tt * 128:(tt + 1) * 128], rhs=WQR[:, c].rearrange("p h d -> p (h d)"), start=(c == 0), stop=(c == 1)), [FT, WQR], [ps])
                    kb.op("act", lambda e, ps=ps, qq=qq: e.activation(out=qq[:], in_=ps[:, 0:256], func=AF.Copy), [ps], [qq])
                    rope_tile(g, qq, qr, CS, tt, 4, tms)
                    ps = g.PS[7]
                    psb = ps[:].bitcast(BF16)
                    for c in range(2):
                        kb.op("pe", lambda e, c=c, qr=qr, psb=psb: e.transpose(out=psb[:, c * 128:(c + 1) * 128], in_=qr[:, c * 128:(c + 1) * 128], identity=g.ident_b[:]), [qr, g.ident_b], [ps])
                    kb.op("dve", lambda e, tt=tt, psb=psb: e.tensor_copy(out=QRT[:, :, tt * 128:(tt + 1) * 128], in_=psb[:, 0:256].rearrange("p (k t) -> p k t", k=2)), [ps], [QRT])
                kb.barrier()

        if "dbgB" in g.debug:
            dft = nc.dram_tensor("dbgFT", [128, 4, T], BF16, kind="ExternalOutput").ap()
            dqr = nc.dram_tensor("dbgQRT", [128, 2, T], BF16, kind="ExternalOutput").ap()
            dqn = nc.dram_tensor("dbgQNT", [128, 4, T], BF16, kind="ExternalOutput").ap()
            dkn = nc.dram_tensor("dbgKNT", [128, 4, T], BF16, kind="ExternalOutput").ap()
            dvb = nc.dram_tensor("dbgVB", [128, NT, 4, 129], BF16, kind="ExternalOutput").ap()
            dma(lambda e: e.dma_start(out=dft[:], in_=FT[:]), [FT], [])
            dma(lambda e: e.dma_start(out=dqr[:], in_=QRT[:]), [QRT], [])
            dma(lambda e: e.dma_start(out=dqn[:], in_=QNT[:]), [QNT], [])
            dma(lambda e: e.dma_start(out=dkn[:], in_=KNT[:]), [KNT], [])
            dma(lambda e: e.dma_start(out=dvb[:], in_=VB[:]), [VB], [])

        def qk_chunks(h, s):
            p0 = (h % 2) * 64
            return [(lambda lo, n: QNT[:, h, lo:lo + n], lambda kt: KNT[:, h, kt * 128:(kt + 1) * 128], [QNT, KNT]),
                    (lambda lo, n: QRT[p0:p0 + 64, h // 2, lo:lo + n], lambda kt: FT[p0:p0 + 64, 3, kt * 128:(kt + 1) * 128], [QRT, FT])]

        with nc.sbuf_tensor(_nm(), [128, 2, 4], F32) as r_t, nc.sbuf_tensor(_nm(), [128, 2, 4, 128], BF16) as o_t:
            cnt = [0]
            rs_ = [Buf(r_t[:, i], "r") for i in range(2)]; obs_ = [Buf(o_t[:, i], "ob") for i in range(2)]

            def post(h, q_lo, q_n, ofl):
                O1 = ofl[0]
                i = cnt[0] % 2
                cnt[0] += 1
                ns = q_n // 128
                r = rs_[i]; ob = obs_[i]
                kb.op("dve", lambda e: e.reciprocal(out=r[:, 0:ns], in_=O1[:, 0:ns, 128]), [O1], [r])
                kb.op("dve", lambda e: e.tensor_tensor(out=ob[:, 0:ns, :], in0=O1[:, 0:ns, 0:128], in1=r[:, 0:ns].unsqueeze(2).to_broadcast([128, ns, 128]), op=ALU.mult), [O1, r], [ob])
                t0 = q_lo // 128
                dma(lambda e: e.dma_start(out=OBv[:, t0:t0 + ns, 512 + h * 128:512 + (h + 1) * 128], in_=ob[:, 0:ns, :]), [ob], [g.OB])

            groups = [(i * 512, 512, list(range(NT))) for i in range(4)]
            if ctx_out:
                groups.append((2048, 256, [16, 17]))
            attention(g, 4, 128, 192.0 ** -0.5, VB, qk_chunks, groups, post, npass=1)
            kb.barrier()


def phase_mixer_d(g, l, ctx_out):
    nc, kb = g.nc, g.kb
    dma = lambda fn, r=(), w=(): kb.dma("sp", fn, r, w)
    Pv = g.P.t.rearrange("(t p) c -> p t c", p=128)
    OBv = g.OB.t.rearrange("(t p) c -> p t c", p=128)
    scale = 1.0 / 8.0
    with nc.sbuf_tensor(_nm(), [128, 6, T], BF16) as qkd_t, nc.sbuf_tensor(_nm(), [128, NT, 2, 65], BF16) as vd_t, \
            nc.sbuf_tensor(_nm(), [128, 8], F32) as es_t, nc.sbuf_tensor(_nm(), [128, 2, 128], BF16) as mk_t, \
            nc.sbuf_tensor(_nm(), [128, 2, 128], F32) as mkf_t:
        QKD = Buf(qkd_t, "QKD"); VD = Buf(vd_t, "VD"); ES = Buf(es_t, "ES"); MK = Buf(mk_t, "MK"); MKF = Buf(mkf_t, "MKF")
        kb.op("pool", lambda e: e.memset(VD[:, :, :, 64:65], 1.0), [], [VD])
        for tt in range(NT):
            dma(lambda e, tt=tt: e.dma_start(out=VD[:, tt, :, 0:64], in_=Pv[:, tt, 5184:5312].rearrange("p (h d) -> p h d", h=2)), [g.P], [VD])
        dma(lambda e: e.dma_start(out=MKF[:, 0, :], in_=g.mprev_in[:, :]), [], [MKF])
        dma(lambda e: e.dma_start(out=MKF[:, 1, :], in_=g.mnext_in[:, :]), [], [MKF])
        kb.op("dve", lambda e: e.tensor_copy(out=MK[:], in_=MKF[:]), [MKF], [MK])
        dma(lambda e: e.dma_start(out=ES[:], in_=g.W["win_sink"][l:l + 1, :].partition_broadcast(128)), [], [ES])
        kb.op("act", lambda e: e.activation(out=ES[:], in_=ES[:], func=AF.Exp), [ES], [ES])
        with nc.sbuf_tensor(_nm(), [128, 2, 640], BF16) as pd_t, nc.sbuf_tensor(_nm(), [128, 2, 768], BF16) as rb_t, \
                nc.sbuf_tensor(_nm(), [128, 4, 320], F32) as tm_t, nc.sbuf_tensor(_nm(), [128, NT, 64], F32) as cs_t:
            CS = Buf(cs_t, "CS")
            dma(lambda e: e.dma_start(out=CS[:], in_=g.rope_cs.rearrange("(t p) c -> p t c", p=128)), [], [CS])
            pds = [Buf(pd_t[:, i], "pd%d" % i) for i in range(2)]
            rbs = [Buf(rb_t[:, i], "rb%d" % i) for i in range(2)]
            tms = [Buf(tm_t[:, i], "tm%d" % i) for i in range(4)]
            for tt in range(NT if "d_skip_rope" not in g.debug else 0):
                pd = pds[tt % 2]; rb = rbs[tt % 2]
                dma(lambda e, pd=pd, tt=tt: e.dma_start(out=pd[:], in_=Pv[:, tt, 4544:5184]), [g.P], [pd])
                rope_tile(g, pd, rb, CS, tt, 10, tms)
                kb.op("dve", lambda e, rb=rb: e.tensor_copy(out=rb[:, 640:768].rearrange("p (u d) -> p u d", u=2),
                                                           in_=rb[:, 576:640].unsqueeze(1).to_broadcast([128, 2, 64])), [rb], [rb])
                kb.op("dve", lambda e, rb=rb: e.tensor_copy(out=rb[:, 576:640], in_=rb[:, 512:576]), [rb], [rb])
                ps = g.PS[7]
                psb = ps[:].bitcast(BF16)
                for c in range(6):
                    kb.op("pe", lambda e, c=c, rb=rb, psb=psb: e.transpose(out=psb[:, c * 128:(c + 1) * 128], in_=rb[:, c * 128:(c + 1) * 128], identity=g.ident_b[:]), [rb, g.ident_b], [ps])
                kb.op("act", lambda e, tt=tt, psb=psb: e.activation(out=QKD[:, :, tt * 128:(tt + 1) * 128], in_=psb[:, 0:768].rearrange("p (k t) -> p k t", k=6), func=AF.Copy), [ps], [QKD])
            kb.barrier()
        with nc.sbuf_tensor(_nm(), [128, 3, 512], BF16) as E_t, nc.sbuf_tensor(_nm(), [128, 2, 8, 65], F32) as of_t, \
                nc.sbuf_tensor(_nm(), [128, 2, 2, 8], F32) as r_t, nc.sbuf_tensor(_nm(), [128, 2, 8, 64], BF16) as o_t:
            Es = [Buf(E_t[:, i], "E%d" % i) for i in range(3)]
            Ofs = [Buf(of_t[:, i], "Of%d" % i) for i in range(2)]
            rs_ = [Buf(r_t[:, i], "r%d" % i) for i in range(2)]
            obs_ = [Buf(o_t[:, i], "ob%d" % i) for i in range(2)]
            qtiles = list(range(16)) + ([16, 17] if ctx_out else [])
            if "d_prep_only" in g.debug:
                qtiles = []
            iters = []
            for qn_, qi in enumerate(qtiles):
                if qi < 16:
                    kts = ([(qi - 1, 0)] if qi > 0 else []) + [(qi, None)] + ([(qi + 1, 1)] if qi < 15 else []) + [(16, None), (17, None)]
                else:
                    kts = [(16, None), (17, None)]
                for kvh in range(2):
                    iters.append((qn_, qi, kvh, kts))
            steps = [(ii, ki) for ii, it_ in enumerate(iters) for ki in range(len(it_[3]))]
            Spairs = [(g.PS[0], g.PS[1]), (g.PS[2], g.PS[7])]

            def emit_S(i):
                ii, ki = steps[i]
                qn_, qi, kvh, kts = iters[ii]
                kt, mk = kts[ki]
                Sp = Spairs[i % 2]
                for gq in range(4):
                    hq = kvh * 4 + gq
                    p0 = (hq % 2) * 64
                    S = Sp[gq % 2]
                    c0 = (gq // 2) * 128
                    kb.op("pe", lambda e, S=S, c0=c0, p0=p0, hq=hq: e.matmul(
                        S[:, c0:c0 + 128], lhsT=QKD[p0:p0 + 64, 4 + kvh, kt * 128:(kt + 1) * 128],
                        rhs=QKD[p0:p0 + 64, hq // 2, qi * 128:(qi + 1) * 128], start=True, stop=True, skip_group_check=True), [QKD], [S])

            def emit_rest(i):
                ii, ki = steps[i]
                qn_, qi, kvh, kts = iters[ii]
                kt, mk = kts[ki]
                Sp = Spairs[i % 2]
                E = Es[i % 3]
                Of = Ofs[qn_ % 2]; r = rs_[qn_ % 2]; ob = obs_[qn_ % 2]
                O = g.PS[3 + ii % 4]
                for j in range(2):
                    kb.op("act", lambda e, j=j: e.activation(out=E[:, j * 256:(j + 1) * 256], in_=Sp[j][:, 0:256], func=AF.Exp, scale=scale), [Sp[j]], [E])
                if mk is not None:
                    kb.op("dve", lambda e: e.tensor_tensor(out=E[:].rearrange("p (g q) -> p g q", g=4), in0=E[:].rearrange("p (g q) -> p g q", g=4),
                                                           in1=MKF[:, mk, :].unsqueeze(1).to_broadcast([128, 4, 128]), op=ALU.mult), [E, MKF], [E])
                for gq in range(4):
                    ei = (gq % 2) * 2 + gq // 2
                    kb.op("pe", lambda e, gq=gq, ei=ei: e.matmul(
                        O[:, gq * 65:(gq + 1) * 65], lhsT=E[:, ei * 128:(ei + 1) * 128], rhs=VD[:, kt, kvh, :],
                        start=(ki == 0 and gq == 0), stop=(ki == len(kts) - 1), skip_group_check=True), [E, VD], [O])
                if ki == len(kts) - 1:
                    kb.op("act", lambda e: e.activation(out=Of[:, kvh * 4:(kvh + 1) * 4, :], in_=O[:, 0:260].rearrange("p (g d) -> p g d", g=4), func=AF.Copy), [O], [Of])
                    if kvh == 1:
                        kb.op("dve", lambda e: e.tensor_tensor(out=r[:, 0, :], in0=Of[:, :, 64], in1=ES[:], op=ALU.add), [Of, ES], [r])
                        kb.op("dve", lambda e: e.reciprocal(out=r[:, 1, :], in_=r[:, 0, :]), [r], [r])
                        kb.op("dve", lambda e: e.tensor_tensor(out=ob[:], in0=Of[:, :, 0:64], in1=r[:, 1, :].unsqueeze(2).to_broadcast([128, 8, 64]), op=ALU.mult), [Of, r], [ob])
                        dma(lambda e: e.dma_start(out=OBv[:, qi, 1536:2048], in_=ob[:].rearrange("p h d -> p (h d)")), [ob], [g.OB])

            if steps:
                emit_S(0)
            for i in range(len(steps)):
                if i + 1 < len(steps):
                    emit_S(i + 1)
                emit_rest(i)
                if i % 4 == 3:
                    bgstep(g)
            kb.barrier()


def phase_mixer_c(g, l, ctx_out):
    nc, kb = g.nc, g.kb
    from contextlib import ExitStack
    dma = lambda fn, r=(), w=(): kb.dma("sp", fn, r, w)
    orders = [[32, 33, 34, 35] + list(range(32)), [35, 34, 33, 32] + list(range(31, -1, -1))]
    ODST = [g.OF, g.OBK]
    with ExitStack() as es:
        al = lambda shape, dt: es.enter_context(nc.sbuf_tensor(_nm(), shape, dt))
        TRI = Buf(al([64, 4, 64], F32), "TRI"); LB = Buf(al([64, 2, 2, 512], F32), "LB")
        S_t = al([128, 2, 4, 128], F32); Sb_t = al([128, 2, 4, 128], BF16)
        Ss = [Buf(S_t[:, d], "S%d" % d) for d in range(2)]; Sbs = [Buf(Sb_t[:, d], "Sb%d" % d) for d in range(2)]
        dma(lambda e: e.dma_start(out=TRI[:], in_=g.tri_in.rearrange("p (a t) -> p a t", a=4)), [], [TRI])
        if l == 0:
            kb.op("dve", lambda e: e.memset(LB[:, :, 0, :], 0.0), [], [LB])
            kb.op("dve", lambda e: e.memset(LB[:, :, 1, :], 1.0), [], [LB])
        else:
            for d in range(2):
                dma(lambda e, d=d: e.dma_start(out=LB[:, d, 0, :], in_=g.W["hgrn_lb"][d, 1:2, :].partition_broadcast(64)), [], [LB])
                dma(lambda e, d=d: e.dma_start(out=LB[:, d, 1, :], in_=g.W["hgrn_lb"][d, 0:1, :].partition_broadcast(64)), [], [LB])
                kb.op("dve", lambda e, d=d: e.tensor_tensor(out=LB[:, d, 0, :], in0=LB[:, d, 0, :], in1=LB[:, d, 1, :], op=ALU.subtract), [LB], [LB])
                kb.op("act", lambda e, d=d: e.activation(out=LB[:, d, 0, :], in_=LB[:, d, 0, :], func=AF.Sigmoid), [LB], [LB])
                kb.op("dve", lambda e, d=d: e.tensor_scalar(out=LB[:, d, 1, :], in0=LB[:, d, 0, :], scalar1=-1.0, scalar2=1.0, op0=ALU.mult, op1=ALU.add), [LB], [LB])
        NB = 4
        pc_t = al([64, NB, 2560], BF16); f_t = al([64, NB, 512], F32); lf_t = al([64, NB, 512], F32); kf_t = al([64, NB, 512], BF16)
        bm_t = al([128, NB, 256], F32); e12_t = al([128, NB, 2, 256], F32); em_t = al([128, NB, 8], F32); qf_t = al([128, NB, 256], F32)
        qk_t = al([128, NB, 3, 256], BF16); at_t = al([64, NB, 256], BF16); ed_t = al([64, NB, 512], F32); kh_t = al([64, NB, 512], BF16)
        of_t = al([64, NB, 512], F32)
        mk = lambda t, nm: [Buf(t[:, i], nm + str(i)) for i in range(NB)]
        PCs, Fs, LFs, KFs, BMs, E12s, EMs, QFs, QKs, ATs, EDs, KHs, OFs = [mk(t, n) for t, n in [
            (pc_t, "pc"), (f_t, "f"), (lf_t, "lf"), (kf_t, "kf"), (bm_t, "bm"), (e12_t, "e12"), (em_t, "em"), (qf_t, "qf"),
            (qk_t, "qk"), (at_t, "at"), (ed_t, "ed"), (kh_t, "kh"), (of_t, "of")]]
        psall = g.psall
        BPs = [Buf(psall[:, d, 0:256], "Bp%d" % d) for d in range(2)]
        TPs = [Buf(psall[:, 4 + d, 0:256], "Tp%d" % d) for d in range(2)]
        for d in range(2):
            kb.op("dve", lambda e, d=d: e.memset(Ss[d][:], 0.0), [], [Ss[d]])
            kb.op("dve", lambda e, d=d: e.memset(Sbs[d][:], 0.0), [], [Sbs[d]])
        for a_ in ATs:
            kb.op("dve", lambda e, a_=a_: e.memset(a_[:], 0.0), [], [a_])

        def stage_b(d, S, Sb, pc, qk, at, kh, em, of, need_out, r0):
            if need_out:
                Op = g.PS[6]
                for h in range(4):
                    kb.op("pe", lambda e, h=h: e.matmul(Op[0:64, h * 128:(h + 1) * 128], lhsT=qk[:, 1, h * 64:(h + 1) * 64], rhs=Sb[:, h, :],
                                                        start=(h == 0), stop=False, skip_group_check=True), [qk, Sb], [Op])
                    yield
                    kb.op("pe", lambda e, h=h: e.matmul(Op[0:64, h * 128:(h + 1) * 128], lhsT=at[:, h * 64:(h + 1) * 64], rhs=pc[:, 1536 + h * 128:1536 + (h + 1) * 128],
                                                        start=False, stop=True, skip_group_check=True), [at, pc], [Op])
                    yield
            Up = g.PS[7]
            for h in range(4):
                kb.op("pe", lambda e, h=h: e.matmul(Up[:, h * 128:(h + 1) * 128], lhsT=kh[:, h * 128:(h + 1) * 128], rhs=pc[:, 1536 + h * 128:1536 + (h + 1) * 128],
                                                    start=True, stop=True, skip_group_check=True), [kh, pc], [Up])
                yield
            for h in range(4):
                kb.op("dve", lambda e, h=h: e.scalar_tensor_tensor(out=S[:, h, :], in0=S[:, h, :], scalar=em[:, h:h + 1], in1=Up[:, h * 128:(h + 1) * 128],
                                                                   op0=ALU.mult, op1=ALU.add), [S, em, Up], [S])
                yield
            kb.op("pool", lambda e: e.tensor_copy(out=Sb[:], in_=S[:]), [S], [Sb])
            yield
            if need_out:
                kb.op("act", lambda e: e.activation(out=of[:], in_=Op[0:64, :], func=AF.Copy), [Op], [of])
                yield
                dma(lambda e: e.dma_start(out=ODST[d][r0:r0 + 64, :], in_=of[:]), [of], [ODST[d]])
                yield


        def step(d, ci, part):
            ch = orders[d][ci]
            ti_incl, ti_strict = (0, 2) if d == 0 else (1, 3)
            t_end, t_mid = (63, 31) if d == 0 else (0, 32)
            zc = 512 if d == 0 else 1024
            S, Sb = Ss[d], Sbs[d]
            i = d * 2 + (ci % 2)
            pc, f, lf, kf, bm, e12, em, qf, qk, at, ed, kh, of = [x[i] for x in (PCs, Fs, LFs, KFs, BMs, E12s, EMs, QFs, QKs, ATs, EDs, KHs, OFs)]
            need_out = (ch < 32) or ctx_out
            r0 = ch * 64
            if part == 1:
                yield from stage_b(d, S, Sb, pc, qk, at, kh, em, of, need_out, r0)
                return
            dma(lambda e: e.dma_start(out=pc[:], in_=g.P[r0:r0 + 64, 1984:4544]), [g.P], [pc])
            yield
            yield
            kb.op("act", lambda e: e.activation(out=f[:], in_=pc[:, zc:zc + 512], func=AF.Sigmoid), [pc], [f])
            yield
            kb.op("dve", lambda e: e.tensor_tensor(out=f[:], in0=f[:], in1=LB[:, d, 1, :], op=ALU.mult), [f, LB], [f])
            yield
            kb.op("dve", lambda e: e.tensor_tensor(out=f[:], in0=f[:], in1=LB[:, d, 0, :], op=ALU.add), [f, LB], [f])
            yield
            kb.op("act", lambda e: e.activation(out=lf[:], in_=f[:], func=AF.Ln), [f], [lf])
            yield
            kb.op("pool", lambda e: e.tensor_scalar(out=kf[:], in0=f[:], scalar1=-1.0, scalar2=1.0, op0=ALU.mult, op1=ALU.add), [f], [kf])
            yield
            yield
            Bp = BPs[d]
            for h in range(4):
                kb.op("pe", lambda e, h=h: e.matmul(Bp[:, h * 64:(h + 1) * 64], lhsT=lf[:, h * 128:(h + 1) * 128], rhs=TRI[:, ti_incl, :],
                                                    start=True, stop=True, skip_group_check=True), [lf, TRI], [Bp])
            Dp = g.PS[2 + d]
            kb.op("pe", lambda e: e.matmul(Dp[0:64, :], lhsT=TRI[:, ti_strict, :], rhs=lf[:], start=True, stop=True), [lf, TRI], [Dp])
            yield
            yield
            Tp = TPs[d]
            tpb = Tp[:].bitcast(BF16)
            for h in range(4):
                kb.op("pe", lambda e, h=h: e.transpose(out=tpb[:, h * 64:(h + 1) * 64], in_=pc[:, h * 128:(h + 1) * 128], identity=g.ident_b[0:64, 0:64]), [pc, g.ident_b], [Tp])
            for h in range(4):
                kb.op("pe", lambda e, h=h: e.transpose(out=tpb[:, 256 + h * 64:256 + (h + 1) * 64], in_=kf[:, h * 128:(h + 1) * 128], identity=g.ident_b[0:64, 0:64]), [kf, g.ident_b], [Tp])
            b3 = Bp[:].rearrange("p (h t) -> p h t", h=4)
            kb.op("act", lambda e: e.activation(out=em[:, 0:4], in_=b3[:, :, t_mid], func=AF.Copy), [Bp], [em])
            yield
            kb.op("dve", lambda e: e.tensor_tensor(out=bm[:].rearrange("p (h t) -> p h t", h=4), in0=b3, in1=em[:, 0:4].unsqueeze(2).to_broadcast([128, 4, 64]), op=ALU.subtract), [Bp, em], [bm])
            yield
            kb.op("act", lambda e: e.activation(out=e12[:, 0, :], in_=bm[:], func=AF.Exp), [bm], [e12])
            yield
            kb.op("act", lambda e: e.activation(out=e12[:, 1, :], in_=bm[:], func=AF.Exp, scale=-1.0), [bm], [e12])
            yield
            kb.op("act", lambda e: e.activation(out=em[:, 4:8], in_=em[:, 0:4], func=AF.Exp), [em], [em])
            yield
            kb.op("act", lambda e: e.activation(out=em[:, 0:4], in_=b3[:, :, t_end], func=AF.Exp), [Bp], [em])
            yield
            yield
            kb.op("dve", lambda e: e.tensor_tensor(out=qf[:], in0=tpb[:, 0:256], in1=e12[:, 0, :], op=ALU.mult), [Tp, e12], [qf])
            yield
            kb.op("pool", lambda e: e.tensor_copy(out=qk[:, 0, :], in_=qf[:]), [qf], [qk])
            yield
            kb.op("dve", lambda e: e.tensor_tensor(out=qk[:, 1, :].rearrange("p (h t) -> p h t", h=4), in0=qf[:].rearrange("p (h t) -> p h t", h=4),
                                                   in1=em[:, 4:8].unsqueeze(2).to_broadcast([128, 4, 64]), op=ALU.mult), [qf, em], [qk])
            yield
            kb.op("dve", lambda e: e.tensor_tensor(out=qk[:, 2, :], in0=tpb[:, 256:512], in1=e12[:, 1, :], op=ALU.mult), [Tp, e12], [qk])
            yield
            yield
            yield
            Ap = Tp
            if d == 0:
                parts = [((0, 32), (0, 64)), ((32, 64), (32, 64))]
            else:
                parts = [((32, 64), (0, 64)), ((0, 32), (0, 32))]
            for h in range(4):
                for (s0, s1), (c0, c1) in parts:
                    kb.op("pe", lambda e, h=h, s0=s0, s1=s1, c0=c0, c1=c1: e.matmul(
                        Ap[s0:s1, h * 64 + c0:h * 64 + c1], lhsT=qk[:, 2, h * 64 + s0:h * 64 + s1], rhs=qk[:, 0, h * 64 + c0:h * 64 + c1],
                        start=True, stop=True, skip_group_check=True), [qk], [Ap])
            for (s0, s1), (c0, c1) in parts:
                kb.op("dve", lambda e, s0=s0, s1=s1, c0=c0, c1=c1: e.tensor_tensor(
                    out=at[s0:s1, :].rearrange("p (h t) -> p h t", h=4)[:, :, c0:c1],
                    in0=Ap[s0:s1, 0:256].rearrange("p (h t) -> p h t", h=4)[:, :, c0:c1],
                    in1=TRI[s0:s1, ti_incl, c0:c1].unsqueeze(1).to_broadcast([s1 - s0, 4, c1 - c0]), op=ALU.mult), [Ap, TRI], [at])
            yield
            kb.op("act", lambda e: e.activation(out=ed[:], in_=Dp[0:64, :], func=AF.Exp), [Dp], [ed])
            yield
            kb.op("pool", lambda e: e.tensor_tensor(out=kh[:], in0=kf[:], in1=ed[:], op=ALU.mult), [kf, ed], [kh])
            yield

        def lockstep(gens):
            gens = list(gens)
            while gens:
                for g_ in list(gens):
                    try:
                        next(g_)
                    except StopIteration:
                        gens.remove(g_)

        for ci in range(37):
            bgstep(g, 2)
            if ci < 36:
                if "c_nolock" in g.debug:
                    lockstep([step(0, ci, 0)])
                    lockstep([step(1, ci, 0)])
                else:
                    lockstep([step(0, ci, 0), step(1, ci, 0)])
            if ci >= 1:
                lockstep([step(0, ci - 1, 1)])
                lockstep([step(1, ci - 1, 1)])
        kb.barrier()
    with ExitStack() as es:
        al = lambda shape, dt: es.enter_context(nc.sbuf_tensor(_nm(), shape, dt))
        NGh = Buf(al([128, 128], F32), "NGh")
        dma(lambda e: e.dma_start(out=NGh[:], in_=g.W["hgrn_norm_g"][l:l + 1, :].partition_broadcast(128)), [], [NGh])
        a_t = al([128, 2, 512], F32); b_t = al([128, 2, 512], F32); gt_t = al([128, 2, 512], BF16); ro_t = al([128, 2, 3, 512], F32)
        rr_t = al([128, 2, 8], F32); oo_t = al([128, 2, 512], BF16)
        mk2 = lambda t, nm: [Buf(t[:, i], nm + str(i)) for i in range(2)]
        As, Bs, GTs, ROs, RRs, OOs = mk2(a_t, "a"), mk2(b_t, "b"), mk2(gt_t, "gt"), mk2(ro_t, "ro"), mk2(rr_t, "rr"), mk2(oo_t, "oo")
        for tt in range(NT if ctx_out else 16):
            i = tt % 2
            a, b, gt, ro, rr, oo = As[i], Bs[i], GTs[i], ROs[i], RRs[i], OOs[i]
            r0 = tt * 128
            dma(lambda e, a=a, r0=r0: e.dma_start(out=a[:], in_=g.OF[r0:r0 + 128, :]), [g.OF], [a])
            dma(lambda e, b=b, r0=r0: e.dma_start(out=b[:], in_=g.OBK[r0:r0 + 128, :]), [g.OBK], [b])
            dma(lambda e, gt=gt, r0=r0: e.dma_start(out=gt[:], in_=g.P[r0:r0 + 128, 4032:4544]), [g.P], [gt])
            o3 = lambda bb, j: bb[:, j, :].rearrange("p (h v) -> p h v", h=4)
            kb.op("dve", lambda e, a=a, b=b, ro=ro: e.tensor_tensor(out=ro[:, 0, :], in0=a[:], in1=b[:], op=ALU.add), [a, b], [ro])
            kb.op("pool", lambda e, ro=ro: e.tensor_tensor(out=ro[:, 1, :], in0=ro[:, 0, :], in1=ro[:, 0, :], op=ALU.mult), [ro], [ro])
            kb.op("dve", lambda e, ro=ro, rr=rr: e.tensor_reduce(out=rr[:, 0:4], in_=o3(ro, 1), axis=AX.X, op=ALU.add), [ro], [rr])
            kb.op("dve", lambda e, rr=rr: e.tensor_scalar(out=rr[:, 0:4], in0=rr[:, 0:4], scalar1=1.0 / 128, scalar2=EPS, op0=ALU.mult, op1=ALU.add), [rr], [rr])
            kb.op("act", lambda e, rr=rr: e.activation(out=rr[:, 4:8], in_=rr[:, 0:4], func=AF.Sqrt), [rr], [rr])
            kb.op("dve", lambda e, rr=rr: e.reciprocal(out=rr[:, 0:4], in_=rr[:, 4:8]), [rr], [rr])
            kb.op("dve", lambda e, ro=ro, rr=rr: e.tensor_tensor(out=o3(ro, 1), in0=o3(ro, 0), in1=rr[:, 0:4].unsqueeze(2).to_broadcast([128, 4, 128]), op=ALU.mult), [ro, rr], [ro])
            kb.op("act", lambda e, ro=ro, gt=gt: e.activation(out=ro[:, 2, :], in_=gt[:], func=AF.Silu), [gt], [ro])
            kb.op("pool", lambda e, ro=ro: e.tensor_tensor(out=o3(ro, 0), in0=o3(ro, 1), in1=NGh[:].unsqueeze(1).to_broadcast([128, 4, 128]), op=ALU.mult), [ro, NGh], [ro])
            kb.op("dve", lambda e, ro=ro, oo=oo: e.tensor_tensor(out=oo[:], in0=ro[:, 0, :], in1=ro[:, 2, :], op=ALU.mult), [ro], [oo])
            dma(lambda e, oo=oo, r0=r0: e.dma_start(out=g.OB[r0:r0 + 128, 1024:1536], in_=oo[:]), [oo], [g.OB])
        kb.barrier()


def phase_merge(g, l, ctx_out):
    nc, kb = g.nc, g.kb
    from contextlib import ExitStack
    dma = lambda fn, r=(), w=(): kb.dma("sp", fn, r, w)
    Pv = g.P.t.rearrange("(t p) c -> p t c", p=128)
    OBv = g.OB.t.rearrange("(t p) c -> p t c", p=128)
    with ExitStack() as es:
        al = lambda shape, dt: es.enter_context(nc.sbuf_tensor(_nm(), shape, dt))
        WBR = Buf(al([128, 16, 1024], BF16), "WBR"); WO = Buf(al([128, 8, 1024], BF16), "WO")
        G1 = [Buf(al([128, 1024], F32), "G1x"), Buf(al([128, 1024], F32), "G1c")]
        ob_t = al([128, 2, 2048], BF16); gt_t = al([128, 2, 4096], BF16); obt_t = al([128, 2, 16, 128], BF16)
        sg_t = al([128, 3, 512], F32); tp_t = al([128, 3, 512], F32); y_t = al([128, 2, 1024], F32)
        yb_t = al([128, 2, 1024], BF16); yt_t = al([128, 2, 8, 128], BF16)
        mk = lambda t, nm, n: [Buf(t[:, i], nm + str(i)) for i in range(n)]
        OBs, GTs, OBTs, SGs, TPs, Ys, YBs, YTs = mk(ob_t, "ob", 2), mk(gt_t, "gt", 2), mk(obt_t, "obt", 2), mk(sg_t, "sg", 3), mk(tp_t, "tp", 3), mk(y_t, "y", 2), mk(yb_t, "yb", 2), mk(yt_t, "yt", 2)
        wsrc = g.W["w_branch"][l].rearrange("j (c p) n -> p j c n", p=128)
        for j in range(4):
            kb.dma("pool", lambda e, j=j: e.dma_start(out=WBR[:, j * 4:(j + 1) * 4, :], in_=wsrc[:, j]), [], [WBR])
        wo = g.W["w_out"][l].rearrange("(k p) n -> p k n", p=128)
        for j in range(2):
            kb.dma("pool", lambda e, j=j: e.dma_start(out=WO[:, j * 4:(j + 1) * 4, :], in_=wo[:, j * 4:(j + 1) * 4, :]), [], [WO])
        load_bcast(g, G1[0], g.MODS[l, 0:1, 2 * D:3 * D])
        load_bcast(g, G1[1], g.MODS[l, 1:2, 2 * D:3 * D])
        pi = 0
        si = 0
        for tt in range(NT if ctx_out else 16):
            i = tt % 2
            ob, gt, obt, y, yb, yt = OBs[i], GTs[i], OBTs[i], Ys[i], YBs[i], YTs[i]
            G = G1[0] if tt < 16 else G1[1]
            dma(lambda e, ob=ob, tt=tt: e.dma_start(out=ob[:], in_=OBv[:, tt, :]), [g.OB], [ob])
            dma(lambda e, gt=gt, tt=tt: e.dma_start(out=gt[:], in_=Pv[:, tt, 5312:9408]), [g.P], [gt])
            for hf in range(2):
                ps = g.PS[6 + hf]
                psb = ps[:].bitcast(BF16)
                for c in range(8):
                    kb.op("pe", lambda e, c=c, hf=hf, psb=psb, ob=ob: e.transpose(out=psb[:, c * 128:(c + 1) * 128], in_=ob[:, (hf * 8 + c) * 128:(hf * 8 + c + 1) * 128], identity=g.ident_b[:]), [ob, g.ident_b], [ps])
                kb.op("act", lambda e, hf=hf, psb=psb, obt=obt: e.activation(out=obt[:, hf * 8:(hf + 1) * 8, :], in_=psb.rearrange("p (k t) -> p k t", k=8), func=AF.Copy), [ps], [obt])
            for hf in range(2):
                for j in range(4):
                    ps = g.PS[pi % 4]; pi += 1
                    for c in range(4):
                        kb.op("pe", lambda e, c=c, j=j, hf=hf, ps=ps, obt=obt: e.matmul(ps[:], lhsT=obt[:, j * 4 + c, :], rhs=WBR[:, j * 4 + c, hf * 512:(hf + 1) * 512], start=(c == 0), stop=(c == 3)), [obt, WBR], [ps])
                    sg = SGs[si % 3]; tp = TPs[si % 3]; si += 1
                    kb.op("act", lambda e, sg=sg, gt=gt, j=j, hf=hf: e.activation(out=sg[:], in_=gt[:, j * 1024 + hf * 512:j * 1024 + (hf + 1) * 512], func=AF.Sigmoid), [gt], [sg])
                    ysl = (slice(None), slice(hf * 512, (hf + 1) * 512))
                    if j == 0:
                        kb.op("dve", lambda e, ps=ps, sg=sg, y=y, ysl=ysl: e.tensor_tensor(out=y[ysl], in0=ps[:], in1=sg[:], op=ALU.mult), [ps, sg], [y])
                    else:
                        kb.op("dve", lambda e, ps=ps, sg=sg, tp=tp: e.tensor_tensor(out=tp[:], in0=ps[:], in1=sg[:], op=ALU.mult), [ps, sg], [tp])
                        if j < 3:
                            kb.op("pool", lambda e, tp=tp, y=y, ysl=ysl: e.tensor_tensor(out=y[ysl], in0=y[ysl], in1=tp[:], op=ALU.add), [y, tp], [y])
                        else:
                            kb.op("pool", lambda e, tp=tp, y=y, yb=yb, ysl=ysl: e.tensor_tensor(out=yb[ysl], in0=y[ysl], in1=tp[:], op=ALU.add), [y, tp], [yb])
            ps = g.PS[6]
            psb = ps[:].bitcast(BF16)
            for c in range(8):
                kb.op("pe", lambda e, c=c, psb=psb, yb=yb: e.transpose(out=psb[:, c * 128:(c + 1) * 128], in_=yb[:, c * 128:(c + 1) * 128], identity=g.ident_b[:]), [yb, g.ident_b], [ps])
            kb.op("act", lambda e, psb=psb, yt=yt: e.activation(out=yt[:], in_=psb.rearrange("p (k t) -> p k t", k=8), func=AF.Copy), [ps], [yt])
            for hf in range(2):
                ps = g.PS[pi % 4]; pi += 1
                for k in range(8):
                    kb.op("pe", lambda e, k=k, hf=hf, ps=ps, yt=yt: e.matmul(ps[:], lhsT=yt[:, k, :], rhs=WO[:, k, hf * 512:(hf + 1) * 512], start=(k == 0), stop=(k == 7)), [yt, WO], [ps])
                tp = TPs[si % 3]; si += 1
                xs = g.X[tt]
                kb.op("dve", lambda e, ps=ps, tp=tp, G=G, hf=hf: e.tensor_tensor(out=tp[:], in0=ps[:], in1=G[:, hf * 512:(hf + 1) * 512], op=ALU.mult), [ps, G], [tp])
                kb.op("pool", lambda e, tp=tp, xs=xs, hf=hf: e.tensor_tensor(out=xs[:, hf * 512:(hf + 1) * 512], in0=xs[:, hf * 512:(hf + 1) * 512], in1=tp[:], op=ALU.add), [xs, tp], [xs])
        kb.barrier()


def phase_final(g):
    nc, kb = g.nc, g.kb
    from contextlib import ExitStack
    dma = lambda fn, r=(), w=(): kb.dma("sp", fn, r, w)
    with ExitStack() as es:
        al = lambda shape, dt: es.enter_context(nc.sbuf_tensor(_nm(), shape, dt))
        FG = Buf(al([128, 1024], F32), "FG")
        junk = Buf(al([128, 1024], F32), "junk")
        o_t = al([128, 2, 1024], F32); st_t = al([128, 2, 8], F32)
        Os = [Buf(o_t[:, i], "o%d" % i) for i in range(2)]; Ss = [Buf(st_t[:, i], "s%d" % i) for i in range(2)]
        dma(lambda e: e.dma_start(out=FG[:], in_=g.W["final_g"][0:1, :].partition_broadcast(128)), [], [FG])
        for tt in range(16):
            xin = g.X[tt]; st = Ss[tt % 2]; o = Os[tt % 2]
            kb.op("act", lambda e, xin=xin, st=st: e.activation(out=junk[:], in_=xin[:], func=AF.Square, accum_out=st[:, 0:1]), [xin], [junk, st])
            kb.op("dve", lambda e, st=st: e.tensor_scalar(out=st[:, 1:2], in0=st[:, 0:1], scalar1=1.0 / D, scalar2=EPS, op0=ALU.mult, op1=ALU.add), [st], [st])
            kb.op("act", lambda e, st=st: e.activation(out=st[:, 2:3], in_=st[:, 1:2], func=AF.Sqrt), [st], [st])
            kb.op("dve", lambda e, st=st: e.reciprocal(out=st[:, 3:4], in_=st[:, 2:3]), [st], [st])
            kb.op("dve", lambda e, xin=xin, st=st, o=o: e.scalar_tensor_tensor(out=o[:], in0=xin[:], scalar=st[:, 3:4], in1=FG[:], op0=ALU.mult, op1=ALU.mult), [xin, st, FG], [o])
            dma(lambda e, o=o, tt=tt: e.dma_start(out=g.out[tt * 128:(tt + 1) * 128, :], in_=o[:]), [o], [])
        kb.barrier()


def phase_peer(g, l, ctx_out):
    nc, kb = g.nc, g.kb
    from contextlib import ExitStack
    dma = lambda fn, r=(), w=(): kb.dma("sp", fn, r, w)
    NEG = -1.0e30
    uv_flat = g.UV.t
    H2P = g.PSP[2]
    with ExitStack() as es:
        al = lambda shape, dt: es.enter_context(nc.sbuf_tensor(_nm(), shape, dt))
        WQ = Buf(al([128, 8, 1024], BF16), "WQ"); KT = Buf(al([128, 8, 128], BF16), "KT")
        GS = Buf(al([128, 1024], F32), "GS"); SH = Buf(al([128, 1024], F32), "SH")
        G2s = [Buf(al([128, 1024], F32), "G2x"), Buf(al([128, 1024], F32), "G2c")]
        IO16 = Buf(al([128, 16], F32), "IO16")
        kb.op("pool", lambda e: e.iota(IO16[:], pattern=[[1, 16]], base=0, channel_multiplier=0, allow_small_or_imprecise_dtypes=True), [], [IO16])
        wq = g.W["peer_wq"][l].rearrange("(k p) n -> p k n", p=128)
        for j in range(2):
            kb.dma("pool", lambda e, j=j: e.dma_start(out=WQ[:, j * 4:(j + 1) * 4, :], in_=wq[:, j * 4:(j + 1) * 4, :]), [], [WQ])
        junk = Buf(al([128, 1024], F32), "junk"); junkb = Buf(al([128, 1024], BF16), "junkb")
        keyf = junk[:].rearrange("p (a d) -> p a d", a=16); keyb = junkb[:].rearrange("p (a d) -> p a d", a=16)
        dma(lambda e: e.dma_start(out=keyf, in_=g.W["peer_keys"][l].rearrange("h p n d -> n (h p) d")), [], [junk])
        kb.op("dve", lambda e: e.tensor_copy(out=keyb, in_=keyf), [junk], [junkb])
        ps = g.PS[0]
        psb = ps[:].bitcast(BF16)
        for h in range(8):
            kb.op("pe", lambda e, h=h: e.transpose(out=psb[:, h * 128:(h + 1) * 128], in_=keyb[:, h * 2:(h + 1) * 2, :].rearrange("n a d -> n (a d)"), identity=g.ident_b[:]), [junkb, g.ident_b], [ps])
        kb.op("act", lambda e: e.activation(out=KT[:], in_=psb.rearrange("p (h n) -> p h n", h=8), func=AF.Copy), [ps], [KT])
        load_bcast(g, G2s[0], g.MODS[l, 0:1, 5 * D:6 * D])
        if ctx_out:
            load_bcast(g, G2s[1], g.MODS[l, 1:2, 5 * D:6 * D])
        hb = Buf(al([128, 1024], BF16), "hb"); hT = Buf(al([128, 8, 128], BF16), "hT"); qT = Buf(al([128, 8, 128], BF16), "qT")
        st = Buf(al([128, 8], F32), "st")
        SC = Buf(al([128, 2, 8, 128], F32), "SC")
        OH4 = SC[:].rearrange("p a h (x y) -> p (a h) x y", x=8)
        TMP = Buf(al([128, 256], F32), "TMP")
        SV = Buf(al([128, 2, 8, 16], F32), "SV"); SI = Buf(al([128, 2, 8, 16], U32), "SI"); SIF = Buf(al([128, 2, 8, 16], F32), "SIF")
        CS_ = Buf(al([128, 8, 256], F32), "CS_")
        TS = Buf(al([128, 8, 16], F32), "TS"); POS = Buf(al([128, 8, 16], U32), "POS")
        PA = Buf(al([128, 2, 128], U32), "PA"); PAF = Buf(al([128, 2, 128], F32), "PAF")
        IAB = Buf(al([128, 2, 128], F32), "IAB"); RZ = Buf(al([128, 16], F32), "RZ")
        idx_t = al([128, 2, 128], I32); gw_t = al([128, 2, 8, 16], F32); h2s_t = al([128, 2, 1024], F32)
        IDXs = [Buf(idx_t[:, i], "IDX%d" % i) for i in range(2)]; GWs = [Buf(gw_t[:, i], "GW%d" % i) for i in range(2)]
        H2Ss = [Buf(h2s_t[:, i], "H2S%d" % i) for i in range(2)]
        DOT = Buf(al([128, 128], F32), "DOT"); WGT = Buf(al([128, 128], F32), "WGT"); WG = Buf(al([128, 128], F32), "WG")
        FIN = Buf(al([128, 512], F32), "FIN")
        NGRP = 3
        GSZ = 4
        gb_t = al([128, NGRP, GSZ, 2048], BF16)
        GB = [[Buf(gb_t[:, i, j], "gb%d_%d" % (i, j)) for j in range(GSZ)] for i in range(NGRP)]
        dg_t = al([128, 4, 128], BF16)
        DGs = [Buf(dg_t[:, i], "dg%d" % i) for i in range(4)]
        ACCB = [g.PS[6], g.PS[7]]
        OHB = SC

        def routing(tt):
            i = tt % 2
            IDX, GW, h2 = IDXs[i], GWs[i], H2Ss[i]
            if tt == 0 or tt == 16:
                r = 0 if tt < 16 else 1
                dma(lambda e: e.dma_start(out=GS[:], in_=g.W["norm2_g"][l:l + 1, :].partition_broadcast(128)), [], [GS])
                load_bcast(g, junk, g.MODS[l, r:r + 1, 4 * D:5 * D])
                kb.op("dve", lambda e: e.scalar_tensor_tensor(out=GS[:], in0=junk[:], scalar=1.0, in1=GS[:], op0=ALU.add, op1=ALU.mult), [junk, GS], [GS])
                load_bcast(g, SH, g.MODS[l, r:r + 1, 3 * D:4 * D])
                yield
            xin = g.X[tt]
            kb.op("act", lambda e: e.activation(out=junk[:], in_=xin[:], func=AF.Square, accum_out=st[:, 0:1]), [xin], [junk, st])
            kb.op("dve", lambda e: e.tensor_scalar(out=st[:, 1:2], in0=st[:, 0:1], scalar1=1.0 / D, scalar2=EPS, op0=ALU.mult, op1=ALU.add), [st], [st])
            yield
            kb.op("act", lambda e: e.activation(out=st[:, 2:3], in_=st[:, 1:2], func=AF.Sqrt), [st], [st])
            kb.op("dve", lambda e: e.reciprocal(out=st[:, 3:4], in_=st[:, 2:3]), [st], [st])
            yield
            kb.op("dve", lambda e: e.scalar_tensor_tensor(out=junk[:], in0=xin[:], scalar=st[:, 3:4], in1=GS[:], op0=ALU.mult, op1=ALU.mult), [xin, st, GS], [junk])
            yield
            kb.op("dve", lambda e: e.tensor_tensor(out=h2[:], in0=junk[:], in1=SH[:], op=ALU.add), [junk, SH], [h2])
            yield
            kb.op("act", lambda e: e.activation(out=hb[:], in_=h2[:], func=AF.Copy), [h2], [hb])
            ps = g.PS[0]
            psb = ps[:].bitcast(BF16)
            for k in range(8):
                kb.op("pe", lambda e, k=k: e.transpose(out=psb[:, k * 128:(k + 1) * 128], in_=hb[:, k * 128:(k + 1) * 128], identity=g.ident_b[:]), [hb, g.ident_b], [ps])
            kb.op("act", lambda e: e.activation(out=hT[:], in_=psb.rearrange("p (k t) -> p k t", k=8), func=AF.Copy), [ps], [hT])
            yield
            for hh in range(2):
                ps = g.PS[0]
                for h4 in range(4):
                    h = hh * 4 + h4
                    for k in range(8):
                        kb.op("pe", lambda e, k=k, h=h, h4=h4, ps=ps: e.matmul(ps[:, h4 * 128:(h4 + 1) * 128], lhsT=WQ[:, k, h * 128:(h + 1) * 128], rhs=hT[:, k, :],
                                                                            start=(k == 0 and h4 == 0), stop=(k == 7), skip_group_check=True), [WQ, hT], [ps])
                kb.op("act", lambda e, hh=hh, ps=ps: e.activation(out=qT[:, hh * 4:(hh + 1) * 4, :], in_=ps[:].rearrange("p (h t) -> p h t", h=4), func=AF.Copy), [ps], [qT])
                yield
            for p in range(2):
                for hh in range(2):
                    ps = g.PS[1 + p]
                    for h4 in range(4):
                        h = hh * 4 + h4
                        kb.op("pe", lambda e, p=p, h=h, h4=h4, ps=ps: e.matmul(ps[:, h4 * 128:(h4 + 1) * 128], lhsT=qT[p * 64:(p + 1) * 64, h, :], rhs=KT[p * 64:(p + 1) * 64, h, :],
                                                                            start=True, stop=True, skip_group_check=True), [qT, KT], [ps])
                    kb.op("act", lambda e, p=p, hh=hh, ps=ps: e.activation(out=SC[:, p, hh * 4:(hh + 1) * 4, :], in_=ps[:].rearrange("p (h t) -> p h t", h=4), func=AF.Copy), [ps], [SC])
                    yield
            for p in range(2):
                for h in range(8):
                    s_ = SC[:, p, h, :]
                    kb.op("dve", lambda e, p=p, h=h, s_=s_: e.max(out=SV[:, p, h, 0:8], in_=s_), [SC], [SV])
                    kb.op("dve", lambda e, p=p, h=h, s_=s_: e.max_index(out=SI[:, p, h, 0:8], in_max=SV[:, p, h, 0:8], in_values=s_), [SC, SV], [SI])
                    yield
                    kb.op("dve", lambda e, p=p, h=h, s_=s_: e.match_replace(out=TMP[:, 0:128], in_to_replace=SV[:, p, h, 0:8], in_values=s_, imm_value=NEG), [SC, SV], [TMP])
                    kb.op("dve", lambda e, p=p, h=h: e.max(out=SV[:, p, h, 8:16], in_=TMP[:, 0:128]), [TMP], [SV])
                    kb.op("dve", lambda e, p=p, h=h: e.max_index(out=SI[:, p, h, 8:16], in_max=SV[:, p, h, 8:16], in_values=TMP[:, 0:128]), [TMP, SV], [SI])
                    yield
            kb.op("dve", lambda e: e.tensor_copy(out=SIF[:], in_=SI[:]), [SI], [SIF])
            c4 = CS_[:].rearrange("p h (a b) -> p h a b", a=16)
            kb.op("dve", lambda e: e.tensor_tensor(out=c4, in0=SV[:, 0].unsqueeze(3).to_broadcast([128, 8, 16, 16]), in1=SV[:, 1].unsqueeze(2).to_broadcast([128, 8, 16, 16]), op=ALU.add), [SV], [CS_])
            yield
            for h in range(8):
                kb.op("dve", lambda e, h=h: e.max(out=TS[:, h, 0:8], in_=CS_[:, h, :]), [CS_], [TS])
                kb.op("dve", lambda e, h=h: e.max_index(out=POS[:, h, 0:8], in_max=TS[:, h, 0:8], in_values=CS_[:, h, :]), [CS_, TS], [POS])
                yield
                kb.op("dve", lambda e, h=h: e.match_replace(out=TMP[:], in_to_replace=TS[:, h, 0:8], in_values=CS_[:, h, :], imm_value=NEG), [CS_, TS], [TMP])
                kb.op("dve", lambda e, h=h: e.max(out=TS[:, h, 8:16], in_=TMP[:]), [TMP], [TS])
                kb.op("dve", lambda e, h=h: e.max_index(out=POS[:, h, 8:16], in_max=TS[:, h, 8:16], in_values=TMP[:]), [TMP, TS], [POS])
                yield
            posf = POS[:].rearrange("p h j -> p (h j)")
            kb.op("dve", lambda e: e.tensor_single_scalar(out=PA[:, 0, :], in_=posf, scalar=4, op=ALU.logical_shift_right), [POS], [PA])
            kb.op("dve", lambda e: e.tensor_single_scalar(out=PA[:, 1, :], in_=posf, scalar=15, op=ALU.bitwise_and), [POS], [PA])
            kb.op("dve", lambda e: e.tensor_copy(out=PAF[:], in_=PA[:]), [PA], [PAF])
            yield
            oh = SC[:].rearrange("p a h k -> p (a h k)").rearrange("p (h j a) -> p h j a", h=8, j=16)
            for w in range(2):
                pa3 = PAF[:, w, :].rearrange("p (h j) -> p h j", h=8)
                kb.op("dve", lambda e, pa3=pa3: e.tensor_tensor(out=oh, in0=pa3.unsqueeze(3).to_broadcast([128, 8, 16, 16]),
                                                               in1=IO16[:].unsqueeze(1).unsqueeze(1).to_broadcast([128, 8, 16, 16]), op=ALU.is_equal), [PAF, IO16], [OHB])
                yield
                kb.op("dve", lambda e, w=w: e.tensor_tensor(out=oh, in0=oh, in1=SIF[:, w].unsqueeze(2).to_broadcast([128, 8, 16, 16]), op=ALU.mult), [OHB, SIF], [OHB])
                yield
                kb.op("dve", lambda e, w=w: e.tensor_reduce(out=IAB[:, w, :].rearrange("p (h j) -> p h j", h=8), in_=oh, axis=AX.X, op=ALU.add), [OHB], [IAB])
                yield
            kb.op("dve", lambda e: e.tensor_scalar(out=IAB[:, 0, :], in0=IAB[:, 0, :], scalar1=128.0, scalar2=float(l * 16384), op0=ALU.mult, op1=ALU.add), [IAB], [IAB])
            kb.op("dve", lambda e: e.tensor_tensor(out=IAB[:, 0, :], in0=IAB[:, 0, :], in1=IAB[:, 1, :], op=ALU.add), [IAB], [IAB])
            kb.op("dve", lambda e: e.tensor_copy(out=IDX[:], in_=IAB[:, 0, :]), [IAB], [IDX])
            yield
            kb.op("dve", lambda e: e.tensor_tensor(out=GW[:], in0=TS[:], in1=TS[:, :, 0:1].to_broadcast([128, 8, 16]), op=ALU.subtract), [TS], [GW])
            kb.op("act", lambda e: e.activation(out=GW[:], in_=GW[:], func=AF.Exp), [GW], [GW])
            kb.op("dve", lambda e: e.tensor_reduce(out=RZ[:, 0:8], in_=GW[:], axis=AX.X, op=ALU.add), [GW], [RZ])
            yield
            kb.op("dve", lambda e: e.reciprocal(out=RZ[:, 8:16], in_=RZ[:, 0:8]), [RZ], [RZ])
            kb.op("dve", lambda e: e.tensor_tensor(out=GW[:], in0=GW[:], in1=RZ[:, 8:16].unsqueeze(2).to_broadcast([128, 8, 16]), op=ALU.mult), [GW, RZ], [GW])
            yield

        def advance(gen, n):
            if gen is None:
                return
            for _ in range(n):
                try:
                    next(gen)
                except StopIteration:
                    return

        def gather_phase(tt, bg):
            i = tt % 2
            IDX, GW, h2s = IDXs[i], GWs[i], H2Ss[i]
            G2 = G2s[0] if tt < 16 else G2s[1]
            xin = g.X[tt]
            kb.op("act", lambda e: e.activation(out=H2P[:], in_=h2s[:], func=AF.Copy), [h2s], [H2P])
            ngr = 128 // GSZ

            def acc_group(k):
                gb = GB[k % NGRP]
                sl = slice(k * GSZ, (k + 1) * GSZ)
                kb.op("dve", lambda e: e.tensor_tensor(out=WGT[:, sl], in0=WG[:, sl], in1=GW[:].rearrange("p h j -> p (h j)")[:, sl], op=ALU.mult), [WG, GW], [WGT])
                for j in range(GSZ):
                    s = k * GSZ + j
                    dg = DGs[s % 4]
                    kb.op("act", lambda e, dg=dg, s=s: e.activation(out=dg[:], in_=g.ident_b[:], func=AF.Copy, scale=WGT[:, s:s + 1]), [g.ident_b, WGT], [dg])
                    for hf in range(2):
                        kb.op("pe", lambda e, dg=dg, j=j, hf=hf, s=s: e.matmul(ACCB[hf][:], lhsT=dg[:], rhs=gb[j][:, 1024 + hf * 512:1024 + (hf + 1) * 512],
                                                                            start=(s == 0), stop=(s == 127)), [dg, gb[j]], [ACCB[hf]])

            for k in range(ngr):
                gb = GB[k % NGRP]
                for j in range(GSZ if "peer_nogather" not in g.debug else 0):
                    s = k * GSZ + j
                    kb.dma("pool", lambda e, j=j, s=s: e.indirect_dma_start(out=gb[j][:], out_offset=None, in_=uv_flat,
                           in_offset=bass.IndirectOffsetOnAxis(ap=IDX[:, s:s + 1], axis=0)), [IDX], [gb[j]])
                for j in range(GSZ if "peer_nodot" not in g.debug else 0):
                    s = k * GSZ + j
                    kb.op("dve", lambda e, j=j, s=s: e.scalar_tensor_tensor(out=junkb[:], in0=gb[j][:, 0:1024], scalar=1.0, in1=H2P[:], op0=ALU.mult, op1=ALU.mult,
                                                                          accum_out=DOT[:, s:s + 1]), [gb[j], H2P], [junkb, DOT])
                sl = slice(k * GSZ, (k + 1) * GSZ)
                kb.op("act", lambda e, sl=sl: e.activation(out=WG[:, sl], in_=DOT[:, sl], func=AF.Gelu), [DOT], [WG])
                if k >= 1:
                    acc_group(k - 1)
                advance(bg, 4)
            acc_group(ngr - 1)
            for hf in range(2):
                kb.op("dve", lambda e, hf=hf: e.tensor_tensor(out=FIN[:], in0=ACCB[hf][:], in1=G2[:, hf * 512:(hf + 1) * 512], op=ALU.mult), [ACCB[hf], G2], [FIN])
                kb.op("dve", lambda e, hf=hf: e.tensor_tensor(out=xin[:, hf * 512:(hf + 1) * 512], in0=xin[:, hf * 512:(hf + 1) * 512], in1=FIN[:], op=ALU.add), [xin, FIN], [xin])
            advance(bg, 100000)

        tiles = list(range(NT if ctx_out else 16))
        advance(routing(tiles[0]), 100000)
        for n_, tt in enumerate(tiles):
            bg = routing(tiles[n_ + 1]) if n_ + 1 < len(tiles) else None
            gather_phase(tt, bg)
        kb.barrier()


def kernel(**inputs):
    nc, g = build_program()
    consts = host_consts()
    shared = {n: np.ascontiguousarray(np.asarray(inputs[n], dtype=np.float32)).reshape(s) for n, s in W_NAMES}
    shared.update(consts)
    shared["c_ctx"] = np.ascontiguousarray(np.asarray(inputs["c_ctx"], dtype=np.float32)).reshape(1, D)
    x = np.asarray(inputs["x"], dtype=np.float32)
    c = np.asarray(inputs["c"], dtype=np.float32)
    ctx = np.asarray(inputs["ctx"], dtype=np.float32)
    nb = x.shape[0]
    in_maps = []
    for b in range(nb):
        m = dict(shared)
        m["x"] = np.ascontiguousarray(x[b])
        m["ctx"] = np.ascontiguousarray(ctx[b])
        m["c"] = np.ascontiguousarray(c[b:b + 1])
        in_maps.append(m)
    res = run_bass_kernel_spmd(nc, in_maps, core_ids=list(range(nb)))
    out = np.stack([np.asarray(r["out"], dtype=np.float32) for r in res.results], axis=0)
    return out


def phase_cast_experts(g, n_layers):
    nc, kb = g.nc, g.kb
    from contextlib import ExitStack
    with ExitStack() as es:
        al = lambda shape, dt: es.enter_context(nc.sbuf_tensor(_nm(), shape, dt))
        f_t = al([128, 2, 8, 1024], F32); b_t = al([128, 2, 8, 1024], BF16)
        Fs = [Buf(f_t[:, i], "cf%d" % i) for i in range(2)]; Bs = [Buf(b_t[:, i], "cb%d" % i) for i in range(2)]
        srcs = [(g.W["peer_u"].rearrange("l e d -> (l e) d"), g.UV[:, 0:D]), (g.W["peer_v"].rearrange("l e d -> (l e) d"), g.UV[:, D:2 * D])]
        n = 0
        engs = ["act", "dve", "pool"]
        for src, dst in srcs:
            for ch in range(n_layers * 16):
                r0 = ch * 1024
                f = Fs[n % 2]; b = Bs[n % 2]
                kb.dma("sp", lambda e, f=f, src=src, r0=r0: e.dma_start(out=f[:], in_=src[r0:r0 + 1024, :].rearrange("(p j) d -> p j d", p=128)), [], [f])
                for q in range(4):
                    en = engs[(n * 4 + q) % 3]
                    if en == "act":
                        kb.op("act", lambda e, f=f, b=b, q=q: e.activation(out=b[:, q * 2:(q + 1) * 2, :], in_=f[:, q * 2:(q + 1) * 2, :], func=AF.Copy), [f], [b])
                    else:
                        kb.op(en, lambda e, f=f, b=b, q=q: e.tensor_copy(out=b[:, q * 2:(q + 1) * 2, :], in_=f[:, q * 2:(q + 1) * 2, :]), [f], [b])
                kb.dma("sp", lambda e, b=b, dst=dst, r0=r0: e.dma_start(out=dst[r0:r0 + 1024, :].rearrange("(p j) d -> p j d", p=128), in_=b[:]), [b], [])
                n += 1
        kb.barrier()


def cast_alloc(g, es):
    nc = g.nc
    f_t = es.enter_context(nc.sbuf_tensor(_nm(), [128, 2, 1024], F32))
    b_t = es.enter_context(nc.sbuf_tensor(_nm(), [128, 2, 1024], BF16))
    return f_t, b_t


def cast_bg(g, l, bufs):
    nc, kb = g.nc, g.kb
    f_t, b_t = bufs
    Fs = [Buf(f_t[:, i], "cf%d" % i) for i in range(2)]; Bs = [Buf(b_t[:, i], "cb%d" % i) for i in range(2)]
    chunks = [(src, c0, ch) for src, c0 in [(g.W["peer_u"][l], 0), (g.W["peer_v"][l], D)] for ch in range(128)]

    def load(n):
        src, c0, ch = chunks[n]
        f = Fs[n % 2]
        kb.dma("sp", lambda e: e.dma_start(out=f[:], in_=src[ch * 128:(ch + 1) * 128, :]), [], [f])

    load(0)
    yield
    for n in range(len(chunks)):
        src, c0, ch = chunks[n]
        f = Fs[n % 2]; b = Bs[n % 2]
        kb.op("pool", lambda e: e.tensor_copy(out=b[:], in_=f[:]), [f], [b])
        if n + 1 < len(chunks):
            load(n + 1)
        r0 = l * 16384 + ch * 128
        kb.dma("sp", lambda e: e.dma_start(out=g.UV[r0:r0 + 128, c0:c0 + D], in_=b[:]), [b], [])
        yield


def bgstep(g, n=1):
    bg = getattr(g, "bg", None)
    if bg is None:
        return
    for _ in range(n):
        try:
            next(bg)
        except StopIteration:
            g.bg = None
            return
```

```python
import numpy as np
import math
import concourse.bass as bass
import concourse.mybir as mybir
from concourse.bass_utils import run_bass_kernel_spmd

F32 = mybir.dt.float32
BF16 = mybir.dt.bfloat16
U32 = mybir.dt.uint32
I32 = mybir.dt.int32
AF = mybir.ActivationFunctionType
ALU = mybir.AluOpType
AX = mybir.AxisListType

NDS = 40


class Buf:
    __slots__ = ("t", "w", "r", "name", "nt")

    def __init__(self, t, name="", nt=False):
        self.t = t
        self.w = None
        self.r = []
        self.name = name
        self.nt = nt

    def __getitem__(self, k):
        return self.t[k]


class KB:
    def __init__(self, nc):
        self.nc = nc
        self.E = {"pe": nc.tensor, "act": nc.scalar, "dve": nc.vector, "pool": nc.gpsimd, "sp": nc.sync}
        self.esem = {n: nc.alloc_semaphore("s_" + n) for n in self.E}
        self.ecnt = {n: 0 for n in self.E}
        self.dsems = [nc.alloc_semaphore("d%d" % i) for i in range(NDS)]
        self.dcnt = [0] * NDS
        self.dnext = 0
        self.known = {n: {} for n in self.E}
        self.nwait = 0
        self.ninst = 0

    def _sem(self, key):
        return self.esem[key] if isinstance(key, str) else self.dsems[key]

    def _need(self, eng, ev):
        key, val, _ = ev
        if self.known[eng].get(key, 0) >= val:
            return
        self.E[eng].wait_ge(self._sem(key), val)
        self.known[eng][key] = val
        self.nwait += 1

    def _deps(self, eng, reads, writes, is_dma=False):
        for b in reads:
            if b.w is not None:
                self._need(eng, b.w)
        for b in writes:
            if b.w is not None and (is_dma or b.w[2] != eng):
                self._need(eng, b.w)
            for r in b.r:
                if is_dma or r[2] != eng:
                    self._need(eng, r)

    def op(self, eng, fn, reads=(), writes=()):
        reads = [b for b in reads if not b.nt]
        writes = [b for b in writes if not b.nt]
        self._deps(eng, reads, writes)
        inst = fn(self.E[eng])
        self.ecnt[eng] += 1
        inst.then_inc(self.esem[eng], 1)
        ev = (eng, self.ecnt[eng], eng)
        for b in reads:
            b.r.append(ev)
        for b in writes:
            b.w = ev
            b.r = []
        self.ninst += 1
        return inst

    def dma(self, q, fn, reads=(), writes=()):
        reads = [b for b in reads if not b.nt]
        writes = [b for b in writes if not b.nt]
        self._deps(q, reads, writes, is_dma=True)
        i = self.dnext
        self.dnext = (self.dnext + 1) % NDS
        if self.dcnt[i] > 0:
            self._need(q, (i, self.dcnt[i], "dma"))
        inst = fn(self.E[q])
        self.dcnt[i] += 16
        inst.then_inc(self.dsems[i], 16)
        ev = (i, self.dcnt[i], "dma")
        for b in reads:
            b.r.append(ev)
        for b in writes:
            b.w = ev
            b.r = []
        self.ninst += 1
        return inst

    def barrier(self):
        evs = [(n, self.ecnt[n], n) for n in self.E if self.ecnt[n] > 0]
        evs += [(i, self.dcnt[i], "dma") for i in range(NDS) if self.dcnt[i] > 0]
        for n in self.E:
            for ev in evs:
                self._need(n, ev)

    def finish(self):
        self.barrier()


D = 1024
NX = 2048
NC_ = 256
T = NX + NC_
NT = T // 128
P_IN = 9408
EPS = 1e-6
OFF = {"a_q1": 0, "a_q2": 256, "a_k1": 512, "a_k2": 768, "a_v": 1024,
       "b_cq": 1536, "b_ckv": 1792, "b_kr": 1920,
       "c_q": 1984, "c_f": 2496, "c_b": 3008, "c_i": 3520, "c_g": 4032,
       "d_q": 4544, "d_k": 5056, "d_v": 5184, "gate": 5312}

W_NAMES = [("w_mod", [2, 1024, 6144]), ("b_mod", [2, 6144]), ("norm1_g", [2, 1024]), ("norm2_g", [2, 1024]),
           ("w_in", [2, 1024, 9408]), ("diff_lam_q1", [2, 64]), ("diff_lam_k1", [2, 64]), ("diff_lam_q2", [2, 64]),
           ("diff_lam_k2", [2, 64]), ("diff_norm_g", [2, 128]), ("mla_qnorm_g", [2, 256]), ("mla_kvnorm_g", [2, 128]),
           ("mla_w_uq", [2, 256, 768]), ("mla_w_ukv", [2, 128, 1024]), ("hgrn_lb", [2, 2, 512]), ("hgrn_norm_g", [2, 128]),
           ("win_sink", [2, 8]), ("w_branch", [2, 4, 512, 1024]), ("w_out", [2, 1024, 1024]), ("peer_wq", [2, 1024, 1024]),
           ("peer_keys", [2, 8, 2, 128, 64]), ("peer_u", [2, 16384, 1024]), ("peer_v", [2, 16384, 1024]), ("final_g", [1, 1024])]


def host_consts():
    n = NX
    rows = np.repeat(np.arange(n // 64, dtype=np.float32), 64)
    cols = np.tile(np.arange(64, dtype=np.float32), n // 64)
    m = 32
    freqs = (10000.0 ** (-2.0 * np.arange(m // 2, dtype=np.float32) / m)).astype(np.float32)
    ang = np.concatenate([rows[:, None] * freqs, cols[:, None] * freqs], axis=-1).astype(np.float32)
    cs = np.zeros((T, 64), np.float32)
    cs[:n, :32] = np.cos(ang)
    cs[:n, 32:] = np.sin(ang)
    cs[n:, :32] = 1.0
    i = np.arange(128)
    c = {}
    c["rope_cs"] = cs
    c["ident"] = np.eye(128, dtype=np.float32)
    c["mask_prev"] = (i[:, None] >= i[None, :]).astype(np.float32)
    c["mask_next"] = (i[:, None] <= i[None, :]).astype(np.float32)
    j = np.arange(64)
    tri = np.zeros((64, 4, 64), np.float32)
    tri[:, 0] = (j[:, None] <= j[None, :])
    tri[:, 1] = (j[:, None] >= j[None, :])
    tri[:, 2] = (j[:, None] > j[None, :])
    tri[:, 3] = (j[:, None] < j[None, :])
    c["tri"] = tri.reshape(64, 256)
    return c


class Ctx:
    pass


_nmc = [0]


def _nm():
    _nmc[0] += 1
    return "tmp%d" % _nmc[0]


def build_program(n_layers=2, debug=(), stop_after=None):
    nc = bass.Bass("TRN2", target_bir_lowering=False)
    kb = KB(nc)
    g = Ctx()
    g.nc, g.kb = nc, kb
    g.debug = debug

    def din(name, shape, dt=F32):
        return nc.dram_tensor(name, list(shape), dt, kind="ExternalInput").ap()

    g.x_in = din("x", [NX, D])
    g.c_in = din("c", [1, D])
    g.ctx_in = din("ctx", [NC_, D])
    g.cctx_in = din("c_ctx", [1, D])
    g.W = {n: din(n, s) for n, s in W_NAMES}
    g.rope_cs = din("rope_cs", [T, 64])
    g.ident_in = din("ident", [128, 128])
    g.mprev_in = din("mask_prev", [128, 128])
    g.mnext_in = din("mask_next", [128, 128])
    g.tri_in = din("tri", [64, 256])
    g.out = nc.dram_tensor("out", [NX, D], F32, kind="ExternalOutput").ap()

    def scratch(name, shape, dt):
        kind = "ExternalOutput" if name in debug else "Internal"
        return Buf(nc.dram_tensor(name, list(shape), dt, kind=kind).ap(), name, nt=True)

    g.P = scratch("P", [T, P_IN], BF16)
    g.OB = scratch("OB", [T, 2048], BF16)
    g.MODS = scratch("MODS", [2, 2, 6144], F32)
    g.OF = scratch("OF", [T, 512], F32)
    g.OBK = scratch("OBK", [T, 512], F32)
    g.UV = scratch("UV", [2 * 16384, 2 * D], BF16)

    cnt = [0]

    def sb(shape, dt, name=None):
        cnt[0] += 1
        return Buf(nc.alloc_sbuf_tensor("%s_%d" % (name or "t", cnt[0]), list(shape), dt), name or "t")
    g.sb = sb

    g.X = [Buf(None, "X%d" % i) for i in range(NT)]
    xall = nc.alloc_sbuf_tensor("Xall", [128, NT, D], F32)
    for i in range(NT):
        g.X[i].t = xall[:, i, :]
    psall = nc.alloc_psum_tensor("psall", [128, 8, 512], F32)
    g.psall = psall
    g.PS = [Buf(psall[:, i, :], "ps%d" % i) for i in range(8)]
    g.PSP = [Buf(psall[:, 2 * i:2 * i + 2, :].rearrange("p b c -> p (b c)"), "psp%d" % i) for i in range(4)]
    g.ident_f = sb([128, 128], F32, "identf")
    g.ident_b = sb([128, 128], BF16, "identb")
    g.ones_f = sb([128, 128], F32, "onesf")
    g.SCT = sb([128, 8, 2], F32, "sct")

    dma = lambda fn, r=(), w=(): kb.dma("sp", fn, r, w)

    for i in range(16):
        dma(lambda e, i=i: e.dma_start(out=g.X[i][:], in_=g.x_in[i * 128:(i + 1) * 128, :]), [], [g.X[i]])
    for i in range(2):
        dma(lambda e, i=i: e.dma_start(out=g.X[16 + i][:], in_=g.ctx_in[i * 128:(i + 1) * 128, :]), [], [g.X[16 + i]])
    dma(lambda e: e.dma_start(out=g.ident_f[:], in_=g.ident_in[:, :]), [], [g.ident_f])
    kb.op("dve", lambda e: e.tensor_copy(out=g.ident_b[:], in_=g.ident_f[:]), [g.ident_f], [g.ident_b])
    kb.op("dve", lambda e: e.memset(g.ones_f[:], 1.0), [], [g.ones_f])
    craw = sb([128, 8, 2], F32, "craw")
    with nc.allow_non_contiguous_dma(reason="tiny conditioning vector load"):
        dma(lambda e: e.dma_start(out=craw[:, :, 0], in_=g.c_in.rearrange("o (k p) -> p (o k)", p=128)), [], [craw])
        dma(lambda e: e.dma_start(out=craw[:, :, 1], in_=g.cctx_in.rearrange("o (k p) -> p (o k)", p=128)), [], [craw])
    kb.op("act", lambda e: e.activation(out=g.SCT[:], in_=craw[:], func=AF.Silu), [craw], [g.SCT])

    for l in range(n_layers):
        last = (l == 1)
        from contextlib import ExitStack
        bg_es = ExitStack()
        g.bg = cast_bg(g, l, cast_alloc(g, bg_es))
        with nc.named_scope("mod%d" % l):
            phase_mod(g, l)
        if stop_after == ("mod", l):
            break
        if "skip_proj" not in debug:
            with nc.named_scope("proj%d" % l):
                phase_norm_proj(g, l)
        if stop_after == ("proj", l):
            break
        ctx_out = not last
        if "skip_a" not in debug:
            with nc.named_scope("a%d" % l):
                phase_mixer_a(g, l, ctx_out)
        if stop_after == ("a", l):
            break
        if "skip_b" not in debug:
            with nc.named_scope("b%d" % l):
                phase_mixer_b(g, l, ctx_out)
        if stop_after == ("b", l):
            break
        if "skip_d" not in debug:
            with nc.named_scope("d%d" % l):
                phase_mixer_d(g, l, ctx_out)
        if stop_after == ("d", l):
            break
        if "skip_c" not in debug:
            with nc.named_scope("c%d" % l):
                phase_mixer_c(g, l, ctx_out)
        if stop_after == ("c", l):
            break
        if "skip_merge" not in debug:
            with nc.named_scope("merge%d" % l):
                phase_merge(g, l, ctx_out)
        if "Xdbg" in debug and stop_after == ("merge", l):
            xd = nc.dram_tensor("Xdbg", [NT, 128, D], F32, kind="ExternalOutput").ap()
            for i in range(NT):
                dma(lambda e, i=i: e.dma_start(out=xd[i], in_=g.X[i][:]), [g.X[i]], [])
        if stop_after == ("merge", l):
            break
        with nc.named_scope("castdrain%d" % l):
            bgstep(g, 100000)
            kb.barrier()
            bg_es.close()
        with nc.named_scope("peer%d" % l):
            phase_peer(g, l, ctx_out)
        if "Xdbg" in debug and stop_after == ("peer", l):
            xd = nc.dram_tensor("Xdbg", [NT, 128, D], F32, kind="ExternalOutput").ap()
            for i in range(NT):
                dma(lambda e, i=i: e.dma_start(out=xd[i], in_=g.X[i][:]), [g.X[i]], [])
        if stop_after == ("peer", l):
            break

    if stop_after is None:
        with nc.named_scope("final"):
            phase_final(g)
    kb.finish()
    g.nc = nc
    return nc, g


def phase_mod(g, l):
    nc, kb = g.nc, g.kb
    dma = lambda fn, r=(), w=(): kb.dma("sp", fn, r, w)
    with nc.sbuf_tensor(_nm(), [128, 2, 8, 512], F32) as wm_t, nc.sbuf_tensor(_nm(), [1, 6144], F32) as bm_t, \
            nc.sbuf_tensor(_nm(), [2, 2, 512], F32) as ev_t:
        wm = [Buf(wm_t[:, i], "wm%d" % i) for i in range(2)]
        bm = Buf(bm_t, "bm")
        ev = [Buf(ev_t[:, i], "ev%d" % i) for i in range(2)]
        dma(lambda e: e.dma_start(out=bm[:], in_=g.W["b_mod"][l:l + 1, :]), [], [bm])
        wsrc = g.W["w_mod"][l].rearrange("(k p) c -> p k c", p=128)
        for m in range(12):
            w = wm[m % 2]
            dma(lambda e, w=w, m=m: e.dma_start(out=w[:], in_=wsrc[:, :, m * 512:(m + 1) * 512]), [], [w])
            ps = g.PS[m % 2]
            for k in range(8):
                kb.op("pe", lambda e, w=w, k=k, ps=ps: e.matmul(ps[0:2, :], lhsT=g.SCT[:, k, :], rhs=w[:, k, :],
                                                              start=(k == 0), stop=False), [g.SCT, w], [ps])
            kb.op("pe", lambda e, ps=ps, m=m: e.matmul(ps[0:2, :], lhsT=g.ones_f[0:1, 0:2], rhs=bm[0:1, m * 512:(m + 1) * 512],
                                                     start=False, stop=True), [g.ones_f, bm], [ps])
            o = ev[m % 2]
            kb.op("act", lambda e, o=o, ps=ps: e.activation(out=o[:], in_=ps[0:2, :], func=AF.Copy), [ps], [o])
            dma(lambda e, o=o, m=m: e.dma_start(out=g.MODS[l, :, m * 512:(m + 1) * 512], in_=o[:]), [o], [g.MODS])
        kb.barrier()


def load_bcast(g, dst, src_row_ap):
    g.kb.dma("sp", lambda e: e.dma_start(out=dst[:], in_=src_row_ap.partition_broadcast(128)), [g.MODS], [dst])


def norm_tile(g, xin, GS, SH, hb, scr):
    kb = g.kb
    junk, st = scr
    kb.op("act", lambda e: e.activation(out=junk[:], in_=xin[:], func=AF.Square, accum_out=st[:, 0:1]), [xin], [junk, st])
    kb.op("dve", lambda e: e.tensor_scalar(out=st[:, 1:2], in0=st[:, 0:1], scalar1=1.0 / D, scalar2=EPS, op0=ALU.mult, op1=ALU.add), [st], [st])
    kb.op("act", lambda e: e.activation(out=st[:, 2:3], in_=st[:, 1:2], func=AF.Sqrt), [st], [st])
    kb.op("dve", lambda e: e.reciprocal(out=st[:, 3:4], in_=st[:, 2:3]), [st], [st])
    kb.op("dve", lambda e: e.scalar_tensor_tensor(out=junk[:], in0=xin[:], scalar=st[:, 3:4], in1=GS[:], op0=ALU.mult, op1=ALU.mult), [xin, st, GS], [junk])
    kb.op("dve", lambda e: e.tensor_tensor(out=hb[:], in0=junk[:], in1=SH[:], op=ALU.add), [junk, SH], [hb])


def phase_norm_proj(g, l):
    nc, kb = g.nc, g.kb
    dma = lambda fn, r=(), w=(): kb.dma("sp", fn, r, w)
    with nc.sbuf_tensor(_nm(), [128, 8, T], BF16) as hT_t:
        hT = Buf(hT_t, "hT")
        with nc.sbuf_tensor(_nm(), [128, 5, D], F32) as bc_t, nc.sbuf_tensor(_nm(), [128, D], F32) as junk_t, \
                nc.sbuf_tensor(_nm(), [128, 2, 8], F32) as st_t, nc.sbuf_tensor(_nm(), [128, 2, D], BF16) as hb_t:
            G1, SCx, SHx, SCc, SHc = [Buf(bc_t[:, i], "bc%d" % i) for i in range(5)]
            junk = Buf(junk_t, "junk")
            sts = [Buf(st_t[:, i], "st%d" % i) for i in range(2)]
            hbs = [Buf(hb_t[:, i], "hb%d" % i) for i in range(2)]
            dma(lambda e: e.dma_start(out=G1[:], in_=g.W["norm1_g"][l:l + 1, :].partition_broadcast(128)), [], [G1])
            load_bcast(g, SHx, g.MODS[l, 0:1, 0:D])
            load_bcast(g, SCx, g.MODS[l, 0:1, D:2 * D])
            load_bcast(g, SHc, g.MODS[l, 1:2, 0:D])
            load_bcast(g, SCc, g.MODS[l, 1:2, D:2 * D])
            for S in (SCx, SCc):
                kb.op("dve", lambda e, S=S: e.scalar_tensor_tensor(out=S[:], in0=S[:], scalar=1.0, in1=G1[:], op0=ALU.add, op1=ALU.mult), [S, G1], [S])
            for tt in range(NT):
                GS, SH = (SCx, SHx) if tt < 16 else (SCc, SHc)
                hb = hbs[tt % 2]
                norm_tile(g, g.X[tt], GS, SH, hb, (junk, sts[tt % 2]))
                ps = g.PS[tt % 2]
                psb = ps[:].bitcast(BF16)
                for k in range(8):
                    kb.op("pe", lambda e, k=k, hb=hb, psb=psb: e.transpose(out=psb[:, k * 128:(k + 1) * 128], in_=hb[:, k * 128:(k + 1) * 128], identity=g.ident_b[:]), [hb, g.ident_b], [ps])
                kb.op("act", lambda e, tt=tt, psb=psb: e.activation(out=hT[:, :, tt * 128:(tt + 1) * 128], in_=psb.rearrange("p (k t) -> p k t", k=8), func=AF.Copy), [ps], [hT])
            kb.barrier()
        with nc.sbuf_tensor(_nm(), [128, 2, 8, 512], BF16) as wb_t, nc.sbuf_tensor(_nm(), [128, 2, 6, 512], BF16) as po_t:
            wbs = [Buf(wb_t[:, i], "wb%d" % i) for i in range(2)]
            pos = [Buf(po_t[:, i], "po%d" % i) for i in range(2)]
            wsrc = g.W["w_in"][l].rearrange("(k p) c -> p k c", p=128)
            pdst = g.P.t.rearrange("(t p) c -> p t c", p=128)
            nct = (P_IN + 511) // 512
            cw = lambda ct: min(512, P_IN - ct * 512)

            def load_w(ct):
                w = wbs[ct % 2]
                kb.dma("pool", lambda e: e.dma_start(out=w[:, :, 0:cw(ct)], in_=wsrc[:, :, ct * 512:ct * 512 + cw(ct)]), [], [w])
            load_w(0)
            oi = 0
            pi = 0
            for ct in range(nct):
                if ct + 1 < nct:
                    load_w(ct + 1)
                w = wbs[ct % 2]
                n = cw(ct)
                for tg in range(3):
                    po = pos[oi % 2]
                    oi += 1
                    for j in range(6):
                        tt = tg * 6 + j
                        ps = g.PS[2 + pi % 4]
                        pi += 1
                        for k in range(8):
                            kb.op("pe", lambda e, k=k, tt=tt, ps=ps, w=w, n=n: e.matmul(ps[:, 0:n], lhsT=hT[:, k, tt * 128:(tt + 1) * 128], rhs=w[:, k, 0:n],
                                                                                     start=(k == 0), stop=(k == 7)), [hT, w], [ps])
                        if pi % 2 == 0:
                            kb.op("act", lambda e, j=j, ps=ps, po=po, n=n: e.activation(out=po[:, j, 0:n], in_=ps[:, 0:n], func=AF.Copy), [ps], [po])
                        else:
                            kb.op("dve", lambda e, j=j, ps=ps, po=po, n=n: e.tensor_copy(out=po[:, j, 0:n], in_=ps[:, 0:n]), [ps], [po])
                    dma(lambda e, po=po, tg=tg, ct=ct, n=n: e.dma_start(out=pdst[:, tg * 6:(tg + 1) * 6, ct * 512:ct * 512 + n], in_=po[:, :, 0:n]), [po], [g.P])
            kb.barrier()


def attention(g, nh, dv, scale, V, qk_chunks, groups, post, npass=1):
    nc, kb = g.nc, g.kb
    assert dv + 1 <= 256
    with nc.sbuf_tensor(_nm(), [128, 3, 512], BF16) as E_t, nc.sbuf_tensor(_nm(), [128, 4, 4, dv + 1], F32) as of_t:
        Es = [Buf(E_t[:, i], "E%d" % i) for i in range(3)]
        Ofs = [Buf(of_t[:, i], "Of%d" % i) for i in range(4)]
        iters = []
        for h in range(nh):
            for (q_lo, q_n, key_tiles) in groups:
                for s in range(npass):
                    iters.append((h, q_lo, q_n, key_tiles, s))
        steps = [(ii, ki) for ii, it_ in enumerate(iters) for ki in range(len(it_[3]))]

        def emit_S(i):
            ii, ki = steps[i]
            h, q_lo, q_n, kts, s = iters[ii]
            kt = kts[ki]
            S = g.PS[i % 3]
            chunks = qk_chunks(h, s)
            for ci, (qf, kf, bufs) in enumerate(chunks):
                kb.op("pe", lambda e, qf=qf, kf=kf, ci=ci: e.matmul(
                    S[:, 0:q_n], lhsT=kf(kt), rhs=qf(q_lo, q_n), start=(ci == 0), stop=(ci == len(chunks) - 1)), bufs, [S])

        ofl = []

        def emit_rest(i):
            ii, ki = steps[i]
            h, q_lo, q_n, kts, s = iters[ii]
            kt = kts[ki]
            nsub = q_n // 128
            S = g.PS[i % 3]; E = Es[i % 3]
            obanks = [g.PS[3 + 2 * (ii % 2)], g.PS[4 + 2 * (ii % 2)]]
            Of = Ofs[ii % 4]
            kb.op("act", lambda e: e.activation(out=E[:, 0:q_n], in_=S[:, 0:q_n], func=AF.Exp, scale=scale), [S], [E])
            for sub in range(nsub):
                ob = obanks[sub // 2]
                c0 = (sub % 2) * (dv + 1)
                kb.op("pe", lambda e, ob=ob, c0=c0, sub=sub: e.matmul(
                    ob[:, c0:c0 + dv + 1], lhsT=E[:, sub * 128:(sub + 1) * 128], rhs=V[:, kt, h, :],
                    start=(ki == 0 and sub % 2 == 0), stop=(ki == len(kts) - 1), skip_group_check=True), [E, V], [ob])
            if ki == len(kts) - 1:
                for bi in range((nsub + 1) // 2):
                    nb = min(2, nsub - bi * 2)
                    ob = obanks[bi]
                    src = ob[:, 0:nb * (dv + 1)].rearrange("p (s d) -> p s d", s=nb)
                    kb.op("dve", lambda e, bi=bi, nb=nb, src=src: e.tensor_copy(out=Of[:, bi * 2:bi * 2 + nb, :], in_=src), [ob], [Of])
                ofl.append(Of)
                if s == npass - 1:
                    post(h, q_lo, q_n, list(ofl))
                    del ofl[:]

        emit_S(0)
        for i in range(len(steps)):
            if i + 1 < len(steps):
                emit_S(i + 1)
            emit_rest(i)
            if i % 4 == 3:
                bgstep(g)


def phase_mixer_a(g, l, ctx_out):
    nc, kb = g.nc, g.kb
    dma = lambda fn, r=(), w=(): kb.dma("sp", fn, r, w)
    lam_init = 0.8 - 0.6 * math.exp(-0.3 * l)
    Pv = g.P.t.rearrange("(t p) c -> p t c", p=128)
    OBv = g.OB.t.rearrange("(t p) c -> p t c", p=128)
    with nc.sbuf_tensor(_nm(), [128, 8, T], BF16) as qkt_t, nc.sbuf_tensor(_nm(), [128, NT, 4, 129], BF16) as va_t, \
            nc.sbuf_tensor(_nm(), [128, NT, 64], F32) as cs_t, nc.sbuf_tensor(_nm(), [128, 8], F32) as lam_t, \
            nc.sbuf_tensor(_nm(), [128, 128], F32) as ng_t:
        QKT = Buf(qkt_t, "QKT"); VA = Buf(va_t, "VA"); CS = Buf(cs_t, "CS"); LAM = Buf(lam_t, "LAM"); NG = Buf(ng_t, "NG")
        dma(lambda e: e.dma_start(out=CS[:], in_=g.rope_cs.rearrange("(t p) c -> p t c", p=128)), [], [CS])
        for tt in range(NT):
            dma(lambda e, tt=tt: e.dma_start(out=VA[:, tt, :, 0:128], in_=Pv[:, tt, 1024:1536].rearrange("p (h d) -> p h d", h=4)), [g.P], [VA])
        kb.op("pool", lambda e: e.memset(VA[:, :, :, 128:129], 1.0), [], [VA])
        with nc.sbuf_tensor(_nm(), [128, 4, 64], F32) as lv_t:
            LV = Buf(lv_t, "LV")
            for i, nm in enumerate(["diff_lam_q1", "diff_lam_k1", "diff_lam_q2", "diff_lam_k2"]):
                dma(lambda e, i=i, nm=nm: e.dma_start(out=LV[:, i, :], in_=g.W[nm][l:l + 1, :].partition_broadcast(128)), [], [LV])
            kb.op("dve", lambda e: e.tensor_tensor(out=LV[:, 0, :], in0=LV[:, 0, :], in1=LV[:, 1, :], op=ALU.mult), [LV], [LV])
            kb.op("dve", lambda e: e.tensor_tensor(out=LV[:, 2, :], in0=LV[:, 2, :], in1=LV[:, 3, :], op=ALU.mult), [LV], [LV])
            kb.op("dve", lambda e: e.tensor_reduce(out=LAM[:, 0:1], in_=LV[:, 0, :], axis=AX.X, op=ALU.add), [LV], [LAM])
            kb.op("dve", lambda e: e.tensor_reduce(out=LAM[:, 1:2], in_=LV[:, 2, :], axis=AX.X, op=ALU.add), [LV], [LAM])
            kb.op("act", lambda e: e.activation(out=LAM[:, 2:4], in_=LAM[:, 0:2], func=AF.Exp), [LAM], [LAM])
            kb.op("dve", lambda e: e.tensor_tensor(out=LAM[:, 4:5], in0=LAM[:, 2:3], in1=LAM[:, 3:4], op=ALU.subtract), [LAM], [LAM])
            kb.op("dve", lambda e: e.tensor_scalar(out=LAM[:, 5:6], in0=LAM[:, 4:5], scalar1=lam_init, scalar2=None, op0=ALU.add), [LAM], [LAM])
            dma(lambda e: e.dma_start(out=NG[:], in_=g.W["diff_norm_g"][l:l + 1, :].partition_broadcast(128)), [], [NG])
            kb.op("dve", lambda e: e.tensor_scalar(out=NG[:], in0=NG[:], scalar1=1.0 - lam_init, scalar2=None, op0=ALU.mult), [NG], [NG])
            kb.barrier()
        lam = LAM[:, 5:6]
        with nc.sbuf_tensor(_nm(), [128, 2, 1024], BF16) as pa_t, nc.sbuf_tensor(_nm(), [128, 2, 1024], BF16) as rb_t, \
                nc.sbuf_tensor(_nm(), [128, 4, 512], F32) as tm_t:
            pas = [Buf(pa_t[:, i], "pa%d" % i) for i in range(2)]
            rbs = [Buf(rb_t[:, i], "rb%d" % i) for i in range(2)]
            tms = [Buf(tm_t[:, i], "tm%d" % i) for i in range(4)]
            for tt in range(NT):
                pa = pas[tt % 2]; rb = rbs[tt % 2]
                dma(lambda e, pa=pa, tt=tt: e.dma_start(out=pa[:], in_=Pv[:, tt, 0:1024]), [g.P], [pa])
                rope_tile(g, pa, rb, CS, tt, 16, tms)
                ps = g.PS[7]
                psb = ps[:].bitcast(BF16)
                for c in range(8):
                    kb.op("pe", lambda e, c=c, rb=rb, psb=psb: e.transpose(out=psb[:, c * 128:(c + 1) * 128], in_=rb[:, c * 128:(c + 1) * 128], identity=g.ident_b[:]), [rb, g.ident_b], [ps])
                kb.op("act", lambda e, tt=tt, psb=psb: e.activation(out=QKT[:, :, tt * 128:(tt + 1) * 128], in_=psb.rearrange("p (k t) -> p k t", k=8), func=AF.Copy), [ps], [QKT])
            kb.barrier()

        def qk_chunks(h, s):
            p0 = (h % 2) * 64
            qc = s * 2 + h // 2
            kc = 4 + s * 2 + h // 2
            return [(lambda lo, n: QKT[p0:p0 + 64, qc, lo:lo + n], lambda kt: QKT[p0:p0 + 64, kc, kt * 128:(kt + 1) * 128], [QKT])]

        with nc.sbuf_tensor(_nm(), [128, 2, 8, 4], F32) as r_t, nc.sbuf_tensor(_nm(), [128, 2, 3, 4, 128], F32) as w_t, \
                nc.sbuf_tensor(_nm(), [128, 2, 4, 128], BF16) as o_t:
            cnt = [0]
            rs_ = [Buf(r_t[:, i], "r") for i in range(2)]; obs_ = [Buf(o_t[:, i], "ob") for i in range(2)]
            ws_ = [[Buf(w_t[:, i, j], "w%d" % j) for j in range(3)] for i in range(2)]

            def post(h, q_lo, q_n, ofl):
                O1, O2 = ofl
                i = cnt[0] % 2
                cnt[0] += 1
                ns = q_n // 128
                r = rs_[i]; w0, w1, w2 = ws_[i]; ob = obs_[i]
                bc = lambda ap: ap.unsqueeze(2).to_broadcast([128, ns, 128])
                kb.op("dve", lambda e: e.reciprocal(out=r[:, 0, 0:ns], in_=O1[:, 0:ns, 128]), [O1], [r])
                kb.op("dve", lambda e: e.reciprocal(out=r[:, 1, 0:ns], in_=O2[:, 0:ns, 128]), [O2], [r])
                kb.op("dve", lambda e: e.tensor_scalar(out=r[:, 2, 0:ns], in0=r[:, 1, 0:ns], scalar1=lam, scalar2=None, op0=ALU.mult), [r, LAM], [r])
                kb.op("dve", lambda e: e.tensor_tensor(out=w0[:, 0:ns, :], in0=O1[:, 0:ns, 0:128], in1=bc(r[:, 0, 0:ns]), op=ALU.mult), [O1, r], [w0])
                kb.op("dve", lambda e: e.tensor_tensor(out=w1[:, 0:ns, :], in0=O2[:, 0:ns, 0:128], in1=bc(r[:, 2, 0:ns]), op=ALU.mult), [O2, r], [w1])
                kb.op("dve", lambda e: e.tensor_tensor(out=w0[:, 0:ns, :], in0=w0[:, 0:ns, :], in1=w1[:, 0:ns, :], op=ALU.subtract), [w0, w1], [w0])
                kb.op("pool", lambda e: e.tensor_tensor(out=w2[:, 0:ns, :], in0=w0[:, 0:ns, :], in1=w0[:, 0:ns, :], op=ALU.mult), [w0], [w2])
                kb.op("dve", lambda e: e.tensor_reduce(out=r[:, 3, 0:ns], in_=w2[:, 0:ns, :], axis=AX.X, op=ALU.add), [w2], [r])
                kb.op("dve", lambda e: e.tensor_scalar(out=r[:, 4, 0:ns], in0=r[:, 3, 0:ns], scalar1=1.0 / 128, scalar2=EPS, op0=ALU.mult, op1=ALU.add), [r], [r])
                kb.op("act", lambda e: e.activation(out=r[:, 5, 0:ns], in_=r[:, 4, 0:ns], func=AF.Sqrt), [r], [r])
                kb.op("dve", lambda e: e.reciprocal(out=r[:, 6, 0:ns], in_=r[:, 5, 0:ns]), [r], [r])
                kb.op("dve", lambda e: e.tensor_tensor(out=w1[:, 0:ns, :], in0=w0[:, 0:ns, :], in1=bc(r[:, 6, 0:ns]), op=ALU.mult), [w0, r], [w1])
                kb.op("pool", lambda e: e.tensor_tensor(out=ob[:, 0:ns, :], in0=w1[:, 0:ns, :], in1=NG[:].unsqueeze(1).to_broadcast([128, ns, 128]), op=ALU.mult), [w1, NG], [ob])
                t0 = q_lo // 128
                dma(lambda e: e.dma_start(out=OBv[:, t0:t0 + ns, h * 128:(h + 1) * 128], in_=ob[:, 0:ns, :]), [ob], [g.OB])

            groups = [(i * 512, 512, list(range(NT))) for i in range(4)]
            if ctx_out:
                groups.append((2048, 256, [16, 17]))
            attention(g, 4, 128, 1.0 / 8.0, VA, qk_chunks, groups, post, npass=2)
            kb.barrier()


def rope_tile(g, pa, rb, CS, tt, ng, tms, col0=0):
    kb = g.kb
    n = ng * 64
    xv = pa[:, col0:col0 + n].rearrange("p (g a f j) -> p g a f j", g=ng, a=2, f=2, j=16)
    ov = rb[:, col0:col0 + n].rearrange("p (g a f j) -> p g a f j", g=ng, a=2, f=2, j=16)
    x1, x2 = xv[:, :, :, 0, :], xv[:, :, :, 1, :]
    cosb = CS[:, tt, 0:32].rearrange("p (a j) -> p a j", a=2).unsqueeze(1).to_broadcast([128, ng, 2, 16])
    sinb = CS[:, tt, 32:64].rearrange("p (a j) -> p a j", a=2).unsqueeze(1).to_broadcast([128, ng, 2, 16])
    tv = [t[:, 0:ng * 32].rearrange("p (g a j) -> p g a j", g=ng, a=2) for t in tms]
    kb.op("dve", lambda e: e.tensor_tensor(out=tv[0], in0=x1, in1=cosb, op=ALU.mult), [pa, CS], [tms[0]])
    kb.op("pool", lambda e: e.tensor_tensor(out=tv[1], in0=x2, in1=sinb, op=ALU.mult), [pa, CS], [tms[1]])
    kb.op("dve", lambda e: e.tensor_tensor(out=tv[2], in0=x1, in1=sinb, op=ALU.mult), [pa, CS], [tms[2]])
    kb.op("pool", lambda e: e.tensor_tensor(out=tv[3], in0=x2, in1=cosb, op=ALU.mult), [pa, CS], [tms[3]])
    kb.op("dve", lambda e: e.tensor_tensor(out=ov[:, :, :, 0, :], in0=tv[0], in1=tv[1], op=ALU.subtract), [tms[0], tms[1]], [rb])
    kb.op("pool", lambda e: e.tensor_tensor(out=ov[:, :, :, 1, :], in0=tv[2], in1=tv[3], op=ALU.add), [tms[2], tms[3]], [rb])


def small_rms(g, src_ap, src_buf, n, nh, r, col):
    kb = g.kb
    return None


def phase_mixer_b(g, l, ctx_out):
    nc, kb = g.nc, g.kb
    dma = lambda fn, r=(), w=(): kb.dma("sp", fn, r, w)
    Pv = g.P.t.rearrange("(t p) c -> p t c", p=128)
    OBv = g.OB.t.rearrange("(t p) c -> p t c", p=128)
    TQ = T if ctx_out else NX
    with nc.sbuf_tensor(_nm(), [128, 4, T], BF16) as ft_t, nc.sbuf_tensor(_nm(), [128, 4, T], BF16) as qnt_t, \
            nc.sbuf_tensor(_nm(), [128, 4, T], BF16) as knt_t, nc.sbuf_tensor(_nm(), [128, 2, T], BF16) as qrt_t, \
            nc.sbuf_tensor(_nm(), [128, NT, 4, 129], BF16) as vb_t:
        FT = Buf(ft_t, "FT"); QNT = Buf(qnt_t, "QNT"); KNT = Buf(knt_t, "KNT"); QRT = Buf(qrt_t, "QRT"); VB = Buf(vb_t, "VB")
        kb.op("pool", lambda e: e.memset(VB[:, :, :, 128:129], 1.0), [], [VB])
        with nc.sbuf_tensor(_nm(), [128, 2, 4, 128], BF16) as wqn_t, nc.sbuf_tensor(_nm(), [128, 2, 4, 64], BF16) as wqr_t, \
                nc.sbuf_tensor(_nm(), [128, 4, 128], BF16) as wkn_t, nc.sbuf_tensor(_nm(), [128, 4, 128], BF16) as wv_t, \
                nc.sbuf_tensor(_nm(), [128, 384], F32) as gn_t, nc.sbuf_tensor(_nm(), [128, NT, 64], F32) as cs_t:
            WQN = Buf(wqn_t, "WQN"); WQR = Buf(wqr_t, "WQR"); WKN = Buf(wkn_t, "WKN"); WV = Buf(wv_t, "WV"); GN = Buf(gn_t, "GN"); CS = Buf(cs_t, "CS")
            dma(lambda e: e.dma_start(out=CS[:], in_=g.rope_cs.rearrange("(t p) c -> p t c", p=128)), [], [CS])
            wuq = g.W["mla_w_uq"][l].rearrange("(c p) (h d) -> p c h d", p=128, h=4)
            wukv = g.W["mla_w_ukv"][l].rearrange("p (h d) -> p h d", h=4)
            for c in range(2):
                kb.dma("pool", lambda e, c=c: e.dma_start(out=WQN[:, c], in_=wuq[:, c, :, 0:128]), [], [WQN])
                kb.dma("pool", lambda e, c=c: e.dma_start(out=WQR[:, c], in_=wuq[:, c, :, 128:192]), [], [WQR])
            kb.dma("pool", lambda e: e.dma_start(out=WKN[:], in_=wukv[:, :, 0:128]), [], [WKN])
            kb.dma("pool", lambda e: e.dma_start(out=WV[:], in_=wukv[:, :, 128:256]), [], [WV])
            dma(lambda e: e.dma_start(out=GN[:, 0:256], in_=g.W["mla_qnorm_g"][l:l + 1, :].partition_broadcast(128)), [], [GN])
            dma(lambda e: e.dma_start(out=GN[:, 256:384], in_=g.W["mla_kvnorm_g"][l:l + 1, :].partition_broadcast(128)), [], [GN])
            with nc.sbuf_tensor(_nm(), [128, 2, 448], BF16) as pb_t, nc.sbuf_tensor(_nm(), [128, 2, 512], BF16) as tb_t, \
                    nc.sbuf_tensor(_nm(), [128, 2, 512], F32) as jk_t, nc.sbuf_tensor(_nm(), [128, 2, 8], F32) as st_t, \
                    nc.sbuf_tensor(_nm(), [128, 4, 128], F32) as tm_t, nc.sbuf_tensor(_nm(), [128, 2, 64], BF16) as kr_t:
                tms = [Buf(tm_t[:, i], "tm%d" % i) for i in range(4)]
                pbs = [Buf(pb_t[:, i], "pb") for i in range(2)]; tbs = [Buf(tb_t[:, i], "tb") for i in range(2)]
                jks = [Buf(jk_t[:, i], "jk") for i in range(2)]; sts = [Buf(st_t[:, i], "st") for i in range(2)]
                for tt in range(NT):
                    i = tt % 2
                    pb = pbs[i]; tb = tbs[i]; jk = jks[i]; st = sts[i]
                    dma(lambda e, pb=pb, tt=tt: e.dma_start(out=pb[:], in_=Pv[:, tt, 1536:1984]), [g.P], [pb])
                    kb.op("act", lambda e: e.activation(out=jk[:, 0:256], in_=pb[:, 0:256], func=AF.Square, accum_out=st[:, 0:1]), [pb], [jk, st])
                    kb.op("act", lambda e: e.activation(out=jk[:, 256:384], in_=pb[:, 256:384], func=AF.Square, accum_out=st[:, 1:2]), [pb], [jk, st])
                    kb.op("dve", lambda e: e.tensor_scalar(out=st[:, 2:3], in0=st[:, 0:1], scalar1=1.0 / 256, scalar2=EPS, op0=ALU.mult, op1=ALU.add), [st], [st])
                    kb.op("dve", lambda e: e.tensor_scalar(out=st[:, 3:4], in0=st[:, 1:2], scalar1=1.0 / 128, scalar2=EPS, op0=ALU.mult, op1=ALU.add), [st], [st])
                    kb.op("act", lambda e: e.activation(out=st[:, 4:6], in_=st[:, 2:4], func=AF.Sqrt), [st], [st])
                    kb.op("dve", lambda e: e.reciprocal(out=st[:, 6:8], in_=st[:, 4:6]), [st], [st])
                    kb.op("dve", lambda e: e.scalar_tensor_tensor(out=tb[:, 0:256], in0=pb[:, 0:256], scalar=st[:, 6:7], in1=GN[:, 0:256], op0=ALU.mult, op1=ALU.mult), [pb, st, GN], [tb])
                    kb.op("dve", lambda e: e.scalar_tensor_tensor(out=tb[:, 256:384], in0=pb[:, 256:384], scalar=st[:, 7:8], in1=GN[:, 256:384], op0=ALU.mult, op1=ALU.mult), [pb, st, GN], [tb])
                    rope_tile(g, pb, tb, CS, tt, 1, tms, col0=384)
                    kb.op("pool", lambda e: e.tensor_copy(out=tb[:, 448:512], in_=tb[:, 384:448]), [tb], [tb])
                    ps = g.PS[7]
                    psb = ps[:].bitcast(BF16)
                    for c in range(4):
                        kb.op("pe", lambda e, c=c, tb=tb, psb=psb: e.transpose(out=psb[:, c * 128:(c + 1) * 128], in_=tb[:, c * 128:(c + 1) * 128], identity=g.ident_b[:]), [tb, g.ident_b], [ps])
                    kb.op("act", lambda e, tt=tt, psb=psb: e.activation(out=FT[:, :, tt * 128:(tt + 1) * 128], in_=psb[:, 0:512].rearrange("p (k t) -> p k t", k=4), func=AF.Copy), [ps], [FT])
                kb.barrier()
            pi = 0
            ngr = (T + 511) // 512
            for gi in range(ngr):
                lo = gi * 512
                n = min(512, T - lo)
                for h in range(4):
                    ps = g.PS[pi % 4]; pi += 1
                    for c in range(2):
                        kb.op("pe", lambda e, c=c, h=h, ps=ps, lo=lo, n=n: e.matmul(ps[:, 0:n], lhsT=WQN[:, c, h, :], rhs=FT[:, c, lo:lo + n], start=(c == 0), stop=(c == 1)), [WQN, FT], [ps])
                    kb.op("act", lambda e, h=h, ps=ps, lo=lo, n=n: e.activation(out=QNT[:, h, lo:lo + n], in_=ps[:, 0:n], func=AF.Copy), [ps], [QNT])
                    ps = g.PS[pi % 4]; pi += 1
                    kb.op("pe", lambda e, h=h, ps=ps, lo=lo, n=n: e.matmul(ps[:, 0:n], lhsT=WKN[:, h, :], rhs=FT[:, 2, lo:lo + n], start=True, stop=True), [WKN, FT], [ps])
                    kb.op("dve", lambda e, h=h, ps=ps, lo=lo, n=n: e.tensor_copy(out=KNT[:, h, lo:lo + n], in_=ps[:, 0:n]), [ps], [KNT])
            with nc.sbuf_tensor(_nm(), [128, 2, 256], BF16) as qr_t, nc.sbuf_tensor(_nm(), [128, 2, 256], BF16) as qq_t, \
                    nc.sbuf_tensor(_nm(), [128, 4, 128], F32) as tm_t:
                tms = [Buf(tm_t[:, i], "tm%d" % i) for i in range(4)]
                qrs = [Buf(qr_t[:, i], "qr") for i in range(2)]; qqs = [Buf(qq_t[:, i], "qq") for i in range(2)]
                for tt in range(NT):
                    i = tt % 2
                    qr = qrs[i]; qq = qqs[i]
                    ps = g.PS[pi % 4]; pi += 1
                    kb.op("pe", lambda e, ps=ps, tt=tt: e.matmul(ps[:, 0:512], lhsT=FT[:, 2, tt * 128:(tt + 1) * 128], rhs=WV[:].rearrange("p h d -> p (h d)"), start=True, stop=True), [FT, WV], [ps])
                    kb.op("act", lambda e, ps=ps, tt=tt: e.activation(out=VB[:, tt, :, 0:128], in_=ps[:, 0:512].rearrange("p (h d) -> p h d", h=4), func=AF.Copy), [ps], [VB])
                    ps = g.PS[pi % 4]; pi += 1
                    for c in range(2):
                        kb.op("pe", lambda e, c=c, ps=ps, tt=tt: e.matmul(ps[:, 0:256], lhsT=FT[:, c, tt * 128:(tt + 1) * 128], rhs=WQR[:, c].rearrange("p h d -> p (h d)"), start=(c == 0), stop=(c == 1)), [FT, WQR], [ps])
                    kb.op("act", lambda e, ps=ps, qq=qq: e.activation(out=qq[:], in_=ps[:, 0:256], func=AF.Copy), [ps], [qq])
                    rope_tile(g, qq, qr, CS, tt, 4, tms)
                    ps = g.PS[7]
                    psb = ps[:].bitcast(BF16)
                    for c in range(2):
                        kb.op("pe", lambda e, c=c, qr=qr, psb=psb: e.transpose(out=psb[:, c * 128:(c + 1) * 128], in_=qr[:, c * 128:(c + 1) * 128], identity=g.ident_b[:]), [qr, g.ident_b], [ps])
                    kb.op("dve", lambda e, tt=tt, psb=psb: e.tensor_copy(out=QRT[:, :, tt * 128:(tt + 1) * 128], in_=psb[:, 0:256].rearrange("p (k t) -> p k t", k=2)), [ps], [QRT])
                kb.barrier()

        if "dbgB" in g.debug:
            dft = nc.dram_tensor("dbgFT", [128, 4, T], BF16, kind="ExternalOutput").ap()
            dqr = nc.dram_tensor("dbgQRT", [128, 2, T], BF16, kind="ExternalOutput").ap()
            dqn = nc.dram_tensor("dbgQNT", [128, 4, T], BF16, kind="ExternalOutput").ap()
            dkn = nc.dram_tensor("dbgKNT", [128, 4, T], BF16, kind="ExternalOutput").ap()
            dvb = nc.dram_tensor("dbgVB", [128, NT, 4, 129], BF16, kind="ExternalOutput").ap()
            dma(lambda e: e.dma_start(out=dft[:], in_=FT[:]), [FT], [])
            dma(lambda e: e.dma_start(out=dqr[:], in_=QRT[:]), [QRT], [])
            dma(lambda e: e.dma_start(out=dqn[:], in_=QNT[:]), [QNT], [])
            dma(lambda e: e.dma_start(out=dkn[:], in_=KNT[:]), [KNT], [])
            dma(lambda e: e.dma_start(out=dvb[:], in_=VB[:]), [VB], [])

        def qk_chunks(h, s):
            p0 = (h % 2) * 64
            return [(lambda lo, n: QNT[:, h, lo:lo + n], lambda kt: KNT[:, h, kt * 128:(kt + 1) * 128], [QNT, KNT]),
                    (lambda lo, n: QRT[p0:p0 + 64, h // 2, lo:lo + n], lambda kt: FT[p0:p0 + 64, 3, kt * 128:(kt + 1) * 128], [QRT, FT])]

        with nc.sbuf_tensor(_nm(), [128, 2, 4], F32) as r_t, nc.sbuf_tensor(_nm(), [128, 2, 4, 128], BF16) as o_t:
            cnt = [0]
            rs_ = [Buf(r_t[:, i], "r") for i in range(2)]; obs_ = [Buf(o_t[:, i], "ob") for i in range(2)]

            def post(h, q_lo, q_n, ofl):
                O1 = ofl[0]
                i = cnt[0] % 2
                cnt[0] += 1
                ns = q_n // 128
                r = rs_[i]; ob = obs_[i]
                kb.op("dve", lambda e: e.reciprocal(out=r[:, 0:ns], in_=O1[:, 0:ns, 128]), [O1], [r])
                kb.op("dve", lambda e: e.tensor_tensor(out=ob[:, 0:ns, :], in0=O1[:, 0:ns, 0:128], in1=r[:, 0:ns].unsqueeze(2).to_broadcast([128, ns, 128]), op=ALU.mult), [O1, r], [ob])
                t0 = q_lo // 128
                dma(lambda e: e.dma_start(out=OBv[:, t0:t0 + ns, 512 + h * 128:512 + (h + 1) * 128], in_=ob[:, 0:ns, :]), [ob], [g.OB])

            groups = [(i * 512, 512, list(range(NT))) for i in range(4)]
            if ctx_out:
                groups.append((2048, 256, [16, 17]))
            attention(g, 4, 128, 192.0 ** -0.5, VB, qk_chunks, groups, post, npass=1)
            kb.barrier()


def phase_mixer_d(g, l, ctx_out):
    nc, kb = g.nc, g.kb
    dma = lambda fn, r=(), w=(): kb.dma("sp", fn, r, w)
    Pv = g.P.t.rearrange("(t p) c -> p t c", p=128)
    OBv = g.OB.t.rearrange("(t p) c -> p t c", p=128)
    scale = 1.0 / 8.0
    with nc.sbuf_tensor(_nm(), [128, 6, T], BF16) as qkd_t, nc.sbuf_tensor(_nm(), [128, NT, 2, 65], BF16) as vd_t, \
            nc.sbuf_tensor(_nm(), [128, 8], F32) as es_t, nc.sbuf_tensor(_nm(), [128, 2, 128], BF16) as mk_t, \
            nc.sbuf_tensor(_nm(), [128, 2, 128], F32) as mkf_t:
        QKD = Buf(qkd_t, "QKD"); VD = Buf(vd_t, "VD"); ES = Buf(es_t, "ES"); MK = Buf(mk_t, "MK"); MKF = Buf(mkf_t, "MKF")
        kb.op("pool", lambda e: e.memset(VD[:, :, :, 64:65], 1.0), [], [VD])
        for tt in range(NT):
            dma(lambda e, tt=tt: e.dma_start(out=VD[:, tt, :, 0:64], in_=Pv[:, tt, 5184:5312].rearrange("p (h d) -> p h d", h=2)), [g.P], [VD])
        dma(lambda e: e.dma_start(out=MKF[:, 0, :], in_=g.mprev_in[:, :]), [], [MKF])
        dma(lambda e: e.dma_start(out=MKF[:, 1, :], in_=g.mnext_in[:, :]), [], [MKF])
        kb.op("dve", lambda e: e.tensor_copy(out=MK[:], in_=MKF[:]), [MKF], [MK])
        dma(lambda e: e.dma_start(out=ES[:], in_=g.W["win_sink"][l:l + 1, :].partition_broadcast(128)), [], [ES])
        kb.op("act", lambda e: e.activation(out=ES[:], in_=ES[:], func=AF.Exp), [ES], [ES])
        with nc.sbuf_tensor(_nm(), [128, 2, 640], BF16) as pd_t, nc.sbuf_tensor(_nm(), [128, 2, 768], BF16) as rb_t, \
                nc.sbuf_tensor(_nm(), [128, 4, 320], F32) as tm_t, nc.sbuf_tensor(_nm(), [128, NT, 64], F32) as cs_t:
            CS = Buf(cs_t, "CS")
            dma(lambda e: e.dma_start(out=CS[:], in_=g.rope_cs.rearrange("(t p) c -> p t c", p=128)), [], [CS])
            pds = [Buf(pd_t[:, i], "pd%d" % i) for i in range(2)]
            rbs = [Buf(rb_t[:, i], "rb%d" % i) for i in range(2)]
            tms = [Buf(tm_t[:, i], "tm%d" % i) for i in range(4)]
            for tt in range(NT if "d_skip_rope" not in g.debug else 0):
                pd = pds[tt % 2]; rb = rbs[tt % 2]
                dma(lambda e, pd=pd, tt=tt: e.dma_start(out=pd[:], in_=Pv[:, tt, 4544:5184]), [g.P], [pd])
                rope_tile(g, pd, rb, CS, tt, 10, tms)
                kb.op("dve", lambda e, rb=rb: e.tensor_copy(out=rb[:, 640:768].rearrange("p (u d) -> p u d", u=2),
                                                           in_=rb[:, 576:640].unsqueeze(1).to_broadcast([128, 2, 64])), [rb], [rb])
                kb.op("dve", lambda e, rb=rb: e.tensor_copy(out=rb[:, 576:640], in_=rb[:, 512:576]), [rb], [rb])
                ps = g.PS[7]
                psb = ps[:].bitcast(BF16)
                for c in range(6):
                    kb.op("pe", lambda e, c=c, rb=rb, psb=psb: e.transpose(out=psb[:, c * 128:(c + 1) * 128], in_=rb[:, c * 128:(c + 1) * 128], identity=g.ident_b[:]), [rb, g.ident_b], [ps])
                kb.op("act", lambda e, tt=tt, psb=psb: e.activation(out=QKD[:, :, tt * 128:(tt + 1) * 128], in_=psb[:, 0:768].rearrange("p (k t) -> p k t", k=6), func=AF.Copy), [ps], [QKD])
            kb.barrier()
        with nc.sbuf_tensor(_nm(), [128, 3, 512], BF16) as E_t, nc.sbuf_tensor(_nm(), [128, 2, 8, 65], F32) as of_t, \
                nc.sbuf_tensor(_nm(), [128, 2, 2, 8], F32) as r_t, nc.sbuf_tensor(_nm(), [128, 2, 8, 64], BF16) as o_t:
            Es = [Buf(E_t[:, i], "E%d" % i) for i in range(3)]
            Ofs = [Buf(of_t[:, i], "Of%d" % i) for i in range(2)]
            rs_ = [Buf(r_t[:, i], "r%d" % i) for i in range(2)]
            obs_ = [Buf(o_t[:, i], "ob%d" % i) for i in range(2)]
            qtiles = list(range(16)) + ([16, 17] if ctx_out else [])
            if "d_prep_only" in g.debug:
                qtiles = []
            iters = []
            for qn_, qi in enumerate(qtiles):
                if qi < 16:
                    kts = ([(qi - 1, 0)] if qi > 0 else []) + [(qi, None)] + ([(qi + 1, 1)] if qi < 15 else []) + [(16, None), (17, None)]
                else:
                    kts = [(16, None), (17, None)]
                for kvh in range(2):
                    iters.append((qn_, qi, kvh, kts))
            steps = [(ii, ki) for ii, it_ in enumerate(iters) for ki in range(len(it_[3]))]
            Spairs = [(g.PS[0], g.PS[1]), (g.PS[2], g.PS[7])]

            def emit_S(i):
                ii, ki = steps[i]
                qn_, qi, kvh, kts = iters[ii]
                kt, mk = kts[ki]
                Sp = Spairs[i % 2]
                for gq in range(4):
                    hq = kvh * 4 + gq
                    p0 = (hq % 2) * 64
                    S = Sp[gq % 2]
                    c0 = (gq // 2) * 128
                    kb.op("pe", lambda e, S=S, c0=c0, p0=p0, hq=hq: e.matmul(
                        S[:, c0:c0 + 128], lhsT=QKD[p0:p0 + 64, 4 + kvh, kt * 128:(kt + 1) * 128],
                        rhs=QKD[p0:p0 + 64, hq // 2, qi * 128:(qi + 1) * 128], start=True, stop=True, skip_group_check=True), [QKD], [S])

            def emit_rest(i):
                ii, ki = steps[i]
                qn_, qi, kvh, kts = iters[ii]
                kt, mk = kts[ki]
                Sp = Spairs[i % 2]
                E = Es[i % 3]
                Of = Ofs[qn_ % 2]; r = rs_[qn_ % 2]; ob = obs_[qn_ % 2]
                O = g.PS[3 + ii % 4]
                for j in range(2):
                    kb.op("act", lambda e, j=j: e.activation(out=E[:, j * 256:(j + 1) * 256], in_=Sp[j][:, 0:256], func=AF.Exp, scale=scale), [Sp[j]], [E])
                if mk is not None:
                    kb.op("dve", lambda e: e.tensor_tensor(out=E[:].rearrange("p (g q) -> p g q", g=4), in0=E[:].rearrange("p (g q) -> p g q", g=4),
                                                           in1=MKF[:, mk, :].unsqueeze(1).to_broadcast([128, 4, 128]), op=ALU.mult), [E, MKF], [E])
                for gq in range(4):
                    ei = (gq % 2) * 2 + gq // 2
                    kb.op("pe", lambda e, gq=gq, ei=ei: e.matmul(
                        O[:, gq * 65:(gq + 1) * 65], lhsT=E[:, ei * 128:(ei + 1) * 128], rhs=VD[:, kt, kvh, :],
                        start=(ki == 0 and gq == 0), stop=(ki == len(kts) - 1), skip_group_check=True), [E, VD], [O])
                if ki == len(kts) - 1:
                    kb.op("act", lambda e: e.activation(out=Of[:, kvh * 4:(kvh + 1) * 4, :], in_=O[:, 0:260].rearrange("p (g d) -> p g d", g=4), func=AF.Copy), [O], [Of])
                    if kvh == 1:
                        kb.op("dve", lambda e: e.tensor_tensor(out=r[:, 0, :], in0=Of[:, :, 64], in1=ES[:], op=ALU.add), [Of, ES], [r])
                        kb.op("dve", lambda e: e.reciprocal(out=r[:, 1, :], in_=r[:, 0, :]), [r], [r])
                        kb.op("dve", lambda e: e.tensor_tensor(out=ob[:], in0=Of[:, :, 0:64], in1=r[:, 1, :].unsqueeze(2).to_broadcast([128, 8, 64]), op=ALU.mult), [Of, r], [ob])
                        dma(lambda e: e.dma_start(out=OBv[:, qi, 1536:2048], in_=ob[:].rearrange("p h d -> p (h d)")), [ob], [g.OB])

            if steps:
                emit_S(0)
            for i in range(len(steps)):
                if i + 1 < len(steps):
                    emit_S(i + 1)
                emit_rest(i)
                if i % 4 == 3:
                    bgstep(g)
            kb.barrier()


def phase_mixer_c(g, l, ctx_out):
    nc, kb = g.nc, g.kb
    from contextlib import ExitStack
    dma = lambda fn, r=(), w=(): kb.dma("sp", fn, r, w)
    orders = [[32, 33, 34, 35] + list(range(32)), [35, 34, 33, 32] + list(range(31, -1, -1))]
    ODST = [g.OF, g.OBK]
    with ExitStack() as es:
        al = lambda shape, dt: es.enter_context(nc.sbuf_tensor(_nm(), shape, dt))
        TRI = Buf(al([64, 4, 64], F32), "TRI"); LB = Buf(al([64, 2, 2, 512], F32), "LB")
        S_t = al([128, 2, 4, 128], F32); Sb_t = al([128, 2, 4, 128], BF16)
        Ss = [Buf(S_t[:, d], "S%d" % d) for d in range(2)]; Sbs = [Buf(Sb_t[:, d], "Sb%d" % d) for d in range(2)]
        dma(lambda e: e.dma_start(out=TRI[:], in_=g.tri_in.rearrange("p (a t) -> p a t", a=4)), [], [TRI])
        if l == 0:
            kb.op("dve", lambda e: e.memset(LB[:, :, 0, :], 0.0), [], [LB])
            kb.op("dve", lambda e: e.memset(LB[:, :, 1, :], 1.0), [], [LB])
        else:
            for d in range(2):
                dma(lambda e, d=d: e.dma_start(out=LB[:, d, 0, :], in_=g.W["hgrn_lb"][d, 1:2, :].partition_broadcast(64)), [], [LB])
                dma(lambda e, d=d: e.dma_start(out=LB[:, d, 1, :], in_=g.W["hgrn_lb"][d, 0:1, :].partition_broadcast(64)), [], [LB])
                kb.op("dve", lambda e, d=d: e.tensor_tensor(out=LB[:, d, 0, :], in0=LB[:, d, 0, :], in1=LB[:, d, 1, :], op=ALU.subtract), [LB], [LB])
                kb.op("act", lambda e, d=d: e.activation(out=LB[:, d, 0, :], in_=LB[:, d, 0, :], func=AF.Sigmoid), [LB], [LB])
                kb.op("dve", lambda e, d=d: e.tensor_scalar(out=LB[:, d, 1, :], in0=LB[:, d, 0, :], scalar1=-1.0, scalar2=1.0, op0=ALU.mult, op1=ALU.add), [LB], [LB])
        NB = 4
        pc_t = al([64, NB, 2560], BF16); f_t = al([64, NB, 512], F32); lf_t = al([64, NB, 512], F32); kf_t = al([64, NB, 512], BF16)
        bm_t = al([128, NB, 256], F32); e12_t = al([128, NB, 2, 256], F32); em_t = al([128, NB, 8], F32); qf_t = al([128, NB, 256], F32)
        qk_t = al([128, NB, 3, 256], BF16); at_t = al([64, NB, 256], BF16); ed_t = al([64, NB, 512], F32); kh_t = al([64, NB, 512], BF16)
        of_t = al([64, NB, 512], F32)
        mk = lambda t, nm: [Buf(t[:, i], nm + str(i)) for i in range(NB)]
        PCs, Fs, LFs, KFs, BMs, E12s, EMs, QFs, QKs, ATs, EDs, KHs, OFs = [mk(t, n) for t, n in [
            (pc_t, "pc"), (f_t, "f"), (lf_t, "lf"), (kf_t, "kf"), (bm_t, "bm"), (e12_t, "e12"), (em_t, "em"), (qf_t, "qf"),
            (qk_t, "qk"), (at_t, "at"), (ed_t, "ed"), (kh_t, "kh"), (of_t, "of")]]
        psall = g.psall
        BPs = [Buf(psall[:, d, 0:256], "Bp%d" % d) for d in range(2)]
        TPs = [Buf(psall[:, 4 + d, 0:256], "Tp%d" % d) for d in range(2)]
        for d in range(2):
            kb.op("dve", lambda e, d=d: e.memset(Ss[d][:], 0.0), [], [Ss[d]])
            kb.op("dve", lambda e, d=d: e.memset(Sbs[d][:], 0.0), [], [Sbs[d]])
        for a_ in ATs:
            kb.op("dve", lambda e, a_=a_: e.memset(a_[:], 0.0), [], [a_])

        def stage_b(d, S, Sb, pc, qk, at, kh, em, of, need_out, r0):
            if need_out:
                Op = g.PS[6]
                for h in range(4):
                    kb.op("pe", lambda e, h=h: e.matmul(Op[0:64, h * 128:(h + 1) * 128], lhsT=qk[:, 1, h * 64:(h + 1) * 64], rhs=Sb[:, h, :],
                                                        start=(h == 0), stop=False, skip_group_check=True), [qk, Sb], [Op])
                    yield
                    kb.op("pe", lambda e, h=h: e.matmul(Op[0:64, h * 128:(h + 1) * 128], lhsT=at[:, h * 64:(h + 1) * 64], rhs=pc[:, 1536 + h * 128:1536 + (h + 1) * 128],
                                                        start=False, stop=True, skip_group_check=True), [at, pc], [Op])
                    yield
            Up = g.PS[7]
            for h in range(4):
                kb.op("pe", lambda e, h=h: e.matmul(Up[:, h * 128:(h + 1) * 128], lhsT=kh[:, h * 128:(h + 1) * 128], rhs=pc[:, 1536 + h * 128:1536 + (h + 1) * 128],
                                                    start=True, stop=True, skip_group_check=True), [kh, pc], [Up])
                yield
            for h in range(4):
                kb.op("dve", lambda e, h=h: e.scalar_tensor_tensor(out=S[:, h, :], in0=S[:, h, :], scalar=em[:, h:h + 1], in1=Up[:, h * 128:(h + 1) * 128],
                                                                   op0=ALU.mult, op1=ALU.add), [S, em, Up], [S])
                yield
            kb.op("pool", lambda e: e.tensor_copy(out=Sb[:], in_=S[:]), [S], [Sb])
            yield
            if need_out:
                kb.op("act", lambda e: e.activation(out=of[:], in_=Op[0:64, :], func=AF.Copy), [Op], [of])
                yield
                dma(lambda e: e.dma_start(out=ODST[d][r0:r0 + 64, :], in_=of[:]), [of], [ODST[d]])
                yield


        def step(d, ci, part):
            ch = orders[d][ci]
            ti_incl, ti_strict = (0, 2) if d == 0 else (1, 3)
            t_end, t_mid = (63, 31) if d == 0 else (0, 32)
            zc = 512 if d == 0 else 1024
            S, Sb = Ss[d], Sbs[d]
            i = d * 2 + (ci % 2)
            pc, f, lf, kf, bm, e12, em, qf, qk, at, ed, kh, of = [x[i] for x in (PCs, Fs, LFs, KFs, BMs, E12s, EMs, QFs, QKs, ATs, EDs, KHs, OFs)]
            need_out = (ch < 32) or ctx_out
            r0 = ch * 64
            if part == 1:
                yield from stage_b(d, S, Sb, pc, qk, at, kh, em, of, need_out, r0)
                return
            dma(lambda e: e.dma_start(out=pc[:], in_=g.P[r0:r0 + 64, 1984:4544]), [g.P], [pc])
            yield
            yield
            kb.op("act", lambda e: e.activation(out=f[:], in_=pc[:, zc:zc + 512], func=AF.Sigmoid), [pc], [f])
            yield
            if l > 0:
                kb.op("dve", lambda e: e.tensor_tensor(out=f[:], in0=f[:], in1=LB[:, d, 1, :], op=ALU.mult), [f, LB], [f])
                yield
                kb.op("dve", lambda e: e.tensor_tensor(out=f[:], in0=f[:], in1=LB[:, d, 0, :], op=ALU.add), [f, LB], [f])
                yield
            kb.op("act", lambda e: e.activation(out=lf[:], in_=f[:], func=AF.Ln), [f], [lf])
            yield
            kb.op("pool", lambda e: e.tensor_scalar(out=kf[:], in0=f[:], scalar1=-1.0, scalar2=1.0, op0=ALU.mult, op1=ALU.add), [f], [kf])
            yield
            yield
            Bp = BPs[d]
            for h in range(4):
                kb.op("pe", lambda e, h=h: e.matmul(Bp[:, h * 64:(h + 1) * 64], lhsT=lf[:, h * 128:(h + 1) * 128], rhs=TRI[:, ti_incl, :],
                                                    start=True, stop=True, skip_group_check=True), [lf, TRI], [Bp])
            Dp = g.PS[2 + d]
            kb.op("pe", lambda e: e.matmul(Dp[0:64, :], lhsT=TRI[:, ti_strict, :], rhs=lf[:], start=True, stop=True), [lf, TRI], [Dp])
            yield
            yield
            Tp = TPs[d]
            tpb = Tp[:].bitcast(BF16)
            for h in range(4):
                kb.op("pe", lambda e, h=h: e.transpose(out=tpb[:, h * 64:(h + 1) * 64], in_=pc[:, h * 128:(h + 1) * 128], identity=g.ident_b[0:64, 0:64]), [pc, g.ident_b], [Tp])
            for h in range(4):
                kb.op("pe", lambda e, h=h: e.transpose(out=tpb[:, 256 + h * 64:256 + (h + 1) * 64], in_=kf[:, h * 128:(h + 1) * 128], identity=g.ident_b[0:64, 0:64]), [kf, g.ident_b], [Tp])
            b3 = Bp[:].rearrange("p (h t) -> p h t", h=4)
            kb.op("act", lambda e: e.activation(out=em[:, 0:4], in_=b3[:, :, t_mid], func=AF.Copy), [Bp], [em])
            yield
            kb.op("dve", lambda e: e.tensor_tensor(out=bm[:].rearrange("p (h t) -> p h t", h=4), in0=b3, in1=em[:, 0:4].unsqueeze(2).to_broadcast([128, 4, 64]), op=ALU.subtract), [Bp, em], [bm])
            yield
            kb.op("act", lambda e: e.activation(out=e12[:, 0, :], in_=bm[:], func=AF.Exp), [bm], [e12])
            yield
            kb.op("act", lambda e: e.activation(out=e12[:, 1, :], in_=bm[:], func=AF.Exp, scale=-1.0), [bm], [e12])
            yield
            kb.op("act", lambda e: e.activation(out=em[:, 4:8], in_=em[:, 0:4], func=AF.Exp), [em], [em])
            yield
            kb.op("act", lambda e: e.activation(out=em[:, 0:4], in_=b3[:, :, t_end], func=AF.Exp), [Bp], [em])
            yield
            yield
            kb.op("dve", lambda e: e.tensor_tensor(out=qf[:], in0=tpb[:, 0:256], in1=e12[:, 0, :], op=ALU.mult), [Tp, e12], [qf])
            yield
            kb.op("pool", lambda e: e.tensor_copy(out=qk[:, 0, :], in_=qf[:]), [qf], [qk])
            yield
            kb.op("dve", lambda e: e.tensor_tensor(out=qk[:, 1, :].rearrange("p (h t) -> p h t", h=4), in0=qf[:].rearrange("p (h t) -> p h t", h=4),
                                                   in1=em[:, 4:8].unsqueeze(2).to_broadcast([128, 4, 64]), op=ALU.mult), [qf, em], [qk])
            yield
            kb.op("dve", lambda e: e.tensor_tensor(out=qk[:, 2, :], in0=tpb[:, 256:512], in1=e12[:, 1, :], op=ALU.mult), [Tp, e12], [qk])
            yield
            yield
            yield
            Ap = Tp
            if d == 0:
                parts = [((0, 32), (0, 64)), ((32, 64), (32, 64))]
            else:
                parts = [((32, 64), (0, 64)), ((0, 32), (0, 32))]
            for h in range(4):
                for (s0, s1), (c0, c1) in parts:
                    kb.op("pe", lambda e, h=h, s0=s0, s1=s1, c0=c0, c1=c1: e.matmul(
                        Ap[s0:s1, h * 64 + c0:h * 64 + c1], lhsT=qk[:, 2, h * 64 + s0:h * 64 + s1], rhs=qk[:, 0, h * 64 + c0:h * 64 + c1],
                        start=True, stop=True, skip_group_check=True), [qk], [Ap])
            for (s0, s1), (c0, c1) in parts:
                kb.op("dve", lambda e, s0=s0, s1=s1, c0=c0, c1=c1: e.tensor_tensor(
                    out=at[s0:s1, :].rearrange("p (h t) -> p h t", h=4)[:, :, c0:c1],
                    in0=Ap[s0:s1, 0:256].rearrange("p (h t) -> p h t", h=4)[:, :, c0:c1],
                    in1=TRI[s0:s1, ti_incl, c0:c1].unsqueeze(1).to_broadcast([s1 - s0, 4, c1 - c0]), op=ALU.mult), [Ap, TRI], [at])
            yield
            kb.op("act", lambda e: e.activation(out=ed[:], in_=Dp[0:64, :], func=AF.Exp), [Dp], [ed])
            yield
            kb.op("pool", lambda e: e.tensor_tensor(out=kh[:], in0=kf[:], in1=ed[:], op=ALU.mult), [kf, ed], [kh])
            yield

        def lockstep(gens):
            gens = list(gens)
            while gens:
                for g_ in list(gens):
                    try:
                        next(g_)
                    except StopIteration:
                        gens.remove(g_)

        for ci in range(37):
            bgstep(g, 2)
            if ci < 36:
                if "c_nolock" in g.debug:
                    lockstep([step(0, ci, 0)])
                    lockstep([step(1, ci, 0)])
                else:
                    lockstep([step(0, ci, 0), step(1, ci, 0)])
            if ci >= 1:
                lockstep([step(0, ci - 1, 1)])
                lockstep([step(1, ci - 1, 1)])
        kb.barrier()
    with ExitStack() as es:
        al = lambda shape, dt: es.enter_context(nc.sbuf_tensor(_nm(), shape, dt))
        NGh = Buf(al([128, 128], F32), "NGh")
        dma(lambda e: e.dma_start(out=NGh[:], in_=g.W["hgrn_norm_g"][l:l + 1, :].partition_broadcast(128)), [], [NGh])
        a_t = al([128, 2, 512], F32); b_t = al([128, 2, 512], F32); gt_t = al([128, 2, 512], BF16); ro_t = al([128, 2, 3, 512], F32)
        rr_t = al([128, 2, 8], F32); oo_t = al([128, 2, 512], BF16)
        mk2 = lambda t, nm: [Buf(t[:, i], nm + str(i)) for i in range(2)]
        As, Bs, GTs, ROs, RRs, OOs = mk2(a_t, "a"), mk2(b_t, "b"), mk2(gt_t, "gt"), mk2(ro_t, "ro"), mk2(rr_t, "rr"), mk2(oo_t, "oo")
        for tt in range(NT if ctx_out else 16):
            i = tt % 2
            a, b, gt, ro, rr, oo = As[i], Bs[i], GTs[i], ROs[i], RRs[i], OOs[i]
            r0 = tt * 128
            dma(lambda e, a=a, r0=r0: e.dma_start(out=a[:], in_=g.OF[r0:r0 + 128, :]), [g.OF], [a])
            dma(lambda e, b=b, r0=r0: e.dma_start(out=b[:], in_=g.OBK[r0:r0 + 128, :]), [g.OBK], [b])
            dma(lambda e, gt=gt, r0=r0: e.dma_start(out=gt[:], in_=g.P[r0:r0 + 128, 4032:4544]), [g.P], [gt])
            o3 = lambda bb, j: bb[:, j, :].rearrange("p (h v) -> p h v", h=4)
            kb.op("dve", lambda e, a=a, b=b, ro=ro: e.tensor_tensor(out=ro[:, 0, :], in0=a[:], in1=b[:], op=ALU.add), [a, b], [ro])
            kb.op("pool", lambda e, ro=ro: e.tensor_tensor(out=ro[:, 1, :], in0=ro[:, 0, :], in1=ro[:, 0, :], op=ALU.mult), [ro], [ro])
            kb.op("dve", lambda e, ro=ro, rr=rr: e.tensor_reduce(out=rr[:, 0:4], in_=o3(ro, 1), axis=AX.X, op=ALU.add), [ro], [rr])
            kb.op("dve", lambda e, rr=rr: e.tensor_scalar(out=rr[:, 0:4], in0=rr[:, 0:4], scalar1=1.0 / 128, scalar2=EPS, op0=ALU.mult, op1=ALU.add), [rr], [rr])
            kb.op("act", lambda e, rr=rr: e.activation(out=rr[:, 4:8], in_=rr[:, 0:4], func=AF.Sqrt), [rr], [rr])
            kb.op("dve", lambda e, rr=rr: e.reciprocal(out=rr[:, 0:4], in_=rr[:, 4:8]), [rr], [rr])
            kb.op("dve", lambda e, ro=ro, rr=rr: e.tensor_tensor(out=o3(ro, 1), in0=o3(ro, 0), in1=rr[:, 0:4].unsqueeze(2).to_broadcast([128, 4, 128]), op=ALU.mult), [ro, rr], [ro])
            kb.op("act", lambda e, ro=ro, gt=gt: e.activation(out=ro[:, 2, :], in_=gt[:], func=AF.Silu), [gt], [ro])
            kb.op("pool", lambda e, ro=ro: e.tensor_tensor(out=o3(ro, 0), in0=o3(ro, 1), in1=NGh[:].unsqueeze(1).to_broadcast([128, 4, 128]), op=ALU.mult), [ro, NGh], [ro])
            kb.op("dve", lambda e, ro=ro, oo=oo: e.tensor_tensor(out=oo[:], in0=ro[:, 0, :], in1=ro[:, 2, :], op=ALU.mult), [ro], [oo])
            dma(lambda e, oo=oo, r0=r0: e.dma_start(out=g.OB[r0:r0 + 128, 1024:1536], in_=oo[:]), [oo], [g.OB])
        kb.barrier()


def phase_merge(g, l, ctx_out):
    nc, kb = g.nc, g.kb
    from contextlib import ExitStack
    dma = lambda fn, r=(), w=(): kb.dma("sp", fn, r, w)
    Pv = g.P.t.rearrange("(t p) c -> p t c", p=128)
    OBv = g.OB.t.rearrange("(t p) c -> p t c", p=128)
    with ExitStack() as es:
        al = lambda shape, dt: es.enter_context(nc.sbuf_tensor(_nm(), shape, dt))
        WBR = Buf(al([128, 16, 1024], BF16), "WBR"); WO = Buf(al([128, 8, 1024], BF16), "WO")
        G1 = [Buf(al([128, 1024], F32), "G1x"), Buf(al([128, 1024], F32), "G1c")]
        ob_t = al([128, 2, 2048], BF16); gt_t = al([128, 2, 4096], BF16); obt_t = al([128, 2, 16, 128], BF16)
        sg_t = al([128, 3, 512], F32); tp_t = al([128, 3, 512], F32); y_t = al([128, 2, 1024], F32)
        yb_t = al([128, 2, 1024], BF16); yt_t = al([128, 2, 8, 128], BF16)
        mk = lambda t, nm, n: [Buf(t[:, i], nm + str(i)) for i in range(n)]
        OBs, GTs, OBTs, SGs, TPs, Ys, YBs, YTs = mk(ob_t, "ob", 2), mk(gt_t, "gt", 2), mk(obt_t, "obt", 2), mk(sg_t, "sg", 3), mk(tp_t, "tp", 3), mk(y_t, "y", 2), mk(yb_t, "yb", 2), mk(yt_t, "yt", 2)
        wsrc = g.W["w_branch"][l].rearrange("j (c p) n -> p j c n", p=128)
        for j in range(4):
            kb.dma("pool", lambda e, j=j: e.dma_start(out=WBR[:, j * 4:(j + 1) * 4, :], in_=wsrc[:, j]), [], [WBR])
        wo = g.W["w_out"][l].rearrange("(k p) n -> p k n", p=128)
        for j in range(2):
            kb.dma("pool", lambda e, j=j: e.dma_start(out=WO[:, j * 4:(j + 1) * 4, :], in_=wo[:, j * 4:(j + 1) * 4, :]), [], [WO])
        load_bcast(g, G1[0], g.MODS[l, 0:1, 2 * D:3 * D])
        load_bcast(g, G1[1], g.MODS[l, 1:2, 2 * D:3 * D])
        pi = 0
        si = 0
        for tt in range(NT if ctx_out else 16):
            i = tt % 2
            ob, gt, obt, y, yb, yt = OBs[i], GTs[i], OBTs[i], Ys[i], YBs[i], YTs[i]
            G = G1[0] if tt < 16 else G1[1]
            dma(lambda e, ob=ob, tt=tt: e.dma_start(out=ob[:], in_=OBv[:, tt, :]), [g.OB], [ob])
            dma(lambda e, gt=gt, tt=tt: e.dma_start(out=gt[:], in_=Pv[:, tt, 5312:9408]), [g.P], [gt])
            for hf in range(2):
                ps = g.PS[6 + hf]
                psb = ps[:].bitcast(BF16)
                for c in range(8):
                    kb.op("pe", lambda e, c=c, hf=hf, psb=psb, ob=ob: e.transpose(out=psb[:, c * 128:(c + 1) * 128], in_=ob[:, (hf * 8 + c) * 128:(hf * 8 + c + 1) * 128], identity=g.ident_b[:]), [ob, g.ident_b], [ps])
                kb.op("act", lambda e, hf=hf, psb=psb, obt=obt: e.activation(out=obt[:, hf * 8:(hf + 1) * 8, :], in_=psb.rearrange("p (k t) -> p k t", k=8), func=AF.Copy), [ps], [obt])
            for hf in range(2):
                for j in range(4):
                    ps = g.PS[pi % 4]; pi += 1
                    for c in range(4):
                        kb.op("pe", lambda e, c=c, j=j, hf=hf, ps=ps, obt=obt: e.matmul(ps[:], lhsT=obt[:, j * 4 + c, :], rhs=WBR[:, j * 4 + c, hf * 512:(hf + 1) * 512], start=(c == 0), stop=(c == 3)), [obt, WBR], [ps])
                    sg = SGs[si % 3]; tp = TPs[si % 3]; si += 1
                    kb.op("act", lambda e, sg=sg, gt=gt, j=j, hf=hf: e.activation(out=sg[:], in_=gt[:, j * 1024 + hf * 512:j * 1024 + (hf + 1) * 512], func=AF.Sigmoid), [gt], [sg])
                    ysl = (slice(None), slice(hf * 512, (hf + 1) * 512))
                    if j == 0:
                        kb.op("dve", lambda e, ps=ps, sg=sg, y=y, ysl=ysl: e.tensor_tensor(out=y[ysl], in0=ps[:], in1=sg[:], op=ALU.mult), [ps, sg], [y])
                    else:
                        kb.op("dve", lambda e, ps=ps, sg=sg, tp=tp: e.tensor_tensor(out=tp[:], in0=ps[:], in1=sg[:], op=ALU.mult), [ps, sg], [tp])
                        if j < 3:
                            kb.op("pool", lambda e, tp=tp, y=y, ysl=ysl: e.tensor_tensor(out=y[ysl], in0=y[ysl], in1=tp[:], op=ALU.add), [y, tp], [y])
                        else:
                            kb.op("pool", lambda e, tp=tp, y=y, yb=yb, ysl=ysl: e.tensor_tensor(out=yb[ysl], in0=y[ysl], in1=tp[:], op=ALU.add), [y, tp], [yb])
            ps = g.PS[6]
            psb = ps[:].bitcast(BF16)
            for c in range(8):
                kb.op("pe", lambda e, c=c, psb=psb, yb=yb: e.transpose(out=psb[:, c * 128:(c + 1) * 128], in_=yb[:, c * 128:(c + 1) * 128], identity=g.ident_b[:]), [yb, g.ident_b], [ps])
            kb.op("act", lambda e, psb=psb, yt=yt: e.activation(out=yt[:], in_=psb.rearrange("p (k t) -> p k t", k=8), func=AF.Copy), [ps], [yt])
            for hf in range(2):
                ps = g.PS[pi % 4]; pi += 1
                for k in range(8):
                    kb.op("pe", lambda e, k=k, hf=hf, ps=ps, yt=yt: e.matmul(ps[:], lhsT=yt[:, k, :], rhs=WO[:, k, hf * 512:(hf + 1) * 512], start=(k == 0), stop=(k == 7)), [yt, WO], [ps])
                tp = TPs[si % 3]; si += 1
                xs = g.X[tt]
                kb.op("dve", lambda e, ps=ps, tp=tp, G=G, hf=hf: e.tensor_tensor(out=tp[:], in0=ps[:], in1=G[:, hf * 512:(hf + 1) * 512], op=ALU.mult), [ps, G], [tp])
                kb.op("pool", lambda e, tp=tp, xs=xs, hf=hf: e.tensor_tensor(out=xs[:, hf * 512:(hf + 1) * 512], in0=xs[:, hf * 512:(hf + 1) * 512], in1=tp[:], op=ALU.add), [xs, tp], [xs])
        kb.barrier()


def phase_final(g):
    nc, kb = g.nc, g.kb
    from contextlib import ExitStack
    dma = lambda fn, r=(), w=(): kb.dma("sp", fn, r, w)
    with ExitStack() as es:
        al = lambda shape, dt: es.enter_context(nc.sbuf_tensor(_nm(), shape, dt))
        FG = Buf(al([128, 1024], F32), "FG")
        junk = Buf(al([128, 1024], F32), "junk")
        o_t = al([128, 2, 1024], F32); st_t = al([128, 2, 8], F32)
        Os = [Buf(o_t[:, i], "o%d" % i) for i in range(2)]; Ss = [Buf(st_t[:, i], "s%d" % i) for i in range(2)]
        dma(lambda e: e.dma_start(out=FG[:], in_=g.W["final_g"][0:1, :].partition_broadcast(128)), [], [FG])
        for tt in range(16):
            xin = g.X[tt]; st = Ss[tt % 2]; o = Os[tt % 2]
            kb.op("act", lambda e, xin=xin, st=st: e.activation(out=junk[:], in_=xin[:], func=AF.Square, accum_out=st[:, 0:1]), [xin], [junk, st])
            kb.op("dve", lambda e, st=st: e.tensor_scalar(out=st[:, 1:2], in0=st[:, 0:1], scalar1=1.0 / D, scalar2=EPS, op0=ALU.mult, op1=ALU.add), [st], [st])
            kb.op("act", lambda e, st=st: e.activation(out=st[:, 2:3], in_=st[:, 1:2], func=AF.Sqrt), [st], [st])
            kb.op("dve", lambda e, st=st: e.reciprocal(out=st[:, 3:4], in_=st[:, 2:3]), [st], [st])
            kb.op("dve", lambda e, xin=xin, st=st, o=o: e.scalar_tensor_tensor(out=o[:], in0=xin[:], scalar=st[:, 3:4], in1=FG[:], op0=ALU.mult, op1=ALU.mult), [xin, st, FG], [o])
            dma(lambda e, o=o, tt=tt: e.dma_start(out=g.out[tt * 128:(tt + 1) * 128, :], in_=o[:]), [o], [])
        kb.barrier()


def phase_peer(g, l, ctx_out):
    nc, kb = g.nc, g.kb
    from contextlib import ExitStack
    dma = lambda fn, r=(), w=(): kb.dma("sp", fn, r, w)
    NEG = -1.0e30
    uv_flat = g.UV.t
    H2P = g.PSP[2]
    with ExitStack() as es:
        al = lambda shape, dt: es.enter_context(nc.sbuf_tensor(_nm(), shape, dt))
        WQ = Buf(al([128, 8, 1024], BF16), "WQ"); KT = Buf(al([128, 8, 128], BF16), "KT")
        GS = Buf(al([128, 1024], F32), "GS"); SH = Buf(al([128, 1024], F32), "SH")
        G2s = [Buf(al([128, 1024], F32), "G2x"), Buf(al([128, 1024], F32), "G2c")]
        IO16 = Buf(al([128, 16], F32), "IO16")
        kb.op("pool", lambda e: e.iota(IO16[:], pattern=[[1, 16]], base=0, channel_multiplier=0, allow_small_or_imprecise_dtypes=True), [], [IO16])
        wq = g.W["peer_wq"][l].rearrange("(k p) n -> p k n", p=128)
        for j in range(2):
            kb.dma("pool", lambda e, j=j: e.dma_start(out=WQ[:, j * 4:(j + 1) * 4, :], in_=wq[:, j * 4:(j + 1) * 4, :]), [], [WQ])
        junk = Buf(al([128, 1024], F32), "junk"); junkb = Buf(al([128, 1024], BF16), "junkb")
        keyf = junk[:].rearrange("p (a d) -> p a d", a=16); keyb = junkb[:].rearrange("p (a d) -> p a d", a=16)
        dma(lambda e: e.dma_start(out=keyf, in_=g.W["peer_keys"][l].rearrange("h p n d -> n (h p) d")), [], [junk])
        kb.op("dve", lambda e: e.tensor_copy(out=keyb, in_=keyf), [junk], [junkb])
        ps = g.PS[0]
        psb = ps[:].bitcast(BF16)
        for h in range(8):
            kb.op("pe", lambda e, h=h: e.transpose(out=psb[:, h * 128:(h + 1) * 128], in_=keyb[:, h * 2:(h + 1) * 2, :].rearrange("n a d -> n (a d)"), identity=g.ident_b[:]), [junkb, g.ident_b], [ps])
        kb.op("act", lambda e: e.activation(out=KT[:], in_=psb.rearrange("p (h n) -> p h n", h=8), func=AF.Copy), [ps], [KT])
        load_bcast(g, G2s[0], g.MODS[l, 0:1, 5 * D:6 * D])
        if ctx_out:
            load_bcast(g, G2s[1], g.MODS[l, 1:2, 5 * D:6 * D])
        hb = Buf(al([128, 1024], BF16), "hb"); hT = Buf(al([128, 8, 128], BF16), "hT"); qT = Buf(al([128, 8, 128], BF16), "qT")
        st = Buf(al([128, 8], F32), "st")
        SC = Buf(al([128, 2, 8, 128], F32), "SC")
        OH4 = SC[:].rearrange("p a h (x y) -> p (a h) x y", x=8)
        TMP = Buf(al([128, 256], F32), "TMP")
        SV = Buf(al([128, 2, 8, 16], F32), "SV"); SI = Buf(al([128, 2, 8, 16], U32), "SI"); SIF = Buf(al([128, 2, 8, 16], F32), "SIF")
        CS_ = Buf(al([128, 8, 256], F32), "CS_")
        TS = Buf(al([128, 8, 16], F32), "TS"); POS = Buf(al([128, 8, 16], U32), "POS")
        PA = Buf(al([128, 2, 128], U32), "PA"); PAF = Buf(al([128, 2, 128], F32), "PAF")
        IAB = Buf(al([128, 2, 128], F32), "IAB"); RZ = Buf(al([128, 16], F32), "RZ")
        idx_t = al([128, 2, 128], I32); gw_t = al([128, 2, 8, 16], F32); h2s_t = al([128, 2, 1024], F32)
        IDXs = [Buf(idx_t[:, i], "IDX%d" % i) for i in range(2)]; GWs = [Buf(gw_t[:, i], "GW%d" % i) for i in range(2)]
        H2Ss = [Buf(h2s_t[:, i], "H2S%d" % i) for i in range(2)]
        DOT = Buf(al([128, 128], F32), "DOT"); WGT = Buf(al([128, 128], F32), "WGT"); WG = Buf(al([128, 128], F32), "WG")
        FIN = Buf(al([128, 512], F32), "FIN")
        NGRP = 3
        GSZ = 4
        gb_t = al([128, NGRP, GSZ, 2048], BF16)
        GB = [[Buf(gb_t[:, i, j], "gb%d_%d" % (i, j)) for j in range(GSZ)] for i in range(NGRP)]
        dg_t = al([128, 4, 128], BF16)
        DGs = [Buf(dg_t[:, i], "dg%d" % i) for i in range(4)]
        ACCB = [g.PS[6], g.PS[7]]
        OHB = SC

        def routing(tt):
            i = tt % 2
            IDX, GW, h2 = IDXs[i], GWs[i], H2Ss[i]
            if tt == 0 or tt == 16:
                r = 0 if tt < 16 else 1
                dma(lambda e: e.dma_start(out=GS[:], in_=g.W["norm2_g"][l:l + 1, :].partition_broadcast(128)), [], [GS])
                load_bcast(g, junk, g.MODS[l, r:r + 1, 4 * D:5 * D])
                kb.op("dve", lambda e: e.scalar_tensor_tensor(out=GS[:], in0=junk[:], scalar=1.0, in1=GS[:], op0=ALU.add, op1=ALU.mult), [junk, GS], [GS])
                load_bcast(g, SH, g.MODS[l, r:r + 1, 3 * D:4 * D])
                yield
            xin = g.X[tt]
            kb.op("act", lambda e: e.activation(out=junk[:], in_=xin[:], func=AF.Square, accum_out=st[:, 0:1]), [xin], [junk, st])
            kb.op("dve", lambda e: e.tensor_scalar(out=st[:, 1:2], in0=st[:, 0:1], scalar1=1.0 / D, scalar2=EPS, op0=ALU.mult, op1=ALU.add), [st], [st])
            yield
            kb.op("act", lambda e: e.activation(out=st[:, 2:3], in_=st[:, 1:2], func=AF.Sqrt), [st], [st])
            kb.op("dve", lambda e: e.reciprocal(out=st[:, 3:4], in_=st[:, 2:3]), [st], [st])
            yield
            kb.op("dve", lambda e: e.scalar_tensor_tensor(out=junk[:], in0=xin[:], scalar=st[:, 3:4], in1=GS[:], op0=ALU.mult, op1=ALU.mult), [xin, st, GS], [junk])
            yield
            kb.op("dve", lambda e: e.tensor_tensor(out=h2[:], in0=junk[:], in1=SH[:], op=ALU.add), [junk, SH], [h2])
            yield
            kb.op("act", lambda e: e.activation(out=hb[:], in_=h2[:], func=AF.Copy), [h2], [hb])
            ps = g.PS[0]
            psb = ps[:].bitcast(BF16)
            for k in range(8):
                kb.op("pe", lambda e, k=k: e.transpose(out=psb[:, k * 128:(k + 1) * 128], in_=hb[:, k * 128:(k + 1) * 128], identity=g.ident_b[:]), [hb, g.ident_b], [ps])
            kb.op("act", lambda e: e.activation(out=hT[:], in_=psb.rearrange("p (k t) -> p k t", k=8), func=AF.Copy), [ps], [hT])
            yield
            for hh in range(2):
                ps = g.PS[0]
                for h4 in range(4):
                    h = hh * 4 + h4
                    for k in range(8):
                        kb.op("pe", lambda e, k=k, h=h, h4=h4, ps=ps: e.matmul(ps[:, h4 * 128:(h4 + 1) * 128], lhsT=WQ[:, k, h * 128:(h + 1) * 128], rhs=hT[:, k, :],
                                                                            start=(k == 0 and h4 == 0), stop=(k == 7), skip_group_check=True), [WQ, hT], [ps])
                kb.op("act", lambda e, hh=hh, ps=ps: e.activation(out=qT[:, hh * 4:(hh + 1) * 4, :], in_=ps[:].rearrange("p (h t) -> p h t", h=4), func=AF.Copy), [ps], [qT])
                yield
            for p in range(2):
                for hh in range(2):
                    ps = g.PS[1 + p]
                    for h4 in range(4):
                        h = hh * 4 + h4
                        kb.op("pe", lambda e, p=p, h=h, h4=h4, ps=ps: e.matmul(ps[:, h4 * 128:(h4 + 1) * 128], lhsT=qT[p * 64:(p + 1) * 64, h, :], rhs=KT[p * 64:(p + 1) * 64, h, :],
                                                                            start=True, stop=True, skip_group_check=True), [qT, KT], [ps])
                    kb.op("act", lambda e, p=p, hh=hh, ps=ps: e.activation(out=SC[:, p, hh * 4:(hh + 1) * 4, :], in_=ps[:].rearrange("p (h t) -> p h t", h=4), func=AF.Copy), [ps], [SC])
                    yield
            for p in range(2):
                for h in range(8):
                    s_ = SC[:, p, h, :]
                    kb.op("dve", lambda e, p=p, h=h, s_=s_: e.max(out=SV[:, p, h, 0:8], in_=s_), [SC], [SV])
                    kb.op("dve", lambda e, p=p, h=h, s_=s_: e.max_index(out=SI[:, p, h, 0:8], in_max=SV[:, p, h, 0:8], in_values=s_), [SC, SV], [SI])
                    yield
                    kb.op("dve", lambda e, p=p, h=h, s_=s_: e.match_replace(out=TMP[:, 0:128], in_to_replace=SV[:, p, h, 0:8], in_values=s_, imm_value=NEG), [SC, SV], [TMP])
                    kb.op("dve", lambda e, p=p, h=h: e.max(out=SV[:, p, h, 8:16], in_=TMP[:, 0:128]), [TMP], [SV])
                    kb.op("dve", lambda e, p=p, h=h: e.max_index(out=SI[:, p, h, 8:16], in_max=SV[:, p, h, 8:16], in_values=TMP[:, 0:128]), [TMP, SV], [SI])
                    yield
            kb.op("dve", lambda e: e.tensor_copy(out=SIF[:], in_=SI[:]), [SI], [SIF])
            c4 = CS_[:].rearrange("p h (a b) -> p h a b", a=16)
            kb.op("dve", lambda e: e.tensor_tensor(out=c4, in0=SV[:, 0].unsqueeze(3).to_broadcast([128, 8, 16, 16]), in1=SV[:, 1].unsqueeze(2).to_broadcast([128, 8, 16, 16]), op=ALU.add), [SV], [CS_])
            yield
            for h in range(8):
                kb.op("dve", lambda e, h=h: e.max(out=TS[:, h, 0:8], in_=CS_[:, h, :]), [CS_], [TS])
                kb.op("dve", lambda e, h=h: e.max_index(out=POS[:, h, 0:8], in_max=TS[:, h, 0:8], in_values=CS_[:, h, :]), [CS_, TS], [POS])
                yield
                kb.op("dve", lambda e, h=h: e.match_replace(out=TMP[:], in_to_replace=TS[:, h, 0:8], in_values=CS_[:, h, :], imm_value=NEG), [CS_, TS], [TMP])
                kb.op("dve", lambda e, h=h: e.max(out=TS[:, h, 8:16], in_=TMP[:]), [TMP], [TS])
                kb.op("dve", lambda e, h=h: e.max_index(out=POS[:, h, 8:16], in_max=TS[:, h, 8:16], in_values=TMP[:]), [TMP, TS], [POS])
                yield
            posf = POS[:].rearrange("p h j -> p (h j)")
            kb.op("dve", lambda e: e.tensor_single_scalar(out=PA[:, 0, :], in_=posf, scalar=4, op=ALU.logical_shift_right), [POS], [PA])
            kb.op("dve", lambda e: e.tensor_single_scalar(out=PA[:, 1, :], in_=posf, scalar=15, op=ALU.bitwise_and), [POS], [PA])
            kb.op("dve", lambda e: e.tensor_copy(out=PAF[:], in_=PA[:]), [PA], [PAF])
            yield
            oh = SC[:].rearrange("p a h k -> p (a h k)").rearrange("p (h j a) -> p h j a", h=8, j=16)
            for w in range(2):
                pa3 = PAF[:, w, :].rearrange("p (h j) -> p h j", h=8)
                kb.op("dve", lambda e, pa3=pa3: e.tensor_tensor(out=oh, in0=pa3.unsqueeze(3).to_broadcast([128, 8, 16, 16]),
                                                               in1=IO16[:].unsqueeze(1).unsqueeze(1).to_broadcast([128, 8, 16, 16]), op=ALU.is_equal), [PAF, IO16], [OHB])
                yield
                kb.op("dve", lambda e, w=w: e.tensor_tensor(out=oh, in0=oh, in1=SIF[:, w].unsqueeze(2).to_broadcast([128, 8, 16, 16]), op=ALU.mult), [OHB, SIF], [OHB])
                yield
                kb.op("dve", lambda e, w=w: e.tensor_reduce(out=IAB[:, w, :].rearrange("p (h j) -> p h j", h=8), in_=oh, axis=AX.X, op=ALU.add), [OHB], [IAB])
                yield
            kb.op("dve", lambda e: e.tensor_scalar(out=IAB[:, 0, :], in0=IAB[:, 0, :], scalar1=128.0, scalar2=float(l * 16384), op0=ALU.mult, op1=ALU.add), [IAB], [IAB])
            kb.op("dve", lambda e: e.tensor_tensor(out=IAB[:, 0, :], in0=IAB[:, 0, :], in1=IAB[:, 1, :], op=ALU.add), [IAB], [IAB])
            kb.op("dve", lambda e: e.tensor_copy(out=IDX[:], in_=IAB[:, 0, :]), [IAB], [IDX])
            yield
            kb.op("dve", lambda e: e.tensor_tensor(out=GW[:], in0=TS[:], in1=TS[:, :, 0:1].to_broadcast([128, 8, 16]), op=ALU.subtract), [TS], [GW])
            kb.op("act", lambda e: e.activation(out=GW[:], in_=GW[:], func=AF.Exp), [GW], [GW])
            kb.op("dve", lambda e: e.tensor_reduce(out=RZ[:, 0:8], in_=GW[:], axis=AX.X, op=ALU.add), [GW], [RZ])
            yield
            kb.op("dve", lambda e: e.reciprocal(out=RZ[:, 8:16], in_=RZ[:, 0:8]), [RZ], [RZ])
            kb.op("dve", lambda e: e.tensor_tensor(out=GW[:], in0=GW[:], in1=RZ[:, 8:16].unsqueeze(2).to_broadcast([128, 8, 16]), op=ALU.mult), [GW, RZ], [GW])
            yield

        def advance(gen, n):
            if gen is None:
                return
            for _ in range(n):
                try:
                    next(gen)
                except StopIteration:
                    return

        def gather_phase(tt, bg):
            i = tt % 2
            IDX, GW, h2s = IDXs[i], GWs[i], H2Ss[i]
            G2 = G2s[0] if tt < 16 else G2s[1]
            xin = g.X[tt]
            kb.op("act", lambda e: e.activation(out=H2P[:], in_=h2s[:], func=AF.Copy), [h2s], [H2P])
            ngr = 128 // GSZ

            def acc_group(k):
                gb = GB[k % NGRP]
                sl = slice(k * GSZ, (k + 1) * GSZ)
                kb.op("dve", lambda e: e.tensor_tensor(out=WGT[:, sl], in0=WG[:, sl], in1=GW[:].rearrange("p h j -> p (h j)")[:, sl], op=ALU.mult), [WG, GW], [WGT])
                for j in range(GSZ):
                    s = k * GSZ + j
                    dg = DGs[s % 4]
                    kb.op("act", lambda e, dg=dg, s=s: e.activation(out=dg[:], in_=g.ident_b[:], func=AF.Copy, scale=WGT[:, s:s + 1]), [g.ident_b, WGT], [dg])
                    for hf in range(2):
                        kb.op("pe", lambda e, dg=dg, j=j, hf=hf, s=s: e.matmul(ACCB[hf][:], lhsT=dg[:], rhs=gb[j][:, 1024 + hf * 512:1024 + (hf + 1) * 512],
                                                                            start=(s == 0), stop=(s == 127)), [dg, gb[j]], [ACCB[hf]])

            for k in range(ngr):
                gb = GB[k % NGRP]
                for j in range(GSZ if "peer_nogather" not in g.debug else 0):
                    s = k * GSZ + j
                    kb.dma("pool", lambda e, j=j, s=s: e.indirect_dma_start(out=gb[j][:], out_offset=None, in_=uv_flat,
                           in_offset=bass.IndirectOffsetOnAxis(ap=IDX[:, s:s + 1], axis=0)), [IDX], [gb[j]])
                for j in range(GSZ if "peer_nodot" not in g.debug else 0):
                    s = k * GSZ + j
                    kb.op("dve", lambda e, j=j, s=s: e.scalar_tensor_tensor(out=junkb[:], in0=gb[j][:, 0:1024], scalar=1.0, in1=H2P[:], op0=ALU.mult, op1=ALU.mult,
                                                                          accum_out=DOT[:, s:s + 1]), [gb[j], H2P], [junkb, DOT])
                sl = slice(k * GSZ, (k + 1) * GSZ)
                kb.op("act", lambda e, sl=sl: e.activation(out=WG[:, sl], in_=DOT[:, sl], func=AF.Gelu), [DOT], [WG])
                if k >= 1:
                    acc_group(k - 1)
                advance(bg, 4)
            acc_group(ngr - 1)
            for hf in range(2):
                kb.op("dve", lambda e, hf=hf: e.tensor_tensor(out=FIN[:], in0=ACCB[hf][:], in1=G2[:, hf * 512:(hf + 1) * 512], op=ALU.mult), [ACCB[hf], G2], [FIN])
                kb.op("dve", lambda e, hf=hf: e.tensor_tensor(out=xin[:, hf * 512:(hf + 1) * 512], in0=xin[:, hf * 512:(hf + 1) * 512], in1=FIN[:], op=ALU.add), [xin, FIN], [xin])
            advance(bg, 100000)

        tiles = list(range(NT if ctx_out else 16))
        advance(routing(tiles[0]), 100000)
        for n_, tt in enumerate(tiles):
            bg = routing(tiles[n_ + 1]) if n_ + 1 < len(tiles) else None
            gather_phase(tt, bg)
        kb.barrier()


def kernel(**inputs):
    nc, g = build_program()
    consts = host_consts()
    shared = {n: np.ascontiguousarray(np.asarray(inputs[n], dtype=np.float32)).reshape(s) for n, s in W_NAMES}
    shared.update(consts)
    shared["c_ctx"] = np.ascontiguousarray(np.asarray(inputs["c_ctx"], dtype=np.float32)).reshape(1, D)
    x = np.asarray(inputs["x"], dtype=np.float32)
    c = np.asarray(inputs["c"], dtype=np.float32)
    ctx = np.asarray(inputs["ctx"], dtype=np.float32)
    nb = x.shape[0]
    in_maps = []
    for b in range(nb):
        m = dict(shared)
        m["x"] = np.ascontiguousarray(x[b])
        m["ctx"] = np.ascontiguousarray(ctx[b])
        m["c"] = np.ascontiguousarray(c[b:b + 1])
        in_maps.append(m)
    res = run_bass_kernel_spmd(nc, in_maps, core_ids=list(range(nb)))
    out = np.stack([np.asarray(r["out"], dtype=np.float32) for r in res.results], axis=0)
    return out


def phase_cast_experts(g, n_layers):
    nc, kb = g.nc, g.kb
    from contextlib import ExitStack
    with ExitStack() as es:
        al = lambda shape, dt: es.enter_context(nc.sbuf_tensor(_nm(), shape, dt))
        f_t = al([128, 2, 8, 1024], F32); b_t = al([128, 2, 8, 1024], BF16)
        Fs = [Buf(f_t[:, i], "cf%d" % i) for i in range(2)]; Bs = [Buf(b_t[:, i], "cb%d" % i) for i in range(2)]
        srcs = [(g.W["peer_u"].rearrange("l e d -> (l e) d"), g.UV[:, 0:D]), (g.W["peer_v"].rearrange("l e d -> (l e) d"), g.UV[:, D:2 * D])]
        n = 0
        engs = ["act", "dve", "pool"]
        for src, dst in srcs:
            for ch in range(n_layers * 16):
                r0 = ch * 1024
                f = Fs[n % 2]; b = Bs[n % 2]
                kb.dma("sp", lambda e, f=f, src=src, r0=r0: e.dma_start(out=f[:], in_=src[r0:r0 + 1024, :].rearrange("(p j) d -> p j d", p=128)), [], [f])
                for q in range(4):
                    en = engs[(n * 4 + q) % 3]
                    if en == "act":
                        kb.op("act", lambda e, f=f, b=b, q=q: e.activation(out=b[:, q * 2:(q + 1) * 2, :], in_=f[:, q * 2:(q + 1) * 2, :], func=AF.Copy), [f], [b])
                    else:
                        kb.op(en, lambda e, f=f, b=b, q=q: e.tensor_copy(out=b[:, q * 2:(q + 1) * 2, :], in_=f[:, q * 2:(q + 1) * 2, :]), [f], [b])
                kb.dma("sp", lambda e, b=b, dst=dst, r0=r0: e.dma_start(out=dst[r0:r0 + 1024, :].rearrange("(p j) d -> p j d", p=128), in_=b[:]), [b], [])
                n += 1
        kb.barrier()


def cast_alloc(g, es):
    nc = g.nc
    f_t = es.enter_context(nc.sbuf_tensor(_nm(), [128, 2, 1024], F32))
    b_t = es.enter_context(nc.sbuf_tensor(_nm(), [128, 2, 1024], BF16))
    return f_t, b_t


def cast_bg(g, l, bufs):
    nc, kb = g.nc, g.kb
    f_t, b_t = bufs
    Fs = [Buf(f_t[:, i], "cf%d" % i) for i in range(2)]; Bs = [Buf(b_t[:, i], "cb%d" % i) for i in range(2)]
    chunks = [(src, c0, ch) for src, c0 in [(g.W["peer_u"][l], 0), (g.W["peer_v"][l], D)] for ch in range(128)]

    def load(n):
        src, c0, ch = chunks[n]
        f = Fs[n % 2]
        kb.dma("sp", lambda e: e.dma_start(out=f[:], in_=src[ch * 128:(ch + 1) * 128, :]), [], [f])

    load(0)
    yield
    for n in range(len(chunks)):
        src, c0, ch = chunks[n]
        f = Fs[n % 2]; b = Bs[n % 2]
        kb.op("pool", lambda e: e.tensor_copy(out=b[:], in_=f[:]), [f], [b])
        if n + 1 < len(chunks):
            load(n + 1)
        r0 = l * 16384 + ch * 128
        kb.dma("sp", lambda e: e.dma_start(out=g.UV[r0:r0 + 128, c0:c0 + D], in_=b[:]), [b], [])
        yield


def bgstep(g, n=1):
    bg = getattr(g, "bg", None)
    if bg is None:
        return
    for _ in range(n):
        try:
            next(bg)
        except StopIteration:
            g.bg = None
            return
```
